# Optimizing a Trainium2 kernel written in Bass

```python
import math
import jax
import jax.numpy as jnp
from jax import lax
import numpy as np

D_MODEL = 1024
BATCH = 4
SEQ = 8192
DEPTH = 2

N_MIXERS = 4
N_HEADS = 16
GROUP_HEADS = N_HEADS // N_MIXERS
HEAD_DIM = D_MODEL // N_HEADS
GROUP_WIDTH = GROUP_HEADS * HEAD_DIM

MOBA_BLOCK = 256
MOBA_TOPK = 3

NSA_KV_HEADS = 1
NSA_CMP_LEN = 32
NSA_CMP_STRIDE = 16
NSA_CMP_HIDDEN = 2 * HEAD_DIM
NSA_SEL_BLOCK = 64
NSA_SEL_TOPN = 16
NSA_WINDOW = 512

DIFF_QK_DIM = HEAD_DIM // 2

SWA_KV_HEADS = 2
SWA_WINDOW = 128

D_FF = 4 * D_MODEL
CONV_WIDTH = 3

Q_BLOCK = 128
GATHER_Q_BLOCK = 32
NORM_EPS = 1e-6
NEG_INF = -1e30
TINY = 1e-30
FORCED_SCORE = 1e4

NSA_KV_WIDTH = NSA_KV_HEADS * HEAD_DIM
SWA_KV_WIDTH = SWA_KV_HEADS * HEAD_DIM
IN_SPLITS = (GROUP_WIDTH, GROUP_WIDTH, GROUP_WIDTH,
             GROUP_WIDTH, NSA_KV_WIDTH, NSA_KV_WIDTH, NSA_KV_WIDTH,
             NSA_KV_WIDTH, NSA_KV_WIDTH, NSA_KV_WIDTH, 3 * GROUP_HEADS,
             GROUP_WIDTH, GROUP_WIDTH, GROUP_WIDTH,
             GROUP_WIDTH, SWA_KV_WIDTH, SWA_KV_WIDTH)
D_IN = sum(IN_SPLITS)

kernel_name = 'hybrid_moba_nsa_diff_swa_convffn'


def _rms_norm(x, gain):
    xf = x.astype(jnp.float32)
    y = xf * lax.rsqrt(jnp.mean(xf * xf, axis=-1, keepdims=True) + NORM_EPS)
    return (y * gain.astype(jnp.float32)).astype(x.dtype)


def _alibi_slopes(mixer):
    slopes = np.power(np.float32(2.0), np.arange(1, N_HEADS + 1, dtype=np.float32) * np.float32(-8.0 / N_HEADS))
    return jnp.asarray(slopes[mixer::N_MIXERS], dtype=jnp.float32)


def _masked_softmax(s, mask, sink=None):
    s = jnp.where(mask, s, NEG_INF)
    m = jnp.max(s, axis=-1, keepdims=True)
    if sink is not None:
        m = jnp.maximum(m, sink)
    p = jnp.where(mask, jnp.exp(s - m), 0.0)
    denom = jnp.sum(p, axis=-1, keepdims=True)
    if sink is not None:
        denom = denom + jnp.exp(sink - m)
    return p / jnp.maximum(denom, TINY)


def _heads(t, n):
    b, s, _ = t.shape
    return t.reshape(b, s, n, -1).transpose(0, 2, 1, 3)


def _merge(t):
    b, h, s, d = t.shape
    return t.transpose(0, 2, 1, 3).reshape(b, s, h * d)


def _to_chunks(t, qb, axis):
    shp = t.shape
    t = t.reshape(shp[:axis] + (shp[axis] // qb, qb) + shp[axis + 1:])
    return jnp.moveaxis(t, axis, 0)


def _from_chunks(t, axis):
    t = jnp.moveaxis(t, 0, axis)
    shp = t.shape
    return t.reshape(shp[:axis] + (shp[axis] * shp[axis + 1],) + shp[axis + 2:])


def _gather_blocks(blocks, idx):
    g = jax.vmap(jax.vmap(lambda bl, i: bl[i]))(blocks, idx)
    b, h, q, k, l, d = g.shape
    return g.reshape(b, h, q, k * l, d)


def _banded_attention(q, k, v, slopes, window, sinks=None):
    b, g, r, s, dh = q.shape
    nb = s // Q_BLOCK
    n_prev = -(-window // Q_BLOCK)

    def bands(t):
        tp = jnp.pad(t, ((0, 0), (0, 0), (n_prev * Q_BLOCK, 0), (0, 0))).reshape(b, g, nb + n_prev, Q_BLOCK, dh)
        return jnp.concatenate([tp[:, :, p:p + nb] for p in range(n_prev + 1)], axis=3)

    kb, vb = bands(k), bands(v)
    qpos = jnp.arange(s).reshape(nb, Q_BLOCK)
    kpos = (jnp.arange(nb)[:, None] - n_prev) * Q_BLOCK + jnp.arange((n_prev + 1) * Q_BLOCK)
    dist = qpos[:, :, None] - kpos[:, None, :]
    mask = (kpos[:, None, :] >= 0) & (dist >= 0) & (dist < window)
    qr = q.reshape(b, g, r, nb, Q_BLOCK, dh)
    sc = jnp.einsum('bgrnqd,bgnkd->bgrnqk', qr, kb).astype(jnp.float32) * (dh ** -0.5)
    sc = sc - slopes[:, :, None, None, None] * dist.astype(jnp.float32)
    sink = None if sinks is None else sinks.astype(jnp.float32)[:, :, None, None, None]
    p = _masked_softmax(sc, mask, sink)
    o = jnp.einsum('bgrnqk,bgnkd->bgrnqd', p.astype(vb.dtype), vb)
    return o.reshape(b, g, r, s, dh)


def _moba_attention(q, k, v, slopes):
    b, h, s, dh = q.shape
    nb = -(-s // MOBA_BLOCK)
    pad = nb * MOBA_BLOCK - s
    kp = jnp.pad(k, ((0, 0), (0, 0), (0, pad), (0, 0))).reshape(b, h, nb, MOBA_BLOCK, dh)
    vp = jnp.pad(v, ((0, 0), (0, 0), (0, pad), (0, 0))).reshape(b, h, nb, MOBA_BLOCK, dh)
    pos = jnp.arange(s)
    qblk = pos // MOBA_BLOCK
    own = jnp.broadcast_to(qblk[None, None, :, None], (b, h, s, 1))
    n_top = min(MOBA_TOPK, nb - 1)
    if n_top > 0:
        kmean = jnp.mean(kp.astype(jnp.float32), axis=3)
        gate = jnp.einsum('bhsd,bhnd->bhsn', q.astype(jnp.float32), kmean)
        past = jnp.arange(nb)[None, :] < qblk[:, None]
        gate = jnp.where(past, gate, NEG_INF)
        top_val, top_idx = lax.top_k(gate, n_top)
        sel_idx = jnp.concatenate([top_idx, own], axis=-1)
        sel_ok = jnp.concatenate([top_val > 0.5 * NEG_INF, jnp.ones(own.shape, dtype=bool)], axis=-1)
    else:
        sel_idx = own
        sel_ok = jnp.ones(own.shape, dtype=bool)
    scale = dh ** -0.5
    offs = jnp.arange(MOBA_BLOCK)

    def chunk(args):
        qc, ic, okc, tc = args
        kg = _gather_blocks(kp, ic)
        vg = _gather_blocks(vp, ic)
        kpos = (ic[..., None] * MOBA_BLOCK + offs).reshape(ic.shape[:3] + (-1,))
        ok = jnp.repeat(okc, MOBA_BLOCK, axis=-1)
        sc = jnp.einsum('bhqd,bhqkd->bhqk', qc, kg).astype(jnp.float32) * scale
        sc = sc - slopes[None, :, None, None] * (tc[:, None] - kpos).astype(jnp.float32)
        p = _masked_softmax(sc, ok & (kpos <= tc[:, None]))
        return jnp.einsum('bhqk,bhqkd->bhqd', p.astype(vg.dtype), vg)

    out = lax.map(chunk, (_to_chunks(q, GATHER_Q_BLOCK, 2), _to_chunks(sel_idx, GATHER_Q_BLOCK, 2),
                          _to_chunks(sel_ok, GATHER_Q_BLOCK, 2), pos.reshape(-1, GATHER_Q_BLOCK)))
    return _from_chunks(out, 2)


def _nsa_compress(x, pos_emb, w1, b1, w2):
    b, g, s, dh = x.shape
    n_sub = NSA_CMP_LEN // NSA_CMP_STRIDE
    c = x.reshape(b, g, s // NSA_CMP_STRIDE, NSA_CMP_STRIDE, dh)
    nc = c.shape[2] - n_sub + 1
    win = jnp.concatenate([c[:, :, i:i + nc] for i in range(n_sub)], axis=3)
    hid = jax.nn.gelu(jnp.einsum('bgnld,ldf->bgnf', win + pos_emb, w1) + b1)
    return jnp.einsum('bgnf,fd->bgnd', hid, w2)


def _cmp_to_sel_matrix(nc, nsel):
    cs = np.arange(nc)[:, None] * NSA_CMP_STRIDE
    bs = np.arange(nsel)[None, :] * NSA_SEL_BLOCK
    ov = np.clip(np.minimum(cs + NSA_CMP_LEN, bs + NSA_SEL_BLOCK) - np.maximum(cs, bs), 0, None)
    return (ov / NSA_CMP_LEN).astype(np.float32)


def _nsa_attention(q, k_cmp, v_cmp, k_slc, v_slc, k_win, v_win, gates, slopes, cmp_k, cmp_v):
    b, h, s, dh = q.shape
    g = NSA_KV_HEADS
    r = h // g
    qg = q.reshape(b, g, r, s, dh)
    sl = slopes.reshape(g, r)
    kc = _nsa_compress(k_cmp, *cmp_k)
    vc = _nsa_compress(v_cmp, *cmp_v)
    nc = kc.shape[2]
    cmp_end = jnp.arange(nc) * NSA_CMP_STRIDE + NSA_CMP_LEN - 1
    nsel = s // NSA_SEL_BLOCK
    n_top = min(NSA_SEL_TOPN, nsel)
    overlap = jnp.asarray(_cmp_to_sel_matrix(nc, nsel))
    ks = k_slc.reshape(b, g, nsel, NSA_SEL_BLOCK, dh)
    vs = v_slc.reshape(b, g, nsel, NSA_SEL_BLOCK, dh)
    blk_ids = jnp.arange(nsel)
    offs = jnp.arange(NSA_SEL_BLOCK)
    scale = dh ** -0.5

    def chunk(args):
        qc, tc = args
        sc = jnp.einsum('bgrqd,bgnd->bgrqn', qc, kc).astype(jnp.float32) * scale
        sc = sc - sl[:, :, None, None] * (tc[:, None] - cmp_end).astype(jnp.float32)
        p_c = _masked_softmax(sc, cmp_end <= tc[:, None])
        o_c = jnp.einsum('bgrqn,bgnd->bgrqd', p_c.astype(vc.dtype), vc)
        imp = jnp.einsum('bgrqn,ns->bgqs', p_c, overlap)
        qblk = tc // NSA_SEL_BLOCK
        forced = (blk_ids == 0) | (blk_ids == qblk[:, None]) | (blk_ids == qblk[:, None] - 1)
        score = jnp.where(forced, FORCED_SCORE, imp)
        score = jnp.where(blk_ids * NSA_SEL_BLOCK <= tc[:, None], score, NEG_INF)
        top_val, top_idx = lax.top_k(score, n_top)
        ok = jnp.repeat(top_val > 0.5 * NEG_INF, NSA_SEL_BLOCK, axis=-1)
        kg = _gather_blocks(ks, top_idx)
        vg = _gather_blocks(vs, top_idx)
        kpos = (top_idx[..., None] * NSA_SEL_BLOCK + offs).reshape(top_idx.shape[:3] + (-1,))
        dist = (tc[:, None] - kpos)[:, :, None]
        s2 = jnp.einsum('bgrqd,bgqkd->bgrqk', qc, kg).astype(jnp.float32) * scale
        s2 = s2 - sl[:, :, None, None] * dist.astype(jnp.float32)
        p_s = _masked_softmax(s2, ok[:, :, None] & (dist >= 0))
        o_s = jnp.einsum('bgrqk,bgqkd->bgrqd', p_s.astype(vg.dtype), vg)
        return o_c, o_s

    o_c, o_s = lax.map(chunk, (_to_chunks(qg, GATHER_Q_BLOCK, 3), jnp.arange(s).reshape(-1, GATHER_Q_BLOCK)))
    o_c = _from_chunks(o_c, 3).reshape(b, h, s, dh)
    o_s = _from_chunks(o_s, 3).reshape(b, h, s, dh)
    o_w = _banded_attention(qg, k_win, v_win, sl, NSA_WINDOW).reshape(b, h, s, dh)
    return gates[..., 0:1] * o_c + gates[..., 1:2] * o_s + gates[..., 2:3] * o_w


def _diff_attention(q1, q2, k1, k2, v, slopes, lq1, lk1, lq2, lk2, subln, lambda_init):
    b, h, s, dq = q1.shape
    scale = dq ** -0.5
    lam = (jnp.exp(jnp.sum(lq1.astype(jnp.float32) * lk1.astype(jnp.float32)))
           - jnp.exp(jnp.sum(lq2.astype(jnp.float32) * lk2.astype(jnp.float32))) + lambda_init)
    kpos = jnp.arange(s)

    def chunk(args):
        a1, a2, tc = args
        dist = (tc[:, None] - kpos).astype(jnp.float32)
        mask = dist >= 0
        bias = -slopes[:, None, None] * dist
        s1 = jnp.einsum('bhqd,bhkd->bhqk', a1, k1).astype(jnp.float32) * scale + bias
        s2 = jnp.einsum('bhqd,bhkd->bhqk', a2, k2).astype(jnp.float32) * scale + bias
        p = _masked_softmax(s1, mask) - lam * _masked_softmax(s2, mask)
        return jnp.einsum('bhqk,bhkd->bhqd', p.astype(v.dtype), v)

    o = lax.map(chunk, (_to_chunks(q1, Q_BLOCK, 2), _to_chunks(q2, Q_BLOCK, 2), kpos.reshape(-1, Q_BLOCK)))
    o = _from_chunks(o, 2)
    return _rms_norm(o, subln) * (1.0 - lambda_init)


def _swa_attention(q, k, v, slopes, sinks):
    b, h, s, dh = q.shape
    g = SWA_KV_HEADS
    r = h // g
    o = _banded_attention(q.reshape(b, g, r, s, dh), k, v, slopes.reshape(g, r), SWA_WINDOW, sinks.reshape(g, r))
    return o.reshape(b, h, s, dh)


def _conv_ffn(x, w_gate, w_up, conv_w, conv_b, w_down):
    a = x @ w_gate
    a = lax.conv_general_dilated(a, conv_w[:, None, :], window_strides=(1,), padding=[(CONV_WIDTH - 1, 0)],
                                 dimension_numbers=('NWC', 'WIO', 'NWC'), feature_group_count=a.shape[-1]) + conv_b
    return (jax.nn.gelu(a, approximate=True) * (x @ w_up)) @ w_down


def setup_inputs(seed: int = 0) -> dict:
    key = jax.random.key(seed)
    keys = iter(jax.random.split(key, 40))

    def nrm(shape, scale):
        return jax.random.normal(next(keys), shape, jnp.float32) * scale

    L = DEPTH
    return {
        'x': nrm((BATCH, SEQ, D_MODEL), 1.0),
        'attn_pre_norm': 1.0 + nrm((L, D_MODEL), 0.02),
        'attn_post_norm': 1.0 + nrm((L, D_MODEL), 0.02),
        'ffn_pre_norm': 1.0 + nrm((L, D_MODEL), 0.02),
        'ffn_post_norm': 1.0 + nrm((L, D_MODEL), 0.02),
        'w_in': nrm((L, D_MODEL, D_IN), D_MODEL ** -0.5),
        'w_out': nrm((L, D_MODEL, D_MODEL), D_MODEL ** -0.5),
        'nsa_cmp_pos_k': nrm((L, NSA_CMP_LEN, HEAD_DIM), 0.1),
        'nsa_cmp_w1_k': nrm((L, NSA_CMP_LEN, HEAD_DIM, NSA_CMP_HIDDEN), (NSA_CMP_LEN * HEAD_DIM) ** -0.5),
        'nsa_cmp_b1_k': nrm((L, NSA_CMP_HIDDEN), 0.01),
        'nsa_cmp_w2_k': nrm((L, NSA_CMP_HIDDEN, HEAD_DIM), NSA_CMP_HIDDEN ** -0.5),
        'nsa_cmp_pos_v': nrm((L, NSA_CMP_LEN, HEAD_DIM), 0.1),
        'nsa_cmp_w1_v': nrm((L, NSA_CMP_LEN, HEAD_DIM, NSA_CMP_HIDDEN), (NSA_CMP_LEN * HEAD_DIM) ** -0.5),
        'nsa_cmp_b1_v': nrm((L, NSA_CMP_HIDDEN), 0.01),
        'nsa_cmp_w2_v': nrm((L, NSA_CMP_HIDDEN, HEAD_DIM), NSA_CMP_HIDDEN ** -0.5),
        'diff_lambda_q1': nrm((L, DIFF_QK_DIM), 0.1),
        'diff_lambda_k1': nrm((L, DIFF_QK_DIM), 0.1),
        'diff_lambda_q2': nrm((L, DIFF_QK_DIM), 0.1),
        'diff_lambda_k2': nrm((L, DIFF_QK_DIM), 0.1),
        'diff_subln': 1.0 + nrm((L, HEAD_DIM), 0.02),
        'swa_sinks': nrm((L, GROUP_HEADS), 0.5),
        'ffn_w_gate': nrm((L, D_MODEL, D_FF), D_MODEL ** -0.5),
        'ffn_w_up': nrm((L, D_MODEL, D_FF), D_MODEL ** -0.5),
        'ffn_conv_w': nrm((L, CONV_WIDTH, D_FF), CONV_WIDTH ** -0.5),
        'ffn_conv_b': nrm((L, D_FF), 0.01),
        'ffn_w_down': nrm((L, D_FF, D_MODEL), D_FF ** -0.5),
    }


def reference(x, attn_pre_norm, attn_post_norm, ffn_pre_norm, ffn_post_norm, w_in, w_out,
              nsa_cmp_pos_k, nsa_cmp_w1_k, nsa_cmp_b1_k, nsa_cmp_w2_k,
              nsa_cmp_pos_v, nsa_cmp_w1_v, nsa_cmp_b1_v, nsa_cmp_w2_v,
              diff_lambda_q1, diff_lambda_k1, diff_lambda_q2, diff_lambda_k2, diff_subln,
              swa_sinks, ffn_w_gate, ffn_w_up, ffn_conv_w, ffn_conv_b, ffn_w_down):
    b, s, _ = x.shape
    splits = np.cumsum(IN_SPLITS)[:-1].tolist()
    for l in range(DEPTH):
        h = _rms_norm(x, attn_pre_norm[l])
        (mq, mk, mv, nq, nkc, nvc, nks, nvs, nkw, nvw, ngate,
         dq, dk, dv, sq, sk, sv) = jnp.split(h @ w_in[l], splits, axis=-1)
        o_moba = _moba_attention(_heads(mq, GROUP_HEADS), _heads(mk, GROUP_HEADS), _heads(mv, GROUP_HEADS),
                                 _alibi_slopes(0))
        gates = jax.nn.sigmoid(ngate.reshape(b, s, GROUP_HEADS, 3).transpose(0, 2, 1, 3))
        o_nsa = _nsa_attention(_heads(nq, GROUP_HEADS), _heads(nkc, NSA_KV_HEADS), _heads(nvc, NSA_KV_HEADS),
                               _heads(nks, NSA_KV_HEADS), _heads(nvs, NSA_KV_HEADS),
                               _heads(nkw, NSA_KV_HEADS), _heads(nvw, NSA_KV_HEADS), gates, _alibi_slopes(1),
                               (nsa_cmp_pos_k[l], nsa_cmp_w1_k[l], nsa_cmp_b1_k[l], nsa_cmp_w2_k[l]),
                               (nsa_cmp_pos_v[l], nsa_cmp_w1_v[l], nsa_cmp_b1_v[l], nsa_cmp_w2_v[l]))
        dq4 = _heads(dq, GROUP_HEADS)
        dk4 = _heads(dk, GROUP_HEADS)
        lambda_init = 0.8 - 0.6 * math.exp(-0.3 * l)
        o_diff = _diff_attention(dq4[..., :DIFF_QK_DIM], dq4[..., DIFF_QK_DIM:],
                                 dk4[..., :DIFF_QK_DIM], dk4[..., DIFF_QK_DIM:], _heads(dv, GROUP_HEADS),
                                 _alibi_slopes(2), diff_lambda_q1[l], diff_lambda_k1[l],
                                 diff_lambda_q2[l], diff_lambda_k2[l], diff_subln[l], lambda_init)
        o_swa = _swa_attention(_heads(sq, GROUP_HEADS), _heads(sk, SWA_KV_HEADS), _heads(sv, SWA_KV_HEADS),
                               _alibi_slopes(3), swa_sinks[l])
        mix = jnp.concatenate([_merge(o_moba), _merge(o_nsa), _merge(o_diff), _merge(o_swa)], axis=-1)
        x = x + _rms_norm(mix @ w_out[l], attn_post_norm[l])
        h = _rms_norm(x, ffn_pre_norm[l])
        x = x + _rms_norm(_conv_ffn(h, ffn_w_gate[l], ffn_w_up[l], ffn_conv_w[l], ffn_conv_b[l], ffn_w_down[l]),
                          ffn_post_norm[l])
    return x
```

```python
import math
import numpy as np
import ml_dtypes
from contextlib import ExitStack
import concourse.bass as bass
import concourse.mybir as mybir
from concourse.bass_utils import run_bass_kernel_spmd

F32 = mybir.dt.float32
BF16 = mybir.dt.bfloat16
AF = mybir.ActivationFunctionType
ALU = mybir.AluOpType
AX = mybir.AxisListType
NPBF = ml_dtypes.bfloat16

D = 1024
S_LEN = 8192
NT = 64
NCH = 16
DEPTH = 2
DFF = 4096
NEG = -30000.0
EPS = 1e-6

OFF = dict(mq=0, mk=256, mv=512, nq=768, nkc=1024, nvc=1088, nks=1152, nvs=1216, nkw=1280, nvw=1344,
           ng=1408, dq=1420, dk=1676, dv=1932, sq=2188, sk=2444, sv=2572)
FRr = dict(mq=0, mk=128, nq=256, nkc=512, nvc=576, nks=640, nkw=704, dq=768, dk=896, sq=1024, sk=1152)
NF = 1280
VCr = dict(mv=0, nvs=128, nvw=192, dv=256, sv=384, ng=448)
NV = 460
NCF = 16


def role_heads(r, m=0):
    if m == 3:
        return [2 * r, 2 * r + 1], [2 * (1 - r), 2 * (1 - r) + 1]
    return [r, r + 2], [1 - r, 3 - r]


def far_skip(m, j, min_dist):
    sl = min(SLOPES[4 * role_heads(0, m)[0][j] + m], SLOPES[4 * role_heads(1, m)[0][j] + m])
    return min_dist > 0 and sl * min_dist >= 200.0


def role_cols(r):
    own0 = role_heads(r, 0)[0]
    own1, oth1 = role_heads(r, 1)
    own2 = role_heads(r, 2)[0]
    own3 = role_heads(r, 3)[0]

    def hc(base, heads, w=64):
        return np.concatenate([np.arange(base + w * h, base + w * (h + 1)) for h in heads])

    f = np.concatenate([hc(OFF["mq"], own0), hc(OFF["mk"], own0), hc(OFF["nq"], own1 + oth1),
                        np.arange(OFF["nkc"], OFF["nkc"] + 64), np.arange(OFF["nvc"], OFF["nvc"] + 64),
                        np.arange(OFF["nks"], OFF["nks"] + 64), np.arange(OFF["nkw"], OFF["nkw"] + 64),
                        hc(OFF["dq"], own2), hc(OFF["dk"], own2), hc(OFF["sq"], own3), hc(OFF["sk"], [r])])
    v = np.concatenate([hc(OFF["mv"], own0), np.arange(OFF["nvs"], OFF["nvs"] + 64), np.arange(OFF["nvw"], OFF["nvw"] + 64),
                        hc(OFF["dv"], own2), hc(OFF["sv"], [r]), hc(OFF["ng"], own1 + oth1, 3)])
    assert len(f) == 1216 and len(v) == NV
    return f, v


SLOPES = np.power(np.float32(2.0), np.arange(1, 17, dtype=np.float32) * np.float32(-0.5)).astype(np.float64)


class Buf:
    __slots__ = ("name", "w", "rs", "dsem", "dcnt")

    def __init__(self, name):
        self.name = name
        self.w = None
        self.rs = []
        self.dsem = None
        self.dcnt = 0


class Sched:
    def __init__(self, nc, stack):
        self.nc = nc
        self.stack = stack
        self.eng = {"pe": nc.tensor, "act": nc.scalar, "dve": nc.vector, "pool": nc.gpsimd, "sp": nc.sync}
        self.sem, self.cnt, self.seen = {}, {}, {}
        for k in self.eng:
            self.sem[k] = stack.enter_context(nc.semaphore("s_" + k))
            self.cnt[k] = 0
            self.seen[k] = {}
        self.dma_seen = {k: {} for k in self.eng}
        self.free_sems = []
        self.live = []
        self.nsem = len(self.eng)
        self.ninstr = 0

    def _wait(self, ek, dep):
        if dep is None:
            return
        e = self.eng[ek]
        if dep[0] == "dma":
            _, sem, val = dep
            d = self.dma_seen[ek]
            if d.get(id(sem), 0) >= val:
                return
            e.wait_ge(sem, val)
            d[id(sem)] = val
        else:
            pk, c = dep
            if self.seen[ek].get(pk, 0) >= c:
                return
            e.wait_ge(self.sem[pk], c)
            self.seen[ek][pk] = c
        self.ninstr += 1

    def _deps(self, ek, reads, writes, accum):
        for b in reads:
            self._wait(ek, b.w)
        for b in writes:
            if not (accum and b.w is not None and b.w[0] == ek):
                self._wait(ek, b.w)
            for r in b.rs:
                self._wait(ek, r)

    def op(self, ek, fn, reads=(), writes=(), accum=False):
        self._deps(ek, reads, writes, accum)
        ins = fn()
        self.cnt[ek] += 1
        ins.then_inc(self.sem[ek], 1)
        tag = (ek, self.cnt[ek])
        for b in reads:
            b.rs.append(tag)
        for b in writes:
            b.w = tag
            b.rs = []
        self.ninstr += 1
        return ins

    def _dsem(self, b):
        if b.dsem is None:
            if self.free_sems:
                b.dsem, b.dcnt = self.free_sems.pop()
            else:
                b.dsem = self.stack.enter_context(self.nc.semaphore("d%d" % self.nsem))
                b.dcnt = 0
                self.nsem += 1
            self.live.append(b)

    def load(self, qk, out_ap, in_ap, buf):
        self._deps(qk, (), (buf,), False)
        self._dsem(buf)
        ins = self.eng[qk].dma_start(out=out_ap, in_=in_ap)
        buf.dcnt += 16
        ins.then_inc(buf.dsem, 16)
        buf.w = ("dma", buf.dsem, buf.dcnt)
        buf.rs = []
        self.ninstr += 1

    def store(self, qk, out_ap, in_ap, buf):
        self._deps(qk, (buf,), (), False)
        self._dsem(buf)
        ins = self.eng[qk].dma_start(out=out_ap, in_=in_ap)
        buf.dcnt += 16
        ins.then_inc(buf.dsem, 16)
        buf.rs.append(("dma", buf.dsem, buf.dcnt))
        self.ninstr += 1

    def barrier(self):
        for ek in self.eng:
            for pk in self.eng:
                if pk != ek and self.cnt[pk] > 0:
                    self._wait(ek, (pk, self.cnt[pk]))
            for b in self.live:
                self._wait(ek, ("dma", b.dsem, b.dcnt))
        for b in self.live:
            self.free_sems.append((b.dsem, b.dcnt))
            b.dsem = None
        self.live = []


class T:
    def __init__(self, nc, stack, name, shape, dtype, psum=False):
        alloc = nc.psum_tensor if psum else nc.sbuf_tensor
        self.t = stack.enter_context(alloc(name, shape, dtype))
        self.b = Buf(name)

    def __getitem__(self, idx):
        return self.t[idx]


def _split3(v):
    v = np.asarray(v, np.float64)
    hi = v.astype(NPBF)
    r = v - hi.astype(np.float64)
    mid = r.astype(NPBF)
    r = r - mid.astype(np.float64)
    lo = r.astype(NPBF)
    return hi, mid, lo


def make_consts(role=0):
    locs = [sum(role_heads(role, m), []) for m in range(4)]
    c = {}
    c["ident"] = np.eye(128, dtype=np.float32).astype(NPBF)
    iq = np.arange(512, dtype=np.float64)
    p = np.arange(128, dtype=np.float64)
    aug = np.zeros((16, 3, 512), NPBF)
    for m in range(4):
        scale = 32 ** -0.5 if m == 2 else 64 ** -0.5
        for j in range(4):
            sl = SLOPES[4 * locs[m][j] + m]
            hi, mid, lo = _split3(-sl * iq / scale)
            aug[4 * m + j, 0], aug[4 * m + j, 1], aug[4 * m + j, 2] = hi, mid, lo
    c["aug"] = aug
    kb = np.zeros((128, 16, 64), np.float32)
    r = np.arange(-60, 4, dtype=np.float64)
    for m in range(4):
        for j in range(4):
            sl = SLOPES[4 * locs[m][j] + m]
            kb[:, 4 * m + j, :] = (sl * (p[:, None] + 128.0 * r[None, :])).astype(np.float32)
    c["kb"] = kb
    kbc = np.zeros((128, 4, 16, 4), np.float32)
    for j in range(4):
        sl = SLOPES[4 * locs[1][j] + 1]
        for cc in range(16):
            for kt in range(4):
                kbc[:, j, cc, kt] = (sl * (16.0 * (128 * kt + p) + 31.0 - 512.0 * cc)).astype(np.float32)
    c["kbc"] = kbc
    P = p[:, None, None]
    Q = iq[None, None, :]
    k4 = np.arange(4, dtype=np.float64)[None, :, None]
    c["caus"] = np.where(Q >= 128 * k4 + P, 0.0, NEG).astype(np.float32).astype(NPBF)
    r8 = (np.arange(8, dtype=np.float64) - 4)[None, :, None]
    dd = Q - 128 * r8 - P
    c["wmask"] = np.where((dd >= 0) & (dd < 512), 0.0, NEG).astype(np.float32).astype(NPBF)
    r5 = (np.arange(5, dtype=np.float64) - 1)[None, :, None]
    dd = Q - 128 * r5 - P
    c["swamask"] = np.where((dd >= 0) & (dd < 128), 0.0, NEG).astype(np.float32).astype(NPBF)
    d5 = (512.0 * np.arange(5, dtype=np.float64))[None, :, None]
    c["cmask"] = np.where(16 * P + 31 <= d5 + Q, 0.0, NEG).astype(np.float32).astype(NPBF)
    j = np.arange(S_LEN)
    c["em"] = (j[None, :] // 256 == np.arange(32)[:, None]).astype(np.float32).astype(NPBF)
    c["es"] = (j[None, :] // 64 == np.arange(128)[:, None]).astype(np.float32).astype(NPBF)
    ncmp, nsel = 511, 128
    cs = np.arange(512)[:, None] * 16
    bs = np.arange(nsel)[None, :] * 64
    ov = np.clip(np.minimum(cs + 32, bs + 64) - np.maximum(cs, bs), 0, None) / 32.0
    ov[ncmp:, :] = 0.0
    c["ov"] = ov.reshape(4, 128, 128).transpose(1, 0, 2).astype(np.float32).astype(NPBF)
    n32 = np.arange(32, dtype=np.float32)
    qi = np.arange(4)
    c["bi"] = np.broadcast_to((n32[None, None, :] - (qi // 2)[None, :, None].astype(np.float32)), (128, 4, 32)).astype(np.float32).copy()
    s128 = np.arange(128)[None, None, None, :]
    cc = np.arange(16)[:, None, None, None]
    pp = np.arange(128)[None, :, None, None]
    qq = np.arange(4)[None, None, :, None]
    qblk = 8 * cc + 2 * qq + (pp >= 64)
    forced = (s128 == 0) | (s128 == qblk) | (s128 == qblk - 1)
    valid = s128 <= qblk
    c["f1e4"] = np.where(forced, 1e4, 0.0).astype(np.float32)
    c["negv"] = np.where(valid, 0.0, -1e30).astype(np.float32)
    c["ones3"] = np.ones((3, 4 * S_LEN), np.float32).astype(NPBF)
    return c


CONST_SPECS = [("ident", [128, 128], BF16), ("aug", [16, 3, 512], BF16), ("kb", [128, 16, 64], F32),
               ("kbc", [128, 4, 16, 4], F32), ("caus", [128, 4, 512], BF16), ("wmask", [128, 8, 512], BF16),
               ("swamask", [128, 5, 512], BF16), ("cmask", [128, 5, 512], BF16), ("em", [32, S_LEN], BF16),
               ("es", [128, S_LEN], BF16), ("ov", [128, 4, 128], BF16), ("bi", [128, 4, 32], F32),
               ("f1e4", [16, 128, 4, 128], F32), ("negv", [16, 128, 4, 128], F32), ("ones3", [3, 4 * S_LEN], BF16)]

PARAM_SPECS = [("wf", [DEPTH, 128, 8, NF]), ("wv", [DEPTH, 128, 8, NV]), ("wo", [DEPTH, 128, 8, D]),
               ("wg", [DEPTH, NCF, 128, 8, 128]), ("wu", [DEPTH, NCF, 128, 8, 128]), ("wd", [DEPTH, NCF, 128, D]),
               ("g_apre", [DEPTH, D]), ("g_apost", [DEPTH, D]), ("g_fpre", [DEPTH, D]), ("g_fpost", [DEPTH, D]),
               ("cw", [DEPTH, 128, NCF, 3]), ("cb", [DEPTH, 128, NCF]),
               ("posk", [DEPTH, 64, 32]), ("w1k", [DEPTH, 64, 32, 128]), ("b1k", [DEPTH, 128, 1]), ("w2k", [DEPTH, 128, 64]),
               ("posv", [DEPTH, 64, 32]), ("w1v", [DEPTH, 64, 32, 128]), ("b1v", [DEPTH, 128, 1]), ("w2v", [DEPTH, 128, 64]),
               ("lq1", [DEPTH, 32]), ("lk1", [DEPTH, 32]), ("lq2", [DEPTH, 32]), ("lk2", [DEPTH, 32]),
               ("subln", [DEPTH, 64]), ("sinks", [DEPTH, 4])]


def layout_params(inp, role=0):
    o = {}
    w_in = inp["w_in"]
    fc, vc = role_cols(role)
    wf = np.zeros((DEPTH, D, NF), np.float32)
    wf[:, :, :len(fc)] = w_in[:, :, fc]
    o["wf"] = wf.reshape(DEPTH, 8, 128, NF).transpose(0, 2, 1, 3)
    o["wv"] = w_in[:, :, vc].reshape(DEPTH, 8, 128, NV).transpose(0, 2, 1, 3)
    rows = np.concatenate([np.arange(256 * m + 64 * h, 256 * m + 64 * (h + 1)) for rr in range(2) for m in range(4)
                           for h in role_heads(rr, m)[0]])
    o["wo"] = inp["w_out"][:, rows, :].reshape(DEPTH, 8, 128, D).transpose(0, 2, 1, 3)
    cfs = slice(NCF * role, NCF * (role + 1))
    o["wg"] = inp["ffn_w_gate"].reshape(DEPTH, 8, 128, 32, 128).transpose(0, 3, 2, 1, 4)[:, cfs]
    o["wu"] = inp["ffn_w_up"].reshape(DEPTH, 8, 128, 32, 128).transpose(0, 3, 2, 1, 4)[:, cfs]
    o["wd"] = inp["ffn_w_down"].reshape(DEPTH, 32, 128, D)[:, cfs]
    o["g_apre"], o["g_apost"] = inp["attn_pre_norm"], inp["attn_post_norm"]
    o["g_fpre"], o["g_fpost"] = inp["ffn_pre_norm"], inp["ffn_post_norm"]
    o["cw"] = inp["ffn_conv_w"].reshape(DEPTH, 3, 32, 128).transpose(0, 3, 2, 1)[:, :, cfs, :]
    o["cb"] = inp["ffn_conv_b"].reshape(DEPTH, 32, 128).transpose(0, 2, 1)[:, :, cfs]
    for sfx in "kv":
        o["pos" + sfx] = inp["nsa_cmp_pos_" + sfx].transpose(0, 2, 1)
        o["w1" + sfx] = inp["nsa_cmp_w1_" + sfx].transpose(0, 2, 1, 3)
        o["b1" + sfx] = inp["nsa_cmp_b1_" + sfx].reshape(DEPTH, 128, 1)
        o["w2" + sfx] = inp["nsa_cmp_w2_" + sfx]
    o["lq1"], o["lk1"] = inp["diff_lambda_q1"], inp["diff_lambda_k1"]
    o["lq2"], o["lk2"] = inp["diff_lambda_q2"], inp["diff_lambda_k2"]
    o["subln"] = inp["diff_subln"]
    o["sinks"] = inp["swa_sinks"][:, sum(role_heads(role, 3), [])]
    return {k: np.ascontiguousarray(v, dtype=np.float32) for k, v in o.items()}


class Prog:
    def __init__(self, phases=("A", "B", "C"), layers=(0, 1), mixers=(0, 1, 2, 3), debug=False, nchunks=NCH, ncores=8):
        self.phases, self.layers, self.mixers, self.debug, self.nch = phases, layers, mixers, debug, nchunks
        self.groups = [[2 * i, 2 * i + 1] for i in range(ncores // 2)]
        nc = bass.Bass("TRN2", target_bir_lowering=False)
        self.nc = nc
        self.x_in = nc.dram_tensor("x", [S_LEN, D], F32, kind="ExternalInput").ap()
        self.y = nc.dram_tensor("y", [S_LEN, D], F32, kind="ExternalOutput").ap()
        self.dc = {n: nc.dram_tensor("c_" + n, shp, dt, kind="ExternalInput").ap() for n, shp, dt in CONST_SPECS}
        self.dp = {n: nc.dram_tensor("p_" + n, shp, F32, kind="ExternalInput").ap() for n, shp in PARAM_SPECS}
        kind = "ExternalOutput" if debug else "Internal"
        self.featT = nc.dram_tensor("featT", [NF, S_LEN], BF16, kind=kind).ap()
        self.vtok = nc.dram_tensor("vtok", [S_LEN, NV], BF16, kind=kind).ap()
        self.mix_t = nc.dram_tensor("mix", [S_LEN, 512], BF16)
        self.mix = self.mix_t.ap()
        self.mixall_t = nc.dram_tensor("mixall", [2 * S_LEN, 512], BF16)
        self.mixall = self.mixall_t.ap()
        self.part = nc.dram_tensor("part", [S_LEN, D], F32).ap()
        self.red = nc.dram_tensor("red", [S_LEN, D], F32).ap()
        self.x1s = nc.dram_tensor("x1s", [S_LEN, D], F32).ap()
        self.xs = nc.dram_tensor("xs", [S_LEN, D], F32).ap()
        self.wgs = nc.dram_tensor("wgs", [NCF, 128, 8, 128], BF16).ap()
        self.wus = nc.dram_tensor("wus", [NCF, 128, 8, 128], BF16).ap()
        self.wds = nc.dram_tensor("wds", [NCF, 128, D], BF16).ap()
        self.cp_i = 0
        with ExitStack() as st:
            self.st = st
            self.S = Sched(nc, st)
            self.cc_sem = st.enter_context(nc.semaphore("cc_sem"))
            self.cc_cnt = 0
            self.build()
            print("built: instr", self.S.ninstr, "sems", self.S.nsem, flush=True)

    def collective(self, kind, op, in_ap, out_ap):
        ins = self.nc.gpsimd.collective_compute(kind, op, replica_groups=self.groups, ins=[in_ap], outs=[out_ap])
        self.cc_cnt += 1
        ins.then_inc(self.cc_sem, 1)
        self.S.ninstr += 1
        return ("dma", self.cc_sem, self.cc_cnt)

    def tile(self, stack, name, shape, dtype, psum=False):
        self.tile_i = getattr(self, "tile_i", 0) + 1
        return T(self.nc, stack, "%s_%d" % (name, self.tile_i), shape, dtype, psum)

    def copy(self, out_ap, in_ap, reads, writes, eng=None):
        nc = self.nc
        if eng is None:
            eng = "act" if (self.cp_i % 2 == 0) else "dve"
            self.cp_i += 1
        if eng == "act":
            self.S.op("act", lambda: nc.scalar.activation(out=out_ap, in_=in_ap, func=AF.Copy), reads=reads, writes=writes)
        elif eng == "dve":
            self.S.op("dve", lambda: nc.vector.tensor_copy(out=out_ap, in_=in_ap), reads=reads, writes=writes)
        else:
            self.S.op("pool", lambda: nc.gpsimd.tensor_copy(out=out_ap, in_=in_ap), reads=reads, writes=writes)

    def load_cast(self, stack, dst, dst_ap_fn, src_ap_fn, n, ncols):
        stg = [self.tile(stack, "stg%d_%d" % (i, self.S.ninstr), [128, ncols], F32) for i in range(2)]
        for i in range(n):
            s = stg[i % 2]
            self.S.load("sp", s[:, :], src_ap_fn(i), s.b)
            self.copy(dst_ap_fn(i), s[:, :], [s.b], [dst.b])

    def build(self):
        nc, S, st = self.nc, self.S, self.st
        self.ident = self.tile(st, "ident", [128, 128], BF16)
        self.caus = self.tile(st, "caus", [128, 4, 512], BF16)
        self.kb = self.tile(st, "kb", [128, 16, 64], F32)
        self.epsT = self.tile(st, "epsT", [128, 1], F32)
        S.load("sp", self.ident[:, :], self.dc["ident"], self.ident.b)
        S.load("sp", self.caus[:, :, :], self.dc["caus"], self.caus.b)
        S.load("sp", self.kb[:, :, :], self.dc["kb"], self.kb.b)
        S.op("dve", lambda: nc.vector.memset(self.epsT[:, :], EPS), writes=[self.epsT.b])
        for l in self.layers:
            x_src = self.x_in if l == 0 else self.xs
            x_dst = self.y if l == self.layers[-1] else self.xs
            if "A" in self.phases:
                self.phase_A(l, x_src)
                S.barrier()
            if "B" in self.phases:
                for m in self.mixers:
                    [self.moba, self.nsa, self.diff, self.swa][m](l)
                    S.barrier()
            if "C" in self.phases:
                tags = []
                for k in range((self.nch * 512 + 2047) // 2048):
                    tags.append(self.collective("AllGather", ALU.bypass, self.mix[k * 2048:(k + 1) * 2048, :].opt(),
                                                self.mixall[k * 4096:(k + 1) * 4096, :].opt()))
                self.prep_ffn(l)
                for ek in S.eng:
                    S._wait(ek, tags[-1])
                S.barrier()
                self.phase_C(l, x_src, x_dst)
                S.barrier()
        S.barrier()
        if self.debug:
            nc = self.nc
            dbg = {}
            for nm, src, rows, cols, dt in (("d_mixall", self.mixall, 4096, 512, BF16), ("d_part", self.part, 2048, D, F32),
                                            ("d_red", self.red, 2048, D, F32), ("d_x1s", self.x1s, 2048, D, F32), ("d_mix", self.mix, 2048, 512, BF16)):
                dst = nc.dram_tensor(nm, [rows, cols], dt, kind="ExternalOutput").ap()
                b = Buf(nm)
                S._dsem(b)
                ins = nc.sync.dma_start(out=dst[:, :], in_=src[0:rows, :])
                b.dcnt += 16
                ins.then_inc(b.dsem, 16)
                S._wait("sp", ("dma", b.dsem, b.dcnt))

    def rstd_of(self, x_ap, xbuf, junk, ss, sd, rstd, n=D):
        nc, S = self.nc, self.S
        S.op("dve", lambda: nc.vector.scalar_tensor_tensor(out=junk[:, 0:n], in0=x_ap, scalar=1.0, in1=x_ap,
                                                           op0=ALU.mult, op1=ALU.mult, accum_out=ss[:, 0:1]),
             reads=[xbuf], writes=[junk.b, ss.b])
        S.op("act", lambda: nc.scalar.activation(out=sd[:, 0:1], in_=ss[:, 0:1], func=AF.Sqrt,
                                                 bias=self.epsT[:, 0:1], scale=1.0 / n),
             reads=[ss.b, self.epsT.b], writes=[sd.b])
        S.op("dve", lambda: nc.vector.reciprocal(out=rstd[:, 0:1], in_=sd[:, 0:1]), reads=[sd.b], writes=[rstd.b])

    def phase_A(self, l, x_src):
        nc, S = self.nc, self.S
        with ExitStack() as ph:
            WF = self.tile(ph, "WF", [128, 8, NF], BF16)
            WV = self.tile(ph, "WV", [128, 8, NV], BF16)
            gain = self.tile(ph, "gainA", [128, D], F32)
            with ExitStack() as tmp:
                self.load_cast(tmp, WF, lambda i: WF[:, i, :], lambda i: self.dp["wf"][l, :, i, :], 8, NF)
                self.load_cast(tmp, WV, lambda i: WV[:, i, :], lambda i: self.dp["wv"][l, :, i, :], 8, NV)
                S.barrier()
            S.load("sp", gain[:, :], self.dp["g_apre"][l].partition_broadcast(128), gain.b)
            xts = [self.tile(ph, "xtA%d" % i, [128, D], F32) for i in range(3)]
            junk = self.tile(ph, "junkA", [128, D], F32)
            ss = self.tile(ph, "ssA", [128, 1], F32)
            sd = self.tile(ph, "sdA", [128, 1], F32)
            rstd = self.tile(ph, "rstdA", [128, 1], F32)
            hb = [self.tile(ph, "hbA%d" % i, [128, D], BF16) for i in range(2)]
            hT = [self.tile(ph, "hTA%d" % i, [128, 8, 512], BF16) for i in range(2)]
            tp = [self.tile(ph, "tpA%d" % i, [128, D], BF16, psum=True) for i in range(2)]
            psF = [self.tile(ph, "psF%d" % i, [128, 512], F32, psum=True) for i in range(2)]
            psV0 = self.tile(ph, "psV0", [128, 512], F32, psum=True)
            fst = [self.tile(ph, "fst%d" % i, [128, 512], BF16) for i in range(3)]
            vst = [self.tile(ph, "vst%d" % i, [128, NV], BF16) for i in range(2)]
            it = 0
            for c in range(self.nch):
                hTc = hT[c % 2]
                for ti in range(4):
                    tok = (4 * c + ti) * 128
                    xt = xts[it % 3]
                    h = hb[it % 2]
                    tpp = tp[it % 2]
                    it += 1
                    S.load("sp", xt[:, :], x_src[tok:tok + 128, :], xt.b)
                    self.rstd_of(xt[:, :], xt.b, junk, ss, sd, rstd)
                    S.op("dve", lambda: nc.vector.scalar_tensor_tensor(out=h[:, :], in0=xt[:, :], scalar=rstd[:, 0:1],
                                                                       in1=gain[:, :], op0=ALU.mult, op1=ALU.mult),
                         reads=[xt.b, rstd.b, gain.b], writes=[h.b])
                    for kc in range(8):
                        S.op("pe", lambda: nc.tensor.transpose(out=tpp[:, kc * 128:(kc + 1) * 128],
                                                               in_=h[:, kc * 128:(kc + 1) * 128], identity=self.ident[:, :]),
                             reads=[h.b, self.ident.b], writes=[tpp.b], accum=(kc > 0))
                    self.copy(hTc[:, :, ti * 128:(ti + 1) * 128], tpp[:, :].rearrange("p (k t) -> p k t", k=8),
                              [tpp.b], [hTc.b])
                for g in range(NF // 128):
                    ps = psF[g % 2]
                    for kc in range(8):
                        S.op("pe", lambda: nc.tensor.matmul(ps[:, :], lhsT=WF[:, kc, g * 128:(g + 1) * 128], rhs=hTc[:, kc, :],
                                                            start=(kc == 0), stop=(kc == 7)),
                             reads=[WF.b, hTc.b], writes=[ps.b], accum=(kc > 0))
                    f = fst[g % 3]
                    self.copy(f[:, :], ps[:, :], [ps.b], [f.b])
                    S.store("pool", self.featT[g * 128:(g + 1) * 128, c * 512:(c + 1) * 512], f[:, :], f.b)
                for ti in range(4):
                    tok = (4 * c + ti) * 128
                    for kc in range(8):
                        S.op("pe", lambda: nc.tensor.matmul(psV0[:, 0:NV], lhsT=hTc[:, kc, ti * 128:(ti + 1) * 128], rhs=WV[:, kc, 0:NV],
                                                            start=(kc == 0), stop=(kc == 7)),
                             reads=[WV.b, hTc.b], writes=[psV0.b], accum=(kc > 0))
                    v = vst[ti % 2]
                    self.copy(v[:, 0:NV], psV0[:, 0:NV], [psV0.b], [v.b])
                    S.store("pool", self.vtok[tok:tok + 128, :], v[:, :], v.b)
            S.barrier()

    def attn_res(self, ph, nps=3, npt=3, no=3):
        R = type("R", (), {})()
        R.ps = [self.tile(ph, "ps_s%d" % i, [128, 512], F32, psum=True) for i in range(nps)]
        R.pt = [self.tile(ph, "pT%d" % i, [128, 512], BF16) for i in range(npt)]
        R.O = [self.tile(ph, "O%d" % i, [128, 512], F32, psum=True) for i in range(no)]
        R.ips = R.ipt = R.io = 0
        return R

    def attend(self, R, q_ap, qbufs, tiles, O, scale):
        nc, S = self.nc, self.S
        Ov = O[:, 0:260].rearrange("p (a b) -> p a b", a=4)
        n = len(tiles)

        def pv(pt, tl, first, last):
            for qi in range(4):
                S.op("pe", lambda: nc.tensor.matmul(Ov[:, qi, :], lhsT=pt[:, qi * 128:(qi + 1) * 128], rhs=tl["v"],
                                                    start=(first and qi == 0), stop=last),
                     reads=[pt.b] + tl["kbufs"], writes=[O.b], accum=not (first and qi == 0))
            if tl.get("post") is not None:
                tl["post"](pt, first, last)

        prev = None
        for i, tl in enumerate(tiles):
            ps = R.ps[R.ips % len(R.ps)]
            R.ips += 1
            ex = tl.get("extra", [])
            S.op("pe", lambda: nc.tensor.matmul(ps[:, :], lhsT=tl["kT"], rhs=q_ap, start=True, stop=(len(ex) == 0)),
                 reads=qbufs + tl["kbufs"], writes=[ps.b])
            for j, (lh, rh, bufs) in enumerate(ex):
                S.op("pe", lambda: nc.tensor.matmul(ps[:, :], lhsT=lh, rhs=rh, start=False, stop=(j == len(ex) - 1)),
                     reads=bufs, writes=[ps.b], accum=True)
            pt = R.pt[R.ipt % len(R.pt)]
            R.ipt += 1
            S.op("act", lambda: nc.scalar.activation(out=pt[:, :], in_=ps[:, :], func=AF.Exp, bias=tl["bias"], scale=scale),
                 reads=[ps.b] + tl["bbufs"], writes=[pt.b])
            if prev is not None:
                pv(prev[0], prev[1], prev[2] == 0, False)
            prev = (pt, tl, i)
        pv(prev[0], prev[1], prev[2] == 0, True)
        return Ov

    def load_kv(self, KT, V, krows, nh_k, k_row0, v_col0, nh_v, kd=64):
        nc, S = self.nc, self.S
        S.op("pool", lambda: nc.gpsimd.memset(V[:, :, :, 64:65], 1.0), writes=[V.b])
        for h in range(nh_k):
            S.load("sp", KT[0:kd, h, :], self.featT[k_row0 + h * kd:k_row0 + (h + 1) * kd, :], KT.b)
        S.load("sp", KT[kd:kd + 3, :, :], self.dc["ones3"][:, 0:nh_k * S_LEN].rearrange("r (h s) -> r h s", h=nh_k), KT.b)
        for h in range(nh_v):
            S.load("sp", V[:, h, :, 0:64],
                   self.vtok[:, v_col0 + h * 64:v_col0 + (h + 1) * 64].rearrange("(kt p) d -> p kt d", p=128), V.b)

    def recip_den(self, Ov, O, den, add_ap=None, add_bufs=()):
        nc, S = self.nc, self.S
        if add_ap is None:
            S.op("dve", lambda: nc.vector.tensor_scalar(out=den[:, 0:4], in0=Ov[:, :, 64], scalar1=1e-30, scalar2=None,
                                                        op0=ALU.max), reads=[O.b], writes=[den.b])
        else:
            S.op("dve", lambda: nc.vector.tensor_scalar(out=den[:, 0:4], in0=Ov[:, :, 64], scalar1=add_ap, scalar2=1e-30,
                                                        op0=ALU.add, op1=ALU.max), reads=[O.b] + list(add_bufs), writes=[den.b])
        S.op("dve", lambda: nc.vector.reciprocal(out=den[:, 0:4], in_=den[:, 0:4]), reads=[den.b], writes=[den.b])

    def moba(self, l):
        nc, S = self.nc, self.S
        with ExitStack() as ph:
            KT = self.tile(ph, "KTm", [128, 2, S_LEN], BF16)
            V = self.tile(ph, "Vm", [128, 2, NT, 65], BF16)
            EM = self.tile(ph, "EMm", [32, S_LEN], BF16)
            BI = self.tile(ph, "BIm", [128, 4, 32], F32)
            S.load("sp", EM[:, :], self.dc["em"], EM.b)
            S.load("sp", BI[:, :, :], self.dc["bi"], BI.b)
            self.load_kv(KT, V, 67, 2, FRr["mk"], VCr["mv"], 2)
            R = self.attn_res(ph)
            QT = [self.tile(ph, "QTm%d" % i, [128, 2, 512], BF16) for i in range(2)]
            for q in QT:
                S.load("sp", q[64:67, :, :], self.dc["aug"][0:2].rearrange("h r q -> r h q"), q.b)
            kms = self.tile(ph, "kms", [64, 2, 32], F32)
            kmh = self.tile(ph, "kmh", [64, 2, 32], BF16)
            kml = self.tile(ph, "kml", [64, 2, 32], BF16)
            kmr = self.tile(ph, "kmr", [64, 2, 32], F32)
            for h in range(2):
                S.op("dve", lambda: nc.vector.tensor_reduce(out=kms[:, h, :], in_=KT[0:64, h, :].rearrange("p (n k) -> p n k", k=256),
                                                            axis=AX.X, op=ALU.add), reads=[KT.b], writes=[kms.b])
            S.op("dve", lambda: nc.vector.tensor_scalar(out=kms[:, :, :], in0=kms[:, :, :], scalar1=1.0 / 256, scalar2=None, op0=ALU.mult),
                 reads=[kms.b], writes=[kms.b])
            S.op("dve", lambda: nc.vector.tensor_copy(out=kmh[:, :, :], in_=kms[:, :, :]), reads=[kms.b], writes=[kmh.b])
            S.op("dve", lambda: nc.vector.tensor_tensor(out=kmr[:, :, :], in0=kms[:, :, :], in1=kmh[:, :, :], op=ALU.subtract),
                 reads=[kms.b, kmh.b], writes=[kmr.b])
            S.op("dve", lambda: nc.vector.tensor_copy(out=kml[:, :, :], in_=kmr[:, :, :]), reads=[kmr.b], writes=[kml.b])
            gps = self.tile(ph, "gps", [128, 512], F32, psum=True)
            tps = self.tile(ph, "tpsm", [128, 1024], BF16, psum=True)
            past = self.tile(ph, "past", [128, 4, 32], F32)
            own = self.tile(ph, "own", [128, 4, 32], F32)
            negp = self.tile(ph, "negp", [128, 4, 32], F32)
            gm = self.tile(ph, "gm", [128, 4, 32], F32)
            m8 = self.tile(ph, "m8", [128, 4, 8], F32)
            sel = self.tile(ph, "sel", [128, 4, 32], F32)
            nsel = self.tile(ph, "nsel", [128, 4, 32], BF16)
            nselT = [self.tile(ph, "nselT%d" % i, [32, 512], BF16) for i in range(2)]
            den = self.tile(ph, "denm", [128, 4], F32)
            mst = [self.tile(ph, "mstm%d" % i, [128, 4, 128], BF16) for i in range(2)]
            ih = 0
            for c in range(self.nch):
                q = QT[c % 2]
                for h in range(2):
                    S.load("sp", q[0:64, h, :], self.featT[FRr["mq"] + 64 * h:FRr["mq"] + 64 * (h + 1), c * 512:(c + 1) * 512], q.b)
                S.op("dve", lambda: nc.vector.tensor_scalar(out=past[:, :, :], in0=BI[:, :, :], scalar1=float(2 * c), scalar2=None, op0=ALU.is_lt),
                     reads=[BI.b], writes=[past.b])
                S.op("dve", lambda: nc.vector.tensor_scalar(out=own[:, :, :], in0=BI[:, :, :], scalar1=float(2 * c), scalar2=None, op0=ALU.is_equal),
                     reads=[BI.b], writes=[own.b])
                S.op("dve", lambda: nc.vector.tensor_scalar(out=negp[:, :, :], in0=past[:, :, :], scalar1=-1.0, scalar2=1e30, op0=ALU.add, op1=ALU.mult),
                     reads=[past.b], writes=[negp.b])
                ms = mst[c % 2]
                for h in range(2):
                    gv = gps[:, 0:128].rearrange("p (a b) -> p a b", a=4)
                    for qi in range(4):
                        S.op("pe", lambda: nc.tensor.matmul(gv[:, qi, :], lhsT=q[0:64, h, qi * 128:(qi + 1) * 128], rhs=kmh[:, h, :], start=True, stop=False),
                             reads=[q.b, kmh.b], writes=[gps.b], accum=(qi > 0))
                        S.op("pe", lambda: nc.tensor.matmul(gv[:, qi, :], lhsT=q[0:64, h, qi * 128:(qi + 1) * 128], rhs=kml[:, h, :], start=False, stop=True),
                             reads=[q.b, kml.b], writes=[gps.b], accum=True)
                    S.op("dve", lambda: nc.vector.tensor_tensor(out=gm[:, :, :], in0=gv, in1=negp[:, :, :], op=ALU.add),
                         reads=[gps.b, negp.b], writes=[gm.b])
                    for qi in range(4):
                        S.op("dve", lambda: nc.vector.max(out=m8[:, qi, :], in_=gm[:, qi, :]), reads=[gm.b], writes=[m8.b])
                    for qi in range(4):
                        S.op("dve", lambda: nc.vector.tensor_scalar(out=sel[:, qi, :], in0=gm[:, qi, :], scalar1=m8[:, qi, 2:3], scalar2=None, op0=ALU.is_ge),
                             reads=[gm.b, m8.b], writes=[sel.b])
                    S.op("dve", lambda: nc.vector.tensor_tensor(out=sel[:, :, :], in0=sel[:, :, :], in1=past[:, :, :], op=ALU.mult),
                         reads=[sel.b, past.b], writes=[sel.b])
                    S.op("dve", lambda: nc.vector.tensor_tensor(out=sel[:, :, :], in0=sel[:, :, :], in1=own[:, :, :], op=ALU.add),
                         reads=[sel.b, own.b], writes=[sel.b])
                    S.op("dve", lambda: nc.vector.tensor_scalar(out=nsel[:, :, :], in0=sel[:, :, :], scalar1=-1.0, scalar2=-NEG, op0=ALU.add, op1=ALU.mult),
                         reads=[sel.b], writes=[nsel.b])
                    nT = nselT[ih % 2]
                    ih += 1
                    for qi in range(4):
                        S.op("pe", lambda: nc.tensor.transpose(out=tps[0:32, qi * 128:(qi + 1) * 128], in_=nsel[:, qi, :], identity=self.ident[:, :]),
                             reads=[nsel.b, self.ident.b], writes=[tps.b], accum=(qi > 0))
                    self.copy(nT[:, :], tps[0:32, 0:512], [tps.b], [nT.b], eng="dve")
                    tiles = []
                    for kt in range(4 * c + 4):
                        if far_skip(0, h, 512 * c - (128 * kt + 127)):
                            continue
                        r = kt - 4 * c
                        ex = [(EM[:, kt * 128:(kt + 1) * 128], nT[:, :], [EM.b, nT.b])]
                        if r >= 0:
                            ex.append((self.ident[:, :], self.caus[:, r, :], [self.ident.b, self.caus.b]))
                        tiles.append(dict(kT=KT[0:67, h, kt * 128:(kt + 1) * 128], v=V[:, h, kt, :], kbufs=[KT.b, V.b],
                                          bias=self.kb[:, 0 + h, r + 60:r + 61], bbufs=[self.kb.b], extra=ex))
                    O = R.O[R.io % len(R.O)]
                    R.io += 1
                    Ov = self.attend(R, q[0:67, h, :], [q.b], tiles, O, 0.125)
                    self.recip_den(Ov, O, den)
                    for qi in range(4):
                        S.op("dve", lambda: nc.vector.tensor_scalar(out=ms[:, qi, h * 64:(h + 1) * 64], in0=Ov[:, qi, 0:64], scalar1=den[:, qi:qi + 1],
                                                                    scalar2=None, op0=ALU.mult), reads=[O.b, den.b], writes=[ms.b])
                S.store("pool", self.mix[c * 512:(c + 1) * 512, 0:128].rearrange("(a p) d -> p a d", p=128), ms[:, :, :], ms.b)
            S.barrier()

    def swa(self, l):
        nc, S = self.nc, self.S
        with ExitStack() as ph:
            KT = self.tile(ph, "KTs", [128, 1, S_LEN], BF16)
            V = self.tile(ph, "Vs", [128, 1, NT, 65], BF16)
            SM = self.tile(ph, "SMs", [128, 5, 512], BF16)
            S.load("sp", SM[:, :, :], self.dc["swamask"], SM.b)
            self.load_kv(KT, V, 67, 1, FRr["sk"], VCr["sv"], 1)
            sk = self.tile(ph, "sinks", [128, 4], F32)
            esk = self.tile(ph, "esinks", [128, 4], F32)
            S.load("sp", sk[:, :], self.dp["sinks"][l].partition_broadcast(128), sk.b)
            S.op("act", lambda: nc.scalar.activation(out=esk[:, :], in_=sk[:, :], func=AF.Exp), reads=[sk.b], writes=[esk.b])
            R = self.attn_res(ph)
            QT = [self.tile(ph, "QTs%d" % i, [128, 2, 512], BF16) for i in range(2)]
            for q in QT:
                S.load("sp", q[64:67, :, :], self.dc["aug"][12:14].rearrange("h r q -> r h q"), q.b)
            den = self.tile(ph, "dens", [128, 4], F32)
            mst = [self.tile(ph, "msts%d" % i, [128, 4, 128], BF16) for i in range(2)]
            for c in range(self.nch):
                q = QT[c % 2]
                for h in range(2):
                    S.load("sp", q[0:64, h, :], self.featT[FRr["sq"] + 64 * h:FRr["sq"] + 64 * (h + 1), c * 512:(c + 1) * 512], q.b)
                ms = mst[c % 2]
                for h in range(2):
                    g = 0
                    tiles = []
                    for r in range(-1, 4):
                        kt = 4 * c + r
                        if kt < 0:
                            continue
                        tiles.append(dict(kT=KT[0:67, g, kt * 128:(kt + 1) * 128], v=V[:, g, kt, :], kbufs=[KT.b, V.b],
                                          bias=self.kb[:, 12 + h, r + 60:r + 61], bbufs=[self.kb.b],
                                          extra=[(self.ident[:, :], SM[:, r + 1, :], [self.ident.b, SM.b])]))
                    O = R.O[R.io % len(R.O)]
                    R.io += 1
                    Ov = self.attend(R, q[0:67, h, :], [q.b], tiles, O, 0.125)
                    self.recip_den(Ov, O, den, add_ap=esk[:, h:h + 1], add_bufs=[esk.b])
                    for qi in range(4):
                        S.op("dve", lambda: nc.vector.tensor_scalar(out=ms[:, qi, h * 64:(h + 1) * 64], in0=Ov[:, qi, 0:64], scalar1=den[:, qi:qi + 1],
                                                                    scalar2=None, op0=ALU.mult), reads=[O.b, den.b], writes=[ms.b])
                S.store("pool", self.mix[c * 512:(c + 1) * 512, 384:512].rearrange("(a p) d -> p a d", p=128), ms[:, :, :], ms.b)
            S.barrier()

    def diff(self, l):
        nc, S = self.nc, self.S
        lambda_init = 0.8 - 0.6 * math.exp(-0.3 * l)
        sc = 32 ** -0.5
        with ExitStack() as ph:
            KT = self.tile(ph, "KTd", [128, 2, S_LEN], BF16)
            V = self.tile(ph, "Vd", [128, 2, NT, 65], BF16)
            S.op("pool", lambda: nc.gpsimd.memset(V[:, :, :, 64:65], 1.0), writes=[V.b])
            for h in range(2):
                r0 = FRr["dk"] + 64 * h
                S.load("sp", KT[0:32, h, :], self.featT[r0:r0 + 32, :], KT.b)
                S.load("sp", KT[64:96, h, :], self.featT[r0 + 32:r0 + 64, :], KT.b)
                S.load("sp", V[:, h, :, 0:64], self.vtok[:, VCr["dv"] + h * 64:VCr["dv"] + (h + 1) * 64].rearrange("(kt p) d -> p kt d", p=128), V.b)
            ones = self.dc["ones3"][:, 0:2 * S_LEN].rearrange("r (h s) -> r h s", h=2)
            S.load("sp", KT[32:35, :, :], ones, KT.b)
            S.load("sp", KT[96:99, :, :], ones, KT.b)
            lam = self.tile(ph, "lam", [128, 4], F32)
            lt = self.tile(ph, "lamt", [128, 4, 32], F32)
            lj = self.tile(ph, "lamj", [128, 32], F32)
            for i, nme in enumerate(["lq1", "lk1", "lq2", "lk2"]):
                S.load("sp", lt[:, i, :], self.dp[nme][l].partition_broadcast(128), lt.b)
            for i in range(2):
                S.op("dve", lambda: nc.vector.scalar_tensor_tensor(out=lj[:, :], in0=lt[:, 2 * i, :], scalar=1.0, in1=lt[:, 2 * i + 1, :],
                                                                   op0=ALU.mult, op1=ALU.mult, accum_out=lam[:, i:i + 1]),
                     reads=[lt.b], writes=[lj.b, lam.b])
            S.op("act", lambda: nc.scalar.activation(out=lam[:, 0:2], in_=lam[:, 0:2], func=AF.Exp), reads=[lam.b], writes=[lam.b])
            S.op("dve", lambda: nc.vector.tensor_tensor(out=lam[:, 2:3], in0=lam[:, 1:2], in1=lam[:, 0:1], op=ALU.subtract),
                 reads=[lam.b], writes=[lam.b])
            S.op("dve", lambda: nc.vector.tensor_scalar(out=lam[:, 2:3], in0=lam[:, 2:3], scalar1=-lambda_init, scalar2=None, op0=ALU.add),
                 reads=[lam.b], writes=[lam.b])
            gs = self.tile(ph, "gsub", [128, 64], F32)
            S.load("sp", gs[:, :], self.dp["subln"][l].partition_broadcast(128), gs.b)
            S.op("dve", lambda: nc.vector.tensor_scalar(out=gs[:, :], in0=gs[:, :], scalar1=1.0 - lambda_init, scalar2=None, op0=ALU.mult),
                 reads=[gs.b], writes=[gs.b])
            R = self.attn_res(ph, no=4)
            QT = [self.tile(ph, "QTd%d" % i, [128, 2, 512], BF16) for i in range(2)]
            for q in QT:
                a = self.dc["aug"][8:10].rearrange("h r q -> r h q")
                S.load("sp", q[32:35, :, :], a, q.b)
                S.load("sp", q[96:99, :, :], a, q.b)
            den1 = self.tile(ph, "den1", [128, 4], F32)
            den2 = self.tile(ph, "den2", [128, 4], F32)
            o1 = self.tile(ph, "o1d", [128, 4, 64], F32)
            od = self.tile(ph, "od", [128, 4, 64], F32)
            jk = self.tile(ph, "jkd", [128, 64], F32)
            ssd = self.tile(ph, "ssd", [128, 4], F32)
            mst = [self.tile(ph, "mstd%d" % i, [128, 4, 128], BF16) for i in range(2)]
            for c in range(self.nch):
                q = QT[c % 2]
                for h in range(2):
                    r0 = FRr["dq"] + 64 * h
                    S.load("sp", q[0:32, h, :], self.featT[r0:r0 + 32, c * 512:(c + 1) * 512], q.b)
                    S.load("sp", q[64:96, h, :], self.featT[r0 + 32:r0 + 64, c * 512:(c + 1) * 512], q.b)
                ms = mst[c % 2]
                for h in range(2):
                    Os = []
                    for j in range(2):
                        b0 = 64 * j
                        tiles = []
                        for kt in range(4 * c + 4):
                            if far_skip(2, h, 512 * c - (128 * kt + 127)):
                                continue
                            r = kt - 4 * c
                            ex = []
                            if r >= 0:
                                ex.append((self.ident[:, :], self.caus[:, r, :], [self.ident.b, self.caus.b]))
                            tiles.append(dict(kT=KT[b0:b0 + 35, h, kt * 128:(kt + 1) * 128], v=V[:, h, kt, :], kbufs=[KT.b, V.b],
                                              bias=self.kb[:, 8 + h, r + 60:r + 61], bbufs=[self.kb.b], extra=ex))
                        O = R.O[R.io % len(R.O)]
                        R.io += 1
                        Ov = self.attend(R, q[b0:b0 + 35, h, :], [q.b], tiles, O, sc)
                        Os.append((O, Ov))
                    (O1, Ov1), (O2, Ov2) = Os
                    self.recip_den(Ov1, O1, den1)
                    self.recip_den(Ov2, O2, den2)
                    S.op("dve", lambda: nc.vector.tensor_scalar(out=den2[:, :], in0=den2[:, :], scalar1=lam[:, 2:3], scalar2=None, op0=ALU.mult),
                         reads=[den2.b, lam.b], writes=[den2.b])
                    for qi in range(4):
                        S.op("dve", lambda: nc.vector.tensor_scalar(out=o1[:, qi, :], in0=Ov1[:, qi, 0:64], scalar1=den1[:, qi:qi + 1], scalar2=None, op0=ALU.mult),
                             reads=[O1.b, den1.b], writes=[o1.b])
                        S.op("dve", lambda: nc.vector.scalar_tensor_tensor(out=od[:, qi, :], in0=Ov2[:, qi, 0:64], scalar=den2[:, qi:qi + 1], in1=o1[:, qi, :],
                                                                           op0=ALU.mult, op1=ALU.add), reads=[O2.b, den2.b, o1.b], writes=[od.b])
                        S.op("dve", lambda: nc.vector.scalar_tensor_tensor(out=jk[:, :], in0=od[:, qi, :], scalar=1.0, in1=od[:, qi, :],
                                                                           op0=ALU.mult, op1=ALU.mult, accum_out=ssd[:, qi:qi + 1]),
                             reads=[od.b], writes=[jk.b, ssd.b])
                    S.op("act", lambda: nc.scalar.activation(out=ssd[:, :], in_=ssd[:, :], func=AF.Ln, bias=self.epsT[:, 0:1], scale=1.0 / 64),
                         reads=[ssd.b, self.epsT.b], writes=[ssd.b])
                    S.op("act", lambda: nc.scalar.activation(out=ssd[:, :], in_=ssd[:, :], func=AF.Exp, scale=-0.5), reads=[ssd.b], writes=[ssd.b])
                    for qi in range(4):
                        S.op("dve", lambda: nc.vector.scalar_tensor_tensor(out=ms[:, qi, h * 64:(h + 1) * 64], in0=od[:, qi, :], scalar=ssd[:, qi:qi + 1],
                                                                           in1=gs[:, :], op0=ALU.mult, op1=ALU.mult),
                             reads=[od.b, ssd.b, gs.b], writes=[ms.b])
                S.store("pool", self.mix[c * 512:(c + 1) * 512, 256:384].rearrange("(a p) d -> p a d", p=128), ms[:, :, :], ms.b)
            S.barrier()

    def nsa(self, l):
        nc, S = self.nc, self.S
        with ExitStack() as ph:
            KCT = self.tile(ph, "KCT", [128, 512], BF16)
            VCt = self.tile(ph, "VCt", [128, 4, 65], BF16)
            S.op("pool", lambda: nc.gpsimd.memset(VCt[:, :, :], 1.0), writes=[VCt.b])
            S.load("sp", KCT[64:67, :], self.dc["ones3"][:, 0:512], KCT.b)
            with ExitStack() as cs:
                XT = self.tile(cs, "XTc", [64, S_LEN], BF16)
                w1 = self.tile(cs, "w1c", [64, 32, 128], BF16)
                w2 = self.tile(cs, "w2c", [128, 64], BF16)
                pos = self.tile(cs, "posc", [64, 32], BF16)
                b1 = self.tile(cs, "b1c", [128, 1], F32)
                cbias = self.tile(cs, "cbias", [128, 1], F32)
                hid = self.tile(cs, "hidc", [128, 512], BF16)
                sg = self.tile(cs, "sgc", [128, 4096], F32)
                hp = self.tile(cs, "hpc", [128, 512], F32, psum=True)
                cp = self.tile(cs, "cpc", [128, 512], F32, psum=True)
                op_ = self.tile(cs, "opc", [128, 512], F32, psum=True)
                for s_i, sfx in enumerate("kv"):
                    S.load("sp", XT[:, :], self.featT[FRr["nkc" if sfx == "k" else "nvc"]:FRr["nkc" if sfx == "k" else "nvc"] + 64, :], XT.b)
                    S.load("sp", sg[0:64, 0:4096], self.dp["w1" + sfx][l].rearrange("d l f -> d (l f)"), sg.b)
                    self.copy(w1[:, :, :], sg[0:64, 0:4096].rearrange("d (l f) -> d l f", l=32), [sg.b], [w1.b])
                    S.load("sp", sg[:, 0:64], self.dp["w2" + sfx][l], sg.b)
                    self.copy(w2[:, :], sg[:, 0:64], [sg.b], [w2.b])
                    S.load("sp", sg[0:64, 0:32], self.dp["pos" + sfx][l], sg.b)
                    self.copy(pos[:, :], sg[0:64, 0:32], [sg.b], [pos.b])
                    S.load("sp", b1[:, :], self.dp["b1" + sfx][l], b1.b)
                    for li in range(32):
                        S.op("pe", lambda: nc.tensor.matmul(hp[:, 0:511], lhsT=w1[:, li, :], rhs=XT[:, li:li + 16 * 510 + 1:16], start=(li == 0), stop=(li == 31)),
                             reads=[w1.b, XT.b], writes=[hp.b], accum=(li > 0))
                    for li in range(32):
                        S.op("pe", lambda: nc.tensor.matmul(cp[:, 0:1], lhsT=w1[:, li, :], rhs=pos[:, li:li + 1], start=(li == 0), stop=(li == 31)),
                             reads=[w1.b, pos.b], writes=[cp.b], accum=(li > 0))
                    S.op("dve", lambda: nc.vector.tensor_tensor(out=cbias[:, :], in0=cp[:, 0:1], in1=b1[:, :], op=ALU.add),
                         reads=[cp.b, b1.b], writes=[cbias.b])
                    S.op("pool", lambda: nc.gpsimd.memset(hid[:, :], 0.0), writes=[hid.b])
                    S.op("act", lambda: nc.scalar.activation(out=hid[:, 0:511], in_=hp[:, 0:511], func=AF.Gelu_apprx_tanh, bias=cbias[:, 0:1]),
                         reads=[hp.b, cbias.b], writes=[hid.b])
                    if sfx == "k":
                        S.op("pe", lambda: nc.tensor.matmul(op_[0:64, 0:512], lhsT=w2[:, :], rhs=hid[:, :], start=True, stop=True),
                             reads=[w2.b, hid.b], writes=[op_.b])
                        self.copy(KCT[0:64, :], op_[0:64, 0:512], [op_.b], [KCT.b])
                    else:
                        ov_ = op_[:, 0:256].rearrange("p (a b) -> p a b", a=4)
                        for kt in range(4):
                            S.op("pe", lambda: nc.tensor.matmul(ov_[:, kt, :], lhsT=hid[:, kt * 128:(kt + 1) * 128], rhs=w2[:, :], start=True, stop=True),
                                 reads=[w2.b, hid.b], writes=[op_.b], accum=(kt > 0))
                        self.copy(VCt[:, :, 0:64], ov_, [op_.b], [VCt.b])
                S.barrier()
            KTs = self.tile(ph, "KTsl", [128, 1, S_LEN], BF16)
            KTw = self.tile(ph, "KTwn", [128, 1, S_LEN], BF16)
            Vs = self.tile(ph, "Vsl", [128, 1, NT, 65], BF16)
            Vw = self.tile(ph, "Vwn", [128, 1, NT, 65], BF16)
            self.load_kv(KTs, Vs, 67, 1, FRr["nks"], VCr["nvs"], 1)
            self.load_kv(KTw, Vw, 67, 1, FRr["nkw"], VCr["nvw"], 1)
            ES = self.tile(ph, "ESn", [128, S_LEN], BF16)
            WM = self.tile(ph, "WMn", [128, 8, 512], BF16)
            CM = self.tile(ph, "CMn", [128, 5, 512], BF16)
            OVt = self.tile(ph, "OVn", [128, 4, 128], BF16)
            KBC = self.tile(ph, "KBCn", [128, 4, 16, 4], F32)
            S.load("sp", ES[:, :], self.dc["es"], ES.b)
            S.load("sp", WM[:, :, :], self.dc["wmask"], WM.b)
            S.load("sp", CM[:, :, :], self.dc["cmask"], CM.b)
            S.load("sp", OVt[:, :, :], self.dc["ov"], OVt.b)
            S.load("sp", KBC[:, :, :, :], self.dc["kbc"], KBC.b)
            R = self.attn_res(ph)
            QT = [self.tile(ph, "QTn%d" % i, [128, 4, 512], BF16) for i in range(2)]
            for q in QT:
                S.load("sp", q[64:67, :, :], self.dc["aug"][4:8].rearrange("h r q -> r h q"), q.b)
            imps = self.tile(ph, "imps", [128, 512], F32, psum=True)
            tps = self.tile(ph, "tpsn", [128, 1024], BF16, psum=True)
            impa = self.tile(ph, "impa", [128, 4, 128], F32)
            f1 = [self.tile(ph, "f1e4_%d" % i, [128, 4, 128], F32) for i in range(2)]
            nv = [self.tile(ph, "negv_%d" % i, [128, 4, 128], F32) for i in range(2)]
            gr = [self.tile(ph, "graw%d" % i, [128, 4, 12], BF16) for i in range(2)]
            sgm = self.tile(ph, "sgm", [128, 4, 12], F32)
            scm = self.tile(ph, "scm", [128, 4, 128], F32)
            tmpm = self.tile(ph, "tmpm", [128, 4, 128], F32)
            m8a = self.tile(ph, "m8a", [128, 4, 8], F32)
            m8b = self.tile(ph, "m8b", [128, 4, 8], F32)
            sel = self.tile(ph, "seln", [128, 4, 128], F32)
            val = self.tile(ph, "valn", [128, 4, 128], F32)
            nsel = self.tile(ph, "nseln", [128, 4, 128], BF16)
            nselT = self.tile(ph, "nselTn", [128, 512], BF16)
            den = self.tile(ph, "denn", [128, 4], F32)
            acc = self.tile(ph, "accn", [128, 2, 4, 64], F32)
            mst = [self.tile(ph, "mstn%d" % i, [128, 4, 128], BF16) for i in range(2)]

            def fold(O, Ov, h, gcol, first):
                self.recip_den(Ov, O, den)
                S.op("dve", lambda: nc.vector.tensor_tensor(out=den[:, :], in0=den[:, :], in1=sgm[:, :, gcol], op=ALU.mult),
                     reads=[den.b, sgm.b], writes=[den.b])
                for qi in range(4):
                    if first:
                        S.op("dve", lambda: nc.vector.tensor_scalar(out=acc[:, h, qi, :], in0=Ov[:, qi, 0:64], scalar1=den[:, qi:qi + 1], scalar2=None, op0=ALU.mult),
                             reads=[O.b, den.b], writes=[acc.b])
                    else:
                        S.op("dve", lambda: nc.vector.scalar_tensor_tensor(out=acc[:, h, qi, :], in0=Ov[:, qi, 0:64], scalar=den[:, qi:qi + 1], in1=acc[:, h, qi, :],
                                                                           op0=ALU.mult, op1=ALU.add), reads=[O.b, den.b, acc.b], writes=[acc.b])

            for c in range(self.nch):
                q = QT[c % 2]
                for h in range(4):
                    S.load("sp", q[0:64, h, :], self.featT[FRr["nq"] + 64 * h:FRr["nq"] + 64 * (h + 1), c * 512:(c + 1) * 512], q.b)
                f1c, nvc, grc = f1[c % 2], nv[c % 2], gr[c % 2]
                S.load("sp", f1c[:, :, :], self.dc["f1e4"][c], f1c.b)
                S.load("sp", nvc[:, :, :], self.dc["negv"][c], nvc.b)
                S.load("sp", grc[:, :, :], self.vtok[c * 512:(c + 1) * 512, VCr["ng"]:VCr["ng"] + 12].rearrange("(a p) g -> p a g", p=128), grc.b)
                S.op("act", lambda: nc.scalar.activation(out=sgm[:, :, :], in_=grc[:, :, :], func=AF.Exp, scale=-1.0), reads=[grc.b], writes=[sgm.b])
                S.op("dve", lambda: nc.vector.tensor_scalar(out=sgm[:, :, :], in0=sgm[:, :, :], scalar1=1.0, scalar2=None, op0=ALU.add),
                     reads=[sgm.b], writes=[sgm.b])
                S.op("dve", lambda: nc.vector.reciprocal(out=sgm[:, :, :], in_=sgm[:, :, :]), reads=[sgm.b], writes=[sgm.b])
                ms = mst[c % 2]
                kb_ = c // 4
                iv = imps[:, :].rearrange("p (a b) -> p a b", a=4)
                for h in range(4):
                    tiles = []
                    ntl = kb_ + 1

                    def mkpost(kt, ntl=ntl):
                        def post(pt, first, last):
                            for qi in range(4):
                                S.op("pe", lambda: nc.tensor.matmul(iv[:, qi, :], lhsT=pt[:, qi * 128:(qi + 1) * 128], rhs=OVt[:, kt, :],
                                                                    start=(first and qi == 0), stop=last),
                                     reads=[pt.b, OVt.b], writes=[imps.b], accum=not (first and qi == 0))
                        return post
                    for kt in range(ntl):
                        ex = []
                        if kt == kb_:
                            ex.append((self.ident[:, :], CM[:, c % 4, :], [self.ident.b, CM.b]))
                        elif kt == kb_ - 1 and c % 4 == 0:
                            ex.append((self.ident[:, :], CM[:, 4, :], [self.ident.b, CM.b]))
                        tiles.append(dict(kT=KCT[0:67, kt * 128:(kt + 1) * 128], v=VCt[:, kt, :], kbufs=[KCT.b, VCt.b],
                                          bias=KBC[:, h, c, kt:kt + 1], bbufs=[KBC.b], extra=ex, post=mkpost(kt)))
                    O = R.O[R.io % len(R.O)]
                    R.io += 1
                    Ov = self.attend(R, q[0:67, h, :], [q.b], tiles, O, 0.125)
                    self.recip_den(Ov, O, den)
                    for qi in range(4):
                        if h == 0:
                            S.op("dve", lambda: nc.vector.tensor_scalar(out=impa[:, qi, :], in0=iv[:, qi, :], scalar1=den[:, qi:qi + 1], scalar2=None, op0=ALU.mult),
                                 reads=[imps.b, den.b], writes=[impa.b])
                        else:
                            S.op("dve", lambda: nc.vector.scalar_tensor_tensor(out=impa[:, qi, :], in0=iv[:, qi, :], scalar=den[:, qi:qi + 1], in1=impa[:, qi, :],
                                                                               op0=ALU.mult, op1=ALU.add), reads=[imps.b, den.b, impa.b], writes=[impa.b])
                    if h < 2:
                        fold(O, Ov, h, 3 * h + 0, True)
                S.op("dve", lambda: nc.vector.tensor_tensor(out=scm[:, :, :], in0=impa[:, :, :], in1=f1c[:, :, :], op=ALU.max),
                     reads=[impa.b, f1c.b], writes=[scm.b])
                S.op("dve", lambda: nc.vector.tensor_tensor(out=scm[:, :, :], in0=scm[:, :, :], in1=nvc[:, :, :], op=ALU.add),
                     reads=[scm.b, nvc.b], writes=[scm.b])
                for qi in range(4):
                    S.op("dve", lambda: nc.vector.max(out=m8a[:, qi, :], in_=scm[:, qi, :]), reads=[scm.b], writes=[m8a.b])
                    S.op("dve", lambda: nc.vector.match_replace(out=tmpm[:, qi, :], in_to_replace=m8a[:, qi, :], in_values=scm[:, qi, :], imm_value=-1e30),
                         reads=[scm.b, m8a.b], writes=[tmpm.b])
                    S.op("dve", lambda: nc.vector.max(out=m8b[:, qi, :], in_=tmpm[:, qi, :]), reads=[tmpm.b], writes=[m8b.b])
                    S.op("dve", lambda: nc.vector.tensor_scalar(out=sel[:, qi, :], in0=scm[:, qi, :], scalar1=m8b[:, qi, 7:8], scalar2=None, op0=ALU.is_ge),
                         reads=[scm.b, m8b.b], writes=[sel.b])
                S.op("dve", lambda: nc.vector.tensor_scalar(out=val[:, :, :], in0=nvc[:, :, :], scalar1=-1.0, scalar2=None, op0=ALU.is_ge),
                     reads=[nvc.b], writes=[val.b])
                S.op("dve", lambda: nc.vector.tensor_tensor(out=sel[:, :, :], in0=sel[:, :, :], in1=val[:, :, :], op=ALU.mult),
                     reads=[sel.b, val.b], writes=[sel.b])
                S.op("dve", lambda: nc.vector.tensor_scalar(out=nsel[:, :, :], in0=sel[:, :, :], scalar1=-1.0, scalar2=-NEG, op0=ALU.add, op1=ALU.mult),
                     reads=[sel.b], writes=[nsel.b])
                for qi in range(4):
                    S.op("pe", lambda: nc.tensor.transpose(out=tps[:, qi * 128:(qi + 1) * 128], in_=nsel[:, qi, :], identity=self.ident[:, :]),
                         reads=[nsel.b, self.ident.b], writes=[tps.b], accum=(qi > 0))
                self.copy(nselT[:, :], tps[:, 0:512], [tps.b], [nselT.b], eng="dve")
                for h in range(2):
                    tiles = []
                    for kt in range(4 * c + 4):
                        if far_skip(1, h, 512 * c - (128 * kt + 127)):
                            continue
                        r = kt - 4 * c
                        ex = [(ES[:, kt * 128:(kt + 1) * 128], nselT[:, :], [ES.b, nselT.b])]
                        if r >= 0:
                            ex.append((self.ident[:, :], self.caus[:, r, :], [self.ident.b, self.caus.b]))
                        tiles.append(dict(kT=KTs[0:67, 0, kt * 128:(kt + 1) * 128], v=Vs[:, 0, kt, :], kbufs=[KTs.b, Vs.b],
                                          bias=self.kb[:, 4 + h, r + 60:r + 61], bbufs=[self.kb.b], extra=ex))
                    O = R.O[R.io % len(R.O)]
                    R.io += 1
                    Ov = self.attend(R, q[0:67, h, :], [q.b], tiles, O, 0.125)
                    fold(O, Ov, h, 3 * h + 1, False)
                    tiles = []
                    for r in range(-4, 4):
                        kt = 4 * c + r
                        if kt < 0:
                            continue
                        tiles.append(dict(kT=KTw[0:67, 0, kt * 128:(kt + 1) * 128], v=Vw[:, 0, kt, :], kbufs=[KTw.b, Vw.b],
                                          bias=self.kb[:, 4 + h, r + 60:r + 61], bbufs=[self.kb.b],
                                          extra=[(self.ident[:, :], WM[:, r + 4, :], [self.ident.b, WM.b])]))
                    O = R.O[R.io % len(R.O)]
                    R.io += 1
                    Ov = self.attend(R, q[0:67, h, :], [q.b], tiles, O, 0.125)
                    fold(O, Ov, h, 3 * h + 2, False)
                    S.op("dve", lambda: nc.vector.tensor_copy(out=ms[:, :, h * 64:(h + 1) * 64], in_=acc[:, h, :, :]), reads=[acc.b], writes=[ms.b])
                S.store("pool", self.mix[c * 512:(c + 1) * 512, 128:256].rearrange("(a p) d -> p a d", p=128), ms[:, :, :], ms.b)
            S.barrier()

    def prep_ffn(self, l):
        S = self.S
        with ExitStack() as ph:
            stg = [self.tile(ph, "pstg%d" % i, [128, 4096], F32) for i in range(2)]
            sb = [self.tile(ph, "psb%d" % i, [128, 4096], BF16) for i in range(2)]
            i = 0
            for src, dst in ((self.dp["wg"], self.wgs), (self.dp["wu"], self.wus), (self.dp["wd"], self.wds)):
                for g in range(NCF // 4):
                    s_, b_ = stg[i % 2], sb[i % 2]
                    i += 1
                    if len(src.shape) == 5:
                        sap = src[l, 4 * g:4 * g + 4].rearrange("c p k n -> p c (k n)")
                        dap = dst[4 * g:4 * g + 4].rearrange("c p k n -> p c (k n)")
                    else:
                        sap = src[l, 4 * g:4 * g + 4].rearrange("c p n -> p c n")
                        dap = dst[4 * g:4 * g + 4].rearrange("c p n -> p c n")
                    S.load("sp", s_[:, :].rearrange("p (c n) -> p c n", c=4), sap, s_.b)
                    self.copy(b_[:, :], s_[:, :], [s_.b], [b_.b])
                    S.store("pool", dap, b_[:, :].rearrange("p (c n) -> p c n", c=4), b_.b)
            S.barrier()

    def phase_C(self, l, x_src, x_dst):
        nc, S = self.nc, self.S
        with ExitStack() as ph:
            WO = self.tile(ph, "WO", [128, 8, D], BF16)
            with ExitStack() as tmp:
                self.load_cast(tmp, WO, lambda i: WO[:, i, :], lambda i: self.dp["wo"][l, :, i, :], 8, D)
                S.barrier()
            g1 = self.tile(ph, "g_apost", [128, D], F32)
            g2 = self.tile(ph, "g_fpre", [128, D], F32)
            g3 = self.tile(ph, "g_fpost", [128, D], F32)
            S.load("sp", g1[:, :], self.dp["g_apost"][l].partition_broadcast(128), g1.b)
            S.load("sp", g2[:, :], self.dp["g_fpre"][l].partition_broadcast(128), g2.b)
            S.load("sp", g3[:, :], self.dp["g_fpost"][l].partition_broadcast(128), g3.b)
            cw = self.tile(ph, "cw", [128, NCF, 3], F32)
            cb = self.tile(ph, "cb", [128, NCF], F32)
            S.load("sp", cw[:, :, :], self.dp["cw"][l], cw.b)
            S.load("sp", cb[:, :], self.dp["cb"][l], cb.b)
            halo = self.tile(ph, "halo", [128, NCF, 2], F32)
            S.op("pool", lambda: nc.gpsimd.memset(halo[:, :, :], 0.0), writes=[halo.b])
            bank = [self.tile(ph, "bk%d" % i, [128, 512], F32, psum=True) for i in range(6)]
            tpb = [self.tile(ph, "tpC%d" % i, [128, D], BF16, psum=True) for i in range(2)]
            mixt = [self.tile(ph, "mixt%d" % i, [128, D], BF16) for i in range(2)]
            mT = [self.tile(ph, "mT%d" % i, [128, 8, 128], BF16) for i in range(2)]
            xt = [self.tile(ph, "xtC%d" % i, [128, D], F32) for i in range(2)]
            x1 = [self.tile(ph, "x1C%d" % i, [128, D], F32) for i in range(2)]
            yt = self.tile(ph, "ytC", [128, D], F32)
            junk = self.tile(ph, "junkC", [128, D], F32)
            ss = self.tile(ph, "ssC", [128, 1], F32)
            sd = self.tile(ph, "sdC", [128, 1], F32)
            rstd = self.tile(ph, "rstdC", [128, 1], F32)
            hb = [self.tile(ph, "hbC%d" % i, [128, D], BF16) for i in range(2)]
            h2T = self.tile(ph, "h2T", [128, 8, 512], BF16)
            gT = self.tile(ph, "gT", [128, NCF, 512], BF16)
            wgt = [self.tile(ph, "wgt%d" % i, [128, 8, 128], BF16) for i in range(3)]
            wut = [self.tile(ph, "wut%d" % i, [128, 8, 128], BF16) for i in range(3)]
            wdt = [self.tile(ph, "wdt%d" % i, [128, D], BF16) for i in range(3)]
            aT = [self.tile(ph, "aT%d" % i, [128, 514], F32) for i in range(2)]
            cacc = [self.tile(ph, "cacc%d" % i, [128, 512], F32) for i in range(2)]
            ga = [self.tile(ph, "ga%d" % i, [128, 512], F32) for i in range(2)]
            pst = [self.tile(ph, "pstC%d" % i, [128, D], F32) for i in range(2)]
            rt = [self.tile(ph, "rtC%d" % i, [128, D], F32) for i in range(2)]
            x1r = [self.tile(ph, "x1rC%d" % i, [128, D], F32) for i in range(2)]
            ot = [self.tile(ph, "otC%d" % i, [128, D], F32) for i in range(2)]
            ss3 = self.tile(ph, "ss3C", [128, 1], F32)
            sd3 = self.tile(ph, "sd3C", [128, 1], F32)
            rstd3 = self.tile(ph, "rstd3C", [128, 1], F32)
            junk3 = self.tile(ph, "junk3C", [128, D], F32)
            it = 0
            i3 = 0
            RNG = 2
            pending = []
            store_tags = []

            def finish_range(tag, t0, t1):
                nonlocal i3
                S._wait("sp", tag)
                for tt in range(t0, t1):
                    tok = tt * 128
                    r_, x_, o_ = rt[i3 % 2], x1r[i3 % 2], ot[i3 % 2]
                    i3 += 1
                    S.load("sp", r_[:, :], self.red[tok:tok + 128, :], r_.b)
                    S.load("sp", x_[:, :], self.x1s[tok:tok + 128, :], x_.b)
                    self.rstd_of(r_[:, :], r_.b, junk3, ss3, sd3, rstd3)
                    S.op("dve", lambda: nc.vector.scalar_tensor_tensor(out=r_[:, :], in0=r_[:, :], scalar=rstd3[:, 0:1], in1=g3[:, :], op0=ALU.mult, op1=ALU.mult),
                         reads=[r_.b, rstd3.b, g3.b], writes=[r_.b])
                    S.op("pool", lambda: nc.gpsimd.tensor_tensor(out=o_[:, :], in0=r_[:, :], in1=x_[:, :], op=ALU.add),
                         reads=[r_.b, x_.b], writes=[o_.b])
                    S.store("pool", x_dst[tok:tok + 128, :], o_[:, :], o_.b)

            h2Ts = [h2T, self.tile(ph, "h2Tb", [128, 8, 512], BF16)]

            def seg_a(c, ti):
                nonlocal it
                tok = (4 * c + ti) * 128
                mt, mTt, xtt, h, tpp, x1t = mixt[it % 2], mT[it % 2], xt[it % 2], hb[it % 2], tpb[it % 2], x1[it % 2]
                it += 1
                mrow = (tok // 2048) * 4096 + (tok % 2048)
                S.load("sp", mt[:, 0:512], self.mixall[mrow:mrow + 128, :], mt.b)
                S.load("sp", mt[:, 512:1024], self.mixall[2048 + mrow:2048 + mrow + 128, :], mt.b)
                S.load("sp", xtt[:, :], x_src[tok:tok + 128, :], xtt.b)
                for kc in range(8):
                    S.op("pe", lambda: nc.tensor.transpose(out=tpp[:, kc * 128:(kc + 1) * 128], in_=mt[:, kc * 128:(kc + 1) * 128], identity=self.ident[:, :]),
                         reads=[mt.b, self.ident.b], writes=[tpp.b], accum=(kc > 0))
                self.copy(mTt[:, :, :], tpp[:, :].rearrange("p (k t) -> p k t", k=8), [tpp.b], [mTt.b])
                pa, pb = bank[4], bank[5]
                for half, pp in enumerate((pa, pb)):
                    for kc in range(8):
                        S.op("pe", lambda: nc.tensor.matmul(pp[:, :], lhsT=mTt[:, kc, :], rhs=WO[:, kc, half * 512:(half + 1) * 512],
                                                            start=(kc == 0), stop=(kc == 7)), reads=[mTt.b, WO.b], writes=[pp.b], accum=(kc > 0))
                self.copy(yt[:, 0:512], pa[:, :], [pa.b], [yt.b], eng="act")
                self.copy(yt[:, 512:D], pb[:, :], [pb.b], [yt.b], eng="act")
                self.rstd_of(yt[:, :], yt.b, junk, ss, sd, rstd)
                S.op("dve", lambda: nc.vector.scalar_tensor_tensor(out=yt[:, :], in0=yt[:, :], scalar=rstd[:, 0:1], in1=g1[:, :], op0=ALU.mult, op1=ALU.mult),
                     reads=[yt.b, rstd.b, g1.b], writes=[yt.b])
                S.op("pool", lambda: nc.gpsimd.tensor_tensor(out=x1t[:, :], in0=yt[:, :], in1=xtt[:, :], op=ALU.add),
                     reads=[yt.b, xtt.b], writes=[x1t.b])
                S.store("pool", self.x1s[tok:tok + 128, :], x1t[:, :], x1t.b)
                store_tags.append(("dma", x1t.b.dsem, x1t.b.dcnt))
                self.rstd_of(x1t[:, :], x1t.b, junk, ss, sd, rstd)
                S.op("dve", lambda: nc.vector.scalar_tensor_tensor(out=h[:, :], in0=x1t[:, :], scalar=rstd[:, 0:1], in1=g2[:, :], op0=ALU.mult, op1=ALU.mult),
                     reads=[x1t.b, rstd.b, g2.b], writes=[h.b])
                return (h, tpp)

            def seg_b(c, ti, ctx):
                h, tpp = ctx
                hT = h2Ts[c % 2]
                for kc in range(8):
                    S.op("pe", lambda: nc.tensor.transpose(out=tpp[:, kc * 128:(kc + 1) * 128], in_=h[:, kc * 128:(kc + 1) * 128], identity=self.ident[:, :]),
                         reads=[h.b, self.ident.b], writes=[tpp.b], accum=(kc > 0))
                self.copy(hT[:, :, ti * 128:(ti + 1) * 128], tpp[:, :].rearrange("p (k t) -> p k t", k=8), [tpp.b], [hT.b])

            def c2a_cf(c, cf):
                hT = h2Ts[c % 2]
                wg_, wu_ = wgt[cf % 3], wut[cf % 3]
                S.load("sp", wg_[:, :, :], self.wgs[cf], wg_.b)
                S.load("sp", wu_[:, :, :], self.wus[cf], wu_.b)
                pa, pu = bank[2 * (cf % 2)], bank[2 * (cf % 2) + 1]
                for kc in range(8):
                    S.op("pe", lambda: nc.tensor.matmul(pa[:, :], lhsT=wg_[:, kc, :], rhs=hT[:, kc, :], start=(kc == 0), stop=(kc == 7)),
                         reads=[wg_.b, hT.b], writes=[pa.b], accum=(kc > 0))
                for kc in range(8):
                    S.op("pe", lambda: nc.tensor.matmul(pu[:, :], lhsT=wu_[:, kc, :], rhs=hT[:, kc, :], start=(kc == 0), stop=(kc == 7)),
                         reads=[wu_.b, hT.b], writes=[pu.b], accum=(kc > 0))
                a, ca, g = aT[cf % 2], cacc[cf % 2], ga[cf % 2]
                e4 = a[:, 0:4]
                S.op("dve", lambda: nc.vector.tensor_copy(out=a[:, 0:2], in_=halo[:, cf, :]), reads=[halo.b], writes=[a.b])
                S.op("dve", lambda: nc.vector.tensor_copy(out=a[:, 2:4], in_=pa[:, 0:2]), reads=[pa.b], writes=[a.b])
                S.op("dve", lambda: nc.vector.tensor_copy(out=halo[:, cf, :], in_=pa[:, 510:512]), reads=[pa.b], writes=[halo.b])
                S.op("dve", lambda: nc.vector.tensor_scalar(out=ca[:, 2:512], in0=pa[:, 0:510], scalar1=cw[:, cf, 0:1], scalar2=None, op0=ALU.mult),
                     reads=[pa.b, cw.b], writes=[ca.b])
                S.op("dve", lambda: nc.vector.scalar_tensor_tensor(out=ca[:, 2:512], in0=pa[:, 1:511], scalar=cw[:, cf, 1:2], in1=ca[:, 2:512], op0=ALU.mult, op1=ALU.add),
                     reads=[pa.b, cw.b, ca.b], writes=[ca.b])
                S.op("dve", lambda: nc.vector.scalar_tensor_tensor(out=ca[:, 2:512], in0=pa[:, 2:512], scalar=cw[:, cf, 2:3], in1=ca[:, 2:512], op0=ALU.mult, op1=ALU.add),
                     reads=[pa.b, cw.b, ca.b], writes=[ca.b])
                S.op("dve", lambda: nc.vector.tensor_scalar(out=ca[:, 0:2], in0=e4[:, 0:2], scalar1=cw[:, cf, 0:1], scalar2=None, op0=ALU.mult),
                     reads=[a.b, cw.b], writes=[ca.b])
                S.op("dve", lambda: nc.vector.scalar_tensor_tensor(out=ca[:, 0:2], in0=e4[:, 1:3], scalar=cw[:, cf, 1:2], in1=ca[:, 0:2], op0=ALU.mult, op1=ALU.add),
                     reads=[a.b, cw.b, ca.b], writes=[ca.b])
                S.op("dve", lambda: nc.vector.scalar_tensor_tensor(out=ca[:, 0:2], in0=e4[:, 2:4], scalar=cw[:, cf, 2:3], in1=ca[:, 0:2], op0=ALU.mult, op1=ALU.add),
                     reads=[a.b, cw.b, ca.b], writes=[ca.b])
                S.op("act", lambda: nc.scalar.activation(out=g[:, :], in_=ca[:, :], func=AF.Gelu_apprx_tanh, bias=cb[:, cf:cf + 1]),
                     reads=[ca.b, cb.b], writes=[g.b])
                S.op("dve", lambda: nc.vector.tensor_tensor(out=gT[:, cf, :], in0=g[:, :], in1=pu[:, :], op=ALU.mult),
                     reads=[g.b, pu.b], writes=[gT.b])

            def c2b(c):
                for tp2 in range(2):
                    accs = [bank[0], bank[1], bank[2], bank[3]]
                    for cf in range(NCF):
                        wd_ = wdt[cf % 3]
                        S.load("sp", wd_[:, :], self.wds[cf], wd_.b)
                        for j in range(2):
                            ti = 2 * tp2 + j
                            for half in range(2):
                                pp = accs[2 * j + half]
                                S.op("pe", lambda: nc.tensor.matmul(pp[:, :], lhsT=gT[:, cf, ti * 128:(ti + 1) * 128], rhs=wd_[:, half * 512:(half + 1) * 512],
                                                                    start=(cf == 0), stop=(cf == NCF - 1)), reads=[gT.b, wd_.b], writes=[pp.b], accum=(cf > 0))
                    for j in range(2):
                        ti = 2 * tp2 + j
                        tok = (4 * c + ti) * 128
                        p_ = pst[j]
                        self.copy(p_[:, 0:512], accs[2 * j][:, :], [accs[2 * j].b], [p_.b], eng="act")
                        self.copy(p_[:, 512:D], accs[2 * j + 1][:, :], [accs[2 * j + 1].b], [p_.b], eng="dve")
                        S.store("pool", self.part[tok:tok + 128, :], p_[:, :], p_.b)
                        store_tags.append(("dma", p_.b.dsem, p_.b.dcnt))

            for ti in range(4):
                seg_b(0, ti, seg_a(0, ti))
            for c in range(self.nch):
                ctx = {}
                for cf in range(NCF):
                    c2a_cf(c, cf)
                    if c + 1 < self.nch and cf % 4 == 3:
                        k = cf // 4
                        ctx[k] = seg_a(c + 1, k)
                        if k > 0:
                            seg_b(c + 1, k - 1, ctx[k - 1])
                if c + 1 < self.nch:
                    seg_b(c + 1, 3, ctx[3])
                c2b(c)
                if c % RNG == RNG - 1 or c == self.nch - 1:
                    c0 = (c // RNG) * RNG
                    r0, r1 = c0 * 512, (c + 1) * 512
                    for tg in store_tags:
                        S._wait("pool", tg)
                    store_tags = []
                    tag = self.collective("AllReduce", ALU.add, self.part[r0:r1, :].opt(), self.red[r0:r1, :].opt())
                    pending.append((tag, r0 // 128, r1 // 128))
                    if len(pending) > 1:
                        finish_range(*pending.pop(0))
            while pending:
                finish_range(*pending.pop(0))
            S.barrier()


_CACHE = {}


def kernel(**inputs):
    x = np.ascontiguousarray(inputs["x"], dtype=np.float32)
    params = [layout_params(inputs, r) for r in range(2)]
    consts = [make_consts(r) for r in range(2)]
    if "prog" not in _CACHE:
        _CACHE["prog"] = Prog()
    prog = _CACHE["prog"]
    in_maps = []
    for core in range(8):
        b, r = core // 2, core % 2
        m = {"x": x[b]}
        for n, _, _ in CONST_SPECS:
            m["c_" + n] = consts[r][n]
        for n, _ in PARAM_SPECS:
            m["p_" + n] = params[r][n]
        in_maps.append(m)
    res = run_bass_kernel_spmd(prog.nc, in_maps, core_ids=list(range(8)))
    out = np.stack([np.asarray(res.results[2 * b]["y"], dtype=np.float32) for b in range(4)], axis=0)
    return out
```

```python
import math
import numpy as np
import ml_dtypes
from contextlib import ExitStack
import concourse.bass as bass
import concourse.mybir as mybir
from concourse.bass_utils import run_bass_kernel_spmd

F32 = mybir.dt.float32
BF16 = mybir.dt.bfloat16
AF = mybir.ActivationFunctionType
ALU = mybir.AluOpType
AX = mybir.AxisListType
NPBF = ml_dtypes.bfloat16

D = 1024
S_LEN = 8192
NT = 64
NCH = 16
DEPTH = 2
DFF = 4096
NEG = -30000.0
EPS = 1e-6

OFF = dict(mq=0, mk=256, mv=512, nq=768, nkc=1024, nvc=1088, nks=1152, nvs=1216, nkw=1280, nvw=1344,
           ng=1408, dq=1420, dk=1676, dv=1932, sq=2188, sk=2444, sv=2572)
FRr = dict(mq=0, mk=128, nq=256, nkc=512, nvc=576, nks=640, nkw=704, dq=768, dk=896, sq=1024, sk=1152)
NF = 1280
VCr = dict(mv=0, nvs=128, nvw=192, dv=256, sv=384, ng=448)
NV = 460
NCF = 16


def role_heads(r, m=0):
    if m == 3:
        return [2 * r, 2 * r + 1], [2 * (1 - r), 2 * (1 - r) + 1]
    return [r, r + 2], [1 - r, 3 - r]


def far_skip(m, j, min_dist):
    sl = min(SLOPES[4 * role_heads(0, m)[0][j] + m], SLOPES[4 * role_heads(1, m)[0][j] + m])
    return min_dist > 0 and sl * min_dist >= 200.0


def role_cols(r):
    own0 = role_heads(r, 0)[0]
    own1, oth1 = role_heads(r, 1)
    own2 = role_heads(r, 2)[0]
    own3 = role_heads(r, 3)[0]

    def hc(base, heads, w=64):
        return np.concatenate([np.arange(base + w * h, base + w * (h + 1)) for h in heads])

    f = np.concatenate([hc(OFF["mq"], own0), hc(OFF["mk"], own0), hc(OFF["nq"], own1 + oth1),
                        np.arange(OFF["nkc"], OFF["nkc"] + 64), np.arange(OFF["nvc"], OFF["nvc"] + 64),
                        np.arange(OFF["nks"], OFF["nks"] + 64), np.arange(OFF["nkw"], OFF["nkw"] + 64),
                        hc(OFF["dq"], own2), hc(OFF["dk"], own2), hc(OFF["sq"], own3), hc(OFF["sk"], [r])])
    v = np.concatenate([hc(OFF["mv"], own0), np.arange(OFF["nvs"], OFF["nvs"] + 64), np.arange(OFF["nvw"], OFF["nvw"] + 64),
                        hc(OFF["dv"], own2), hc(OFF["sv"], [r]), hc(OFF["ng"], own1 + oth1, 3)])
    assert len(f) == 1216 and len(v) == NV
    return f, v


SLOPES = np.power(np.float32(2.0), np.arange(1, 17, dtype=np.float32) * np.float32(-0.5)).astype(np.float64)


class Buf:
    __slots__ = ("name", "w", "rs", "dsem", "dcnt")

    def __init__(self, name):
        self.name = name
        self.w = None
        self.rs = []
        self.dsem = None
        self.dcnt = 0


class Sched:
    def __init__(self, nc, stack):
        self.nc = nc
        self.stack = stack
        self.eng = {"pe": nc.tensor, "act": nc.scalar, "dve": nc.vector, "pool": nc.gpsimd, "sp": nc.sync}
        self.sem, self.cnt, self.seen = {}, {}, {}
        for k in self.eng:
            self.sem[k] = stack.enter_context(nc.semaphore("s_" + k))
            self.cnt[k] = 0
            self.seen[k] = {}
        self.dma_seen = {k: {} for k in self.eng}
        self.free_sems = []
        self.live = []
        self.nsem = len(self.eng)
        self.ninstr = 0

    def _wait(self, ek, dep):
        if dep is None:
            return
        e = self.eng[ek]
        if dep[0] == "dma":
            _, sem, val = dep
            d = self.dma_seen[ek]
            if d.get(id(sem), 0) >= val:
                return
            e.wait_ge(sem, val)
            d[id(sem)] = val
        else:
            pk, c = dep
            if self.seen[ek].get(pk, 0) >= c:
                return
            e.wait_ge(self.sem[pk], c)
            self.seen[ek][pk] = c
        self.ninstr += 1

    def _deps(self, ek, reads, writes, accum):
        for b in reads:
            self._wait(ek, b.w)
        for b in writes:
            if not (accum and b.w is not None and b.w[0] == ek):
                self._wait(ek, b.w)
            for r in b.rs:
                self._wait(ek, r)

    def op(self, ek, fn, reads=(), writes=(), accum=False):
        self._deps(ek, reads, writes, accum)
        ins = fn()
        self.cnt[ek] += 1
        ins.then_inc(self.sem[ek], 1)
        tag = (ek, self.cnt[ek])
        for b in reads:
            b.rs.append(tag)
        for b in writes:
            b.w = tag
            b.rs = []
        self.ninstr += 1
        return ins

    def _dsem(self, b):
        if b.dsem is None:
            if self.free_sems:
                b.dsem, b.dcnt = self.free_sems.pop()
            else:
                b.dsem = self.stack.enter_context(self.nc.semaphore("d%d" % self.nsem))
                b.dcnt = 0
                self.nsem += 1
            self.live.append(b)

    def load(self, qk, out_ap, in_ap, buf):
        self._deps(qk, (), (buf,), False)
        self._dsem(buf)
        ins = self.eng[qk].dma_start(out=out_ap, in_=in_ap)
        buf.dcnt += 16
        ins.then_inc(buf.dsem, 16)
        buf.w = ("dma", buf.dsem, buf.dcnt)
        buf.rs = []
        self.ninstr += 1

    def store(self, qk, out_ap, in_ap, buf):
        self._deps(qk, (buf,), (), False)
        self._dsem(buf)
        ins = self.eng[qk].dma_start(out=out_ap, in_=in_ap)
        buf.dcnt += 16
        ins.then_inc(buf.dsem, 16)
        buf.rs.append(("dma", buf.dsem, buf.dcnt))
        self.ninstr += 1

    def barrier(self):
        for ek in self.eng:
            for pk in self.eng:
                if pk != ek and self.cnt[pk] > 0:
                    self._wait(ek, (pk, self.cnt[pk]))
            for b in self.live:
                self._wait(ek, ("dma", b.dsem, b.dcnt))
        for b in self.live:
            self.free_sems.append((b.dsem, b.dcnt))
            b.dsem = None
        self.live = []


class T:
    def __init__(self, nc, stack, name, shape, dtype, psum=False):
        alloc = nc.psum_tensor if psum else nc.sbuf_tensor
        self.t = stack.enter_context(alloc(name, shape, dtype))
        self.b = Buf(name)

    def __getitem__(self, idx):
        return self.t[idx]


def _split3(v):
    v = np.asarray(v, np.float64)
    hi = v.astype(NPBF)
    r = v - hi.astype(np.float64)
    mid = r.astype(NPBF)
    r = r - mid.astype(np.float64)
    lo = r.astype(NPBF)
    return hi, mid, lo


def make_consts(role=0):
    locs = [sum(role_heads(role, m), []) for m in range(4)]
    c = {}
    c["ident"] = np.eye(128, dtype=np.float32).astype(NPBF)
    iq = np.arange(512, dtype=np.float64)
    p = np.arange(128, dtype=np.float64)
    aug = np.zeros((16, 3, 512), NPBF)
    for m in range(4):
        scale = 32 ** -0.5 if m == 2 else 64 ** -0.5
        for j in range(4):
            sl = SLOPES[4 * locs[m][j] + m]
            hi, mid, lo = _split3(-sl * iq / scale)
            aug[4 * m + j, 0], aug[4 * m + j, 1], aug[4 * m + j, 2] = hi, mid, lo
    c["aug"] = aug
    kb = np.zeros((128, 16, 64), np.float32)
    r = np.arange(-60, 4, dtype=np.float64)
    for m in range(4):
        for j in range(4):
            sl = SLOPES[4 * locs[m][j] + m]
            kb[:, 4 * m + j, :] = (sl * (p[:, None] + 128.0 * r[None, :])).astype(np.float32)
    c["kb"] = kb
    kbc = np.zeros((128, 4, 16, 4), np.float32)
    for j in range(4):
        sl = SLOPES[4 * locs[1][j] + 1]
        for cc in range(16):
            for kt in range(4):
                kbc[:, j, cc, kt] = (sl * (16.0 * (128 * kt + p) + 31.0 - 512.0 * cc)).astype(np.float32)
    c["kbc"] = kbc
    P = p[:, None, None]
    Q = iq[None, None, :]
    k4 = np.arange(4, dtype=np.float64)[None, :, None]
    c["caus"] = np.where(Q >= 128 * k4 + P, 0.0, NEG).astype(np.float32).astype(NPBF)
    r8 = (np.arange(8, dtype=np.float64) - 4)[None, :, None]
    dd = Q - 128 * r8 - P
    c["wmask"] = np.where((dd >= 0) & (dd < 512), 0.0, NEG).astype(np.float32).astype(NPBF)
    r5 = (np.arange(5, dtype=np.float64) - 1)[None, :, None]
    dd = Q - 128 * r5 - P
    c["swamask"] = np.where((dd >= 0) & (dd < 128), 0.0, NEG).astype(np.float32).astype(NPBF)
    d5 = (512.0 * np.arange(5, dtype=np.float64))[None, :, None]
    c["cmask"] = np.where(16 * P + 31 <= d5 + Q, 0.0, NEG).astype(np.float32).astype(NPBF)
    j = np.arange(S_LEN)
    c["em"] = (j[None, :] // 256 == np.arange(32)[:, None]).astype(np.float32).astype(NPBF)
    c["es"] = (j[None, :] // 64 == np.arange(128)[:, None]).astype(np.float32).astype(NPBF)
    ncmp, nsel = 511, 128
    cs = np.arange(512)[:, None] * 16
    bs = np.arange(nsel)[None, :] * 64
    ov = np.clip(np.minimum(cs + 32, bs + 64) - np.maximum(cs, bs), 0, None) / 32.0
    ov[ncmp:, :] = 0.0
    c["ov"] = ov.reshape(4, 128, 128).transpose(1, 0, 2).astype(np.float32).astype(NPBF)
    n32 = np.arange(32, dtype=np.float32)
    qi = np.arange(4)
    c["bi"] = np.broadcast_to((n32[None, None, :] - (qi // 2)[None, :, None].astype(np.float32)), (128, 4, 32)).astype(np.float32).copy()
    s128 = np.arange(128)[None, None, None, :]
    cc = np.arange(16)[:, None, None, None]
    pp = np.arange(128)[None, :, None, None]
    qq = np.arange(4)[None, None, :, None]
    qblk = 8 * cc + 2 * qq + (pp >= 64)
    forced = (s128 == 0) | (s128 == qblk) | (s128 == qblk - 1)
    valid = s128 <= qblk
    c["f1e4"] = np.where(forced, 1e4, 0.0).astype(np.float32)
    c["negv"] = np.where(valid, 0.0, -1e30).astype(np.float32)
    c["ones3"] = np.ones((3, 4 * S_LEN), np.float32).astype(NPBF)
    return c


CONST_SPECS = [("ident", [128, 128], BF16), ("aug", [16, 3, 512], BF16), ("kb", [128, 16, 64], F32),
               ("kbc", [128, 4, 16, 4], F32), ("caus", [128, 4, 512], BF16), ("wmask", [128, 8, 512], BF16),
               ("swamask", [128, 5, 512], BF16), ("cmask", [128, 5, 512], BF16), ("em", [32, S_LEN], BF16),
               ("es", [128, S_LEN], BF16), ("ov", [128, 4, 128], BF16), ("bi", [128, 4, 32], F32),
               ("f1e4", [16, 128, 4, 128], F32), ("negv", [16, 128, 4, 128], F32), ("ones3", [3, 4 * S_LEN], BF16)]

PARAM_SPECS = [("wf", [DEPTH, 128, 8, NF]), ("wv", [DEPTH, 128, 8, NV]), ("wo", [DEPTH, 128, 8, D]),
               ("wg", [DEPTH, NCF, 128, 8, 128]), ("wu", [DEPTH, NCF, 128, 8, 128]), ("wd", [DEPTH, NCF, 128, D]),
               ("g_apre", [DEPTH, D]), ("g_apost", [DEPTH, D]), ("g_fpre", [DEPTH, D]), ("g_fpost", [DEPTH, D]),
               ("cw", [DEPTH, 128, NCF, 3]), ("cb", [DEPTH, 128, NCF]),
               ("posk", [DEPTH, 64, 32]), ("w1k", [DEPTH, 64, 32, 128]), ("b1k", [DEPTH, 128, 1]), ("w2k", [DEPTH, 128, 64]),
               ("posv", [DEPTH, 64, 32]), ("w1v", [DEPTH, 64, 32, 128]), ("b1v", [DEPTH, 128, 1]), ("w2v", [DEPTH, 128, 64]),
               ("lq1", [DEPTH, 32]), ("lk1", [DEPTH, 32]), ("lq2", [DEPTH, 32]), ("lk2", [DEPTH, 32]),
               ("subln", [DEPTH, 64]), ("sinks", [DEPTH, 4])]


def layout_params(inp, role=0):
    o = {}
    w_in = inp["w_in"]
    fc, vc = role_cols(role)
    wf = np.zeros((DEPTH, D, NF), np.float32)
    wf[:, :, :len(fc)] = w_in[:, :, fc]
    o["wf"] = wf.reshape(DEPTH, 8, 128, NF).transpose(0, 2, 1, 3)
    o["wv"] = w_in[:, :, vc].reshape(DEPTH, 8, 128, NV).transpose(0, 2, 1, 3)
    rows = np.concatenate([np.arange(256 * m + 64 * h, 256 * m + 64 * (h + 1)) for rr in range(2) for m in range(4)
                           for h in role_heads(rr, m)[0]])
    o["wo"] = inp["w_out"][:, rows, :].reshape(DEPTH, 8, 128, D).transpose(0, 2, 1, 3)
    cfs = slice(NCF * role, NCF * (role + 1))
    o["wg"] = inp["ffn_w_gate"].reshape(DEPTH, 8, 128, 32, 128).transpose(0, 3, 2, 1, 4)[:, cfs]
    o["wu"] = inp["ffn_w_up"].reshape(DEPTH, 8, 128, 32, 128).transpose(0, 3, 2, 1, 4)[:, cfs]
    o["wd"] = inp["ffn_w_down"].reshape(DEPTH, 32, 128, D)[:, cfs]
    o["g_apre"], o["g_apost"] = inp["attn_pre_norm"], inp["attn_post_norm"]
    o["g_fpre"], o["g_fpost"] = inp["ffn_pre_norm"], inp["ffn_post_norm"]
    o["cw"] = inp["ffn_conv_w"].reshape(DEPTH, 3, 32, 128).transpose(0, 3, 2, 1)[:, :, cfs, :]
    o["cb"] = inp["ffn_conv_b"].reshape(DEPTH, 32, 128).transpose(0, 2, 1)[:, :, cfs]
    for sfx in "kv":
        o["pos" + sfx] = inp["nsa_cmp_pos_" + sfx].transpose(0, 2, 1)
        o["w1" + sfx] = inp["nsa_cmp_w1_" + sfx].transpose(0, 2, 1, 3)
        o["b1" + sfx] = inp["nsa_cmp_b1_" + sfx].reshape(DEPTH, 128, 1)
        o["w2" + sfx] = inp["nsa_cmp_w2_" + sfx]
    o["lq1"], o["lk1"] = inp["diff_lambda_q1"], inp["diff_lambda_k1"]
    o["lq2"], o["lk2"] = inp["diff_lambda_q2"], inp["diff_lambda_k2"]
    o["subln"] = inp["diff_subln"]
    o["sinks"] = inp["swa_sinks"][:, sum(role_heads(role, 3), [])]
    return {k: np.ascontiguousarray(v, dtype=np.float32) for k, v in o.items()}


class Prog:
    def __init__(self, phases=("A", "B", "C"), layers=(0, 1), mixers=(0, 1, 2, 3), debug=False, nchunks=NCH, ncores=8):
        self.phases, self.layers, self.mixers, self.debug, self.nch = phases, layers, mixers, debug, nchunks
        self.groups = [[2 * i, 2 * i + 1] for i in range(ncores // 2)]
        nc = bass.Bass("TRN2", target_bir_lowering=False)
        self.nc = nc
        self.x_in = nc.dram_tensor("x", [S_LEN, D], F32, kind="ExternalInput").ap()
        self.y = nc.dram_tensor("y", [S_LEN, D], F32, kind="ExternalOutput").ap()
        self.dc = {n: nc.dram_tensor("c_" + n, shp, dt, kind="ExternalInput").ap() for n, shp, dt in CONST_SPECS}
        self.dp = {n: nc.dram_tensor("p_" + n, shp, F32, kind="ExternalInput").ap() for n, shp in PARAM_SPECS}
        kind = "ExternalOutput" if debug else "Internal"
        self.featT = nc.dram_tensor("featT", [NF, S_LEN], BF16, kind=kind).ap()
        self.vtok = nc.dram_tensor("vtok", [S_LEN, NV], BF16, kind=kind).ap()
        self.mix_t = nc.dram_tensor("mix", [S_LEN, 512], BF16)
        self.mix = self.mix_t.ap()
        self.mixall_t = nc.dram_tensor("mixall", [2 * S_LEN, 512], BF16)
        self.mixall = self.mixall_t.ap()
        self.part = nc.dram_tensor("part", [S_LEN, D], F32).ap()
        self.red = nc.dram_tensor("red", [S_LEN, D], F32).ap()
        self.x1s = nc.dram_tensor("x1s", [S_LEN, D], F32).ap()
        self.xs = nc.dram_tensor("xs", [S_LEN, D], F32).ap()
        self.wgs = nc.dram_tensor("wgs", [NCF, 128, 8, 128], BF16).ap()
        self.wus = nc.dram_tensor("wus", [NCF, 128, 8, 128], BF16).ap()
        self.wds = nc.dram_tensor("wds", [NCF, 128, D], BF16).ap()
        self.cp_i = 0
        with ExitStack() as st:
            self.st = st
            self.S = Sched(nc, st)
            self.cc_sem = st.enter_context(nc.semaphore("cc_sem"))
            self.cc_cnt = 0
            self.build()
            print("built: instr", self.S.ninstr, "sems", self.S.nsem, flush=True)

    def collective(self, kind, op, in_ap, out_ap):
        ins = self.nc.gpsimd.collective_compute(kind, op, replica_groups=self.groups, ins=[in_ap], outs=[out_ap])
        self.cc_cnt += 1
        ins.then_inc(self.cc_sem, 1)
        self.S.ninstr += 1
        return ("dma", self.cc_sem, self.cc_cnt)

    def tile(self, stack, name, shape, dtype, psum=False):
        self.tile_i = getattr(self, "tile_i", 0) + 1
        return T(self.nc, stack, "%s_%d" % (name, self.tile_i), shape, dtype, psum)

    def copy(self, out_ap, in_ap, reads, writes, eng=None):
        nc = self.nc
        if eng is None:
            eng = "act" if (self.cp_i % 2 == 0) else "dve"
            self.cp_i += 1
        if eng == "act":
            self.S.op("act", lambda: nc.scalar.activation(out=out_ap, in_=in_ap, func=AF.Copy), reads=reads, writes=writes)
        elif eng == "dve":
            self.S.op("dve", lambda: nc.vector.tensor_copy(out=out_ap, in_=in_ap), reads=reads, writes=writes)
        else:
            self.S.op("pool", lambda: nc.gpsimd.tensor_copy(out=out_ap, in_=in_ap), reads=reads, writes=writes)

    def load_cast(self, stack, dst, dst_ap_fn, src_ap_fn, n, ncols):
        stg = [self.tile(stack, "stg%d_%d" % (i, self.S.ninstr), [128, ncols], F32) for i in range(2)]
        for i in range(n):
            s = stg[i % 2]
            self.S.load("sp", s[:, :], src_ap_fn(i), s.b)
            self.copy(dst_ap_fn(i), s[:, :], [s.b], [dst.b])

    def build(self):
        nc, S, st = self.nc, self.S, self.st
        self.ident = self.tile(st, "ident", [128, 128], BF16)
        self.caus = self.tile(st, "caus", [128, 4, 512], BF16)
        self.kb = self.tile(st, "kb", [128, 16, 64], F32)
        self.epsT = self.tile(st, "epsT", [128, 1], F32)
        S.load("sp", self.ident[:, :], self.dc["ident"], self.ident.b)
        S.load("sp", self.caus[:, :, :], self.dc["caus"], self.caus.b)
        S.load("sp", self.kb[:, :, :], self.dc["kb"], self.kb.b)
        S.op("dve", lambda: nc.vector.memset(self.epsT[:, :], EPS), writes=[self.epsT.b])
        for l in self.layers:
            x_src = self.x_in if l == 0 else self.xs
            x_dst = self.y if l == self.layers[-1] else self.xs
            if "A" in self.phases:
                self.phase_A(l, x_src)
                S.barrier()
            if "B" in self.phases:
                for m in self.mixers:
                    [self.moba, self.nsa, self.diff, self.swa][m](l)
                    S.barrier()
            if "C" in self.phases:
                tags = []
                for k in range((self.nch * 512 + 2047) // 2048):
                    tags.append(self.collective("AllGather", ALU.bypass, self.mix[k * 2048:(k + 1) * 2048, :].opt(),
                                                self.mixall[k * 4096:(k + 1) * 4096, :].opt()))
                self.prep_ffn(l)
                for ek in S.eng:
                    S._wait(ek, tags[-1])
                S.barrier()
                self.phase_C(l, x_src, x_dst)
                S.barrier()
        S.barrier()
        if self.debug:
            nc = self.nc
            dbg = {}
            for nm, src, rows, cols, dt in (("d_mixall", self.mixall, 4096, 512, BF16), ("d_part", self.part, 2048, D, F32),
                                            ("d_red", self.red, 2048, D, F32), ("d_x1s", self.x1s, 2048, D, F32), ("d_mix", self.mix, 2048, 512, BF16)):
                dst = nc.dram_tensor(nm, [rows, cols], dt, kind="ExternalOutput").ap()
                b = Buf(nm)
                S._dsem(b)
                ins = nc.sync.dma_start(out=dst[:, :], in_=src[0:rows, :])
                b.dcnt += 16
                ins.then_inc(b.dsem, 16)
                S._wait("sp", ("dma", b.dsem, b.dcnt))

    def rstd_of(self, x_ap, xbuf, junk, ss, sd, rstd, n=D):
        nc, S = self.nc, self.S
        S.op("dve", lambda: nc.vector.scalar_tensor_tensor(out=junk[:, 0:n], in0=x_ap, scalar=1.0, in1=x_ap,
                                                           op0=ALU.mult, op1=ALU.mult, accum_out=ss[:, 0:1]),
             reads=[xbuf], writes=[junk.b, ss.b])
        S.op("act", lambda: nc.scalar.activation(out=sd[:, 0:1], in_=ss[:, 0:1], func=AF.Sqrt,
                                                 bias=self.epsT[:, 0:1], scale=1.0 / n),
             reads=[ss.b, self.epsT.b], writes=[sd.b])
        S.op("dve", lambda: nc.vector.reciprocal(out=rstd[:, 0:1], in_=sd[:, 0:1]), reads=[sd.b], writes=[rstd.b])

    def phase_A(self, l, x_src):
        nc, S = self.nc, self.S
        with ExitStack() as ph:
            WF = self.tile(ph, "WF", [128, 8, NF], BF16)
            WV = self.tile(ph, "WV", [128, 8, NV], BF16)
            gain = self.tile(ph, "gainA", [128, D], F32)
            with ExitStack() as tmp:
                self.load_cast(tmp, WF, lambda i: WF[:, i, :], lambda i: self.dp["wf"][l, :, i, :], 8, NF)
                self.load_cast(tmp, WV, lambda i: WV[:, i, :], lambda i: self.dp["wv"][l, :, i, :], 8, NV)
                S.barrier()
            S.load("sp", gain[:, :], self.dp["g_apre"][l].partition_broadcast(128), gain.b)
            xts = [self.tile(ph, "xtA%d" % i, [128, D], F32) for i in range(3)]
            junk = self.tile(ph, "junkA", [128, D], F32)
            ss = self.tile(ph, "ssA", [128, 1], F32)
            sd = self.tile(ph, "sdA", [128, 1], F32)
            rstd = self.tile(ph, "rstdA", [128, 1], F32)
            hb = [self.tile(ph, "hbA%d" % i, [128, D], BF16) for i in range(2)]
            hT = [self.tile(ph, "hTA%d" % i, [128, 8, 512], BF16) for i in range(2)]
            tp = [self.tile(ph, "tpA%d" % i, [128, D], BF16, psum=True) for i in range(2)]
            psF = [self.tile(ph, "psF%d" % i, [128, 512], F32, psum=True) for i in range(2)]
            psV0 = self.tile(ph, "psV0", [128, 512], F32, psum=True)
            fst = [self.tile(ph, "fst%d" % i, [128, 512], BF16) for i in range(3)]
            vst = [self.tile(ph, "vst%d" % i, [128, NV], BF16) for i in range(2)]
            it = 0
            for c in range(self.nch):
                hTc = hT[c % 2]
                for ti in range(4):
                    tok = (4 * c + ti) * 128
                    xt = xts[it % 3]
                    h = hb[it % 2]
                    tpp = tp[it % 2]
                    it += 1
                    S.load("sp", xt[:, :], x_src[tok:tok + 128, :], xt.b)
                    self.rstd_of(xt[:, :], xt.b, junk, ss, sd, rstd)
                    S.op("dve", lambda: nc.vector.scalar_tensor_tensor(out=h[:, :], in0=xt[:, :], scalar=rstd[:, 0:1],
                                                                       in1=gain[:, :], op0=ALU.mult, op1=ALU.mult),
                         reads=[xt.b, rstd.b, gain.b], writes=[h.b])
                    for kc in range(8):
                        S.op("pe", lambda: nc.tensor.transpose(out=tpp[:, kc * 128:(kc + 1) * 128],
                                                               in_=h[:, kc * 128:(kc + 1) * 128], identity=self.ident[:, :]),
                             reads=[h.b, self.ident.b], writes=[tpp.b], accum=(kc > 0))
                    self.copy(hTc[:, :, ti * 128:(ti + 1) * 128], tpp[:, :].rearrange("p (k t) -> p k t", k=8),
                              [tpp.b], [hTc.b])
                for g in range(NF // 128):
                    ps = psF[g % 2]
                    for kc in range(8):
                        S.op("pe", lambda: nc.tensor.matmul(ps[:, :], lhsT=WF[:, kc, g * 128:(g + 1) * 128], rhs=hTc[:, kc, :],
                                                            start=(kc == 0), stop=(kc == 7)),
                             reads=[WF.b, hTc.b], writes=[ps.b], accum=(kc > 0))
                    f = fst[g % 3]
                    self.copy(f[:, :], ps[:, :], [ps.b], [f.b])
                    S.store("pool", self.featT[g * 128:(g + 1) * 128, c * 512:(c + 1) * 512], f[:, :], f.b)
                for ti in range(4):
                    tok = (4 * c + ti) * 128
                    for kc in range(8):
                        S.op("pe", lambda: nc.tensor.matmul(psV0[:, 0:NV], lhsT=hTc[:, kc, ti * 128:(ti + 1) * 128], rhs=WV[:, kc, 0:NV],
                                                            start=(kc == 0), stop=(kc == 7)),
                             reads=[WV.b, hTc.b], writes=[psV0.b], accum=(kc > 0))
                    v = vst[ti % 2]
                    self.copy(v[:, 0:NV], psV0[:, 0:NV], [psV0.b], [v.b])
                    S.store("pool", self.vtok[tok:tok + 128, :], v[:, :], v.b)
            S.barrier()

    def attn_res(self, ph, nps=3, npt=3, no=3):
        R = type("R", (), {})()
        R.ps = [self.tile(ph, "ps_s%d" % i, [128, 512], F32, psum=True) for i in range(nps)]
        R.pt = [self.tile(ph, "pT%d" % i, [128, 512], BF16) for i in range(npt)]
        R.O = [self.tile(ph, "O%d" % i, [128, 512], F32, psum=True) for i in range(no)]
        R.ips = R.ipt = R.io = 0
        return R

    def attend(self, R, q_ap, qbufs, tiles, O, scale):
        nc, S = self.nc, self.S
        Ov = O[:, 0:260].rearrange("p (a b) -> p a b", a=4)
        n = len(tiles)

        def pv(pt, tl, first, last):
            for qi in range(4):
                S.op("pe", lambda: nc.tensor.matmul(Ov[:, qi, :], lhsT=pt[:, qi * 128:(qi + 1) * 128], rhs=tl["v"],
                                                    start=(first and qi == 0), stop=last),
                     reads=[pt.b] + tl["kbufs"], writes=[O.b], accum=not (first and qi == 0))
            if tl.get("post") is not None:
                tl["post"](pt, first, last)

        prev = None
        for i, tl in enumerate(tiles):
            ps = R.ps[R.ips % len(R.ps)]
            R.ips += 1
            ex = tl.get("extra", [])
            S.op("pe", lambda: nc.tensor.matmul(ps[:, :], lhsT=tl["kT"], rhs=q_ap, start=True, stop=(len(ex) == 0)),
                 reads=qbufs + tl["kbufs"], writes=[ps.b])
            for j, (lh, rh, bufs) in enumerate(ex):
                S.op("pe", lambda: nc.tensor.matmul(ps[:, :], lhsT=lh, rhs=rh, start=False, stop=(j == len(ex) - 1)),
                     reads=bufs, writes=[ps.b], accum=True)
            pt = R.pt[R.ipt % len(R.pt)]
            R.ipt += 1
            S.op("act", lambda: nc.scalar.activation(out=pt[:, :], in_=ps[:, :], func=AF.Exp, bias=tl["bias"], scale=scale),
                 reads=[ps.b] + tl["bbufs"], writes=[pt.b])
            if prev is not None:
                pv(prev[0], prev[1], prev[2] == 0, False)
            prev = (pt, tl, i)
        pv(prev[0], prev[1], prev[2] == 0, True)
        return Ov

    def load_kv(self, KT, V, krows, nh_k, k_row0, v_col0, nh_v, kd=64):
        nc, S = self.nc, self.S
        S.op("pool", lambda: nc.gpsimd.memset(V[:, :, :, 64:65], 1.0), writes=[V.b])
        for h in range(nh_k):
            S.load("sp", KT[0:kd, h, :], self.featT[k_row0 + h * kd:k_row0 + (h + 1) * kd, :], KT.b)
        S.load("sp", KT[kd:kd + 3, :, :], self.dc["ones3"][:, 0:nh_k * S_LEN].rearrange("r (h s) -> r h s", h=nh_k), KT.b)
        for h in range(nh_v):
            S.load("sp", V[:, h, :, 0:64],
                   self.vtok[:, v_col0 + h * 64:v_col0 + (h + 1) * 64].rearrange("(kt p) d -> p kt d", p=128), V.b)

    def recip_den(self, Ov, O, den, add_ap=None, add_bufs=()):
        nc, S = self.nc, self.S
        if add_ap is None:
            S.op("dve", lambda: nc.vector.tensor_scalar(out=den[:, 0:4], in0=Ov[:, :, 64], scalar1=1e-30, scalar2=None,
                                                        op0=ALU.max), reads=[O.b], writes=[den.b])
        else:
            S.op("dve", lambda: nc.vector.tensor_scalar(out=den[:, 0:4], in0=Ov[:, :, 64], scalar1=add_ap, scalar2=1e-30,
                                                        op0=ALU.add, op1=ALU.max), reads=[O.b] + list(add_bufs), writes=[den.b])
        S.op("dve", lambda: nc.vector.reciprocal(out=den[:, 0:4], in_=den[:, 0:4]), reads=[den.b], writes=[den.b])

    def moba(self, l):
        nc, S = self.nc, self.S
        with ExitStack() as ph:
            KT = self.tile(ph, "KTm", [128, 2, S_LEN], BF16)
            V = self.tile(ph, "Vm", [128, 2, NT, 65], BF16)
            EM = self.tile(ph, "EMm", [32, S_LEN], BF16)
            BI = self.tile(ph, "BIm", [128, 4, 32], F32)
            S.load("sp", EM[:, :], self.dc["em"], EM.b)
            S.load("sp", BI[:, :, :], self.dc["bi"], BI.b)
            self.load_kv(KT, V, 67, 2, FRr["mk"], VCr["mv"], 2)
            R = self.attn_res(ph)
            QT = [self.tile(ph, "QTm%d" % i, [128, 2, 512], BF16) for i in range(2)]
            for q in QT:
                S.load("sp", q[64:67, :, :], self.dc["aug"][0:2].rearrange("h r q -> r h q"), q.b)
            kms = self.tile(ph, "kms", [64, 2, 32], F32)
            kmh = self.tile(ph, "kmh", [64, 2, 32], BF16)
            kml = self.tile(ph, "kml", [64, 2, 32], BF16)
            kmr = self.tile(ph, "kmr", [64, 2, 32], F32)
            for h in range(2):
                S.op("dve", lambda: nc.vector.tensor_reduce(out=kms[:, h, :], in_=KT[0:64, h, :].rearrange("p (n k) -> p n k", k=256),
                                                            axis=AX.X, op=ALU.add), reads=[KT.b], writes=[kms.b])
            S.op("dve", lambda: nc.vector.tensor_scalar(out=kms[:, :, :], in0=kms[:, :, :], scalar1=1.0 / 256, scalar2=None, op0=ALU.mult),
                 reads=[kms.b], writes=[kms.b])
            S.op("dve", lambda: nc.vector.tensor_copy(out=kmh[:, :, :], in_=kms[:, :, :]), reads=[kms.b], writes=[kmh.b])
            S.op("dve", lambda: nc.vector.tensor_tensor(out=kmr[:, :, :], in0=kms[:, :, :], in1=kmh[:, :, :], op=ALU.subtract),
                 reads=[kms.b, kmh.b], writes=[kmr.b])
            S.op("dve", lambda: nc.vector.tensor_copy(out=kml[:, :, :], in_=kmr[:, :, :]), reads=[kmr.b], writes=[kml.b])
            gps = self.tile(ph, "gps", [128, 512], F32, psum=True)
            tps = self.tile(ph, "tpsm", [128, 1024], BF16, psum=True)
            past = self.tile(ph, "past", [128, 4, 32], F32)
            own = self.tile(ph, "own", [128, 4, 32], F32)
            negp = self.tile(ph, "negp", [128, 4, 32], F32)
            gm = self.tile(ph, "gm", [128, 4, 32], F32)
            m8 = self.tile(ph, "m8", [128, 4, 8], F32)
            sel = self.tile(ph, "sel", [128, 4, 32], F32)
            nsel = self.tile(ph, "nsel", [128, 4, 32], BF16)
            nselT = [self.tile(ph, "nselT%d" % i, [32, 512], BF16) for i in range(2)]
            den = self.tile(ph, "denm", [128, 4], F32)
            mst = [self.tile(ph, "mstm%d" % i, [128, 4, 128], BF16) for i in range(2)]
            ih = 0
            for c in range(self.nch):
                q = QT[c % 2]
                for h in range(2):
                    S.load("sp", q[0:64, h, :], self.featT[FRr["mq"] + 64 * h:FRr["mq"] + 64 * (h + 1), c * 512:(c + 1) * 512], q.b)
                S.op("dve", lambda: nc.vector.tensor_scalar(out=past[:, :, :], in0=BI[:, :, :], scalar1=float(2 * c), scalar2=None, op0=ALU.is_lt),
                     reads=[BI.b], writes=[past.b])
                S.op("dve", lambda: nc.vector.tensor_scalar(out=own[:, :, :], in0=BI[:, :, :], scalar1=float(2 * c), scalar2=None, op0=ALU.is_equal),
                     reads=[BI.b], writes=[own.b])
                S.op("dve", lambda: nc.vector.tensor_scalar(out=negp[:, :, :], in0=past[:, :, :], scalar1=-1.0, scalar2=1e30, op0=ALU.add, op1=ALU.mult),
                     reads=[past.b], writes=[negp.b])
                ms = mst[c % 2]
                for h in range(2):
                    gv = gps[:, 0:128].rearrange("p (a b) -> p a b", a=4)
                    for qi in range(4):
                        S.op("pe", lambda: nc.tensor.matmul(gv[:, qi, :], lhsT=q[0:64, h, qi * 128:(qi + 1) * 128], rhs=kmh[:, h, :], start=True, stop=False),
                             reads=[q.b, kmh.b], writes=[gps.b], accum=(qi > 0))
                        S.op("pe", lambda: nc.tensor.matmul(gv[:, qi, :], lhsT=q[0:64, h, qi * 128:(qi + 1) * 128], rhs=kml[:, h, :], start=False, stop=True),
                             reads=[q.b, kml.b], writes=[gps.b], accum=True)
                    S.op("dve", lambda: nc.vector.tensor_tensor(out=gm[:, :, :], in0=gv, in1=negp[:, :, :], op=ALU.add),
                         reads=[gps.b, negp.b], writes=[gm.b])
                    for qi in range(4):
                        S.op("dve", lambda: nc.vector.max(out=m8[:, qi, :], in_=gm[:, qi, :]), reads=[gm.b], writes=[m8.b])
                    for qi in range(4):
                        S.op("dve", lambda: nc.vector.tensor_scalar(out=sel[:, qi, :], in0=gm[:, qi, :], scalar1=m8[:, qi, 2:3], scalar2=None, op0=ALU.is_ge),
                             reads=[gm.b, m8.b], writes=[sel.b])
                    S.op("dve", lambda: nc.vector.tensor_tensor(out=sel[:, :, :], in0=sel[:, :, :], in1=past[:, :, :], op=ALU.mult),
                         reads=[sel.b, past.b], writes=[sel.b])
                    S.op("dve", lambda: nc.vector.tensor_tensor(out=sel[:, :, :], in0=sel[:, :, :], in1=own[:, :, :], op=ALU.add),
                         reads=[sel.b, own.b], writes=[sel.b])
                    S.op("dve", lambda: nc.vector.tensor_scalar(out=nsel[:, :, :], in0=sel[:, :, :], scalar1=-1.0, scalar2=-NEG, op0=ALU.add, op1=ALU.mult),
                         reads=[sel.b], writes=[nsel.b])
                    nT = nselT[ih % 2]
                    ih += 1
                    for qi in range(4):
                        S.op("pe", lambda: nc.tensor.transpose(out=tps[0:32, qi * 128:(qi + 1) * 128], in_=nsel[:, qi, :], identity=self.ident[:, :]),
                             reads=[nsel.b, self.ident.b], writes=[tps.b], accum=(qi > 0))
                    self.copy(nT[:, :], tps[0:32, 0:512], [tps.b], [nT.b], eng="dve")
                    tiles = []
                    for kt in range(4 * c + 4):
                        if far_skip(0, h, 512 * c - (128 * kt + 127)):
                            continue
                        r = kt - 4 * c
                        ex = [(EM[:, kt * 128:(kt + 1) * 128], nT[:, :], [EM.b, nT.b])]
                        if r >= 0:
                            ex.append((self.ident[:, :], self.caus[:, r, :], [self.ident.b, self.caus.b]))
                        tiles.append(dict(kT=KT[0:67, h, kt * 128:(kt + 1) * 128], v=V[:, h, kt, :], kbufs=[KT.b, V.b],
                                          bias=self.kb[:, 0 + h, r + 60:r + 61], bbufs=[self.kb.b], extra=ex))
                    O = R.O[R.io % len(R.O)]
                    R.io += 1
                    Ov = self.attend(R, q[0:67, h, :], [q.b], tiles, O, 0.125)
                    self.recip_den(Ov, O, den)
                    for qi in range(4):
                        S.op("dve", lambda: nc.vector.tensor_scalar(out=ms[:, qi, h * 64:(h + 1) * 64], in0=Ov[:, qi, 0:64], scalar1=den[:, qi:qi + 1],
                                                                    scalar2=None, op0=ALU.mult), reads=[O.b, den.b], writes=[ms.b])
                S.store("pool", self.mix[c * 512:(c + 1) * 512, 0:128].rearrange("(a p) d -> p a d", p=128), ms[:, :, :], ms.b)
            S.barrier()

    def swa(self, l):
        nc, S = self.nc, self.S
        with ExitStack() as ph:
            KT = self.tile(ph, "KTs", [128, 1, S_LEN], BF16)
            V = self.tile(ph, "Vs", [128, 1, NT, 65], BF16)
            SM = self.tile(ph, "SMs", [128, 5, 512], BF16)
            S.load("sp", SM[:, :, :], self.dc["swamask"], SM.b)
            self.load_kv(KT, V, 67, 1, FRr["sk"], VCr["sv"], 1)
            sk = self.tile(ph, "sinks", [128, 4], F32)
            esk = self.tile(ph, "esinks", [128, 4], F32)
            S.load("sp", sk[:, :], self.dp["sinks"][l].partition_broadcast(128), sk.b)
            S.op("act", lambda: nc.scalar.activation(out=esk[:, :], in_=sk[:, :], func=AF.Exp), reads=[sk.b], writes=[esk.b])
            R = self.attn_res(ph)
            QT = [self.tile(ph, "QTs%d" % i, [128, 2, 512], BF16) for i in range(2)]
            for q in QT:
                S.load("sp", q[64:67, :, :], self.dc["aug"][12:14].rearrange("h r q -> r h q"), q.b)
            den = self.tile(ph, "dens", [128, 4], F32)
            mst = [self.tile(ph, "msts%d" % i, [128, 4, 128], BF16) for i in range(2)]
            for c in range(self.nch):
                q = QT[c % 2]
                for h in range(2):
                    S.load("sp", q[0:64, h, :], self.featT[FRr["sq"] + 64 * h:FRr["sq"] + 64 * (h + 1), c * 512:(c + 1) * 512], q.b)
                ms = mst[c % 2]
                for h in range(2):
                    g = 0
                    tiles = []
                    for r in range(-1, 4):
                        kt = 4 * c + r
                        if kt < 0:
                            continue
                        tiles.append(dict(kT=KT[0:67, g, kt * 128:(kt + 1) * 128], v=V[:, g, kt, :], kbufs=[KT.b, V.b],
                                          bias=self.kb[:, 12 + h, r + 60:r + 61], bbufs=[self.kb.b],
                                          extra=[(self.ident[:, :], SM[:, r + 1, :], [self.ident.b, SM.b])]))
                    O = R.O[R.io % len(R.O)]
                    R.io += 1
                    Ov = self.attend(R, q[0:67, h, :], [q.b], tiles, O, 0.125)
                    self.recip_den(Ov, O, den, add_ap=esk[:, h:h + 1], add_bufs=[esk.b])
                    for qi in range(4):
                        S.op("dve", lambda: nc.vector.tensor_scalar(out=ms[:, qi, h * 64:(h + 1) * 64], in0=Ov[:, qi, 0:64], scalar1=den[:, qi:qi + 1],
                                                                    scalar2=None, op0=ALU.mult), reads=[O.b, den.b], writes=[ms.b])
                S.store("pool", self.mix[c * 512:(c + 1) * 512, 384:512].rearrange("(a p) d -> p a d", p=128), ms[:, :, :], ms.b)
            S.barrier()

    def diff(self, l):
        nc, S = self.nc, self.S
        lambda_init = 0.8 - 0.6 * math.exp(-0.3 * l)
        sc = 32 ** -0.5
        with ExitStack() as ph:
            KT = self.tile(ph, "KTd", [128, 2, S_LEN], BF16)
            V = self.tile(ph, "Vd", [128, 2, NT, 65], BF16)
            S.op("pool", lambda: nc.gpsimd.memset(V[:, :, :, 64:65], 1.0), writes=[V.b])
            for h in range(2):
                r0 = FRr["dk"] + 64 * h
                S.load("sp", KT[0:32, h, :], self.featT[r0:r0 + 32, :], KT.b)
                S.load("sp", KT[64:96, h, :], self.featT[r0 + 32:r0 + 64, :], KT.b)
                S.load("sp", V[:, h, :, 0:64], self.vtok[:, VCr["dv"] + h * 64:VCr["dv"] + (h + 1) * 64].rearrange("(kt p) d -> p kt d", p=128), V.b)
            ones = self.dc["ones3"][:, 0:2 * S_LEN].rearrange("r (h s) -> r h s", h=2)
            S.load("sp", KT[32:35, :, :], ones, KT.b)
            S.load("sp", KT[96:99, :, :], ones, KT.b)
            lam = self.tile(ph, "lam", [128, 4], F32)
            lt = self.tile(ph, "lamt", [128, 4, 32], F32)
            lj = self.tile(ph, "lamj", [128, 32], F32)
            for i, nme in enumerate(["lq1", "lk1", "lq2", "lk2"]):
                S.load("sp", lt[:, i, :], self.dp[nme][l].partition_broadcast(128), lt.b)
            for i in range(2):
                S.op("dve", lambda: nc.vector.scalar_tensor_tensor(out=lj[:, :], in0=lt[:, 2 * i, :], scalar=1.0, in1=lt[:, 2 * i + 1, :],
                                                                   op0=ALU.mult, op1=ALU.mult, accum_out=lam[:, i:i + 1]),
                     reads=[lt.b], writes=[lj.b, lam.b])
            S.op("act", lambda: nc.scalar.activation(out=lam[:, 0:2], in_=lam[:, 0:2], func=AF.Exp), reads=[lam.b], writes=[lam.b])
            S.op("dve", lambda: nc.vector.tensor_tensor(out=lam[:, 2:3], in0=lam[:, 1:2], in1=lam[:, 0:1], op=ALU.subtract),
                 reads=[lam.b], writes=[lam.b])
            S.op("dve", lambda: nc.vector.tensor_scalar(out=lam[:, 2:3], in0=lam[:, 2:3], scalar1=-lambda_init, scalar2=None, op0=ALU.add),
                 reads=[lam.b], writes=[lam.b])
            gs = self.tile(ph, "gsub", [128, 64], F32)
            S.load("sp", gs[:, :], self.dp["subln"][l].partition_broadcast(128), gs.b)
            S.op("dve", lambda: nc.vector.tensor_scalar(out=gs[:, :], in0=gs[:, :], scalar1=1.0 - lambda_init, scalar2=None, op0=ALU.mult),
                 reads=[gs.b], writes=[gs.b])
            R = self.attn_res(ph, no=4)
            QT = [self.tile(ph, "QTd%d" % i, [128, 2, 512], BF16) for i in range(2)]
            for q in QT:
                a = self.dc["aug"][8:10].rearrange("h r q -> r h q")
                S.load("sp", q[32:35, :, :], a, q.b)
                S.load("sp", q[96:99, :, :], a, q.b)
            den1 = self.tile(ph, "den1", [128, 4], F32)
            den2 = self.tile(ph, "den2", [128, 4], F32)
            o1 = self.tile(ph, "o1d", [128, 4, 64], F32)
            od = self.tile(ph, "od", [128, 4, 64], F32)
            jk = self.tile(ph, "jkd", [128, 64], F32)
            ssd = self.tile(ph, "ssd", [128, 4], F32)
            mst = [self.tile(ph, "mstd%d" % i, [128, 4, 128], BF16) for i in range(2)]
            for c in range(self.nch):
                q = QT[c % 2]
                for h in range(2):
                    r0 = FRr["dq"] + 64 * h
                    S.load("sp", q[0:32, h, :], self.featT[r0:r0 + 32, c * 512:(c + 1) * 512], q.b)
                    S.load("sp", q[64:96, h, :], self.featT[r0 + 32:r0 + 64, c * 512:(c + 1) * 512], q.b)
                ms = mst[c % 2]
                for h in range(2):
                    Os = []
                    for j in range(2):
                        b0 = 64 * j
                        tiles = []
                        for kt in range(4 * c + 4):
                            if far_skip(2, h, 512 * c - (128 * kt + 127)):
                                continue
                            r = kt - 4 * c
                            ex = []
                            if r >= 0:
                                ex.append((self.ident[:, :], self.caus[:, r, :], [self.ident.b, self.caus.b]))
                            tiles.append(dict(kT=KT[b0:b0 + 35, h, kt * 128:(kt + 1) * 128], v=V[:, h, kt, :], kbufs=[KT.b, V.b],
                                              bias=self.kb[:, 8 + h, r + 60:r + 61], bbufs=[self.kb.b], extra=ex))
                        O = R.O[R.io % len(R.O)]
                        R.io += 1
                        Ov = self.attend(R, q[b0:b0 + 35, h, :], [q.b], tiles, O, sc)
                        Os.append((O, Ov))
                    (O1, Ov1), (O2, Ov2) = Os
                    self.recip_den(Ov1, O1, den1)
                    self.recip_den(Ov2, O2, den2)
                    S.op("dve", lambda: nc.vector.tensor_scalar(out=den2[:, :], in0=den2[:, :], scalar1=lam[:, 2:3], scalar2=None, op0=ALU.mult),
                         reads=[den2.b, lam.b], writes=[den2.b])
                    for qi in range(4):
                        S.op("dve", lambda: nc.vector.tensor_scalar(out=o1[:, qi, :], in0=Ov1[:, qi, 0:64], scalar1=den1[:, qi:qi + 1], scalar2=None, op0=ALU.mult),
                             reads=[O1.b, den1.b], writes=[o1.b])
                        S.op("dve", lambda: nc.vector.scalar_tensor_tensor(out=od[:, qi, :], in0=Ov2[:, qi, 0:64], scalar=den2[:, qi:qi + 1], in1=o1[:, qi, :],
                                                                           op0=ALU.mult, op1=ALU.add), reads=[O2.b, den2.b, o1.b], writes=[od.b])
                        S.op("dve", lambda: nc.vector.scalar_tensor_tensor(out=jk[:, :], in0=od[:, qi, :], scalar=1.0, in1=od[:, qi, :],
                                                                           op0=ALU.mult, op1=ALU.mult, accum_out=ssd[:, qi:qi + 1]),
                             reads=[od.b], writes=[jk.b, ssd.b])
                    S.op("act", lambda: nc.scalar.activation(out=ssd[:, :], in_=ssd[:, :], func=AF.Ln, bias=self.epsT[:, 0:1], scale=1.0 / 64),
                         reads=[ssd.b, self.epsT.b], writes=[ssd.b])
                    S.op("act", lambda: nc.scalar.activation(out=ssd[:, :], in_=ssd[:, :], func=AF.Exp, scale=-0.5), reads=[ssd.b], writes=[ssd.b])
                    for qi in range(4):
                        S.op("dve", lambda: nc.vector.scalar_tensor_tensor(out=ms[:, qi, h * 64:(h + 1) * 64], in0=od[:, qi, :], scalar=ssd[:, qi:qi + 1],
                                                                           in1=gs[:, :], op0=ALU.mult, op1=ALU.mult),
                             reads=[od.b, ssd.b, gs.b], writes=[ms.b])
                S.store("pool", self.mix[c * 512:(c + 1) * 512, 256:384].rearrange("(a p) d -> p a d", p=128), ms[:, :, :], ms.b)
            S.barrier()

    def nsa(self, l):
        nc, S = self.nc, self.S
        with ExitStack() as ph:
            KCT = self.tile(ph, "KCT", [128, 512], BF16)
            VCt = self.tile(ph, "VCt", [128, 4, 65], BF16)
            S.op("pool", lambda: nc.gpsimd.memset(VCt[:, :, :], 1.0), writes=[VCt.b])
            S.load("sp", KCT[64:67, :], self.dc["ones3"][:, 0:512], KCT.b)
            with ExitStack() as cs:
                XT = self.tile(cs, "XTc", [64, S_LEN], BF16)
                w1 = self.tile(cs, "w1c", [64, 32, 128], BF16)
                w2 = self.tile(cs, "w2c", [128, 64], BF16)
                pos = self.tile(cs, "posc", [64, 32], BF16)
                b1 = self.tile(cs, "b1c", [128, 1], F32)
                cbias = self.tile(cs, "cbias", [128, 1], F32)
                hid = self.tile(cs, "hidc", [128, 512], BF16)
                sg = self.tile(cs, "sgc", [128, 4096], F32)
                hp = self.tile(cs, "hpc", [128, 512], F32, psum=True)
                cp = self.tile(cs, "cpc", [128, 512], F32, psum=True)
                op_ = self.tile(cs, "opc", [128, 512], F32, psum=True)
                for s_i, sfx in enumerate("kv"):
                    S.load("sp", XT[:, :], self.featT[FRr["nkc" if sfx == "k" else "nvc"]:FRr["nkc" if sfx == "k" else "nvc"] + 64, :], XT.b)
                    S.load("sp", sg[0:64, 0:4096], self.dp["w1" + sfx][l].rearrange("d l f -> d (l f)"), sg.b)
                    self.copy(w1[:, :, :], sg[0:64, 0:4096].rearrange("d (l f) -> d l f", l=32), [sg.b], [w1.b])
                    S.load("sp", sg[:, 0:64], self.dp["w2" + sfx][l], sg.b)
                    self.copy(w2[:, :], sg[:, 0:64], [sg.b], [w2.b])
                    S.load("sp", sg[0:64, 0:32], self.dp["pos" + sfx][l], sg.b)
                    self.copy(pos[:, :], sg[0:64, 0:32], [sg.b], [pos.b])
                    S.load("sp", b1[:, :], self.dp["b1" + sfx][l], b1.b)
                    for li in range(32):
                        S.op("pe", lambda: nc.tensor.matmul(hp[:, 0:511], lhsT=w1[:, li, :], rhs=XT[:, li:li + 16 * 510 + 1:16], start=(li == 0), stop=(li == 31)),
                             reads=[w1.b, XT.b], writes=[hp.b], accum=(li > 0))
                    for li in range(32):
                        S.op("pe", lambda: nc.tensor.matmul(cp[:, 0:1], lhsT=w1[:, li, :], rhs=pos[:, li:li + 1], start=(li == 0), stop=(li == 31)),
                             reads=[w1.b, pos.b], writes=[cp.b], accum=(li > 0))
                    S.op("dve", lambda: nc.vector.tensor_tensor(out=cbias[:, :], in0=cp[:, 0:1], in1=b1[:, :], op=ALU.add),
                         reads=[cp.b, b1.b], writes=[cbias.b])
                    S.op("pool", lambda: nc.gpsimd.memset(hid[:, :], 0.0), writes=[hid.b])
                    S.op("act", lambda: nc.scalar.activation(out=hid[:, 0:511], in_=hp[:, 0:511], func=AF.Gelu_apprx_tanh, bias=cbias[:, 0:1]),
                         reads=[hp.b, cbias.b], writes=[hid.b])
                    if sfx == "k":
                        S.op("pe", lambda: nc.tensor.matmul(op_[0:64, 0:512], lhsT=w2[:, :], rhs=hid[:, :], start=True, stop=True),
                             reads=[w2.b, hid.b], writes=[op_.b])
                        self.copy(KCT[0:64, :], op_[0:64, 0:512], [op_.b], [KCT.b])
                    else:
                        ov_ = op_[:, 0:256].rearrange("p (a b) -> p a b", a=4)
                        for kt in range(4):
                            S.op("pe", lambda: nc.tensor.matmul(ov_[:, kt, :], lhsT=hid[:, kt * 128:(kt + 1) * 128], rhs=w2[:, :], start=True, stop=True),
                                 reads=[w2.b, hid.b], writes=[op_.b], accum=(kt > 0))
                        self.copy(VCt[:, :, 0:64], ov_, [op_.b], [VCt.b])
                S.barrier()
            KTs = self.tile(ph, "KTsl", [128, 1, S_LEN], BF16)
            KTw = self.tile(ph, "KTwn", [128, 1, S_LEN], BF16)
            Vs = self.tile(ph, "Vsl", [128, 1, NT, 65], BF16)
            Vw = self.tile(ph, "Vwn", [128, 1, NT, 65], BF16)
            self.load_kv(KTs, Vs, 67, 1, FRr["nks"], VCr["nvs"], 1)
            self.load_kv(KTw, Vw, 67, 1, FRr["nkw"], VCr["nvw"], 1)
            ES = self.tile(ph, "ESn", [128, S_LEN], BF16)
            WM = self.tile(ph, "WMn", [128, 8, 512], BF16)
            CM = self.tile(ph, "CMn", [128, 5, 512], BF16)
            OVt = self.tile(ph, "OVn", [128, 4, 128], BF16)
            KBC = self.tile(ph, "KBCn", [128, 4, 16, 4], F32)
            S.load("sp", ES[:, :], self.dc["es"], ES.b)
            S.load("sp", WM[:, :, :], self.dc["wmask"], WM.b)
            S.load("sp", CM[:, :, :], self.dc["cmask"], CM.b)
            S.load("sp", OVt[:, :, :], self.dc["ov"], OVt.b)
            S.load("sp", KBC[:, :, :, :], self.dc["kbc"], KBC.b)
            R = self.attn_res(ph)
            QT = [self.tile(ph, "QTn%d" % i, [128, 4, 512], BF16) for i in range(2)]
            for q in QT:
                S.load("sp", q[64:67, :, :], self.dc["aug"][4:8].rearrange("h r q -> r h q"), q.b)
            imps = self.tile(ph, "imps", [128, 512], F32, psum=True)
            tps = self.tile(ph, "tpsn", [128, 1024], BF16, psum=True)
            impa = self.tile(ph, "impa", [128, 4, 128], F32)
            f1 = [self.tile(ph, "f1e4_%d" % i, [128, 4, 128], F32) for i in range(2)]
            nv = [self.tile(ph, "negv_%d" % i, [128, 4, 128], F32) for i in range(2)]
            gr = [self.tile(ph, "graw%d" % i, [128, 4, 12], BF16) for i in range(2)]
            sgm = self.tile(ph, "sgm", [128, 4, 12], F32)
            scm = self.tile(ph, "scm", [128, 4, 128], F32)
            tmpm = self.tile(ph, "tmpm", [128, 4, 128], F32)
            m8a = self.tile(ph, "m8a", [128, 4, 8], F32)
            m8b = self.tile(ph, "m8b", [128, 4, 8], F32)
            sel = self.tile(ph, "seln", [128, 4, 128], F32)
            val = self.tile(ph, "valn", [128, 4, 128], F32)
            nsel = self.tile(ph, "nseln", [128, 4, 128], BF16)
            nselT = self.tile(ph, "nselTn", [128, 512], BF16)
            den = self.tile(ph, "denn", [128, 4], F32)
            acc = self.tile(ph, "accn", [128, 2, 4, 64], F32)
            mst = [self.tile(ph, "mstn%d" % i, [128, 4, 128], BF16) for i in range(2)]

            def fold(O, Ov, h, gcol, first):
                self.recip_den(Ov, O, den)
                S.op("dve", lambda: nc.vector.tensor_tensor(out=den[:, :], in0=den[:, :], in1=sgm[:, :, gcol], op=ALU.mult),
                     reads=[den.b, sgm.b], writes=[den.b])
                for qi in range(4):
                    if first:
                        S.op("dve", lambda: nc.vector.tensor_scalar(out=acc[:, h, qi, :], in0=Ov[:, qi, 0:64], scalar1=den[:, qi:qi + 1], scalar2=None, op0=ALU.mult),
                             reads=[O.b, den.b], writes=[acc.b])
                    else:
                        S.op("dve", lambda: nc.vector.scalar_tensor_tensor(out=acc[:, h, qi, :], in0=Ov[:, qi, 0:64], scalar=den[:, qi:qi + 1], in1=acc[:, h, qi, :],
                                                                           op0=ALU.mult, op1=ALU.add), reads=[O.b, den.b, acc.b], writes=[acc.b])

            for c in range(self.nch):
                q = QT[c % 2]
                for h in range(4):
                    S.load("sp", q[0:64, h, :], self.featT[FRr["nq"] + 64 * h:FRr["nq"] + 64 * (h + 1), c * 512:(c + 1) * 512], q.b)
                f1c, nvc, grc = f1[c % 2], nv[c % 2], gr[c % 2]
                S.load("sp", f1c[:, :, :], self.dc["f1e4"][c], f1c.b)
                S.load("sp", nvc[:, :, :], self.dc["negv"][c], nvc.b)
                S.load("sp", grc[:, :, :], self.vtok[c * 512:(c + 1) * 512, VCr["ng"]:VCr["ng"] + 12].rearrange("(a p) g -> p a g", p=128), grc.b)
                S.op("act", lambda: nc.scalar.activation(out=sgm[:, :, :], in_=grc[:, :, :], func=AF.Exp, scale=-1.0), reads=[grc.b], writes=[sgm.b])
                S.op("dve", lambda: nc.vector.tensor_scalar(out=sgm[:, :, :], in0=sgm[:, :, :], scalar1=1.0, scalar2=None, op0=ALU.add),
                     reads=[sgm.b], writes=[sgm.b])
                S.op("dve", lambda: nc.vector.reciprocal(out=sgm[:, :, :], in_=sgm[:, :, :]), reads=[sgm.b], writes=[sgm.b])
                ms = mst[c % 2]
                kb_ = c // 4
                iv = imps[:, :].rearrange("p (a b) -> p a b", a=4)
                for h in range(4):
                    tiles = []
                    ntl = kb_ + 1

                    def mkpost(kt, ntl=ntl):
                        def post(pt, first, last):
                            for qi in range(4):
                                S.op("pe", lambda: nc.tensor.matmul(iv[:, qi, :], lhsT=pt[:, qi * 128:(qi + 1) * 128], rhs=OVt[:, kt, :],
                                                                    start=(first and qi == 0), stop=last),
                                     reads=[pt.b, OVt.b], writes=[imps.b], accum=not (first and qi == 0))
                        return post
                    for kt in range(ntl):
                        ex = []
                        if kt == kb_:
                            ex.append((self.ident[:, :], CM[:, c % 4, :], [self.ident.b, CM.b]))
                        elif kt == kb_ - 1 and c % 4 == 0:
                            ex.append((self.ident[:, :], CM[:, 4, :], [self.ident.b, CM.b]))
                        tiles.append(dict(kT=KCT[0:67, kt * 128:(kt + 1) * 128], v=VCt[:, kt, :], kbufs=[KCT.b, VCt.b],
                                          bias=KBC[:, h, c, kt:kt + 1], bbufs=[KBC.b], extra=ex, post=mkpost(kt)))
                    O = R.O[R.io % len(R.O)]
                    R.io += 1
                    Ov = self.attend(R, q[0:67, h, :], [q.b], tiles, O, 0.125)
                    self.recip_den(Ov, O, den)
                    for qi in range(4):
                        if h == 0:
                            S.op("dve", lambda: nc.vector.tensor_scalar(out=impa[:, qi, :], in0=iv[:, qi, :], scalar1=den[:, qi:qi + 1], scalar2=None, op0=ALU.mult),
                                 reads=[imps.b, den.b], writes=[impa.b])
                        else:
                            S.op("dve", lambda: nc.vector.scalar_tensor_tensor(out=impa[:, qi, :], in0=iv[:, qi, :], scalar=den[:, qi:qi + 1], in1=impa[:, qi, :],
                                                                               op0=ALU.mult, op1=ALU.add), reads=[imps.b, den.b, impa.b], writes=[impa.b])
                    if h < 2:
                        fold(O, Ov, h, 3 * h + 0, True)
                S.op("dve", lambda: nc.vector.tensor_tensor(out=scm[:, :, :], in0=impa[:, :, :], in1=f1c[:, :, :], op=ALU.max),
                     reads=[impa.b, f1c.b], writes=[scm.b])
                S.op("dve", lambda: nc.vector.tensor_tensor(out=scm[:, :, :], in0=scm[:, :, :], in1=nvc[:, :, :], op=ALU.add),
                     reads=[scm.b, nvc.b], writes=[scm.b])
                for qi in range(4):
                    S.op("dve", lambda: nc.vector.max(out=m8a[:, qi, :], in_=scm[:, qi, :]), reads=[scm.b], writes=[m8a.b])
                    S.op("dve", lambda: nc.vector.match_replace(out=tmpm[:, qi, :], in_to_replace=m8a[:, qi, :], in_values=scm[:, qi, :], imm_value=-1e30),
                         reads=[scm.b, m8a.b], writes=[tmpm.b])
                    S.op("dve", lambda: nc.vector.max(out=m8b[:, qi, :], in_=tmpm[:, qi, :]), reads=[tmpm.b], writes=[m8b.b])
                    S.op("dve", lambda: nc.vector.tensor_scalar(out=sel[:, qi, :], in0=scm[:, qi, :], scalar1=m8b[:, qi, 7:8], scalar2=None, op0=ALU.is_ge),
                         reads=[scm.b, m8b.b], writes=[sel.b])
                S.op("dve", lambda: nc.vector.tensor_scalar(out=val[:, :, :], in0=nvc[:, :, :], scalar1=-1.0, scalar2=None, op0=ALU.is_ge),
                     reads=[nvc.b], writes=[val.b])
                S.op("dve", lambda: nc.vector.tensor_tensor(out=sel[:, :, :], in0=sel[:, :, :], in1=val[:, :, :], op=ALU.mult),
                     reads=[sel.b, val.b], writes=[sel.b])
                S.op("dve", lambda: nc.vector.tensor_scalar(out=nsel[:, :, :], in0=sel[:, :, :], scalar1=-1.0, scalar2=-NEG, op0=ALU.add, op1=ALU.mult),
                     reads=[sel.b], writes=[nsel.b])
                for qi in range(4):
                    S.op("pe", lambda: nc.tensor.transpose(out=tps[:, qi * 128:(qi + 1) * 128], in_=nsel[:, qi, :], identity=self.ident[:, :]),
                         reads=[nsel.b, self.ident.b], writes=[tps.b], accum=(qi > 0))
                self.copy(nselT[:, :], tps[:, 0:512], [tps.b], [nselT.b], eng="dve")
                for h in range(2):
                    tiles = []
                    for kt in range(4 * c + 4):
                        if far_skip(1, h, 512 * c - (128 * kt + 127)):
                            continue
                        r = kt - 4 * c
                        ex = [(ES[:, kt * 128:(kt + 1) * 128], nselT[:, :], [ES.b, nselT.b])]
                        if r >= 0:
                            ex.append((self.ident[:, :], self.caus[:, r, :], [self.ident.b, self.caus.b]))
                        tiles.append(dict(kT=KTs[0:67, 0, kt * 128:(kt + 1) * 128], v=Vs[:, 0, kt, :], kbufs=[KTs.b, Vs.b],
                                          bias=self.kb[:, 4 + h, r + 60:r + 61], bbufs=[self.kb.b], extra=ex))
                    O = R.O[R.io % len(R.O)]
                    R.io += 1
                    Ov = self.attend(R, q[0:67, h, :], [q.b], tiles, O, 0.125)
                    fold(O, Ov, h, 3 * h + 1, False)
                    tiles = []
                    for r in range(-4, 4):
                        kt = 4 * c + r
                        if kt < 0:
                            continue
                        tiles.append(dict(kT=KTw[0:67, 0, kt * 128:(kt + 1) * 128], v=Vw[:, 0, kt, :], kbufs=[KTw.b, Vw.b],
                                          bias=self.kb[:, 4 + h, r + 60:r + 61], bbufs=[self.kb.b],
                                          extra=[(self.ident[:, :], WM[:, r + 4, :], [self.ident.b, WM.b])]))
                    O = R.O[R.io % len(R.O)]
                    R.io += 1
                    Ov = self.attend(R, q[0:67, h, :], [q.b], tiles, O, 0.125)
                    fold(O, Ov, h, 3 * h + 2, False)
                    S.op("dve", lambda: nc.vector.tensor_copy(out=ms[:, :, h * 64:(h + 1) * 64], in_=acc[:, h, :, :]), reads=[acc.b], writes=[ms.b])
                S.store("pool", self.mix[c * 512:(c + 1) * 512, 128:256].rearrange("(a p) d -> p a d", p=128), ms[:, :, :], ms.b)
            S.barrier()

    def prep_ffn(self, l):
        S = self.S
        with ExitStack() as ph:
            stg = [self.tile(ph, "pstg%d" % i, [128, 4096], F32) for i in range(2)]
            sb = [self.tile(ph, "psb%d" % i, [128, 4096], BF16) for i in range(2)]
            i = 0
            for src, dst in ((self.dp["wg"], self.wgs), (self.dp["wu"], self.wus), (self.dp["wd"], self.wds)):
                for g in range(NCF // 4):
                    s_, b_ = stg[i % 2], sb[i % 2]
                    i += 1
                    if len(src.shape) == 5:
                        sap = src[l, 4 * g:4 * g + 4].rearrange("c p k n -> p c (k n)")
                        dap = dst[4 * g:4 * g + 4].rearrange("c p k n -> p c (k n)")
                    else:
                        sap = src[l, 4 * g:4 * g + 4].rearrange("c p n -> p c n")
                        dap = dst[4 * g:4 * g + 4].rearrange("c p n -> p c n")
                    S.load("sp", s_[:, :].rearrange("p (c n) -> p c n", c=4), sap, s_.b)
                    self.copy(b_[:, :], s_[:, :], [s_.b], [b_.b])
                    S.store("pool", dap, b_[:, :].rearrange("p (c n) -> p c n", c=4), b_.b)
            S.barrier()

    def phase_C(self, l, x_src, x_dst):
        nc, S = self.nc, self.S
        with ExitStack() as ph:
            WO = self.tile(ph, "WO", [128, 8, D], BF16)
            with ExitStack() as tmp:
                self.load_cast(tmp, WO, lambda i: WO[:, i, :], lambda i: self.dp["wo"][l, :, i, :], 8, D)
                S.barrier()
            g1 = self.tile(ph, "g_apost", [128, D], F32)
            g2 = self.tile(ph, "g_fpre", [128, D], F32)
            g3 = self.tile(ph, "g_fpost", [128, D], F32)
            S.load("sp", g1[:, :], self.dp["g_apost"][l].partition_broadcast(128), g1.b)
            S.load("sp", g2[:, :], self.dp["g_fpre"][l].partition_broadcast(128), g2.b)
            S.load("sp", g3[:, :], self.dp["g_fpost"][l].partition_broadcast(128), g3.b)
            cw = self.tile(ph, "cw", [128, NCF, 3], F32)
            cb = self.tile(ph, "cb", [128, NCF], F32)
            S.load("sp", cw[:, :, :], self.dp["cw"][l], cw.b)
            S.load("sp", cb[:, :], self.dp["cb"][l], cb.b)
            halo = self.tile(ph, "halo", [128, NCF, 2], F32)
            S.op("pool", lambda: nc.gpsimd.memset(halo[:, :, :], 0.0), writes=[halo.b])
            bank = [self.tile(ph, "bk%d" % i, [128, 512], F32, psum=True) for i in range(6)]
            tpb = [self.tile(ph, "tpC%d" % i, [128, D], BF16, psum=True) for i in range(2)]
            mixt = [self.tile(ph, "mixt%d" % i, [128, D], BF16) for i in range(2)]
            mT = [self.tile(ph, "mT%d" % i, [128, 8, 128], BF16) for i in range(2)]
            xt = [self.tile(ph, "xtC%d" % i, [128, D], F32) for i in range(2)]
            x1 = [self.tile(ph, "x1C%d" % i, [128, D], F32) for i in range(2)]
            yt = self.tile(ph, "ytC", [128, D], F32)
            junk = self.tile(ph, "junkC", [128, D], F32)
            ss = self.tile(ph, "ssC", [128, 1], F32)
            sd = self.tile(ph, "sdC", [128, 1], F32)
            rstd = self.tile(ph, "rstdC", [128, 1], F32)
            hb = [self.tile(ph, "hbC%d" % i, [128, D], BF16) for i in range(4)]
            h2T = self.tile(ph, "h2T", [128, 8, 512], BF16)
            gT = self.tile(ph, "gT", [128, NCF, 512], BF16)
            wgt = [self.tile(ph, "wgt%d" % i, [128, 8, 128], BF16) for i in range(3)]
            wut = [self.tile(ph, "wut%d" % i, [128, 8, 128], BF16) for i in range(3)]
            wdt = [self.tile(ph, "wdt%d" % i, [128, D], BF16) for i in range(3)]
            aT = [self.tile(ph, "aT%d" % i, [128, 514], F32) for i in range(2)]
            cacc = [self.tile(ph, "cacc%d" % i, [128, 512], F32) for i in range(2)]
            ga = [self.tile(ph, "ga%d" % i, [128, 512], F32) for i in range(2)]
            pst = [self.tile(ph, "pstC%d" % i, [128, D], F32) for i in range(2)]
            rt = [self.tile(ph, "rtC%d" % i, [128, D], F32) for i in range(2)]
            x1r = [self.tile(ph, "x1rC%d" % i, [128, D], F32) for i in range(2)]
            ot = [self.tile(ph, "otC%d" % i, [128, D], F32) for i in range(2)]
            ss3 = self.tile(ph, "ss3C", [128, 1], F32)
            sd3 = self.tile(ph, "sd3C", [128, 1], F32)
            rstd3 = self.tile(ph, "rstd3C", [128, 1], F32)
            junk3 = self.tile(ph, "junk3C", [128, D], F32)
            it = 0
            i3 = 0
            RNG = 2
            pending = []
            store_tags = []

            def finish_tile(tag, tt):
                nonlocal i3
                S._wait("sp", tag)
                tok = tt * 128
                r_, x_, o_ = rt[i3 % 2], x1r[i3 % 2], ot[i3 % 2]
                i3 += 1
                S.load("sp", r_[:, :], self.red[tok:tok + 128, :], r_.b)
                S.load("sp", x_[:, :], self.x1s[tok:tok + 128, :], x_.b)
                self.rstd_of(r_[:, :], r_.b, junk3, ss3, sd3, rstd3)
                S.op("dve", lambda: nc.vector.scalar_tensor_tensor(out=r_[:, :], in0=r_[:, :], scalar=rstd3[:, 0:1], in1=g3[:, :], op0=ALU.mult, op1=ALU.mult),
                     reads=[r_.b, rstd3.b, g3.b], writes=[r_.b])
                S.op("pool", lambda: nc.gpsimd.tensor_tensor(out=o_[:, :], in0=r_[:, :], in1=x_[:, :], op=ALU.add),
                     reads=[r_.b, x_.b], writes=[o_.b])
                S.store("pool", x_dst[tok:tok + 128, :], o_[:, :], o_.b)

            h2Ts = [h2T, self.tile(ph, "h2Tb", [128, 8, 512], BF16)]

            def seg_a(c, ti):
                nonlocal it
                tok = (4 * c + ti) * 128
                mt, mTt, xtt, h, tpp, x1t = mixt[it % 2], mT[it % 2], xt[it % 2], hb[it % 4], tpb[it % 2], x1[it % 2]
                it += 1
                mrow = (tok // 2048) * 4096 + (tok % 2048)
                S.load("sp", mt[:, 0:512], self.mixall[mrow:mrow + 128, :], mt.b)
                S.load("sp", mt[:, 512:1024], self.mixall[2048 + mrow:2048 + mrow + 128, :], mt.b)
                S.load("sp", xtt[:, :], x_src[tok:tok + 128, :], xtt.b)
                for kc in range(8):
                    S.op("pe", lambda: nc.tensor.transpose(out=tpp[:, kc * 128:(kc + 1) * 128], in_=mt[:, kc * 128:(kc + 1) * 128], identity=self.ident[:, :]),
                         reads=[mt.b, self.ident.b], writes=[tpp.b], accum=(kc > 0))
                self.copy(mTt[:, :, :], tpp[:, :].rearrange("p (k t) -> p k t", k=8), [tpp.b], [mTt.b])
                pa, pb = bank[4], bank[5]
                for half, pp in enumerate((pa, pb)):
                    for kc in range(8):
                        S.op("pe", lambda: nc.tensor.matmul(pp[:, :], lhsT=mTt[:, kc, :], rhs=WO[:, kc, half * 512:(half + 1) * 512],
                                                            start=(kc == 0), stop=(kc == 7)), reads=[mTt.b, WO.b], writes=[pp.b], accum=(kc > 0))
                self.copy(yt[:, 0:512], pa[:, :], [pa.b], [yt.b], eng="act")
                self.copy(yt[:, 512:D], pb[:, :], [pb.b], [yt.b], eng="act")
                self.rstd_of(yt[:, :], yt.b, junk, ss, sd, rstd)
                S.op("dve", lambda: nc.vector.scalar_tensor_tensor(out=yt[:, :], in0=yt[:, :], scalar=rstd[:, 0:1], in1=g1[:, :], op0=ALU.mult, op1=ALU.mult),
                     reads=[yt.b, rstd.b, g1.b], writes=[yt.b])
                S.op("pool", lambda: nc.gpsimd.tensor_tensor(out=x1t[:, :], in0=yt[:, :], in1=xtt[:, :], op=ALU.add),
                     reads=[yt.b, xtt.b], writes=[x1t.b])
                S.store("pool", self.x1s[tok:tok + 128, :], x1t[:, :], x1t.b)
                store_tags.append(("dma", x1t.b.dsem, x1t.b.dcnt))
                self.rstd_of(x1t[:, :], x1t.b, junk, ss, sd, rstd)
                S.op("dve", lambda: nc.vector.scalar_tensor_tensor(out=h[:, :], in0=x1t[:, :], scalar=rstd[:, 0:1], in1=g2[:, :], op0=ALU.mult, op1=ALU.mult),
                     reads=[x1t.b, rstd.b, g2.b], writes=[h.b])
                return (h, tpp)

            def seg_b(c, ti, ctx):
                h, tpp = ctx
                hT = h2Ts[c % 2]
                for kc in range(8):
                    S.op("pe", lambda: nc.tensor.transpose(out=tpp[:, kc * 128:(kc + 1) * 128], in_=h[:, kc * 128:(kc + 1) * 128], identity=self.ident[:, :]),
                         reads=[h.b, self.ident.b], writes=[tpp.b], accum=(kc > 0))
                self.copy(hT[:, :, ti * 128:(ti + 1) * 128], tpp[:, :].rearrange("p (k t) -> p k t", k=8), [tpp.b], [hT.b])

            def c2a_cf(c, cf):
                hT = h2Ts[c % 2]
                wg_, wu_ = wgt[cf % 3], wut[cf % 3]
                S.load("sp", wg_[:, :, :], self.wgs[cf], wg_.b)
                S.load("sp", wu_[:, :, :], self.wus[cf], wu_.b)
                pa, pu = bank[2 * (cf % 2)], bank[2 * (cf % 2) + 1]
                for kc in range(8):
                    S.op("pe", lambda: nc.tensor.matmul(pa[:, :], lhsT=wg_[:, kc, :], rhs=hT[:, kc, :], start=(kc == 0), stop=(kc == 7)),
                         reads=[wg_.b, hT.b], writes=[pa.b], accum=(kc > 0))
                for kc in range(8):
                    S.op("pe", lambda: nc.tensor.matmul(pu[:, :], lhsT=wu_[:, kc, :], rhs=hT[:, kc, :], start=(kc == 0), stop=(kc == 7)),
                         reads=[wu_.b, hT.b], writes=[pu.b], accum=(kc > 0))
                a, ca, g = aT[cf % 2], cacc[cf % 2], ga[cf % 2]
                e4 = a[:, 0:4]
                S.op("dve", lambda: nc.vector.tensor_copy(out=a[:, 0:2], in_=halo[:, cf, :]), reads=[halo.b], writes=[a.b])
                S.op("dve", lambda: nc.vector.tensor_copy(out=a[:, 2:4], in_=pa[:, 0:2]), reads=[pa.b], writes=[a.b])
                S.op("dve", lambda: nc.vector.tensor_copy(out=halo[:, cf, :], in_=pa[:, 510:512]), reads=[pa.b], writes=[halo.b])
                S.op("dve", lambda: nc.vector.tensor_scalar(out=ca[:, 2:512], in0=pa[:, 0:510], scalar1=cw[:, cf, 0:1], scalar2=None, op0=ALU.mult),
                     reads=[pa.b, cw.b], writes=[ca.b])
                S.op("dve", lambda: nc.vector.scalar_tensor_tensor(out=ca[:, 2:512], in0=pa[:, 1:511], scalar=cw[:, cf, 1:2], in1=ca[:, 2:512], op0=ALU.mult, op1=ALU.add),
                     reads=[pa.b, cw.b, ca.b], writes=[ca.b])
                S.op("dve", lambda: nc.vector.scalar_tensor_tensor(out=ca[:, 2:512], in0=pa[:, 2:512], scalar=cw[:, cf, 2:3], in1=ca[:, 2:512], op0=ALU.mult, op1=ALU.add),
                     reads=[pa.b, cw.b, ca.b], writes=[ca.b])
                S.op("dve", lambda: nc.vector.tensor_scalar(out=ca[:, 0:2], in0=e4[:, 0:2], scalar1=cw[:, cf, 0:1], scalar2=None, op0=ALU.mult),
                     reads=[a.b, cw.b], writes=[ca.b])
                S.op("dve", lambda: nc.vector.scalar_tensor_tensor(out=ca[:, 0:2], in0=e4[:, 1:3], scalar=cw[:, cf, 1:2], in1=ca[:, 0:2], op0=ALU.mult, op1=ALU.add),
                     reads=[a.b, cw.b, ca.b], writes=[ca.b])
                S.op("dve", lambda: nc.vector.scalar_tensor_tensor(out=ca[:, 0:2], in0=e4[:, 2:4], scalar=cw[:, cf, 2:3], in1=ca[:, 0:2], op0=ALU.mult, op1=ALU.add),
                     reads=[a.b, cw.b, ca.b], writes=[ca.b])
                S.op("act", lambda: nc.scalar.activation(out=g[:, :], in_=ca[:, :], func=AF.Gelu_apprx_tanh, bias=cb[:, cf:cf + 1]),
                     reads=[ca.b, cb.b], writes=[g.b])
                S.op("dve", lambda: nc.vector.tensor_tensor(out=gT[:, cf, :], in0=g[:, :], in1=pu[:, :], op=ALU.mult),
                     reads=[g.b, pu.b], writes=[gT.b])

            def c2b(c):
                for tp2 in range(2):
                    accs = [bank[0], bank[1], bank[2], bank[3]]
                    for cf in range(NCF):
                        wd_ = wdt[cf % 3]
                        S.load("sp", wd_[:, :], self.wds[cf], wd_.b)
                        for j in range(2):
                            ti = 2 * tp2 + j
                            for half in range(2):
                                pp = accs[2 * j + half]
                                S.op("pe", lambda: nc.tensor.matmul(pp[:, :], lhsT=gT[:, cf, ti * 128:(ti + 1) * 128], rhs=wd_[:, half * 512:(half + 1) * 512],
                                                                    start=(cf == 0), stop=(cf == NCF - 1)), reads=[gT.b, wd_.b], writes=[pp.b], accum=(cf > 0))
                    for j in range(2):
                        ti = 2 * tp2 + j
                        tok = (4 * c + ti) * 128
                        p_ = pst[j]
                        self.copy(p_[:, 0:512], accs[2 * j][:, :], [accs[2 * j].b], [p_.b], eng="act")
                        self.copy(p_[:, 512:D], accs[2 * j + 1][:, :], [accs[2 * j + 1].b], [p_.b], eng="dve")
                        S.store("pool", self.part[tok:tok + 128, :], p_[:, :], p_.b)
                        store_tags.append(("dma", p_.b.dsem, p_.b.dcnt))
                    if pend_b:
                        seg_b(*pend_b.pop(0))

            pend_b = []
            fifo = []
            for ti in range(4):
                seg_b(0, ti, seg_a(0, ti))
            for c in range(self.nch):
                for cf in range(NCF):
                    c2a_cf(c, cf)
                    if cf % 4 == 3:
                        k = cf // 4
                        if len(pend_b) >= 2:
                            seg_b(*pend_b.pop(0))
                        if c + 1 < self.nch:
                            pend_b.append((c + 1, k, seg_a(c + 1, k)))
                        if fifo and fifo[0][2] <= c:
                            tg, tt, _ = fifo.pop(0)
                            finish_tile(tg, tt)
                c2b(c)
                while pend_b:
                    seg_b(*pend_b.pop(0))
                if c % RNG == RNG - 1 or c == self.nch - 1:
                    c0 = (c // RNG) * RNG
                    r0, r1 = c0 * 512, (c + 1) * 512
                    for tg in store_tags:
                        S._wait("pool", tg)
                    store_tags = []
                    tag = self.collective("AllReduce", ALU.add, self.part[r0:r1, :].opt(), self.red[r0:r1, :].opt())
                    for tt in range(r0 // 128, r1 // 128):
                        fifo.append((tag, tt, c + 2))
            for tg, tt, _ in fifo:
                finish_tile(tg, tt)
            S.barrier()


_CACHE = {}


def kernel(**inputs):
    x = np.ascontiguousarray(inputs["x"], dtype=np.float32)
    params = [layout_params(inputs, r) for r in range(2)]
    consts = [make_consts(r) for r in range(2)]
    if "prog" not in _CACHE:
        _CACHE["prog"] = Prog()
    prog = _CACHE["prog"]
    in_maps = []
    for core in range(8):
        b, r = core // 2, core % 2
        m = {"x": x[b]}
        for n, _, _ in CONST_SPECS:
            m["c_" + n] = consts[r][n]
        for n, _ in PARAM_SPECS:
            m["p_" + n] = params[r][n]
        in_maps.append(m)
    res = run_bass_kernel_spmd(prog.nc, in_maps, core_ids=list(range(8)))
    out = np.stack([np.asarray(res.results[2 * b]["y"], dtype=np.float32) for b in range(4)], axis=0)
    return out
```

```python
import math
import numpy as np
import ml_dtypes
from contextlib import ExitStack
import concourse.bass as bass
import concourse.mybir as mybir
from concourse.bass_utils import run_bass_kernel_spmd

F32 = mybir.dt.float32
BF16 = mybir.dt.bfloat16
AF = mybir.ActivationFunctionType
ALU = mybir.AluOpType
AX = mybir.AxisListType
NPBF = ml_dtypes.bfloat16

D = 1024
S_LEN = 8192
NT = 64
NCH = 16
DEPTH = 2
DFF = 4096
NEG = -30000.0
EPS = 1e-6

OFF = dict(mq=0, mk=256, mv=512, nq=768, nkc=1024, nvc=1088, nks=1152, nvs=1216, nkw=1280, nvw=1344,
           ng=1408, dq=1420, dk=1676, dv=1932, sq=2188, sk=2444, sv=2572)
FRr = dict(mq=0, mk=128, nq=256, nkc=512, nvc=576, nks=640, nkw=704, dq=768, dk=896, sq=1024, sk=1152)
NF = 1280
VCr = dict(mv=0, nvs=128, nvw=192, dv=256, sv=384, ng=448)
NV = 460
NCF = 16


def role_heads(r, m=0):
    if m == 3:
        return [2 * r, 2 * r + 1], [2 * (1 - r), 2 * (1 - r) + 1]
    return [r, r + 2], [1 - r, 3 - r]


def far_skip(m, j, min_dist):
    sl = min(SLOPES[4 * role_heads(0, m)[0][j] + m], SLOPES[4 * role_heads(1, m)[0][j] + m])
    return min_dist > 0 and sl * min_dist >= 200.0


def role_cols(r):
    own0 = role_heads(r, 0)[0]
    own1, oth1 = role_heads(r, 1)
    own2 = role_heads(r, 2)[0]
    own3 = role_heads(r, 3)[0]

    def hc(base, heads, w=64):
        return np.concatenate([np.arange(base + w * h, base + w * (h + 1)) for h in heads])

    f = np.concatenate([hc(OFF["mq"], own0), hc(OFF["mk"], own0), hc(OFF["nq"], own1 + oth1),
                        np.arange(OFF["nkc"], OFF["nkc"] + 64), np.arange(OFF["nvc"], OFF["nvc"] + 64),
                        np.arange(OFF["nks"], OFF["nks"] + 64), np.arange(OFF["nkw"], OFF["nkw"] + 64),
                        hc(OFF["dq"], own2), hc(OFF["dk"], own2), hc(OFF["sq"], own3), hc(OFF["sk"], [r])])
    v = np.concatenate([hc(OFF["mv"], own0), np.arange(OFF["nvs"], OFF["nvs"] + 64), np.arange(OFF["nvw"], OFF["nvw"] + 64),
                        hc(OFF["dv"], own2), hc(OFF["sv"], [r]), hc(OFF["ng"], own1 + oth1, 3)])
    assert len(f) == 1216 and len(v) == NV
    return f, v


SLOPES = np.power(np.float32(2.0), np.arange(1, 17, dtype=np.float32) * np.float32(-0.5)).astype(np.float64)


class Buf:
    __slots__ = ("name", "w", "rs", "dsem", "dcnt")

    def __init__(self, name):
        self.name = name
        self.w = None
        self.rs = []
        self.dsem = None
        self.dcnt = 0


class Sched:
    def __init__(self, nc, stack):
        self.nc = nc
        self.stack = stack
        self.eng = {"pe": nc.tensor, "act": nc.scalar, "dve": nc.vector, "pool": nc.gpsimd, "sp": nc.sync}
        self.sem, self.cnt, self.seen = {}, {}, {}
        for k in self.eng:
            self.sem[k] = stack.enter_context(nc.semaphore("s_" + k))
            self.cnt[k] = 0
            self.seen[k] = {}
        self.dma_seen = {k: {} for k in self.eng}
        self.free_sems = []
        self.live = []
        self.nsem = len(self.eng)
        self.ninstr = 0

    def _wait(self, ek, dep):
        if dep is None:
            return
        e = self.eng[ek]
        if dep[0] == "dma":
            _, sem, val = dep
            d = self.dma_seen[ek]
            if d.get(id(sem), 0) >= val:
                return
            e.wait_ge(sem, val)
            d[id(sem)] = val
        else:
            pk, c = dep
            if self.seen[ek].get(pk, 0) >= c:
                return
            e.wait_ge(self.sem[pk], c)
            self.seen[ek][pk] = c
        self.ninstr += 1

    def _deps(self, ek, reads, writes, accum):
        for b in reads:
            self._wait(ek, b.w)
        for b in writes:
            if not (accum and b.w is not None and b.w[0] == ek):
                self._wait(ek, b.w)
            for r in b.rs:
                self._wait(ek, r)

    def op(self, ek, fn, reads=(), writes=(), accum=False):
        self._deps(ek, reads, writes, accum)
        ins = fn()
        self.cnt[ek] += 1
        ins.then_inc(self.sem[ek], 1)
        tag = (ek, self.cnt[ek])
        for b in reads:
            b.rs.append(tag)
        for b in writes:
            b.w = tag
            b.rs = []
        self.ninstr += 1
        return ins

    def _dsem(self, b):
        if b.dsem is None:
            if self.free_sems:
                b.dsem, b.dcnt = self.free_sems.pop()
            else:
                b.dsem = self.stack.enter_context(self.nc.semaphore("d%d" % self.nsem))
                b.dcnt = 0
                self.nsem += 1
            self.live.append(b)

    def load(self, qk, out_ap, in_ap, buf):
        self._deps(qk, (), (buf,), False)
        self._dsem(buf)
        ins = self.eng[qk].dma_start(out=out_ap, in_=in_ap)
        buf.dcnt += 16
        ins.then_inc(buf.dsem, 16)
        buf.w = ("dma", buf.dsem, buf.dcnt)
        buf.rs = []
        self.ninstr += 1

    def store(self, qk, out_ap, in_ap, buf):
        self._deps(qk, (buf,), (), False)
        self._dsem(buf)
        ins = self.eng[qk].dma_start(out=out_ap, in_=in_ap)
        buf.dcnt += 16
        ins.then_inc(buf.dsem, 16)
        buf.rs.append(("dma", buf.dsem, buf.dcnt))
        self.ninstr += 1

    def barrier(self):
        for ek in self.eng:
            for pk in self.eng:
                if pk != ek and self.cnt[pk] > 0:
                    self._wait(ek, (pk, self.cnt[pk]))
            for b in self.live:
                self._wait(ek, ("dma", b.dsem, b.dcnt))
        for b in self.live:
            self.free_sems.append((b.dsem, b.dcnt))
            b.dsem = None
        self.live = []


class T:
    def __init__(self, nc, stack, name, shape, dtype, psum=False):
        alloc = nc.psum_tensor if psum else nc.sbuf_tensor
        self.t = stack.enter_context(alloc(name, shape, dtype))
        self.b = Buf(name)

    def __getitem__(self, idx):
        return self.t[idx]


def _split3(v):
    v = np.asarray(v, np.float64)
    hi = v.astype(NPBF)
    r = v - hi.astype(np.float64)
    mid = r.astype(NPBF)
    r = r - mid.astype(np.float64)
    lo = r.astype(NPBF)
    return hi, mid, lo


def make_consts(role=0):
    locs = [sum(role_heads(role, m), []) for m in range(4)]
    c = {}
    c["ident"] = np.eye(128, dtype=np.float32).astype(NPBF)
    iq = np.arange(512, dtype=np.float64)
    p = np.arange(128, dtype=np.float64)
    aug = np.zeros((16, 3, 512), NPBF)
    for m in range(4):
        scale = 32 ** -0.5 if m == 2 else 64 ** -0.5
        for j in range(4):
            sl = SLOPES[4 * locs[m][j] + m]
            hi, mid, lo = _split3(-sl * iq / scale)
            aug[4 * m + j, 0], aug[4 * m + j, 1], aug[4 * m + j, 2] = hi, mid, lo
    c["aug"] = aug
    kb = np.zeros((128, 16, 64), np.float32)
    r = np.arange(-60, 4, dtype=np.float64)
    for m in range(4):
        for j in range(4):
            sl = SLOPES[4 * locs[m][j] + m]
            kb[:, 4 * m + j, :] = (sl * (p[:, None] + 128.0 * r[None, :])).astype(np.float32)
    c["kb"] = kb
    kbc = np.zeros((128, 4, 16, 4), np.float32)
    for j in range(4):
        sl = SLOPES[4 * locs[1][j] + 1]
        for cc in range(16):
            for kt in range(4):
                kbc[:, j, cc, kt] = (sl * (16.0 * (128 * kt + p) + 31.0 - 512.0 * cc)).astype(np.float32)
    c["kbc"] = kbc
    P = p[:, None, None]
    Q = iq[None, None, :]
    k4 = np.arange(4, dtype=np.float64)[None, :, None]
    c["caus"] = np.where(Q >= 128 * k4 + P, 0.0, NEG).astype(np.float32).astype(NPBF)
    r8 = (np.arange(8, dtype=np.float64) - 4)[None, :, None]
    dd = Q - 128 * r8 - P
    c["wmask"] = np.where((dd >= 0) & (dd < 512), 0.0, NEG).astype(np.float32).astype(NPBF)
    r5 = (np.arange(5, dtype=np.float64) - 1)[None, :, None]
    dd = Q - 128 * r5 - P
    c["swamask"] = np.where((dd >= 0) & (dd < 128), 0.0, NEG).astype(np.float32).astype(NPBF)
    d5 = (512.0 * np.arange(5, dtype=np.float64))[None, :, None]
    c["cmask"] = np.where(16 * P + 31 <= d5 + Q, 0.0, NEG).astype(np.float32).astype(NPBF)
    j = np.arange(S_LEN)
    c["em"] = (j[None, :] // 256 == np.arange(32)[:, None]).astype(np.float32).astype(NPBF)
    c["es"] = (j[None, :] // 64 == np.arange(128)[:, None]).astype(np.float32).astype(NPBF)
    ncmp, nsel = 511, 128
    cs = np.arange(512)[:, None] * 16
    bs = np.arange(nsel)[None, :] * 64
    ov = np.clip(np.minimum(cs + 32, bs + 64) - np.maximum(cs, bs), 0, None) / 32.0
    ov[ncmp:, :] = 0.0
    c["ov"] = ov.reshape(4, 128, 128).transpose(1, 0, 2).astype(np.float32).astype(NPBF)
    n32 = np.arange(32, dtype=np.float32)
    qi = np.arange(4)
    c["bi"] = np.broadcast_to((n32[None, None, :] - (qi // 2)[None, :, None].astype(np.float32)), (128, 4, 32)).astype(np.float32).copy()
    s128 = np.arange(128)[None, None, None, :]
    cc = np.arange(16)[:, None, None, None]
    pp = np.arange(128)[None, :, None, None]
    qq = np.arange(4)[None, None, :, None]
    qblk = 8 * cc + 2 * qq + (pp >= 64)
    forced = (s128 == 0) | (s128 == qblk) | (s128 == qblk - 1)
    valid = s128 <= qblk
    c["f1e4"] = np.where(forced, 1e4, 0.0).astype(np.float32)
    c["negv"] = np.where(valid, 0.0, -1e30).astype(np.float32)
    c["ones3"] = np.ones((3, 4 * S_LEN), np.float32).astype(NPBF)
    return c


CONST_SPECS = [("ident", [128, 128], BF16), ("aug", [16, 3, 512], BF16), ("kb", [128, 16, 64], F32),
               ("kbc", [128, 4, 16, 4], F32), ("caus", [128, 4, 512], BF16), ("wmask", [128, 8, 512], BF16),
               ("swamask", [128, 5, 512], BF16), ("cmask", [128, 5, 512], BF16), ("em", [32, S_LEN], BF16),
               ("es", [128, S_LEN], BF16), ("ov", [128, 4, 128], BF16), ("bi", [128, 4, 32], F32),
               ("f1e4", [16, 128, 4, 128], F32), ("negv", [16, 128, 4, 128], F32), ("ones3", [3, 4 * S_LEN], BF16)]

PARAM_SPECS = [("wf", [DEPTH, 128, 8, NF]), ("wv", [DEPTH, 128, 8, NV]), ("wo", [DEPTH, 128, 8, D]),
               ("wg", [DEPTH, NCF, 128, 8, 128]), ("wu", [DEPTH, NCF, 128, 8, 128]), ("wd", [DEPTH, NCF, 128, D]),
               ("g_apre", [DEPTH, D]), ("g_apost", [DEPTH, D]), ("g_fpre", [DEPTH, D]), ("g_fpost", [DEPTH, D]),
               ("cw", [DEPTH, 128, NCF, 3]), ("cb", [DEPTH, 128, NCF]),
               ("posk", [DEPTH, 64, 32]), ("w1k", [DEPTH, 64, 32, 128]), ("b1k", [DEPTH, 128, 1]), ("w2k", [DEPTH, 128, 64]),
               ("posv", [DEPTH, 64, 32]), ("w1v", [DEPTH, 64, 32, 128]), ("b1v", [DEPTH, 128, 1]), ("w2v", [DEPTH, 128, 64]),
               ("lq1", [DEPTH, 32]), ("lk1", [DEPTH, 32]), ("lq2", [DEPTH, 32]), ("lk2", [DEPTH, 32]),
               ("subln", [DEPTH, 64]), ("sinks", [DEPTH, 4])]


def layout_params(inp, role=0):
    o = {}
    w_in = inp["w_in"]
    fc, vc = role_cols(role)
    wf = np.zeros((DEPTH, D, NF), np.float32)
    wf[:, :, :len(fc)] = w_in[:, :, fc]
    o["wf"] = wf.reshape(DEPTH, 8, 128, NF).transpose(0, 2, 1, 3)
    o["wv"] = w_in[:, :, vc].reshape(DEPTH, 8, 128, NV).transpose(0, 2, 1, 3)
    rows = np.concatenate([np.arange(256 * m + 64 * h, 256 * m + 64 * (h + 1)) for rr in range(2) for m in range(4)
                           for h in role_heads(rr, m)[0]])
    o["wo"] = inp["w_out"][:, rows, :].reshape(DEPTH, 8, 128, D).transpose(0, 2, 1, 3)
    cfs = slice(NCF * role, NCF * (role + 1))
    o["wg"] = inp["ffn_w_gate"].reshape(DEPTH, 8, 128, 32, 128).transpose(0, 3, 2, 1, 4)[:, cfs]
    o["wu"] = inp["ffn_w_up"].reshape(DEPTH, 8, 128, 32, 128).transpose(0, 3, 2, 1, 4)[:, cfs]
    o["wd"] = inp["ffn_w_down"].reshape(DEPTH, 32, 128, D)[:, cfs]
    o["g_apre"], o["g_apost"] = inp["attn_pre_norm"], inp["attn_post_norm"]
    o["g_fpre"], o["g_fpost"] = inp["ffn_pre_norm"], inp["ffn_post_norm"]
    o["cw"] = inp["ffn_conv_w"].reshape(DEPTH, 3, 32, 128).transpose(0, 3, 2, 1)[:, :, cfs, :]
    o["cb"] = inp["ffn_conv_b"].reshape(DEPTH, 32, 128).transpose(0, 2, 1)[:, :, cfs]
    for sfx in "kv":
        o["pos" + sfx] = inp["nsa_cmp_pos_" + sfx].transpose(0, 2, 1)
        o["w1" + sfx] = inp["nsa_cmp_w1_" + sfx].transpose(0, 2, 1, 3)
        o["b1" + sfx] = inp["nsa_cmp_b1_" + sfx].reshape(DEPTH, 128, 1)
        o["w2" + sfx] = inp["nsa_cmp_w2_" + sfx]
    o["lq1"], o["lk1"] = inp["diff_lambda_q1"], inp["diff_lambda_k1"]
    o["lq2"], o["lk2"] = inp["diff_lambda_q2"], inp["diff_lambda_k2"]
    o["subln"] = inp["diff_subln"]
    o["sinks"] = inp["swa_sinks"][:, sum(role_heads(role, 3), [])]
    return {k: np.ascontiguousarray(v, dtype=np.float32) for k, v in o.items()}


class Prog:
    def __init__(self, phases=("A", "B", "C"), layers=(0, 1), mixers=(0, 1, 2, 3), debug=False, nchunks=NCH, ncores=8):
        self.phases, self.layers, self.mixers, self.debug, self.nch = phases, layers, mixers, debug, nchunks
        self.groups = [[2 * i, 2 * i + 1] for i in range(ncores // 2)]
        nc = bass.Bass("TRN2", target_bir_lowering=False)
        self.nc = nc
        self.x_in = nc.dram_tensor("x", [S_LEN, D], F32, kind="ExternalInput").ap()
        self.y = nc.dram_tensor("y", [S_LEN, D], F32, kind="ExternalOutput").ap()
        self.dc = {n: nc.dram_tensor("c_" + n, shp, dt, kind="ExternalInput").ap() for n, shp, dt in CONST_SPECS}
        self.dp = {n: nc.dram_tensor("p_" + n, shp, F32, kind="ExternalInput").ap() for n, shp in PARAM_SPECS}
        kind = "ExternalOutput" if debug else "Internal"
        self.featT = nc.dram_tensor("featT", [NF, S_LEN], BF16, kind=kind).ap()
        self.vtok = nc.dram_tensor("vtok", [S_LEN, NV], BF16, kind=kind).ap()
        self.mix_t = nc.dram_tensor("mix", [S_LEN, 512], BF16)
        self.mix = self.mix_t.ap()
        self.mixall_t = nc.dram_tensor("mixall", [2 * S_LEN, 512], BF16)
        self.mixall = self.mixall_t.ap()
        self.part = nc.dram_tensor("part", [S_LEN, D], F32).ap()
        self.red = nc.dram_tensor("red", [S_LEN, D], F32).ap()
        self.x1s = nc.dram_tensor("x1s", [S_LEN, D], F32).ap()
        self.xs = nc.dram_tensor("xs", [S_LEN, D], F32).ap()
        self.wgs = nc.dram_tensor("wgs", [NCF, 128, 8, 128], BF16).ap()
        self.wus = nc.dram_tensor("wus", [NCF, 128, 8, 128], BF16).ap()
        self.wds = nc.dram_tensor("wds", [NCF, 128, D], BF16).ap()
        self.cp_i = 0
        with ExitStack() as st:
            self.st = st
            self.S = Sched(nc, st)
            self.cc_sem = st.enter_context(nc.semaphore("cc_sem"))
            self.cc_cnt = 0
            self.build()
            print("built: instr", self.S.ninstr, "sems", self.S.nsem, flush=True)

    def collective(self, kind, op, in_ap, out_ap):
        ins = self.nc.gpsimd.collective_compute(kind, op, replica_groups=self.groups, ins=[in_ap], outs=[out_ap])
        self.cc_cnt += 1
        ins.then_inc(self.cc_sem, 1)
        self.S.ninstr += 1
        return ("dma", self.cc_sem, self.cc_cnt)

    def tile(self, stack, name, shape, dtype, psum=False):
        self.tile_i = getattr(self, "tile_i", 0) + 1
        return T(self.nc, stack, "%s_%d" % (name, self.tile_i), shape, dtype, psum)

    def copy(self, out_ap, in_ap, reads, writes, eng=None):
        nc = self.nc
        if eng is None:
            eng = "act" if (self.cp_i % 2 == 0) else "dve"
            self.cp_i += 1
        if eng == "act":
            self.S.op("act", lambda: nc.scalar.activation(out=out_ap, in_=in_ap, func=AF.Copy), reads=reads, writes=writes)
        elif eng == "dve":
            self.S.op("dve", lambda: nc.vector.tensor_copy(out=out_ap, in_=in_ap), reads=reads, writes=writes)
        else:
            self.S.op("pool", lambda: nc.gpsimd.tensor_copy(out=out_ap, in_=in_ap), reads=reads, writes=writes)

    def load_cast(self, stack, dst, dst_ap_fn, src_ap_fn, n, ncols):
        stg = [self.tile(stack, "stg%d_%d" % (i, self.S.ninstr), [128, ncols], F32) for i in range(2)]
        for i in range(n):
            s = stg[i % 2]
            self.S.load("sp", s[:, :], src_ap_fn(i), s.b)
            self.copy(dst_ap_fn(i), s[:, :], [s.b], [dst.b])

    def build(self):
        nc, S, st = self.nc, self.S, self.st
        self.ident = self.tile(st, "ident", [128, 128], BF16)
        self.caus = self.tile(st, "caus", [128, 4, 512], BF16)
        self.kb = self.tile(st, "kb", [128, 16, 64], F32)
        self.epsT = self.tile(st, "epsT", [128, 1], F32)
        S.load("sp", self.ident[:, :], self.dc["ident"], self.ident.b)
        S.load("sp", self.caus[:, :, :], self.dc["caus"], self.caus.b)
        S.load("sp", self.kb[:, :, :], self.dc["kb"], self.kb.b)
        S.op("dve", lambda: nc.vector.memset(self.epsT[:, :], EPS), writes=[self.epsT.b])
        for l in self.layers:
            x_src = self.x_in if l == 0 else self.xs
            x_dst = self.y if l == self.layers[-1] else self.xs
            if "A" in self.phases:
                self.phase_A(l, x_src)
                S.barrier()
            if "B" in self.phases:
                for m in self.mixers:
                    [self.moba, self.nsa, self.diff, self.swa][m](l)
                    S.barrier()
            if "C" in self.phases:
                tags = []
                for k in range((self.nch * 512 + 2047) // 2048):
                    tags.append(self.collective("AllGather", ALU.bypass, self.mix[k * 2048:(k + 1) * 2048, :].opt(),
                                                self.mixall[k * 4096:(k + 1) * 4096, :].opt()))
                self.prep_ffn(l)
                for ek in S.eng:
                    S._wait(ek, tags[-1])
                S.barrier()
                self.phase_C(l, x_src, x_dst)
                S.barrier()
        S.barrier()
        if self.debug:
            nc = self.nc
            dbg = {}
            for nm, src, rows, cols, dt in (("d_mixall", self.mixall, 4096, 512, BF16), ("d_part", self.part, 2048, D, F32),
                                            ("d_red", self.red, 2048, D, F32), ("d_x1s", self.x1s, 2048, D, F32), ("d_mix", self.mix, 2048, 512, BF16)):
                dst = nc.dram_tensor(nm, [rows, cols], dt, kind="ExternalOutput").ap()
                b = Buf(nm)
                S._dsem(b)
                ins = nc.sync.dma_start(out=dst[:, :], in_=src[0:rows, :])
                b.dcnt += 16
                ins.then_inc(b.dsem, 16)
                S._wait("sp", ("dma", b.dsem, b.dcnt))

    def rstd_of(self, x_ap, xbuf, junk, ss, sd, rstd, n=D):
        nc, S = self.nc, self.S
        S.op("dve", lambda: nc.vector.scalar_tensor_tensor(out=junk[:, 0:n], in0=x_ap, scalar=1.0, in1=x_ap,
                                                           op0=ALU.mult, op1=ALU.mult, accum_out=ss[:, 0:1]),
             reads=[xbuf], writes=[junk.b, ss.b])
        S.op("act", lambda: nc.scalar.activation(out=sd[:, 0:1], in_=ss[:, 0:1], func=AF.Sqrt,
                                                 bias=self.epsT[:, 0:1], scale=1.0 / n),
             reads=[ss.b, self.epsT.b], writes=[sd.b])
        S.op("dve", lambda: nc.vector.reciprocal(out=rstd[:, 0:1], in_=sd[:, 0:1]), reads=[sd.b], writes=[rstd.b])

    def phase_A(self, l, x_src):
        nc, S = self.nc, self.S
        with ExitStack() as ph:
            WF = self.tile(ph, "WF", [128, 8, NF], BF16)
            WV = self.tile(ph, "WV", [128, 8, NV], BF16)
            gain = self.tile(ph, "gainA", [128, D], F32)
            with ExitStack() as tmp:
                self.load_cast(tmp, WF, lambda i: WF[:, i, :], lambda i: self.dp["wf"][l, :, i, :], 8, NF)
                self.load_cast(tmp, WV, lambda i: WV[:, i, :], lambda i: self.dp["wv"][l, :, i, :], 8, NV)
                S.barrier()
            S.load("sp", gain[:, :], self.dp["g_apre"][l].partition_broadcast(128), gain.b)
            xts = [self.tile(ph, "xtA%d" % i, [128, D], F32) for i in range(3)]
            junk = self.tile(ph, "junkA", [128, D], F32)
            ss = self.tile(ph, "ssA", [128, 1], F32)
            sd = self.tile(ph, "sdA", [128, 1], F32)
            rstd = self.tile(ph, "rstdA", [128, 1], F32)
            hb = [self.tile(ph, "hbA%d" % i, [128, D], BF16) for i in range(2)]
            hT = [self.tile(ph, "hTA%d" % i, [128, 8, 512], BF16) for i in range(2)]
            tp = [self.tile(ph, "tpA%d" % i, [128, D], BF16, psum=True) for i in range(2)]
            psF = [self.tile(ph, "psF%d" % i, [128, 512], F32, psum=True) for i in range(2)]
            psV0 = self.tile(ph, "psV0", [128, 512], F32, psum=True)
            fst = [self.tile(ph, "fst%d" % i, [128, 512], BF16) for i in range(3)]
            vst = [self.tile(ph, "vst%d" % i, [128, NV], BF16) for i in range(2)]
            it = 0
            for c in range(self.nch):
                hTc = hT[c % 2]
                for ti in range(4):
                    tok = (4 * c + ti) * 128
                    xt = xts[it % 3]
                    h = hb[it % 2]
                    tpp = tp[it % 2]
                    it += 1
                    S.load("sp", xt[:, :], x_src[tok:tok + 128, :], xt.b)
                    self.rstd_of(xt[:, :], xt.b, junk, ss, sd, rstd)
                    S.op("dve", lambda: nc.vector.scalar_tensor_tensor(out=h[:, :], in0=xt[:, :], scalar=rstd[:, 0:1],
                                                                       in1=gain[:, :], op0=ALU.mult, op1=ALU.mult),
                         reads=[xt.b, rstd.b, gain.b], writes=[h.b])
                    for kc in range(8):
                        S.op("pe", lambda: nc.tensor.transpose(out=tpp[:, kc * 128:(kc + 1) * 128],
                                                               in_=h[:, kc * 128:(kc + 1) * 128], identity=self.ident[:, :]),
                             reads=[h.b, self.ident.b], writes=[tpp.b], accum=(kc > 0))
                    self.copy(hTc[:, :, ti * 128:(ti + 1) * 128], tpp[:, :].rearrange("p (k t) -> p k t", k=8),
                              [tpp.b], [hTc.b])
                for g in range(NF // 128):
                    ps = psF[g % 2]
                    for kc in range(8):
                        S.op("pe", lambda: nc.tensor.matmul(ps[:, :], lhsT=WF[:, kc, g * 128:(g + 1) * 128], rhs=hTc[:, kc, :],
                                                            start=(kc == 0), stop=(kc == 7)),
                             reads=[WF.b, hTc.b], writes=[ps.b], accum=(kc > 0))
                    f = fst[g % 3]
                    self.copy(f[:, :], ps[:, :], [ps.b], [f.b])
                    S.store("pool", self.featT[g * 128:(g + 1) * 128, c * 512:(c + 1) * 512], f[:, :], f.b)
                for ti in range(4):
                    tok = (4 * c + ti) * 128
                    for kc in range(8):
                        S.op("pe", lambda: nc.tensor.matmul(psV0[:, 0:NV], lhsT=hTc[:, kc, ti * 128:(ti + 1) * 128], rhs=WV[:, kc, 0:NV],
                                                            start=(kc == 0), stop=(kc == 7)),
                             reads=[WV.b, hTc.b], writes=[psV0.b], accum=(kc > 0))
                    v = vst[ti % 2]
                    self.copy(v[:, 0:NV], psV0[:, 0:NV], [psV0.b], [v.b])
                    S.store("pool", self.vtok[tok:tok + 128, :], v[:, :], v.b)
            S.barrier()

    def attn_res(self, ph, nps=3, npt=3, no=3):
        R = type("R", (), {})()
        R.ps = [self.tile(ph, "ps_s%d" % i, [128, 512], F32, psum=True) for i in range(nps)]
        R.pt = [self.tile(ph, "pT%d" % i, [128, 512], BF16) for i in range(npt)]
        R.O = [self.tile(ph, "O%d" % i, [128, 512], F32, psum=True) for i in range(no)]
        R.ips = R.ipt = R.io = 0
        return R

    def attend(self, R, q_ap, qbufs, tiles, O, scale):
        nc, S = self.nc, self.S
        Ov = O[:, 0:260].rearrange("p (a b) -> p a b", a=4)
        n = len(tiles)

        def pv(pt, tl, first, last):
            for qi in range(4):
                S.op("pe", lambda: nc.tensor.matmul(Ov[:, qi, :], lhsT=pt[:, qi * 128:(qi + 1) * 128], rhs=tl["v"],
                                                    start=(first and qi == 0), stop=last),
                     reads=[pt.b] + tl["kbufs"], writes=[O.b], accum=not (first and qi == 0))
            if tl.get("post") is not None:
                tl["post"](pt, first, last)

        prev = None
        for i, tl in enumerate(tiles):
            ps = R.ps[R.ips % len(R.ps)]
            R.ips += 1
            ex = tl.get("extra", [])
            S.op("pe", lambda: nc.tensor.matmul(ps[:, :], lhsT=tl["kT"], rhs=q_ap, start=True, stop=(len(ex) == 0)),
                 reads=qbufs + tl["kbufs"], writes=[ps.b])
            for j, (lh, rh, bufs) in enumerate(ex):
                S.op("pe", lambda: nc.tensor.matmul(ps[:, :], lhsT=lh, rhs=rh, start=False, stop=(j == len(ex) - 1)),
                     reads=bufs, writes=[ps.b], accum=True)
            pt = R.pt[R.ipt % len(R.pt)]
            R.ipt += 1
            S.op("act", lambda: nc.scalar.activation(out=pt[:, :], in_=ps[:, :], func=AF.Exp, bias=tl["bias"], scale=scale),
                 reads=[ps.b] + tl["bbufs"], writes=[pt.b])
            if prev is not None:
                pv(prev[0], prev[1], prev[2] == 0, False)
            prev = (pt, tl, i)
        pv(prev[0], prev[1], prev[2] == 0, True)
        return Ov

    def load_kv(self, KT, V, krows, nh_k, k_row0, v_col0, nh_v, kd=64):
        nc, S = self.nc, self.S
        S.op("pool", lambda: nc.gpsimd.memset(V[:, :, :, 64:65], 1.0), writes=[V.b])
        for h in range(nh_k):
            S.load("sp", KT[0:kd, h, :], self.featT[k_row0 + h * kd:k_row0 + (h + 1) * kd, :], KT.b)
        S.load("sp", KT[kd:kd + 3, :, :], self.dc["ones3"][:, 0:nh_k * S_LEN].rearrange("r (h s) -> r h s", h=nh_k), KT.b)
        for h in range(nh_v):
            S.load("sp", V[:, h, :, 0:64],
                   self.vtok[:, v_col0 + h * 64:v_col0 + (h + 1) * 64].rearrange("(kt p) d -> p kt d", p=128), V.b)

    def recip_den(self, Ov, O, den, add_ap=None, add_bufs=()):
        nc, S = self.nc, self.S
        if add_ap is None:
            S.op("dve", lambda: nc.vector.tensor_scalar(out=den[:, 0:4], in0=Ov[:, :, 64], scalar1=1e-30, scalar2=None,
                                                        op0=ALU.max), reads=[O.b], writes=[den.b])
        else:
            S.op("dve", lambda: nc.vector.tensor_scalar(out=den[:, 0:4], in0=Ov[:, :, 64], scalar1=add_ap, scalar2=1e-30,
                                                        op0=ALU.add, op1=ALU.max), reads=[O.b] + list(add_bufs), writes=[den.b])
        S.op("dve", lambda: nc.vector.reciprocal(out=den[:, 0:4], in_=den[:, 0:4]), reads=[den.b], writes=[den.b])

    def moba(self, l):
        nc, S = self.nc, self.S
        with ExitStack() as ph:
            KT = self.tile(ph, "KTm", [128, 2, S_LEN], BF16)
            V = self.tile(ph, "Vm", [128, 2, NT, 65], BF16)
            BI = self.tile(ph, "BIm", [128, 4, 32], F32)
            S.load("sp", BI[:, :, :], self.dc["bi"], BI.b)
            S.op("pool", lambda: nc.gpsimd.memset(V[:, :, :, 64:65], 1.0), writes=[V.b])
            S.op("pool", lambda: nc.gpsimd.memset(KT[64:128, :, :], 0.0), writes=[KT.b])
            for h in range(2):
                S.load("sp", KT[96:128, h, :], self.dc["em"], KT.b)
                S.load("sp", KT[0:64, h, :], self.featT[FRr["mk"] + 64 * h:FRr["mk"] + 64 * (h + 1), :], KT.b)
                S.load("sp", V[:, h, :, 0:64],
                       self.vtok[:, VCr["mv"] + h * 64:VCr["mv"] + (h + 1) * 64].rearrange("(kt p) d -> p kt d", p=128), V.b)
            S.load("sp", KT[64:67, :, :], self.dc["ones3"][:, 0:2 * S_LEN].rearrange("r (h s) -> r h s", h=2), KT.b)
            R = self.attn_res(ph)
            QT = [[self.tile(ph, "QTm%d_%d" % (i, h), [128, 512], BF16) for h in range(2)] for i in range(2)]
            for qs in QT:
                for h in range(2):
                    S.op("pool", lambda: nc.gpsimd.memset(qs[h][64:128, :], 0.0), writes=[qs[h].b])
                    S.load("sp", qs[h][64:67, :], self.dc["aug"][h], qs[h].b)
            kms = self.tile(ph, "kms", [128, 2, 32], F32)
            kmh = self.tile(ph, "kmh", [128, 2, 32], BF16)
            kml = self.tile(ph, "kml", [128, 2, 32], BF16)
            kmr = self.tile(ph, "kmr", [128, 2, 32], F32)
            for h in range(2):
                S.op("dve", lambda: nc.vector.tensor_reduce(out=kms[0:64, h, :], in_=KT[0:64, h, :].rearrange("p (n k) -> p n k", k=256),
                                                            axis=AX.X, op=ALU.add), reads=[KT.b], writes=[kms.b])
            S.op("dve", lambda: nc.vector.tensor_scalar(out=kms[0:64, :, :], in0=kms[0:64, :, :], scalar1=1.0 / 256, scalar2=None, op0=ALU.mult),
                 reads=[kms.b], writes=[kms.b])
            S.op("dve", lambda: nc.vector.tensor_copy(out=kmh[0:64, :, :], in_=kms[0:64, :, :]), reads=[kms.b], writes=[kmh.b])
            S.op("dve", lambda: nc.vector.tensor_tensor(out=kmr[0:64, :, :], in0=kms[0:64, :, :], in1=kmh[0:64, :, :], op=ALU.subtract),
                 reads=[kms.b, kmh.b], writes=[kmr.b])
            S.op("dve", lambda: nc.vector.tensor_copy(out=kml[0:64, :, :], in_=kmr[0:64, :, :]), reads=[kmr.b], writes=[kml.b])
            gps = self.tile(ph, "gps", [128, 512], F32, psum=True)
            tps = self.tile(ph, "tpsm", [128, 1024], BF16, psum=True)
            past = self.tile(ph, "past", [128, 4, 32], F32)
            own = self.tile(ph, "own", [128, 4, 32], F32)
            negp = self.tile(ph, "negp", [128, 4, 32], F32)
            gm = self.tile(ph, "gm", [128, 4, 32], F32)
            m8 = self.tile(ph, "m8", [128, 4, 8], F32)
            sel = self.tile(ph, "sel", [128, 4, 32], F32)
            nsel = self.tile(ph, "nsel", [128, 4, 32], BF16)
            den = self.tile(ph, "denm", [128, 4], F32)
            mst = [self.tile(ph, "mstm%d" % i, [128, 4, 128], BF16) for i in range(2)]
            for c in range(self.nch):
                qs = QT[c % 2]
                for h in range(2):
                    S.load("sp", qs[h][0:64, :], self.featT[FRr["mq"] + 64 * h:FRr["mq"] + 64 * (h + 1), c * 512:(c + 1) * 512], qs[h].b)
                S.op("dve", lambda: nc.vector.tensor_scalar(out=past[:, :, :], in0=BI[:, :, :], scalar1=float(2 * c), scalar2=None, op0=ALU.is_lt),
                     reads=[BI.b], writes=[past.b])
                S.op("dve", lambda: nc.vector.tensor_scalar(out=own[:, :, :], in0=BI[:, :, :], scalar1=float(2 * c), scalar2=None, op0=ALU.is_equal),
                     reads=[BI.b], writes=[own.b])
                S.op("dve", lambda: nc.vector.tensor_scalar(out=negp[:, :, :], in0=past[:, :, :], scalar1=-1.0, scalar2=1e30, op0=ALU.add, op1=ALU.mult),
                     reads=[past.b], writes=[negp.b])
                ms = mst[c % 2]
                for h in range(2):
                    q = qs[h]
                    gv = gps[:, 0:128].rearrange("p (a b) -> p a b", a=4)
                    for qi in range(4):
                        S.op("pe", lambda: nc.tensor.matmul(gv[:, qi, :], lhsT=q[0:64, qi * 128:(qi + 1) * 128], rhs=kmh[0:64, h, :], start=True, stop=False),
                             reads=[q.b, kmh.b], writes=[gps.b], accum=(qi > 0))
                        S.op("pe", lambda: nc.tensor.matmul(gv[:, qi, :], lhsT=q[0:64, qi * 128:(qi + 1) * 128], rhs=kml[0:64, h, :], start=False, stop=True),
                             reads=[q.b, kml.b], writes=[gps.b], accum=True)
                    S.op("dve", lambda: nc.vector.tensor_tensor(out=gm[:, :, :], in0=gv, in1=negp[:, :, :], op=ALU.add),
                         reads=[gps.b, negp.b], writes=[gm.b])
                    for qi in range(4):
                        S.op("dve", lambda: nc.vector.max(out=m8[:, qi, :], in_=gm[:, qi, :]), reads=[gm.b], writes=[m8.b])
                    for qi in range(4):
                        S.op("dve", lambda: nc.vector.tensor_scalar(out=sel[:, qi, :], in0=gm[:, qi, :], scalar1=m8[:, qi, 2:3], scalar2=None, op0=ALU.is_ge),
                             reads=[gm.b, m8.b], writes=[sel.b])
                    S.op("dve", lambda: nc.vector.tensor_tensor(out=sel[:, :, :], in0=sel[:, :, :], in1=past[:, :, :], op=ALU.mult),
                         reads=[sel.b, past.b], writes=[sel.b])
                    S.op("dve", lambda: nc.vector.tensor_tensor(out=sel[:, :, :], in0=sel[:, :, :], in1=own[:, :, :], op=ALU.add),
                         reads=[sel.b, own.b], writes=[sel.b])
                    S.op("dve", lambda: nc.vector.tensor_scalar(out=nsel[:, :, :], in0=sel[:, :, :], scalar1=-1.0, scalar2=-NEG, op0=ALU.add, op1=ALU.mult),
                         reads=[sel.b], writes=[nsel.b])
                    for qi in range(4):
                        S.op("pe", lambda: nc.tensor.transpose(out=tps[0:32, qi * 128:(qi + 1) * 128], in_=nsel[:, qi, :], identity=self.ident[:, :]),
                             reads=[nsel.b, self.ident.b], writes=[tps.b], accum=(qi > 0))
                    self.copy(q[96:128, :], tps[0:32, 0:512], [tps.b], [q.b], eng="dve")
                    tiles = []
                    for kt in range(4 * c + 4):
                        if far_skip(0, h, 512 * c - (128 * kt + 127)):
                            continue
                        r = kt - 4 * c
                        ex = []
                        if r >= 0:
                            ex.append((self.ident[:, :], self.caus[:, r, :], [self.ident.b, self.caus.b]))
                        tiles.append(dict(kT=KT[0:128, h, kt * 128:(kt + 1) * 128], v=V[:, h, kt, :], kbufs=[KT.b, V.b],
                                          bias=self.kb[:, 0 + h, r + 60:r + 61], bbufs=[self.kb.b], extra=ex))
                    O = R.O[R.io % len(R.O)]
                    R.io += 1
                    Ov = self.attend(R, q[0:128, :], [q.b], tiles, O, 0.125)
                    self.recip_den(Ov, O, den)
                    for qi in range(4):
                        S.op("dve", lambda: nc.vector.tensor_scalar(out=ms[:, qi, h * 64:(h + 1) * 64], in0=Ov[:, qi, 0:64], scalar1=den[:, qi:qi + 1],
                                                                    scalar2=None, op0=ALU.mult), reads=[O.b, den.b], writes=[ms.b])
                S.store("pool", self.mix[c * 512:(c + 1) * 512, 0:128].rearrange("(a p) d -> p a d", p=128), ms[:, :, :], ms.b)
            S.barrier()

    def swa(self, l):
        nc, S = self.nc, self.S
        with ExitStack() as ph:
            KT = self.tile(ph, "KTs", [128, 1, S_LEN], BF16)
            V = self.tile(ph, "Vs", [128, 1, NT, 65], BF16)
            SM = self.tile(ph, "SMs", [128, 5, 512], BF16)
            S.load("sp", SM[:, :, :], self.dc["swamask"], SM.b)
            self.load_kv(KT, V, 67, 1, FRr["sk"], VCr["sv"], 1)
            sk = self.tile(ph, "sinks", [128, 4], F32)
            esk = self.tile(ph, "esinks", [128, 4], F32)
            S.load("sp", sk[:, :], self.dp["sinks"][l].partition_broadcast(128), sk.b)
            S.op("act", lambda: nc.scalar.activation(out=esk[:, :], in_=sk[:, :], func=AF.Exp), reads=[sk.b], writes=[esk.b])
            R = self.attn_res(ph)
            QT = [self.tile(ph, "QTs%d" % i, [128, 2, 512], BF16) for i in range(2)]
            for q in QT:
                S.load("sp", q[64:67, :, :], self.dc["aug"][12:14].rearrange("h r q -> r h q"), q.b)
            den = self.tile(ph, "dens", [128, 4], F32)
            mst = [self.tile(ph, "msts%d" % i, [128, 4, 128], BF16) for i in range(2)]
            for c in range(self.nch):
                q = QT[c % 2]
                for h in range(2):
                    S.load("sp", q[0:64, h, :], self.featT[FRr["sq"] + 64 * h:FRr["sq"] + 64 * (h + 1), c * 512:(c + 1) * 512], q.b)
                ms = mst[c % 2]
                for h in range(2):
                    g = 0
                    tiles = []
                    for r in range(-1, 4):
                        kt = 4 * c + r
                        if kt < 0:
                            continue
                        tiles.append(dict(kT=KT[0:67, g, kt * 128:(kt + 1) * 128], v=V[:, g, kt, :], kbufs=[KT.b, V.b],
                                          bias=self.kb[:, 12 + h, r + 60:r + 61], bbufs=[self.kb.b],
                                          extra=[(self.ident[:, :], SM[:, r + 1, :], [self.ident.b, SM.b])]))
                    O = R.O[R.io % len(R.O)]
                    R.io += 1
                    Ov = self.attend(R, q[0:67, h, :], [q.b], tiles, O, 0.125)
                    self.recip_den(Ov, O, den, add_ap=esk[:, h:h + 1], add_bufs=[esk.b])
                    for qi in range(4):
                        S.op("dve", lambda: nc.vector.tensor_scalar(out=ms[:, qi, h * 64:(h + 1) * 64], in0=Ov[:, qi, 0:64], scalar1=den[:, qi:qi + 1],
                                                                    scalar2=None, op0=ALU.mult), reads=[O.b, den.b], writes=[ms.b])
                S.store("pool", self.mix[c * 512:(c + 1) * 512, 384:512].rearrange("(a p) d -> p a d", p=128), ms[:, :, :], ms.b)
            S.barrier()

    def diff(self, l):
        nc, S = self.nc, self.S
        lambda_init = 0.8 - 0.6 * math.exp(-0.3 * l)
        sc = 32 ** -0.5
        with ExitStack() as ph:
            KT = self.tile(ph, "KTd", [128, 2, S_LEN], BF16)
            V = self.tile(ph, "Vd", [128, 2, NT, 65], BF16)
            S.op("pool", lambda: nc.gpsimd.memset(V[:, :, :, 64:65], 1.0), writes=[V.b])
            for h in range(2):
                r0 = FRr["dk"] + 64 * h
                S.load("sp", KT[0:32, h, :], self.featT[r0:r0 + 32, :], KT.b)
                S.load("sp", KT[64:96, h, :], self.featT[r0 + 32:r0 + 64, :], KT.b)
                S.load("sp", V[:, h, :, 0:64], self.vtok[:, VCr["dv"] + h * 64:VCr["dv"] + (h + 1) * 64].rearrange("(kt p) d -> p kt d", p=128), V.b)
            ones = self.dc["ones3"][:, 0:2 * S_LEN].rearrange("r (h s) -> r h s", h=2)
            S.load("sp", KT[32:35, :, :], ones, KT.b)
            S.load("sp", KT[96:99, :, :], ones, KT.b)
            lam = self.tile(ph, "lam", [128, 4], F32)
            lt = self.tile(ph, "lamt", [128, 4, 32], F32)
            lj = self.tile(ph, "lamj", [128, 32], F32)
            for i, nme in enumerate(["lq1", "lk1", "lq2", "lk2"]):
                S.load("sp", lt[:, i, :], self.dp[nme][l].partition_broadcast(128), lt.b)
            for i in range(2):
                S.op("dve", lambda: nc.vector.scalar_tensor_tensor(out=lj[:, :], in0=lt[:, 2 * i, :], scalar=1.0, in1=lt[:, 2 * i + 1, :],
                                                                   op0=ALU.mult, op1=ALU.mult, accum_out=lam[:, i:i + 1]),
                     reads=[lt.b], writes=[lj.b, lam.b])
            S.op("act", lambda: nc.scalar.activation(out=lam[:, 0:2], in_=lam[:, 0:2], func=AF.Exp), reads=[lam.b], writes=[lam.b])
            S.op("dve", lambda: nc.vector.tensor_tensor(out=lam[:, 2:3], in0=lam[:, 1:2], in1=lam[:, 0:1], op=ALU.subtract),
                 reads=[lam.b], writes=[lam.b])
            S.op("dve", lambda: nc.vector.tensor_scalar(out=lam[:, 2:3], in0=lam[:, 2:3], scalar1=-lambda_init, scalar2=None, op0=ALU.add),
                 reads=[lam.b], writes=[lam.b])
            gs = self.tile(ph, "gsub", [128, 64], F32)
            S.load("sp", gs[:, :], self.dp["subln"][l].partition_broadcast(128), gs.b)
            S.op("dve", lambda: nc.vector.tensor_scalar(out=gs[:, :], in0=gs[:, :], scalar1=1.0 - lambda_init, scalar2=None, op0=ALU.mult),
                 reads=[gs.b], writes=[gs.b])
            R = self.attn_res(ph, no=4)
            QT = [self.tile(ph, "QTd%d" % i, [128, 2, 512], BF16) for i in range(2)]
            for q in QT:
                a = self.dc["aug"][8:10].rearrange("h r q -> r h q")
                S.load("sp", q[32:35, :, :], a, q.b)
                S.load("sp", q[96:99, :, :], a, q.b)
            den1 = self.tile(ph, "den1", [128, 4], F32)
            den2 = self.tile(ph, "den2", [128, 4], F32)
            o1 = self.tile(ph, "o1d", [128, 4, 64], F32)
            od = self.tile(ph, "od", [128, 4, 64], F32)
            jk = self.tile(ph, "jkd", [128, 64], F32)
            ssd = self.tile(ph, "ssd", [128, 4], F32)
            mst = [self.tile(ph, "mstd%d" % i, [128, 4, 128], BF16) for i in range(2)]
            for c in range(self.nch):
                q = QT[c % 2]
                for h in range(2):
                    r0 = FRr["dq"] + 64 * h
                    S.load("sp", q[0:32, h, :], self.featT[r0:r0 + 32, c * 512:(c + 1) * 512], q.b)
                    S.load("sp", q[64:96, h, :], self.featT[r0 + 32:r0 + 64, c * 512:(c + 1) * 512], q.b)
                ms = mst[c % 2]
                for h in range(2):
                    Os = []
                    for j in range(2):
                        b0 = 64 * j
                        tiles = []
                        for kt in range(4 * c + 4):
                            if far_skip(2, h, 512 * c - (128 * kt + 127)):
                                continue
                            r = kt - 4 * c
                            ex = []
                            if r >= 0:
                                ex.append((self.ident[:, :], self.caus[:, r, :], [self.ident.b, self.caus.b]))
                            tiles.append(dict(kT=KT[b0:b0 + 35, h, kt * 128:(kt + 1) * 128], v=V[:, h, kt, :], kbufs=[KT.b, V.b],
                                              bias=self.kb[:, 8 + h, r + 60:r + 61], bbufs=[self.kb.b], extra=ex))
                        O = R.O[R.io % len(R.O)]
                        R.io += 1
                        Ov = self.attend(R, q[b0:b0 + 35, h, :], [q.b], tiles, O, sc)
                        Os.append((O, Ov))
                    (O1, Ov1), (O2, Ov2) = Os
                    self.recip_den(Ov1, O1, den1)
                    self.recip_den(Ov2, O2, den2)
                    S.op("dve", lambda: nc.vector.tensor_scalar(out=den2[:, :], in0=den2[:, :], scalar1=lam[:, 2:3], scalar2=None, op0=ALU.mult),
                         reads=[den2.b, lam.b], writes=[den2.b])
                    for qi in range(4):
                        S.op("dve", lambda: nc.vector.tensor_scalar(out=o1[:, qi, :], in0=Ov1[:, qi, 0:64], scalar1=den1[:, qi:qi + 1], scalar2=None, op0=ALU.mult),
                             reads=[O1.b, den1.b], writes=[o1.b])
                        S.op("dve", lambda: nc.vector.scalar_tensor_tensor(out=od[:, qi, :], in0=Ov2[:, qi, 0:64], scalar=den2[:, qi:qi + 1], in1=o1[:, qi, :],
                                                                           op0=ALU.mult, op1=ALU.add), reads=[O2.b, den2.b, o1.b], writes=[od.b])
                        S.op("dve", lambda: nc.vector.scalar_tensor_tensor(out=jk[:, :], in0=od[:, qi, :], scalar=1.0, in1=od[:, qi, :],
                                                                           op0=ALU.mult, op1=ALU.mult, accum_out=ssd[:, qi:qi + 1]),
                             reads=[od.b], writes=[jk.b, ssd.b])
                    S.op("act", lambda: nc.scalar.activation(out=ssd[:, :], in_=ssd[:, :], func=AF.Ln, bias=self.epsT[:, 0:1], scale=1.0 / 64),
                         reads=[ssd.b, self.epsT.b], writes=[ssd.b])
                    S.op("act", lambda: nc.scalar.activation(out=ssd[:, :], in_=ssd[:, :], func=AF.Exp, scale=-0.5), reads=[ssd.b], writes=[ssd.b])
                    for qi in range(4):
                        S.op("dve", lambda: nc.vector.scalar_tensor_tensor(out=ms[:, qi, h * 64:(h + 1) * 64], in0=od[:, qi, :], scalar=ssd[:, qi:qi + 1],
                                                                           in1=gs[:, :], op0=ALU.mult, op1=ALU.mult),
                             reads=[od.b, ssd.b, gs.b], writes=[ms.b])
                S.store("pool", self.mix[c * 512:(c + 1) * 512, 256:384].rearrange("(a p) d -> p a d", p=128), ms[:, :, :], ms.b)
            S.barrier()

    def nsa(self, l):
        nc, S = self.nc, self.S
        with ExitStack() as ph:
            KCT = self.tile(ph, "KCT", [128, 512], BF16)
            VCt = self.tile(ph, "VCt", [128, 4, 65], BF16)
            S.op("pool", lambda: nc.gpsimd.memset(VCt[:, :, :], 1.0), writes=[VCt.b])
            S.load("sp", KCT[64:67, :], self.dc["ones3"][:, 0:512], KCT.b)
            with ExitStack() as cs:
                XT = self.tile(cs, "XTc", [64, S_LEN], BF16)
                w1 = self.tile(cs, "w1c", [64, 32, 128], BF16)
                w2 = self.tile(cs, "w2c", [128, 64], BF16)
                pos = self.tile(cs, "posc", [64, 32], BF16)
                b1 = self.tile(cs, "b1c", [128, 1], F32)
                cbias = self.tile(cs, "cbias", [128, 1], F32)
                hid = self.tile(cs, "hidc", [128, 512], BF16)
                sg = self.tile(cs, "sgc", [128, 4096], F32)
                hp = self.tile(cs, "hpc", [128, 512], F32, psum=True)
                cp = self.tile(cs, "cpc", [128, 512], F32, psum=True)
                op_ = self.tile(cs, "opc", [128, 512], F32, psum=True)
                for s_i, sfx in enumerate("kv"):
                    S.load("sp", XT[:, :], self.featT[FRr["nkc" if sfx == "k" else "nvc"]:FRr["nkc" if sfx == "k" else "nvc"] + 64, :], XT.b)
                    S.load("sp", sg[0:64, 0:4096], self.dp["w1" + sfx][l].rearrange("d l f -> d (l f)"), sg.b)
                    self.copy(w1[:, :, :], sg[0:64, 0:4096].rearrange("d (l f) -> d l f", l=32), [sg.b], [w1.b])
                    S.load("sp", sg[:, 0:64], self.dp["w2" + sfx][l], sg.b)
                    self.copy(w2[:, :], sg[:, 0:64], [sg.b], [w2.b])
                    S.load("sp", sg[0:64, 0:32], self.dp["pos" + sfx][l], sg.b)
                    self.copy(pos[:, :], sg[0:64, 0:32], [sg.b], [pos.b])
                    S.load("sp", b1[:, :], self.dp["b1" + sfx][l], b1.b)
                    for li in range(32):
                        S.op("pe", lambda: nc.tensor.matmul(hp[:, 0:511], lhsT=w1[:, li, :], rhs=XT[:, li:li + 16 * 510 + 1:16], start=(li == 0), stop=(li == 31)),
                             reads=[w1.b, XT.b], writes=[hp.b], accum=(li > 0))
                    for li in range(32):
                        S.op("pe", lambda: nc.tensor.matmul(cp[:, 0:1], lhsT=w1[:, li, :], rhs=pos[:, li:li + 1], start=(li == 0), stop=(li == 31)),
                             reads=[w1.b, pos.b], writes=[cp.b], accum=(li > 0))
                    S.op("dve", lambda: nc.vector.tensor_tensor(out=cbias[:, :], in0=cp[:, 0:1], in1=b1[:, :], op=ALU.add),
                         reads=[cp.b, b1.b], writes=[cbias.b])
                    S.op("pool", lambda: nc.gpsimd.memset(hid[:, :], 0.0), writes=[hid.b])
                    S.op("act", lambda: nc.scalar.activation(out=hid[:, 0:511], in_=hp[:, 0:511], func=AF.Gelu_apprx_tanh, bias=cbias[:, 0:1]),
                         reads=[hp.b, cbias.b], writes=[hid.b])
                    if sfx == "k":
                        S.op("pe", lambda: nc.tensor.matmul(op_[0:64, 0:512], lhsT=w2[:, :], rhs=hid[:, :], start=True, stop=True),
                             reads=[w2.b, hid.b], writes=[op_.b])
                        self.copy(KCT[0:64, :], op_[0:64, 0:512], [op_.b], [KCT.b])
                    else:
                        ov_ = op_[:, 0:256].rearrange("p (a b) -> p a b", a=4)
                        for kt in range(4):
                            S.op("pe", lambda: nc.tensor.matmul(ov_[:, kt, :], lhsT=hid[:, kt * 128:(kt + 1) * 128], rhs=w2[:, :], start=True, stop=True),
                                 reads=[w2.b, hid.b], writes=[op_.b], accum=(kt > 0))
                        self.copy(VCt[:, :, 0:64], ov_, [op_.b], [VCt.b])
                S.barrier()
            KTs = self.tile(ph, "KTsl", [128, 1, S_LEN], BF16)
            KTw = self.tile(ph, "KTwn", [128, 1, S_LEN], BF16)
            Vs = self.tile(ph, "Vsl", [128, 1, NT, 65], BF16)
            Vw = self.tile(ph, "Vwn", [128, 1, NT, 65], BF16)
            self.load_kv(KTs, Vs, 67, 1, FRr["nks"], VCr["nvs"], 1)
            self.load_kv(KTw, Vw, 67, 1, FRr["nkw"], VCr["nvw"], 1)
            ES = self.tile(ph, "ESn", [128, S_LEN], BF16)
            WM = self.tile(ph, "WMn", [128, 8, 512], BF16)
            CM = self.tile(ph, "CMn", [128, 5, 512], BF16)
            OVt = self.tile(ph, "OVn", [128, 4, 128], BF16)
            KBC = self.tile(ph, "KBCn", [128, 4, 16, 4], F32)
            S.load("sp", ES[:, :], self.dc["es"], ES.b)
            S.load("sp", WM[:, :, :], self.dc["wmask"], WM.b)
            S.load("sp", CM[:, :, :], self.dc["cmask"], CM.b)
            S.load("sp", OVt[:, :, :], self.dc["ov"], OVt.b)
            S.load("sp", KBC[:, :, :, :], self.dc["kbc"], KBC.b)
            R = self.attn_res(ph)
            QT = [self.tile(ph, "QTn%d" % i, [128, 4, 512], BF16) for i in range(2)]
            for q in QT:
                S.load("sp", q[64:67, :, :], self.dc["aug"][4:8].rearrange("h r q -> r h q"), q.b)
            imps = self.tile(ph, "imps", [128, 512], F32, psum=True)
            tps = self.tile(ph, "tpsn", [128, 1024], BF16, psum=True)
            impa = self.tile(ph, "impa", [128, 4, 128], F32)
            f1 = [self.tile(ph, "f1e4_%d" % i, [128, 4, 128], F32) for i in range(2)]
            nv = [self.tile(ph, "negv_%d" % i, [128, 4, 128], F32) for i in range(2)]
            gr = [self.tile(ph, "graw%d" % i, [128, 4, 12], BF16) for i in range(2)]
            sgm = self.tile(ph, "sgm", [128, 4, 12], F32)
            scm = self.tile(ph, "scm", [128, 4, 128], F32)
            tmpm = self.tile(ph, "tmpm", [128, 4, 128], F32)
            m8a = self.tile(ph, "m8a", [128, 4, 8], F32)
            m8b = self.tile(ph, "m8b", [128, 4, 8], F32)
            sel = self.tile(ph, "seln", [128, 4, 128], F32)
            val = self.tile(ph, "valn", [128, 4, 128], F32)
            nsel = self.tile(ph, "nseln", [128, 4, 128], BF16)
            nselT = self.tile(ph, "nselTn", [128, 512], BF16)
            den = self.tile(ph, "denn", [128, 4], F32)
            acc = self.tile(ph, "accn", [128, 2, 4, 64], F32)
            mst = [self.tile(ph, "mstn%d" % i, [128, 4, 128], BF16) for i in range(2)]

            def fold(O, Ov, h, gcol, first):
                self.recip_den(Ov, O, den)
                S.op("dve", lambda: nc.vector.tensor_tensor(out=den[:, :], in0=den[:, :], in1=sgm[:, :, gcol], op=ALU.mult),
                     reads=[den.b, sgm.b], writes=[den.b])
                for qi in range(4):
                    if first:
                        S.op("dve", lambda: nc.vector.tensor_scalar(out=acc[:, h, qi, :], in0=Ov[:, qi, 0:64], scalar1=den[:, qi:qi + 1], scalar2=None, op0=ALU.mult),
                             reads=[O.b, den.b], writes=[acc.b])
                    else:
                        S.op("dve", lambda: nc.vector.scalar_tensor_tensor(out=acc[:, h, qi, :], in0=Ov[:, qi, 0:64], scalar=den[:, qi:qi + 1], in1=acc[:, h, qi, :],
                                                                           op0=ALU.mult, op1=ALU.add), reads=[O.b, den.b, acc.b], writes=[acc.b])

            for c in range(self.nch):
                q = QT[c % 2]
                for h in range(4):
                    S.load("sp", q[0:64, h, :], self.featT[FRr["nq"] + 64 * h:FRr["nq"] + 64 * (h + 1), c * 512:(c + 1) * 512], q.b)
                f1c, nvc, grc = f1[c % 2], nv[c % 2], gr[c % 2]
                S.load("sp", f1c[:, :, :], self.dc["f1e4"][c], f1c.b)
                S.load("sp", nvc[:, :, :], self.dc["negv"][c], nvc.b)
                S.load("sp", grc[:, :, :], self.vtok[c * 512:(c + 1) * 512, VCr["ng"]:VCr["ng"] + 12].rearrange("(a p) g -> p a g", p=128), grc.b)
                S.op("act", lambda: nc.scalar.activation(out=sgm[:, :, :], in_=grc[:, :, :], func=AF.Exp, scale=-1.0), reads=[grc.b], writes=[sgm.b])
                S.op("dve", lambda: nc.vector.tensor_scalar(out=sgm[:, :, :], in0=sgm[:, :, :], scalar1=1.0, scalar2=None, op0=ALU.add),
                     reads=[sgm.b], writes=[sgm.b])
                S.op("dve", lambda: nc.vector.reciprocal(out=sgm[:, :, :], in_=sgm[:, :, :]), reads=[sgm.b], writes=[sgm.b])
                ms = mst[c % 2]
                kb_ = c // 4
                iv = imps[:, :].rearrange("p (a b) -> p a b", a=4)
                for h in range(4):
                    tiles = []
                    ntl = kb_ + 1

                    def mkpost(kt, ntl=ntl):
                        def post(pt, first, last):
                            for qi in range(4):
                                S.op("pe", lambda: nc.tensor.matmul(iv[:, qi, :], lhsT=pt[:, qi * 128:(qi + 1) * 128], rhs=OVt[:, kt, :],
                                                                    start=(first and qi == 0), stop=last),
                                     reads=[pt.b, OVt.b], writes=[imps.b], accum=not (first and qi == 0))
                        return post
                    for kt in range(ntl):
                        ex = []
                        if kt == kb_:
                            ex.append((self.ident[:, :], CM[:, c % 4, :], [self.ident.b, CM.b]))
                        elif kt == kb_ - 1 and c % 4 == 0:
                            ex.append((self.ident[:, :], CM[:, 4, :], [self.ident.b, CM.b]))
                        tiles.append(dict(kT=KCT[0:67, kt * 128:(kt + 1) * 128], v=VCt[:, kt, :], kbufs=[KCT.b, VCt.b],
                                          bias=KBC[:, h, c, kt:kt + 1], bbufs=[KBC.b], extra=ex, post=mkpost(kt)))
                    O = R.O[R.io % len(R.O)]
                    R.io += 1
                    Ov = self.attend(R, q[0:67, h, :], [q.b], tiles, O, 0.125)
                    self.recip_den(Ov, O, den)
                    for qi in range(4):
                        if h == 0:
                            S.op("dve", lambda: nc.vector.tensor_scalar(out=impa[:, qi, :], in0=iv[:, qi, :], scalar1=den[:, qi:qi + 1], scalar2=None, op0=ALU.mult),
                                 reads=[imps.b, den.b], writes=[impa.b])
                        else:
                            S.op("dve", lambda: nc.vector.scalar_tensor_tensor(out=impa[:, qi, :], in0=iv[:, qi, :], scalar=den[:, qi:qi + 1], in1=impa[:, qi, :],
                                                                               op0=ALU.mult, op1=ALU.add), reads=[imps.b, den.b, impa.b], writes=[impa.b])
                    if h < 2:
                        fold(O, Ov, h, 3 * h + 0, True)
                S.op("dve", lambda: nc.vector.tensor_tensor(out=scm[:, :, :], in0=impa[:, :, :], in1=f1c[:, :, :], op=ALU.max),
                     reads=[impa.b, f1c.b], writes=[scm.b])
                S.op("dve", lambda: nc.vector.tensor_tensor(out=scm[:, :, :], in0=scm[:, :, :], in1=nvc[:, :, :], op=ALU.add),
                     reads=[scm.b, nvc.b], writes=[scm.b])
                for qi in range(4):
                    S.op("dve", lambda: nc.vector.max(out=m8a[:, qi, :], in_=scm[:, qi, :]), reads=[scm.b], writes=[m8a.b])
                    S.op("dve", lambda: nc.vector.match_replace(out=tmpm[:, qi, :], in_to_replace=m8a[:, qi, :], in_values=scm[:, qi, :], imm_value=-1e30),
                         reads=[scm.b, m8a.b], writes=[tmpm.b])
                    S.op("dve", lambda: nc.vector.max(out=m8b[:, qi, :], in_=tmpm[:, qi, :]), reads=[tmpm.b], writes=[m8b.b])
                    S.op("dve", lambda: nc.vector.tensor_scalar(out=sel[:, qi, :], in0=scm[:, qi, :], scalar1=m8b[:, qi, 7:8], scalar2=None, op0=ALU.is_ge),
                         reads=[scm.b, m8b.b], writes=[sel.b])
                S.op("dve", lambda: nc.vector.tensor_scalar(out=val[:, :, :], in0=nvc[:, :, :], scalar1=-1.0, scalar2=None, op0=ALU.is_ge),
                     reads=[nvc.b], writes=[val.b])
                S.op("dve", lambda: nc.vector.tensor_tensor(out=sel[:, :, :], in0=sel[:, :, :], in1=val[:, :, :], op=ALU.mult),
                     reads=[sel.b, val.b], writes=[sel.b])
                S.op("dve", lambda: nc.vector.tensor_scalar(out=nsel[:, :, :], in0=sel[:, :, :], scalar1=-1.0, scalar2=-NEG, op0=ALU.add, op1=ALU.mult),
                     reads=[sel.b], writes=[nsel.b])
                for qi in range(4):
                    S.op("pe", lambda: nc.tensor.transpose(out=tps[:, qi * 128:(qi + 1) * 128], in_=nsel[:, qi, :], identity=self.ident[:, :]),
                         reads=[nsel.b, self.ident.b], writes=[tps.b], accum=(qi > 0))
                self.copy(nselT[:, :], tps[:, 0:512], [tps.b], [nselT.b], eng="dve")
                for h in range(2):
                    tiles = []
                    for kt in range(4 * c + 4):
                        if far_skip(1, h, 512 * c - (128 * kt + 127)):
                            continue
                        r = kt - 4 * c
                        ex = [(ES[:, kt * 128:(kt + 1) * 128], nselT[:, :], [ES.b, nselT.b])]
                        if r >= 0:
                            ex.append((self.ident[:, :], self.caus[:, r, :], [self.ident.b, self.caus.b]))
                        tiles.append(dict(kT=KTs[0:67, 0, kt * 128:(kt + 1) * 128], v=Vs[:, 0, kt, :], kbufs=[KTs.b, Vs.b],
                                          bias=self.kb[:, 4 + h, r + 60:r + 61], bbufs=[self.kb.b], extra=ex))
                    O = R.O[R.io % len(R.O)]
                    R.io += 1
                    Ov = self.attend(R, q[0:67, h, :], [q.b], tiles, O, 0.125)
                    fold(O, Ov, h, 3 * h + 1, False)
                    tiles = []
                    for r in range(-4, 4):
                        kt = 4 * c + r
                        if kt < 0:
                            continue
                        tiles.append(dict(kT=KTw[0:67, 0, kt * 128:(kt + 1) * 128], v=Vw[:, 0, kt, :], kbufs=[KTw.b, Vw.b],
                                          bias=self.kb[:, 4 + h, r + 60:r + 61], bbufs=[self.kb.b],
                                          extra=[(self.ident[:, :], WM[:, r + 4, :], [self.ident.b, WM.b])]))
                    O = R.O[R.io % len(R.O)]
                    R.io += 1
                    Ov = self.attend(R, q[0:67, h, :], [q.b], tiles, O, 0.125)
                    fold(O, Ov, h, 3 * h + 2, False)
                    S.op("dve", lambda: nc.vector.tensor_copy(out=ms[:, :, h * 64:(h + 1) * 64], in_=acc[:, h, :, :]), reads=[acc.b], writes=[ms.b])
                S.store("pool", self.mix[c * 512:(c + 1) * 512, 128:256].rearrange("(a p) d -> p a d", p=128), ms[:, :, :], ms.b)
            S.barrier()

    def prep_ffn(self, l):
        S = self.S
        with ExitStack() as ph:
            stg = [self.tile(ph, "pstg%d" % i, [128, 4096], F32) for i in range(2)]
            sb = [self.tile(ph, "psb%d" % i, [128, 4096], BF16) for i in range(2)]
            i = 0
            for src, dst in ((self.dp["wg"], self.wgs), (self.dp["wu"], self.wus), (self.dp["wd"], self.wds)):
                for g in range(NCF // 4):
                    s_, b_ = stg[i % 2], sb[i % 2]
                    i += 1
                    if len(src.shape) == 5:
                        sap = src[l, 4 * g:4 * g + 4].rearrange("c p k n -> p c (k n)")
                        dap = dst[4 * g:4 * g + 4].rearrange("c p k n -> p c (k n)")
                    else:
                        sap = src[l, 4 * g:4 * g + 4].rearrange("c p n -> p c n")
                        dap = dst[4 * g:4 * g + 4].rearrange("c p n -> p c n")
                    S.load("sp", s_[:, :].rearrange("p (c n) -> p c n", c=4), sap, s_.b)
                    self.copy(b_[:, :], s_[:, :], [s_.b], [b_.b])
                    S.store("pool", dap, b_[:, :].rearrange("p (c n) -> p c n", c=4), b_.b)
            S.barrier()

    def phase_C(self, l, x_src, x_dst):
        nc, S = self.nc, self.S
        with ExitStack() as ph:
            WO = self.tile(ph, "WO", [128, 8, D], BF16)
            with ExitStack() as tmp:
                self.load_cast(tmp, WO, lambda i: WO[:, i, :], lambda i: self.dp["wo"][l, :, i, :], 8, D)
                S.barrier()
            g1 = self.tile(ph, "g_apost", [128, D], F32)
            g2 = self.tile(ph, "g_fpre", [128, D], F32)
            g3 = self.tile(ph, "g_fpost", [128, D], F32)
            S.load("sp", g1[:, :], self.dp["g_apost"][l].partition_broadcast(128), g1.b)
            S.load("sp", g2[:, :], self.dp["g_fpre"][l].partition_broadcast(128), g2.b)
            S.load("sp", g3[:, :], self.dp["g_fpost"][l].partition_broadcast(128), g3.b)
            cw = self.tile(ph, "cw", [128, NCF, 3], F32)
            cb = self.tile(ph, "cb", [128, NCF], F32)
            S.load("sp", cw[:, :, :], self.dp["cw"][l], cw.b)
            S.load("sp", cb[:, :], self.dp["cb"][l], cb.b)
            halo = self.tile(ph, "halo", [128, NCF, 2], F32)
            S.op("pool", lambda: nc.gpsimd.memset(halo[:, :, :], 0.0), writes=[halo.b])
            bank = [self.tile(ph, "bk%d" % i, [128, 512], F32, psum=True) for i in range(6)]
            tpb = [self.tile(ph, "tpC%d" % i, [128, D], BF16, psum=True) for i in range(2)]
            mixt = [self.tile(ph, "mixt%d" % i, [128, D], BF16) for i in range(2)]
            mT = [self.tile(ph, "mT%d" % i, [128, 8, 128], BF16) for i in range(2)]
            xt = [self.tile(ph, "xtC%d" % i, [128, D], F32) for i in range(2)]
            x1 = [self.tile(ph, "x1C%d" % i, [128, D], F32) for i in range(2)]
            yt = self.tile(ph, "ytC", [128, D], F32)
            junk = self.tile(ph, "junkC", [128, D], F32)
            ss = self.tile(ph, "ssC", [128, 1], F32)
            sd = self.tile(ph, "sdC", [128, 1], F32)
            rstd = self.tile(ph, "rstdC", [128, 1], F32)
            hb = [self.tile(ph, "hbC%d" % i, [128, D], BF16) for i in range(4)]
            h2T = self.tile(ph, "h2T", [128, 8, 512], BF16)
            gT = self.tile(ph, "gT", [128, NCF, 512], BF16)
            wgt = [self.tile(ph, "wgt%d" % i, [128, 8, 128], BF16) for i in range(3)]
            wut = [self.tile(ph, "wut%d" % i, [128, 8, 128], BF16) for i in range(3)]
            wdt = [self.tile(ph, "wdt%d" % i, [128, D], BF16) for i in range(3)]
            aT = [self.tile(ph, "aT%d" % i, [128, 514], F32) for i in range(2)]
            cacc = [self.tile(ph, "cacc%d" % i, [128, 512], F32) for i in range(2)]
            ga = [self.tile(ph, "ga%d" % i, [128, 512], F32) for i in range(2)]
            pst = [self.tile(ph, "pstC%d" % i, [128, D], F32) for i in range(2)]
            rt = [self.tile(ph, "rtC%d" % i, [128, D], F32) for i in range(2)]
            x1r = [self.tile(ph, "x1rC%d" % i, [128, D], F32) for i in range(2)]
            ot = [self.tile(ph, "otC%d" % i, [128, D], F32) for i in range(2)]
            ss3 = self.tile(ph, "ss3C", [128, 1], F32)
            sd3 = self.tile(ph, "sd3C", [128, 1], F32)
            rstd3 = self.tile(ph, "rstd3C", [128, 1], F32)
            junk3 = self.tile(ph, "junk3C", [128, D], F32)
            it = 0
            i3 = 0
            RNG = 2
            pending = []
            store_tags = []

            def finish_tile(tag, tt):
                nonlocal i3
                S._wait("sp", tag)
                tok = tt * 128
                r_, x_, o_ = rt[i3 % 2], x1r[i3 % 2], ot[i3 % 2]
                i3 += 1
                S.load("sp", r_[:, :], self.red[tok:tok + 128, :], r_.b)
                S.load("sp", x_[:, :], self.x1s[tok:tok + 128, :], x_.b)
                self.rstd_of(r_[:, :], r_.b, junk3, ss3, sd3, rstd3)
                S.op("dve", lambda: nc.vector.scalar_tensor_tensor(out=r_[:, :], in0=r_[:, :], scalar=rstd3[:, 0:1], in1=g3[:, :], op0=ALU.mult, op1=ALU.mult),
                     reads=[r_.b, rstd3.b, g3.b], writes=[r_.b])
                S.op("pool", lambda: nc.gpsimd.tensor_tensor(out=o_[:, :], in0=r_[:, :], in1=x_[:, :], op=ALU.add),
                     reads=[r_.b, x_.b], writes=[o_.b])
                S.store("pool", x_dst[tok:tok + 128, :], o_[:, :], o_.b)

            h2Ts = [h2T, self.tile(ph, "h2Tb", [128, 8, 512], BF16)]

            def seg_a(c, ti):
                nonlocal it
                tok = (4 * c + ti) * 128
                mt, mTt, xtt, h, tpp, x1t = mixt[it % 2], mT[it % 2], xt[it % 2], hb[it % 4], tpb[it % 2], x1[it % 2]
                it += 1
                mrow = (tok // 2048) * 4096 + (tok % 2048)
                S.load("sp", mt[:, 0:512], self.mixall[mrow:mrow + 128, :], mt.b)
                S.load("sp", mt[:, 512:1024], self.mixall[2048 + mrow:2048 + mrow + 128, :], mt.b)
                S.load("sp", xtt[:, :], x_src[tok:tok + 128, :], xtt.b)
                for kc in range(8):
                    S.op("pe", lambda: nc.tensor.transpose(out=tpp[:, kc * 128:(kc + 1) * 128], in_=mt[:, kc * 128:(kc + 1) * 128], identity=self.ident[:, :]),
                         reads=[mt.b, self.ident.b], writes=[tpp.b], accum=(kc > 0))
                self.copy(mTt[:, :, :], tpp[:, :].rearrange("p (k t) -> p k t", k=8), [tpp.b], [mTt.b])
                pa, pb = bank[4], bank[5]
                for half, pp in enumerate((pa, pb)):
                    for kc in range(8):
                        S.op("pe", lambda: nc.tensor.matmul(pp[:, :], lhsT=mTt[:, kc, :], rhs=WO[:, kc, half * 512:(half + 1) * 512],
                                                            start=(kc == 0), stop=(kc == 7)), reads=[mTt.b, WO.b], writes=[pp.b], accum=(kc > 0))
                self.copy(yt[:, 0:512], pa[:, :], [pa.b], [yt.b], eng="act")
                self.copy(yt[:, 512:D], pb[:, :], [pb.b], [yt.b], eng="act")
                self.rstd_of(yt[:, :], yt.b, junk, ss, sd, rstd)
                S.op("dve", lambda: nc.vector.scalar_tensor_tensor(out=yt[:, :], in0=yt[:, :], scalar=rstd[:, 0:1], in1=g1[:, :], op0=ALU.mult, op1=ALU.mult),
                     reads=[yt.b, rstd.b, g1.b], writes=[yt.b])
                S.op("pool", lambda: nc.gpsimd.tensor_tensor(out=x1t[:, :], in0=yt[:, :], in1=xtt[:, :], op=ALU.add),
                     reads=[yt.b, xtt.b], writes=[x1t.b])
                S.store("pool", self.x1s[tok:tok + 128, :], x1t[:, :], x1t.b)
                store_tags.append(("dma", x1t.b.dsem, x1t.b.dcnt))
                self.rstd_of(x1t[:, :], x1t.b, junk, ss, sd, rstd)
                S.op("dve", lambda: nc.vector.scalar_tensor_tensor(out=h[:, :], in0=x1t[:, :], scalar=rstd[:, 0:1], in1=g2[:, :], op0=ALU.mult, op1=ALU.mult),
                     reads=[x1t.b, rstd.b, g2.b], writes=[h.b])
                return (h, tpp)

            def seg_b(c, ti, ctx):
                h, tpp = ctx
                hT = h2Ts[c % 2]
                for kc in range(8):
                    S.op("pe", lambda: nc.tensor.transpose(out=tpp[:, kc * 128:(kc + 1) * 128], in_=h[:, kc * 128:(kc + 1) * 128], identity=self.ident[:, :]),
                         reads=[h.b, self.ident.b], writes=[tpp.b], accum=(kc > 0))
                self.copy(hT[:, :, ti * 128:(ti + 1) * 128], tpp[:, :].rearrange("p (k t) -> p k t", k=8), [tpp.b], [hT.b])

            def c2a_cf(c, cf):
                hT = h2Ts[c % 2]
                wg_, wu_ = wgt[cf % 3], wut[cf % 3]
                S.load("sp", wg_[:, :, :], self.wgs[cf], wg_.b)
                S.load("sp", wu_[:, :, :], self.wus[cf], wu_.b)
                pa, pu = bank[2 * (cf % 2)], bank[2 * (cf % 2) + 1]
                for kc in range(8):
                    S.op("pe", lambda: nc.tensor.matmul(pa[:, :], lhsT=wg_[:, kc, :], rhs=hT[:, kc, :], start=(kc == 0), stop=(kc == 7)),
                         reads=[wg_.b, hT.b], writes=[pa.b], accum=(kc > 0))
                for kc in range(8):
                    S.op("pe", lambda: nc.tensor.matmul(pu[:, :], lhsT=wu_[:, kc, :], rhs=hT[:, kc, :], start=(kc == 0), stop=(kc == 7)),
                         reads=[wu_.b, hT.b], writes=[pu.b], accum=(kc > 0))
                a, ca, g = aT[cf % 2], cacc[cf % 2], ga[cf % 2]
                e4 = a[:, 0:4]
                S.op("dve", lambda: nc.vector.tensor_copy(out=a[:, 0:2], in_=halo[:, cf, :]), reads=[halo.b], writes=[a.b])
                S.op("dve", lambda: nc.vector.tensor_copy(out=a[:, 2:4], in_=pa[:, 0:2]), reads=[pa.b], writes=[a.b])
                S.op("dve", lambda: nc.vector.tensor_copy(out=halo[:, cf, :], in_=pa[:, 510:512]), reads=[pa.b], writes=[halo.b])
                S.op("dve", lambda: nc.vector.tensor_scalar(out=ca[:, 2:512], in0=pa[:, 0:510], scalar1=cw[:, cf, 0:1], scalar2=None, op0=ALU.mult),
                     reads=[pa.b, cw.b], writes=[ca.b])
                S.op("dve", lambda: nc.vector.scalar_tensor_tensor(out=ca[:, 2:512], in0=pa[:, 1:511], scalar=cw[:, cf, 1:2], in1=ca[:, 2:512], op0=ALU.mult, op1=ALU.add),
                     reads=[pa.b, cw.b, ca.b], writes=[ca.b])
                S.op("dve", lambda: nc.vector.scalar_tensor_tensor(out=ca[:, 2:512], in0=pa[:, 2:512], scalar=cw[:, cf, 2:3], in1=ca[:, 2:512], op0=ALU.mult, op1=ALU.add),
                     reads=[pa.b, cw.b, ca.b], writes=[ca.b])
                S.op("dve", lambda: nc.vector.tensor_scalar(out=ca[:, 0:2], in0=e4[:, 0:2], scalar1=cw[:, cf, 0:1], scalar2=None, op0=ALU.mult),
                     reads=[a.b, cw.b], writes=[ca.b])
                S.op("dve", lambda: nc.vector.scalar_tensor_tensor(out=ca[:, 0:2], in0=e4[:, 1:3], scalar=cw[:, cf, 1:2], in1=ca[:, 0:2], op0=ALU.mult, op1=ALU.add),
                     reads=[a.b, cw.b, ca.b], writes=[ca.b])
                S.op("dve", lambda: nc.vector.scalar_tensor_tensor(out=ca[:, 0:2], in0=e4[:, 2:4], scalar=cw[:, cf, 2:3], in1=ca[:, 0:2], op0=ALU.mult, op1=ALU.add),
                     reads=[a.b, cw.b, ca.b], writes=[ca.b])
                S.op("act", lambda: nc.scalar.activation(out=g[:, :], in_=ca[:, :], func=AF.Gelu_apprx_tanh, bias=cb[:, cf:cf + 1]),
                     reads=[ca.b, cb.b], writes=[g.b])
                S.op("dve", lambda: nc.vector.tensor_tensor(out=gT[:, cf, :], in0=g[:, :], in1=pu[:, :], op=ALU.mult),
                     reads=[g.b, pu.b], writes=[gT.b])

            def c2b(c):
                for tp2 in range(2):
                    accs = [bank[0], bank[1], bank[2], bank[3]]
                    for cf in range(NCF):
                        wd_ = wdt[cf % 3]
                        S.load("sp", wd_[:, :], self.wds[cf], wd_.b)
                        for j in range(2):
                            ti = 2 * tp2 + j
                            for half in range(2):
                                pp = accs[2 * j + half]
                                S.op("pe", lambda: nc.tensor.matmul(pp[:, :], lhsT=gT[:, cf, ti * 128:(ti + 1) * 128], rhs=wd_[:, half * 512:(half + 1) * 512],
                                                                    start=(cf == 0), stop=(cf == NCF - 1)), reads=[gT.b, wd_.b], writes=[pp.b], accum=(cf > 0))
                    for j in range(2):
                        ti = 2 * tp2 + j
                        tok = (4 * c + ti) * 128
                        p_ = pst[j]
                        self.copy(p_[:, 0:512], accs[2 * j][:, :], [accs[2 * j].b], [p_.b], eng="act")
                        self.copy(p_[:, 512:D], accs[2 * j + 1][:, :], [accs[2 * j + 1].b], [p_.b], eng="dve")
                        S.store("pool", self.part[tok:tok + 128, :], p_[:, :], p_.b)
                        store_tags.append(("dma", p_.b.dsem, p_.b.dcnt))
                    if pend_b:
                        seg_b(*pend_b.pop(0))

            pend_b = []
            fifo = []
            for ti in range(4):
                seg_b(0, ti, seg_a(0, ti))
            for c in range(self.nch):
                for cf in range(NCF):
                    c2a_cf(c, cf)
                    if cf % 4 == 3:
                        k = cf // 4
                        if len(pend_b) >= 2:
                            seg_b(*pend_b.pop(0))
                        if c + 1 < self.nch:
                            pend_b.append((c + 1, k, seg_a(c + 1, k)))
                        if fifo and fifo[0][2] <= c:
                            tg, tt, _ = fifo.pop(0)
                            finish_tile(tg, tt)
                c2b(c)
                while pend_b:
                    seg_b(*pend_b.pop(0))
                if c % RNG == RNG - 1 or c == self.nch - 1:
                    c0 = (c // RNG) * RNG
                    r0, r1 = c0 * 512, (c + 1) * 512
                    for tg in store_tags:
                        S._wait("pool", tg)
                    store_tags = []
                    tag = self.collective("AllReduce", ALU.add, self.part[r0:r1, :].opt(), self.red[r0:r1, :].opt())
                    for tt in range(r0 // 128, r1 // 128):
                        fifo.append((tag, tt, c + 2))
            for tg, tt, _ in fifo:
                finish_tile(tg, tt)
            S.barrier()


_CACHE = {}


def kernel(**inputs):
    x = np.ascontiguousarray(inputs["x"], dtype=np.float32)
    params = [layout_params(inputs, r) for r in range(2)]
    consts = [make_consts(r) for r in range(2)]
    if "prog" not in _CACHE:
        _CACHE["prog"] = Prog()
    prog = _CACHE["prog"]
    in_maps = []
    for core in range(8):
        b, r = core // 2, core % 2
        m = {"x": x[b]}
        for n, _, _ in CONST_SPECS:
            m["c_" + n] = consts[r][n]
        for n, _ in PARAM_SPECS:
            m["p_" + n] = params[r][n]
        in_maps.append(m)
    res = run_bass_kernel_spmd(prog.nc, in_maps, core_ids=list(range(8)))
    out = np.stack([np.asarray(res.results[2 * b]["y"], dtype=np.float32) for b in range(4)], axis=0)
    return out
```

```python
import math
import numpy as np
import ml_dtypes
from contextlib import ExitStack
import concourse.bass as bass
import concourse.mybir as mybir
from concourse.bass_utils import run_bass_kernel_spmd

F32 = mybir.dt.float32
BF16 = mybir.dt.bfloat16
AF = mybir.ActivationFunctionType
ALU = mybir.AluOpType
AX = mybir.AxisListType
NPBF = ml_dtypes.bfloat16

D = 1024
S_LEN = 8192
NT = 64
NCH = 16
DEPTH = 2
DFF = 4096
NEG = -30000.0
EPS = 1e-6

OFF = dict(mq=0, mk=256, mv=512, nq=768, nkc=1024, nvc=1088, nks=1152, nvs=1216, nkw=1280, nvw=1344,
           ng=1408, dq=1420, dk=1676, dv=1932, sq=2188, sk=2444, sv=2572)
FRr = dict(mq=0, mk=128, nq=256, nkc=512, nvc=576, nks=640, nkw=704, dq=768, dk=896, sq=1024, sk=1152)
NF = 1280
VCr = dict(mv=0, nvs=128, nvw=192, dv=256, sv=384, ng=448)
NV = 460
NCF = 16


def role_heads(r, m=0):
    if m == 3:
        return [2 * r, 2 * r + 1], [2 * (1 - r), 2 * (1 - r) + 1]
    return [r, r + 2], [1 - r, 3 - r]


def far_skip(m, j, min_dist):
    sl = min(SLOPES[4 * role_heads(0, m)[0][j] + m], SLOPES[4 * role_heads(1, m)[0][j] + m])
    return min_dist > 0 and sl * min_dist >= 200.0


def role_cols(r):
    own0 = role_heads(r, 0)[0]
    own1, oth1 = role_heads(r, 1)
    own2 = role_heads(r, 2)[0]
    own3 = role_heads(r, 3)[0]

    def hc(base, heads, w=64):
        return np.concatenate([np.arange(base + w * h, base + w * (h + 1)) for h in heads])

    f = np.concatenate([hc(OFF["mq"], own0), hc(OFF["mk"], own0), hc(OFF["nq"], own1 + oth1),
                        np.arange(OFF["nkc"], OFF["nkc"] + 64), np.arange(OFF["nvc"], OFF["nvc"] + 64),
                        np.arange(OFF["nks"], OFF["nks"] + 64), np.arange(OFF["nkw"], OFF["nkw"] + 64),
                        hc(OFF["dq"], own2), hc(OFF["dk"], own2), hc(OFF["sq"], own3), hc(OFF["sk"], [r])])
    v = np.concatenate([hc(OFF["mv"], own0), np.arange(OFF["nvs"], OFF["nvs"] + 64), np.arange(OFF["nvw"], OFF["nvw"] + 64),
                        hc(OFF["dv"], own2), hc(OFF["sv"], [r]), hc(OFF["ng"], own1 + oth1, 3)])
    assert len(f) == 1216 and len(v) == NV
    return f, v


SLOPES = np.power(np.float32(2.0), np.arange(1, 17, dtype=np.float32) * np.float32(-0.5)).astype(np.float64)


class Buf:
    __slots__ = ("name", "w", "rs", "dsem", "dcnt")

    def __init__(self, name):
        self.name = name
        self.w = None
        self.rs = []
        self.dsem = None
        self.dcnt = 0


class Sched:
    def __init__(self, nc, stack):
        self.nc = nc
        self.stack = stack
        self.eng = {"pe": nc.tensor, "act": nc.scalar, "dve": nc.vector, "pool": nc.gpsimd, "sp": nc.sync}
        self.sem, self.cnt, self.seen = {}, {}, {}
        for k in self.eng:
            self.sem[k] = stack.enter_context(nc.semaphore("s_" + k))
            self.cnt[k] = 0
            self.seen[k] = {}
        self.dma_seen = {k: {} for k in self.eng}
        self.free_sems = []
        self.live = []
        self.nsem = len(self.eng)
        self.ninstr = 0

    def _wait(self, ek, dep):
        if dep is None:
            return
        e = self.eng[ek]
        if dep[0] == "dma":
            _, sem, val = dep
            d = self.dma_seen[ek]
            if d.get(id(sem), 0) >= val:
                return
            e.wait_ge(sem, val)
            d[id(sem)] = val
        else:
            pk, c = dep
            if self.seen[ek].get(pk, 0) >= c:
                return
            e.wait_ge(self.sem[pk], c)
            self.seen[ek][pk] = c
        self.ninstr += 1

    def _deps(self, ek, reads, writes, accum):
        for b in reads:
            self._wait(ek, b.w)
        for b in writes:
            if not (accum and b.w is not None and b.w[0] == ek):
                self._wait(ek, b.w)
            for r in b.rs:
                self._wait(ek, r)

    def op(self, ek, fn, reads=(), writes=(), accum=False):
        self._deps(ek, reads, writes, accum)
        ins = fn()
        self.cnt[ek] += 1
        ins.then_inc(self.sem[ek], 1)
        tag = (ek, self.cnt[ek])
        for b in reads:
            b.rs.append(tag)
        for b in writes:
            b.w = tag
            b.rs = []
        self.ninstr += 1
        return ins

    def _dsem(self, b):
        if b.dsem is None:
            if self.free_sems:
                b.dsem, b.dcnt = self.free_sems.pop()
            else:
                b.dsem = self.stack.enter_context(self.nc.semaphore("d%d" % self.nsem))
                b.dcnt = 0
                self.nsem += 1
            self.live.append(b)

    def load(self, qk, out_ap, in_ap, buf):
        self._deps(qk, (), (buf,), False)
        self._dsem(buf)
        ins = self.eng[qk].dma_start(out=out_ap, in_=in_ap)
        buf.dcnt += 16
        ins.then_inc(buf.dsem, 16)
        buf.w = ("dma", buf.dsem, buf.dcnt)
        buf.rs = []
        self.ninstr += 1

    def store(self, qk, out_ap, in_ap, buf):
        self._deps(qk, (buf,), (), False)
        self._dsem(buf)
        ins = self.eng[qk].dma_start(out=out_ap, in_=in_ap)
        buf.dcnt += 16
        ins.then_inc(buf.dsem, 16)
        buf.rs.append(("dma", buf.dsem, buf.dcnt))
        self.ninstr += 1

    def barrier(self):
        for ek in self.eng:
            for pk in self.eng:
                if pk != ek and self.cnt[pk] > 0:
                    self._wait(ek, (pk, self.cnt[pk]))
            for b in self.live:
                self._wait(ek, ("dma", b.dsem, b.dcnt))
        for b in self.live:
            self.free_sems.append((b.dsem, b.dcnt))
            b.dsem = None
        self.live = []


class T:
    def __init__(self, nc, stack, name, shape, dtype, psum=False):
        alloc = nc.psum_tensor if psum else nc.sbuf_tensor
        self.t = stack.enter_context(alloc(name, shape, dtype))
        self.b = Buf(name)

    def __getitem__(self, idx):
        return self.t[idx]


def _split3(v):
    v = np.asarray(v, np.float64)
    hi = v.astype(NPBF)
    r = v - hi.astype(np.float64)
    mid = r.astype(NPBF)
    r = r - mid.astype(np.float64)
    lo = r.astype(NPBF)
    return hi, mid, lo


def make_consts(role=0):
    locs = [sum(role_heads(role, m), []) for m in range(4)]
    c = {}
    c["ident"] = np.eye(128, dtype=np.float32).astype(NPBF)
    iq = np.arange(512, dtype=np.float64)
    p = np.arange(128, dtype=np.float64)
    aug = np.zeros((16, 3, 512), NPBF)
    for m in range(4):
        scale = 32 ** -0.5 if m == 2 else 64 ** -0.5
        for j in range(4):
            sl = SLOPES[4 * locs[m][j] + m]
            hi, mid, lo = _split3(-sl * iq / scale)
            aug[4 * m + j, 0], aug[4 * m + j, 1], aug[4 * m + j, 2] = hi, mid, lo
    c["aug"] = aug
    kb = np.zeros((128, 16, 64), np.float32)
    r = np.arange(-60, 4, dtype=np.float64)
    for m in range(4):
        for j in range(4):
            sl = SLOPES[4 * locs[m][j] + m]
            kb[:, 4 * m + j, :] = (sl * (p[:, None] + 128.0 * r[None, :])).astype(np.float32)
    c["kb"] = kb
    kbc = np.zeros((128, 4, 16, 4), np.float32)
    for j in range(4):
        sl = SLOPES[4 * locs[1][j] + 1]
        for cc in range(16):
            for kt in range(4):
                kbc[:, j, cc, kt] = (sl * (16.0 * (128 * kt + p) + 31.0 - 512.0 * cc)).astype(np.float32)
    c["kbc"] = kbc
    P = p[:, None, None]
    Q = iq[None, None, :]
    k4 = np.arange(4, dtype=np.float64)[None, :, None]
    c["caus"] = np.where(Q >= 128 * k4 + P, 0.0, NEG).astype(np.float32).astype(NPBF)
    r8 = (np.arange(8, dtype=np.float64) - 4)[None, :, None]
    dd = Q - 128 * r8 - P
    c["wmask"] = np.where((dd >= 0) & (dd < 512), 0.0, NEG).astype(np.float32).astype(NPBF)
    r5 = (np.arange(5, dtype=np.float64) - 1)[None, :, None]
    dd = Q - 128 * r5 - P
    c["swamask"] = np.where((dd >= 0) & (dd < 128), 0.0, NEG).astype(np.float32).astype(NPBF)
    d5 = (512.0 * np.arange(5, dtype=np.float64))[None, :, None]
    c["cmask"] = np.where(16 * P + 31 <= d5 + Q, 0.0, NEG).astype(np.float32).astype(NPBF)
    j = np.arange(S_LEN)
    c["em"] = (j[None, :] // 256 == np.arange(32)[:, None]).astype(np.float32).astype(NPBF)
    c["es"] = (j[None, :] // 64 == np.arange(128)[:, None]).astype(np.float32).astype(NPBF)
    ncmp, nsel = 511, 128
    cs = np.arange(512)[:, None] * 16
    bs = np.arange(nsel)[None, :] * 64
    ov = np.clip(np.minimum(cs + 32, bs + 64) - np.maximum(cs, bs), 0, None) / 32.0
    ov[ncmp:, :] = 0.0
    c["ov"] = ov.reshape(4, 128, 128).transpose(1, 0, 2).astype(np.float32).astype(NPBF)
    n32 = np.arange(32, dtype=np.float32)
    qi = np.arange(4)
    c["bi"] = np.broadcast_to((n32[None, None, :] - (qi // 2)[None, :, None].astype(np.float32)), (128, 4, 32)).astype(np.float32).copy()
    s128 = np.arange(128)[None, None, None, :]
    cc = np.arange(16)[:, None, None, None]
    pp = np.arange(128)[None, :, None, None]
    qq = np.arange(4)[None, None, :, None]
    qblk = 8 * cc + 2 * qq + (pp >= 64)
    forced = (s128 == 0) | (s128 == qblk) | (s128 == qblk - 1)
    valid = s128 <= qblk
    c["f1e4"] = np.where(forced, 1e4, 0.0).astype(np.float32)
    c["negv"] = np.where(valid, 0.0, -1e30).astype(np.float32)
    c["ones3"] = np.ones((3, 4 * S_LEN), np.float32).astype(NPBF)
    return c


CONST_SPECS = [("ident", [128, 128], BF16), ("aug", [16, 3, 512], BF16), ("kb", [128, 16, 64], F32),
               ("kbc", [128, 4, 16, 4], F32), ("caus", [128, 4, 512], BF16), ("wmask", [128, 8, 512], BF16),
               ("swamask", [128, 5, 512], BF16), ("cmask", [128, 5, 512], BF16), ("em", [32, S_LEN], BF16),
               ("es", [128, S_LEN], BF16), ("ov", [128, 4, 128], BF16), ("bi", [128, 4, 32], F32),
               ("f1e4", [16, 128, 4, 128], F32), ("negv", [16, 128, 4, 128], F32), ("ones3", [3, 4 * S_LEN], BF16)]

PARAM_SPECS = [("wf", [DEPTH, 128, 8, NF]), ("wv", [DEPTH, 128, 8, NV]), ("wo", [DEPTH, 128, 8, D]),
               ("wg", [DEPTH, NCF, 128, 8, 128]), ("wu", [DEPTH, NCF, 128, 8, 128]), ("wd", [DEPTH, NCF, 128, D]),
               ("g_apre", [DEPTH, D]), ("g_apost", [DEPTH, D]), ("g_fpre", [DEPTH, D]), ("g_fpost", [DEPTH, D]),
               ("cw", [DEPTH, 128, NCF, 3]), ("cb", [DEPTH, 128, NCF]),
               ("posk", [DEPTH, 64, 32]), ("w1k", [DEPTH, 64, 32, 128]), ("b1k", [DEPTH, 128, 1]), ("w2k", [DEPTH, 128, 64]),
               ("posv", [DEPTH, 64, 32]), ("w1v", [DEPTH, 64, 32, 128]), ("b1v", [DEPTH, 128, 1]), ("w2v", [DEPTH, 128, 64]),
               ("lq1", [DEPTH, 32]), ("lk1", [DEPTH, 32]), ("lq2", [DEPTH, 32]), ("lk2", [DEPTH, 32]),
               ("subln", [DEPTH, 64]), ("sinks", [DEPTH, 4])]


def layout_params(inp, role=0):
    o = {}
    w_in = inp["w_in"]
    fc, vc = role_cols(role)
    wf = np.zeros((DEPTH, D, NF), np.float32)
    wf[:, :, :len(fc)] = w_in[:, :, fc]
    o["wf"] = wf.reshape(DEPTH, 8, 128, NF).transpose(0, 2, 1, 3)
    o["wv"] = w_in[:, :, vc].reshape(DEPTH, 8, 128, NV).transpose(0, 2, 1, 3)
    rows = np.concatenate([np.arange(256 * m + 64 * h, 256 * m + 64 * (h + 1)) for rr in range(2) for m in range(4)
                           for h in role_heads(rr, m)[0]])
    o["wo"] = inp["w_out"][:, rows, :].reshape(DEPTH, 8, 128, D).transpose(0, 2, 1, 3)
    cfs = slice(NCF * role, NCF * (role + 1))
    o["wg"] = inp["ffn_w_gate"].reshape(DEPTH, 8, 128, 32, 128).transpose(0, 3, 2, 1, 4)[:, cfs]
    o["wu"] = inp["ffn_w_up"].reshape(DEPTH, 8, 128, 32, 128).transpose(0, 3, 2, 1, 4)[:, cfs]
    o["wd"] = inp["ffn_w_down"].reshape(DEPTH, 32, 128, D)[:, cfs]
    o["g_apre"], o["g_apost"] = inp["attn_pre_norm"], inp["attn_post_norm"]
    o["g_fpre"], o["g_fpost"] = inp["ffn_pre_norm"], inp["ffn_post_norm"]
    o["cw"] = inp["ffn_conv_w"].reshape(DEPTH, 3, 32, 128).transpose(0, 3, 2, 1)[:, :, cfs, :]
    o["cb"] = inp["ffn_conv_b"].reshape(DEPTH, 32, 128).transpose(0, 2, 1)[:, :, cfs]
    for sfx in "kv":
        o["pos" + sfx] = inp["nsa_cmp_pos_" + sfx].transpose(0, 2, 1)
        o["w1" + sfx] = inp["nsa_cmp_w1_" + sfx].transpose(0, 2, 1, 3)
        o["b1" + sfx] = inp["nsa_cmp_b1_" + sfx].reshape(DEPTH, 128, 1)
        o["w2" + sfx] = inp["nsa_cmp_w2_" + sfx]
    o["lq1"], o["lk1"] = inp["diff_lambda_q1"], inp["diff_lambda_k1"]
    o["lq2"], o["lk2"] = inp["diff_lambda_q2"], inp["diff_lambda_k2"]
    o["subln"] = inp["diff_subln"]
    o["sinks"] = inp["swa_sinks"][:, sum(role_heads(role, 3), [])]
    return {k: np.ascontiguousarray(v, dtype=np.float32) for k, v in o.items()}


class Prog:
    def __init__(self, phases=("A", "B", "C"), layers=(0, 1), mixers=(0, 1, 2, 3), debug=False, nchunks=NCH, ncores=8):
        self.phases, self.layers, self.mixers, self.debug, self.nch = phases, layers, mixers, debug, nchunks
        self.groups = [[2 * i, 2 * i + 1] for i in range(ncores // 2)]
        nc = bass.Bass("TRN2", target_bir_lowering=False)
        self.nc = nc
        self.x_in = nc.dram_tensor("x", [S_LEN, D], F32, kind="ExternalInput").ap()
        self.y = nc.dram_tensor("y", [S_LEN, D], F32, kind="ExternalOutput").ap()
        self.dc = {n: nc.dram_tensor("c_" + n, shp, dt, kind="ExternalInput").ap() for n, shp, dt in CONST_SPECS}
        self.dp = {n: nc.dram_tensor("p_" + n, shp, F32, kind="ExternalInput").ap() for n, shp in PARAM_SPECS}
        kind = "ExternalOutput" if debug else "Internal"
        self.featT = nc.dram_tensor("featT", [NF, S_LEN], BF16, kind=kind).ap()
        self.vtok = nc.dram_tensor("vtok", [S_LEN, NV], BF16, kind=kind).ap()
        self.mix_t = nc.dram_tensor("mix", [S_LEN, 512], BF16)
        self.mix = self.mix_t.ap()
        self.mixall_t = nc.dram_tensor("mixall", [2 * S_LEN, 512], BF16)
        self.mixall = self.mixall_t.ap()
        self.part = nc.dram_tensor("part", [S_LEN, D], F32).ap()
        self.red = nc.dram_tensor("red", [S_LEN, D], F32).ap()
        self.x1s = nc.dram_tensor("x1s", [S_LEN, D], F32).ap()
        self.xs = nc.dram_tensor("xs", [S_LEN, D], F32).ap()
        self.wgs = nc.dram_tensor("wgs", [NCF, 128, 8, 128], BF16).ap()
        self.wus = nc.dram_tensor("wus", [NCF, 128, 8, 128], BF16).ap()
        self.wds = nc.dram_tensor("wds", [NCF, 128, D], BF16).ap()
        self.cp_i = 0
        with ExitStack() as st:
            self.st = st
            self.S = Sched(nc, st)
            self.cc_sem = st.enter_context(nc.semaphore("cc_sem"))
            self.cc_cnt = 0
            self.build()
            print("built: instr", self.S.ninstr, "sems", self.S.nsem, flush=True)

    def collective(self, kind, op, in_ap, out_ap):
        ins = self.nc.gpsimd.collective_compute(kind, op, replica_groups=self.groups, ins=[in_ap], outs=[out_ap])
        self.cc_cnt += 1
        ins.then_inc(self.cc_sem, 1)
        self.S.ninstr += 1
        return ("dma", self.cc_sem, self.cc_cnt)

    def tile(self, stack, name, shape, dtype, psum=False):
        self.tile_i = getattr(self, "tile_i", 0) + 1
        return T(self.nc, stack, "%s_%d" % (name, self.tile_i), shape, dtype, psum)

    def copy(self, out_ap, in_ap, reads, writes, eng=None):
        nc = self.nc
        if eng is None:
            eng = "act" if (self.cp_i % 2 == 0) else "dve"
            self.cp_i += 1
        if eng == "act":
            self.S.op("act", lambda: nc.scalar.activation(out=out_ap, in_=in_ap, func=AF.Copy), reads=reads, writes=writes)
        elif eng == "dve":
            self.S.op("dve", lambda: nc.vector.tensor_copy(out=out_ap, in_=in_ap), reads=reads, writes=writes)
        else:
            self.S.op("pool", lambda: nc.gpsimd.tensor_copy(out=out_ap, in_=in_ap), reads=reads, writes=writes)

    def load_cast(self, stack, dst, dst_ap_fn, src_ap_fn, n, ncols):
        stg = [self.tile(stack, "stg%d_%d" % (i, self.S.ninstr), [128, ncols], F32) for i in range(2)]
        for i in range(n):
            s = stg[i % 2]
            self.S.load("sp", s[:, :], src_ap_fn(i), s.b)
            self.copy(dst_ap_fn(i), s[:, :], [s.b], [dst.b])

    def build(self):
        nc, S, st = self.nc, self.S, self.st
        self.ident = self.tile(st, "ident", [128, 128], BF16)
        self.caus = self.tile(st, "caus", [128, 4, 512], BF16)
        self.kb = self.tile(st, "kb", [128, 16, 64], F32)
        self.epsT = self.tile(st, "epsT", [128, 1], F32)
        S.load("sp", self.ident[:, :], self.dc["ident"], self.ident.b)
        S.load("sp", self.caus[:, :, :], self.dc["caus"], self.caus.b)
        S.load("sp", self.kb[:, :, :], self.dc["kb"], self.kb.b)
        S.op("dve", lambda: nc.vector.memset(self.epsT[:, :], EPS), writes=[self.epsT.b])
        for l in self.layers:
            x_src = self.x_in if l == 0 else self.xs
            x_dst = self.y if l == self.layers[-1] else self.xs
            if "A" in self.phases:
                self.phase_A(l, x_src)
                S.barrier()
            if "B" in self.phases:
                for m in self.mixers:
                    [self.moba, self.nsa, self.diff, self.swa][m](l)
                    S.barrier()
            if "C" in self.phases:
                tags = []
                for k in range((self.nch * 512 + 2047) // 2048):
                    tags.append(self.collective("AllGather", ALU.bypass, self.mix[k * 2048:(k + 1) * 2048, :].opt(),
                                                self.mixall[k * 4096:(k + 1) * 4096, :].opt()))
                self.prep_ffn(l)
                for ek in S.eng:
                    S._wait(ek, tags[-1])
                S.barrier()
                self.phase_C(l, x_src, x_dst)
                S.barrier()
        S.barrier()
        if self.debug:
            nc = self.nc
            dbg = {}
            for nm, src, rows, cols, dt in (("d_mixall", self.mixall, 4096, 512, BF16), ("d_part", self.part, 2048, D, F32),
                                            ("d_red", self.red, 2048, D, F32), ("d_x1s", self.x1s, 2048, D, F32), ("d_mix", self.mix, 2048, 512, BF16)):
                dst = nc.dram_tensor(nm, [rows, cols], dt, kind="ExternalOutput").ap()
                b = Buf(nm)
                S._dsem(b)
                ins = nc.sync.dma_start(out=dst[:, :], in_=src[0:rows, :])
                b.dcnt += 16
                ins.then_inc(b.dsem, 16)
                S._wait("sp", ("dma", b.dsem, b.dcnt))

    def rstd_of(self, x_ap, xbuf, junk, ss, sd, rstd, n=D):
        nc, S = self.nc, self.S
        S.op("dve", lambda: nc.vector.scalar_tensor_tensor(out=junk[:, 0:n], in0=x_ap, scalar=1.0, in1=x_ap,
                                                           op0=ALU.mult, op1=ALU.mult, accum_out=ss[:, 0:1]),
             reads=[xbuf], writes=[junk.b, ss.b])
        S.op("act", lambda: nc.scalar.activation(out=sd[:, 0:1], in_=ss[:, 0:1], func=AF.Sqrt,
                                                 bias=self.epsT[:, 0:1], scale=1.0 / n),
             reads=[ss.b, self.epsT.b], writes=[sd.b])
        S.op("dve", lambda: nc.vector.reciprocal(out=rstd[:, 0:1], in_=sd[:, 0:1]), reads=[sd.b], writes=[rstd.b])

    def phase_A(self, l, x_src):
        nc, S = self.nc, self.S
        with ExitStack() as ph:
            WF = self.tile(ph, "WF", [128, 8, NF], BF16)
            WV = self.tile(ph, "WV", [128, 8, NV], BF16)
            gain = self.tile(ph, "gainA", [128, D], F32)
            with ExitStack() as tmp:
                self.load_cast(tmp, WF, lambda i: WF[:, i, :], lambda i: self.dp["wf"][l, :, i, :], 8, NF)
                self.load_cast(tmp, WV, lambda i: WV[:, i, :], lambda i: self.dp["wv"][l, :, i, :], 8, NV)
                S.barrier()
            S.load("sp", gain[:, :], self.dp["g_apre"][l].partition_broadcast(128), gain.b)
            xts = [self.tile(ph, "xtA%d" % i, [128, D], F32) for i in range(3)]
            junk = self.tile(ph, "junkA", [128, D], F32)
            ss = self.tile(ph, "ssA", [128, 1], F32)
            sd = self.tile(ph, "sdA", [128, 1], F32)
            rstd = self.tile(ph, "rstdA", [128, 1], F32)
            hb = [self.tile(ph, "hbA%d" % i, [128, D], BF16) for i in range(2)]
            hT = [self.tile(ph, "hTA%d" % i, [128, 8, 512], BF16) for i in range(2)]
            tp = [self.tile(ph, "tpA%d" % i, [128, D], BF16, psum=True) for i in range(2)]
            psF = [self.tile(ph, "psF%d" % i, [128, 512], F32, psum=True) for i in range(2)]
            psV0 = self.tile(ph, "psV0", [128, 512], F32, psum=True)
            fst = [self.tile(ph, "fst%d" % i, [128, 512], BF16) for i in range(3)]
            vst = [self.tile(ph, "vst%d" % i, [128, NV], BF16) for i in range(2)]
            it = 0
            for c in range(self.nch):
                hTc = hT[c % 2]
                for ti in range(4):
                    tok = (4 * c + ti) * 128
                    xt = xts[it % 3]
                    h = hb[it % 2]
                    tpp = tp[it % 2]
                    it += 1
                    S.load("sp", xt[:, :], x_src[tok:tok + 128, :], xt.b)
                    self.rstd_of(xt[:, :], xt.b, junk, ss, sd, rstd)
                    S.op("dve", lambda: nc.vector.scalar_tensor_tensor(out=h[:, :], in0=xt[:, :], scalar=rstd[:, 0:1],
                                                                       in1=gain[:, :], op0=ALU.mult, op1=ALU.mult),
                         reads=[xt.b, rstd.b, gain.b], writes=[h.b])
                    for kc in range(8):
                        S.op("pe", lambda: nc.tensor.transpose(out=tpp[:, kc * 128:(kc + 1) * 128],
                                                               in_=h[:, kc * 128:(kc + 1) * 128], identity=self.ident[:, :]),
                             reads=[h.b, self.ident.b], writes=[tpp.b], accum=(kc > 0))
                    self.copy(hTc[:, :, ti * 128:(ti + 1) * 128], tpp[:, :].rearrange("p (k t) -> p k t", k=8),
                              [tpp.b], [hTc.b])
                for g in range(NF // 128):
                    ps = psF[g % 2]
                    for kc in range(8):
                        S.op("pe", lambda: nc.tensor.matmul(ps[:, :], lhsT=WF[:, kc, g * 128:(g + 1) * 128], rhs=hTc[:, kc, :],
                                                            start=(kc == 0), stop=(kc == 7)),
                             reads=[WF.b, hTc.b], writes=[ps.b], accum=(kc > 0))
                    f = fst[g % 3]
                    self.copy(f[:, :], ps[:, :], [ps.b], [f.b])
                    S.store("pool", self.featT[g * 128:(g + 1) * 128, c * 512:(c + 1) * 512], f[:, :], f.b)
                for ti in range(4):
                    tok = (4 * c + ti) * 128
                    for kc in range(8):
                        S.op("pe", lambda: nc.tensor.matmul(psV0[:, 0:NV], lhsT=hTc[:, kc, ti * 128:(ti + 1) * 128], rhs=WV[:, kc, 0:NV],
                                                            start=(kc == 0), stop=(kc == 7)),
                             reads=[WV.b, hTc.b], writes=[psV0.b], accum=(kc > 0))
                    v = vst[ti % 2]
                    self.copy(v[:, 0:NV], psV0[:, 0:NV], [psV0.b], [v.b])
                    S.store("pool", self.vtok[tok:tok + 128, :], v[:, :], v.b)
            S.barrier()

    def attn_res(self, ph, nps=3, npt=3, no=3):
        R = type("R", (), {})()
        R.ps = [self.tile(ph, "ps_s%d" % i, [128, 512], F32, psum=True) for i in range(nps)]
        R.pt = [self.tile(ph, "pT%d" % i, [128, 512], BF16) for i in range(npt)]
        R.O = [self.tile(ph, "O%d" % i, [128, 512], F32, psum=True) for i in range(no)]
        R.ips = R.ipt = R.io = 0
        return R

    def attend(self, R, q_ap, qbufs, tiles, O, scale):
        nc, S = self.nc, self.S
        Ov = O[:, 0:260].rearrange("p (a b) -> p a b", a=4)
        n = len(tiles)

        def pv(pt, tl, first, last):
            q0 = tl.get("q0", 0)
            assert not (first and q0)
            for qi in range(q0 // 128, 4):
                S.op("pe", lambda: nc.tensor.matmul(Ov[:, qi, :], lhsT=pt[:, qi * 128:(qi + 1) * 128], rhs=tl["v"],
                                                    start=(first and qi == 0), stop=last),
                     reads=[pt.b] + tl["kbufs"], writes=[O.b], accum=not (first and qi == 0))
            if tl.get("post") is not None:
                tl["post"](pt, first, last)

        prev = None
        for i, tl in enumerate(tiles):
            ps = R.ps[R.ips % len(R.ps)]
            R.ips += 1
            q0 = tl.get("q0", 0)
            ex = tl.get("extra", [])
            S.op("pe", lambda: nc.tensor.matmul(ps[:, q0:512], lhsT=tl["kT"], rhs=q_ap[:, q0:512], start=True, stop=(len(ex) == 0)),
                 reads=qbufs + tl["kbufs"], writes=[ps.b])
            for j, (lh, rh, bufs) in enumerate(ex):
                S.op("pe", lambda: nc.tensor.matmul(ps[:, q0:512], lhsT=lh, rhs=rh[:, q0:512], start=False, stop=(j == len(ex) - 1)),
                     reads=bufs, writes=[ps.b], accum=True)
            pt = R.pt[R.ipt % len(R.pt)]
            R.ipt += 1
            S.op("act", lambda: nc.scalar.activation(out=pt[:, q0:512], in_=ps[:, q0:512], func=AF.Exp, bias=tl["bias"], scale=scale),
                 reads=[ps.b] + tl["bbufs"], writes=[pt.b])
            if prev is not None:
                pv(prev[0], prev[1], prev[2] == 0, False)
            prev = (pt, tl, i)
        pv(prev[0], prev[1], prev[2] == 0, True)
        return Ov

    def load_kv(self, KT, V, krows, nh_k, k_row0, v_col0, nh_v, kd=64):
        nc, S = self.nc, self.S
        S.op("pool", lambda: nc.gpsimd.memset(V[:, :, :, 64:65], 1.0), writes=[V.b])
        for h in range(nh_k):
            S.load("sp", KT[0:kd, h, :], self.featT[k_row0 + h * kd:k_row0 + (h + 1) * kd, :], KT.b)
        S.load("sp", KT[kd:kd + 3, :, :], self.dc["ones3"][:, 0:nh_k * S_LEN].rearrange("r (h s) -> r h s", h=nh_k), KT.b)
        for h in range(nh_v):
            S.load("sp", V[:, h, :, 0:64],
                   self.vtok[:, v_col0 + h * 64:v_col0 + (h + 1) * 64].rearrange("(kt p) d -> p kt d", p=128), V.b)

    def recip_den(self, Ov, O, den, add_ap=None, add_bufs=()):
        nc, S = self.nc, self.S
        if add_ap is None:
            S.op("dve", lambda: nc.vector.tensor_scalar(out=den[:, 0:4], in0=Ov[:, :, 64], scalar1=1e-30, scalar2=None,
                                                        op0=ALU.max), reads=[O.b], writes=[den.b])
        else:
            S.op("dve", lambda: nc.vector.tensor_scalar(out=den[:, 0:4], in0=Ov[:, :, 64], scalar1=add_ap, scalar2=1e-30,
                                                        op0=ALU.add, op1=ALU.max), reads=[O.b] + list(add_bufs), writes=[den.b])
        S.op("dve", lambda: nc.vector.reciprocal(out=den[:, 0:4], in_=den[:, 0:4]), reads=[den.b], writes=[den.b])

    def moba(self, l):
        nc, S = self.nc, self.S
        with ExitStack() as ph:
            KT = self.tile(ph, "KTm", [128, 2, S_LEN], BF16)
            V = self.tile(ph, "Vm", [128, 2, NT, 65], BF16)
            BI = self.tile(ph, "BIm", [128, 4, 32], F32)
            S.load("sp", BI[:, :, :], self.dc["bi"], BI.b)
            S.op("pool", lambda: nc.gpsimd.memset(V[:, :, :, 64:65], 1.0), writes=[V.b])
            S.op("pool", lambda: nc.gpsimd.memset(KT[64:128, :, :], 0.0), writes=[KT.b])
            for h in range(2):
                S.load("sp", KT[96:128, h, :], self.dc["em"], KT.b)
                S.load("sp", KT[0:64, h, :], self.featT[FRr["mk"] + 64 * h:FRr["mk"] + 64 * (h + 1), :], KT.b)
                S.load("sp", V[:, h, :, 0:64],
                       self.vtok[:, VCr["mv"] + h * 64:VCr["mv"] + (h + 1) * 64].rearrange("(kt p) d -> p kt d", p=128), V.b)
            S.load("sp", KT[64:67, :, :], self.dc["ones3"][:, 0:2 * S_LEN].rearrange("r (h s) -> r h s", h=2), KT.b)
            R = self.attn_res(ph)
            QT = [[self.tile(ph, "QTm%d_%d" % (i, h), [128, 512], BF16) for h in range(2)] for i in range(2)]
            for qs in QT:
                for h in range(2):
                    S.op("pool", lambda: nc.gpsimd.memset(qs[h][64:128, :], 0.0), writes=[qs[h].b])
                    S.load("sp", qs[h][64:67, :], self.dc["aug"][h], qs[h].b)
            kms = self.tile(ph, "kms", [128, 2, 32], F32)
            kmh = self.tile(ph, "kmh", [128, 2, 32], BF16)
            kml = self.tile(ph, "kml", [128, 2, 32], BF16)
            kmr = self.tile(ph, "kmr", [128, 2, 32], F32)
            for h in range(2):
                S.op("dve", lambda: nc.vector.tensor_reduce(out=kms[0:64, h, :], in_=KT[0:64, h, :].rearrange("p (n k) -> p n k", k=256),
                                                            axis=AX.X, op=ALU.add), reads=[KT.b], writes=[kms.b])
            S.op("dve", lambda: nc.vector.tensor_scalar(out=kms[0:64, :, :], in0=kms[0:64, :, :], scalar1=1.0 / 256, scalar2=None, op0=ALU.mult),
                 reads=[kms.b], writes=[kms.b])
            S.op("dve", lambda: nc.vector.tensor_copy(out=kmh[0:64, :, :], in_=kms[0:64, :, :]), reads=[kms.b], writes=[kmh.b])
            S.op("dve", lambda: nc.vector.tensor_tensor(out=kmr[0:64, :, :], in0=kms[0:64, :, :], in1=kmh[0:64, :, :], op=ALU.subtract),
                 reads=[kms.b, kmh.b], writes=[kmr.b])
            S.op("dve", lambda: nc.vector.tensor_copy(out=kml[0:64, :, :], in_=kmr[0:64, :, :]), reads=[kmr.b], writes=[kml.b])
            gps = self.tile(ph, "gps", [128, 512], F32, psum=True)
            tps = self.tile(ph, "tpsm", [128, 1024], BF16, psum=True)
            past = self.tile(ph, "past", [128, 4, 32], F32)
            own = self.tile(ph, "own", [128, 4, 32], F32)
            negp = self.tile(ph, "negp", [128, 4, 32], F32)
            gm = self.tile(ph, "gm", [128, 4, 32], F32)
            m8 = self.tile(ph, "m8", [128, 4, 8], F32)
            sel = self.tile(ph, "sel", [128, 4, 32], F32)
            nsel = self.tile(ph, "nsel", [128, 4, 32], BF16)
            den = self.tile(ph, "denm", [128, 4], F32)
            mst = [self.tile(ph, "mstm%d" % i, [128, 4, 128], BF16) for i in range(2)]
            for c in range(self.nch):
                qs = QT[c % 2]
                for h in range(2):
                    S.load("sp", qs[h][0:64, :], self.featT[FRr["mq"] + 64 * h:FRr["mq"] + 64 * (h + 1), c * 512:(c + 1) * 512], qs[h].b)
                S.op("dve", lambda: nc.vector.tensor_scalar(out=past[:, :, :], in0=BI[:, :, :], scalar1=float(2 * c), scalar2=None, op0=ALU.is_lt),
                     reads=[BI.b], writes=[past.b])
                S.op("dve", lambda: nc.vector.tensor_scalar(out=own[:, :, :], in0=BI[:, :, :], scalar1=float(2 * c), scalar2=None, op0=ALU.is_equal),
                     reads=[BI.b], writes=[own.b])
                S.op("dve", lambda: nc.vector.tensor_scalar(out=negp[:, :, :], in0=past[:, :, :], scalar1=-1.0, scalar2=1e30, op0=ALU.add, op1=ALU.mult),
                     reads=[past.b], writes=[negp.b])
                ms = mst[c % 2]
                for h in range(2):
                    q = qs[h]
                    gv = gps[:, 0:128].rearrange("p (a b) -> p a b", a=4)
                    for qi in range(4):
                        S.op("pe", lambda: nc.tensor.matmul(gv[:, qi, :], lhsT=q[0:64, qi * 128:(qi + 1) * 128], rhs=kmh[0:64, h, :], start=True, stop=False),
                             reads=[q.b, kmh.b], writes=[gps.b], accum=(qi > 0))
                        S.op("pe", lambda: nc.tensor.matmul(gv[:, qi, :], lhsT=q[0:64, qi * 128:(qi + 1) * 128], rhs=kml[0:64, h, :], start=False, stop=True),
                             reads=[q.b, kml.b], writes=[gps.b], accum=True)
                    S.op("dve", lambda: nc.vector.tensor_tensor(out=gm[:, :, :], in0=gv, in1=negp[:, :, :], op=ALU.add),
                         reads=[gps.b, negp.b], writes=[gm.b])
                    for qi in range(4):
                        S.op("dve", lambda: nc.vector.max(out=m8[:, qi, :], in_=gm[:, qi, :]), reads=[gm.b], writes=[m8.b])
                    for qi in range(4):
                        S.op("dve", lambda: nc.vector.tensor_scalar(out=sel[:, qi, :], in0=gm[:, qi, :], scalar1=m8[:, qi, 2:3], scalar2=None, op0=ALU.is_ge),
                             reads=[gm.b, m8.b], writes=[sel.b])
                    S.op("dve", lambda: nc.vector.tensor_tensor(out=sel[:, :, :], in0=sel[:, :, :], in1=past[:, :, :], op=ALU.mult),
                         reads=[sel.b, past.b], writes=[sel.b])
                    S.op("dve", lambda: nc.vector.tensor_tensor(out=sel[:, :, :], in0=sel[:, :, :], in1=own[:, :, :], op=ALU.add),
                         reads=[sel.b, own.b], writes=[sel.b])
                    S.op("dve", lambda: nc.vector.tensor_scalar(out=nsel[:, :, :], in0=sel[:, :, :], scalar1=-1.0, scalar2=-NEG, op0=ALU.add, op1=ALU.mult),
                         reads=[sel.b], writes=[nsel.b])
                    for qi in range(4):
                        S.op("pe", lambda: nc.tensor.transpose(out=tps[0:32, qi * 128:(qi + 1) * 128], in_=nsel[:, qi, :], identity=self.ident[:, :]),
                             reads=[nsel.b, self.ident.b], writes=[tps.b], accum=(qi > 0))
                    self.copy(q[96:128, :], tps[0:32, 0:512], [tps.b], [q.b], eng="dve")
                    tiles = []
                    for kt in range(4 * c + 4):
                        if far_skip(0, h, 512 * c - (128 * kt + 127)):
                            continue
                        r = kt - 4 * c
                        ex = []
                        if r >= 0:
                            ex.append((self.ident[:, :], self.caus[:, r, :], [self.ident.b, self.caus.b]))
                        tiles.append(dict(kT=KT[0:128, h, kt * 128:(kt + 1) * 128], v=V[:, h, kt, :], kbufs=[KT.b, V.b],
                                          bias=self.kb[:, 0 + h, r + 60:r + 61], bbufs=[self.kb.b], extra=ex, q0=128 * max(r, 0)))
                    O = R.O[R.io % len(R.O)]
                    R.io += 1
                    Ov = self.attend(R, q[0:128, :], [q.b], tiles, O, 0.125)
                    self.recip_den(Ov, O, den)
                    for qi in range(4):
                        S.op("dve", lambda: nc.vector.tensor_scalar(out=ms[:, qi, h * 64:(h + 1) * 64], in0=Ov[:, qi, 0:64], scalar1=den[:, qi:qi + 1],
                                                                    scalar2=None, op0=ALU.mult), reads=[O.b, den.b], writes=[ms.b])
                S.store("pool", self.mix[c * 512:(c + 1) * 512, 0:128].rearrange("(a p) d -> p a d", p=128), ms[:, :, :], ms.b)
            S.barrier()

    def swa(self, l):
        nc, S = self.nc, self.S
        with ExitStack() as ph:
            KT = self.tile(ph, "KTs", [128, 1, S_LEN], BF16)
            V = self.tile(ph, "Vs", [128, 1, NT, 65], BF16)
            SM = self.tile(ph, "SMs", [128, 5, 512], BF16)
            S.load("sp", SM[:, :, :], self.dc["swamask"], SM.b)
            self.load_kv(KT, V, 67, 1, FRr["sk"], VCr["sv"], 1)
            sk = self.tile(ph, "sinks", [128, 4], F32)
            esk = self.tile(ph, "esinks", [128, 4], F32)
            S.load("sp", sk[:, :], self.dp["sinks"][l].partition_broadcast(128), sk.b)
            S.op("act", lambda: nc.scalar.activation(out=esk[:, :], in_=sk[:, :], func=AF.Exp), reads=[sk.b], writes=[esk.b])
            R = self.attn_res(ph)
            QT = [self.tile(ph, "QTs%d" % i, [128, 2, 512], BF16) for i in range(2)]
            for q in QT:
                S.load("sp", q[64:67, :, :], self.dc["aug"][12:14].rearrange("h r q -> r h q"), q.b)
            den = self.tile(ph, "dens", [128, 4], F32)
            mst = [self.tile(ph, "msts%d" % i, [128, 4, 128], BF16) for i in range(2)]
            for c in range(self.nch):
                q = QT[c % 2]
                for h in range(2):
                    S.load("sp", q[0:64, h, :], self.featT[FRr["sq"] + 64 * h:FRr["sq"] + 64 * (h + 1), c * 512:(c + 1) * 512], q.b)
                ms = mst[c % 2]
                for h in range(2):
                    g = 0
                    tiles = []
                    for r in range(-1, 4):
                        kt = 4 * c + r
                        if kt < 0:
                            continue
                        tiles.append(dict(kT=KT[0:67, g, kt * 128:(kt + 1) * 128], v=V[:, g, kt, :], kbufs=[KT.b, V.b],
                                          bias=self.kb[:, 12 + h, r + 60:r + 61], bbufs=[self.kb.b],
                                          extra=[(self.ident[:, :], SM[:, r + 1, :], [self.ident.b, SM.b])]))
                    O = R.O[R.io % len(R.O)]
                    R.io += 1
                    Ov = self.attend(R, q[0:67, h, :], [q.b], tiles, O, 0.125)
                    self.recip_den(Ov, O, den, add_ap=esk[:, h:h + 1], add_bufs=[esk.b])
                    for qi in range(4):
                        S.op("dve", lambda: nc.vector.tensor_scalar(out=ms[:, qi, h * 64:(h + 1) * 64], in0=Ov[:, qi, 0:64], scalar1=den[:, qi:qi + 1],
                                                                    scalar2=None, op0=ALU.mult), reads=[O.b, den.b], writes=[ms.b])
                S.store("pool", self.mix[c * 512:(c + 1) * 512, 384:512].rearrange("(a p) d -> p a d", p=128), ms[:, :, :], ms.b)
            S.barrier()

    def diff(self, l):
        nc, S = self.nc, self.S
        lambda_init = 0.8 - 0.6 * math.exp(-0.3 * l)
        sc = 32 ** -0.5
        with ExitStack() as ph:
            KT = self.tile(ph, "KTd", [128, 2, S_LEN], BF16)
            V = self.tile(ph, "Vd", [128, 2, NT, 65], BF16)
            S.op("pool", lambda: nc.gpsimd.memset(V[:, :, :, 64:65], 1.0), writes=[V.b])
            for h in range(2):
                r0 = FRr["dk"] + 64 * h
                S.load("sp", KT[0:32, h, :], self.featT[r0:r0 + 32, :], KT.b)
                S.load("sp", KT[64:96, h, :], self.featT[r0 + 32:r0 + 64, :], KT.b)
                S.load("sp", V[:, h, :, 0:64], self.vtok[:, VCr["dv"] + h * 64:VCr["dv"] + (h + 1) * 64].rearrange("(kt p) d -> p kt d", p=128), V.b)
            ones = self.dc["ones3"][:, 0:2 * S_LEN].rearrange("r (h s) -> r h s", h=2)
            S.load("sp", KT[32:35, :, :], ones, KT.b)
            S.load("sp", KT[96:99, :, :], ones, KT.b)
            lam = self.tile(ph, "lam", [128, 4], F32)
            lt = self.tile(ph, "lamt", [128, 4, 32], F32)
            lj = self.tile(ph, "lamj", [128, 32], F32)
            for i, nme in enumerate(["lq1", "lk1", "lq2", "lk2"]):
                S.load("sp", lt[:, i, :], self.dp[nme][l].partition_broadcast(128), lt.b)
            for i in range(2):
                S.op("dve", lambda: nc.vector.scalar_tensor_tensor(out=lj[:, :], in0=lt[:, 2 * i, :], scalar=1.0, in1=lt[:, 2 * i + 1, :],
                                                                   op0=ALU.mult, op1=ALU.mult, accum_out=lam[:, i:i + 1]),
                     reads=[lt.b], writes=[lj.b, lam.b])
            S.op("act", lambda: nc.scalar.activation(out=lam[:, 0:2], in_=lam[:, 0:2], func=AF.Exp), reads=[lam.b], writes=[lam.b])
            S.op("dve", lambda: nc.vector.tensor_tensor(out=lam[:, 2:3], in0=lam[:, 1:2], in1=lam[:, 0:1], op=ALU.subtract),
                 reads=[lam.b], writes=[lam.b])
            S.op("dve", lambda: nc.vector.tensor_scalar(out=lam[:, 2:3], in0=lam[:, 2:3], scalar1=-lambda_init, scalar2=None, op0=ALU.add),
                 reads=[lam.b], writes=[lam.b])
            gs = self.tile(ph, "gsub", [128, 64], F32)
            S.load("sp", gs[:, :], self.dp["subln"][l].partition_broadcast(128), gs.b)
            S.op("dve", lambda: nc.vector.tensor_scalar(out=gs[:, :], in0=gs[:, :], scalar1=1.0 - lambda_init, scalar2=None, op0=ALU.mult),
                 reads=[gs.b], writes=[gs.b])
            R = self.attn_res(ph, no=4)
            QT = [self.tile(ph, "QTd%d" % i, [128, 2, 512], BF16) for i in range(2)]
            for q in QT:
                a = self.dc["aug"][8:10].rearrange("h r q -> r h q")
                S.load("sp", q[32:35, :, :], a, q.b)
                S.load("sp", q[96:99, :, :], a, q.b)
            den1 = self.tile(ph, "den1", [128, 4], F32)
            den2 = self.tile(ph, "den2", [128, 4], F32)
            o1 = self.tile(ph, "o1d", [128, 4, 64], F32)
            od = self.tile(ph, "od", [128, 4, 64], F32)
            jk = self.tile(ph, "jkd", [128, 64], F32)
            ssd = self.tile(ph, "ssd", [128, 4], F32)
            mst = [self.tile(ph, "mstd%d" % i, [128, 4, 128], BF16) for i in range(2)]
            for c in range(self.nch):
                q = QT[c % 2]
                for h in range(2):
                    r0 = FRr["dq"] + 64 * h
                    S.load("sp", q[0:32, h, :], self.featT[r0:r0 + 32, c * 512:(c + 1) * 512], q.b)
                    S.load("sp", q[64:96, h, :], self.featT[r0 + 32:r0 + 64, c * 512:(c + 1) * 512], q.b)
                ms = mst[c % 2]
                for h in range(2):
                    Os = []
                    for j in range(2):
                        b0 = 64 * j
                        tiles = []
                        for kt in range(4 * c + 4):
                            if far_skip(2, h, 512 * c - (128 * kt + 127)):
                                continue
                            r = kt - 4 * c
                            ex = []
                            if r >= 0:
                                ex.append((self.ident[:, :], self.caus[:, r, :], [self.ident.b, self.caus.b]))
                            tiles.append(dict(kT=KT[b0:b0 + 35, h, kt * 128:(kt + 1) * 128], v=V[:, h, kt, :], kbufs=[KT.b, V.b],
                                              bias=self.kb[:, 8 + h, r + 60:r + 61], bbufs=[self.kb.b], extra=ex, q0=128 * max(r, 0)))
                        O = R.O[R.io % len(R.O)]
                        R.io += 1
                        Ov = self.attend(R, q[b0:b0 + 35, h, :], [q.b], tiles, O, sc)
                        Os.append((O, Ov))
                    (O1, Ov1), (O2, Ov2) = Os
                    self.recip_den(Ov1, O1, den1)
                    self.recip_den(Ov2, O2, den2)
                    S.op("dve", lambda: nc.vector.tensor_scalar(out=den2[:, :], in0=den2[:, :], scalar1=lam[:, 2:3], scalar2=None, op0=ALU.mult),
                         reads=[den2.b, lam.b], writes=[den2.b])
                    for qi in range(4):
                        S.op("dve", lambda: nc.vector.tensor_scalar(out=o1[:, qi, :], in0=Ov1[:, qi, 0:64], scalar1=den1[:, qi:qi + 1], scalar2=None, op0=ALU.mult),
                             reads=[O1.b, den1.b], writes=[o1.b])
                        S.op("dve", lambda: nc.vector.scalar_tensor_tensor(out=od[:, qi, :], in0=Ov2[:, qi, 0:64], scalar=den2[:, qi:qi + 1], in1=o1[:, qi, :],
                                                                           op0=ALU.mult, op1=ALU.add), reads=[O2.b, den2.b, o1.b], writes=[od.b])
                        S.op("dve", lambda: nc.vector.scalar_tensor_tensor(out=jk[:, :], in0=od[:, qi, :], scalar=1.0, in1=od[:, qi, :],
                                                                           op0=ALU.mult, op1=ALU.mult, accum_out=ssd[:, qi:qi + 1]),
                             reads=[od.b], writes=[jk.b, ssd.b])
                    S.op("act", lambda: nc.scalar.activation(out=ssd[:, :], in_=ssd[:, :], func=AF.Ln, bias=self.epsT[:, 0:1], scale=1.0 / 64),
                         reads=[ssd.b, self.epsT.b], writes=[ssd.b])
                    S.op("act", lambda: nc.scalar.activation(out=ssd[:, :], in_=ssd[:, :], func=AF.Exp, scale=-0.5), reads=[ssd.b], writes=[ssd.b])
                    for qi in range(4):
                        S.op("dve", lambda: nc.vector.scalar_tensor_tensor(out=ms[:, qi, h * 64:(h + 1) * 64], in0=od[:, qi, :], scalar=ssd[:, qi:qi + 1],
                                                                           in1=gs[:, :], op0=ALU.mult, op1=ALU.mult),
                             reads=[od.b, ssd.b, gs.b], writes=[ms.b])
                S.store("pool", self.mix[c * 512:(c + 1) * 512, 256:384].rearrange("(a p) d -> p a d", p=128), ms[:, :, :], ms.b)
            S.barrier()

    def nsa(self, l):
        nc, S = self.nc, self.S
        with ExitStack() as ph:
            KCT = self.tile(ph, "KCT", [128, 512], BF16)
            VCt = self.tile(ph, "VCt", [128, 4, 65], BF16)
            S.op("pool", lambda: nc.gpsimd.memset(VCt[:, :, :], 1.0), writes=[VCt.b])
            S.load("sp", KCT[64:67, :], self.dc["ones3"][:, 0:512], KCT.b)
            with ExitStack() as cs:
                XT = self.tile(cs, "XTc", [64, S_LEN], BF16)
                w1 = self.tile(cs, "w1c", [64, 32, 128], BF16)
                w2 = self.tile(cs, "w2c", [128, 64], BF16)
                pos = self.tile(cs, "posc", [64, 32], BF16)
                b1 = self.tile(cs, "b1c", [128, 1], F32)
                cbias = self.tile(cs, "cbias", [128, 1], F32)
                hid = self.tile(cs, "hidc", [128, 512], BF16)
                sg = self.tile(cs, "sgc", [128, 4096], F32)
                hp = self.tile(cs, "hpc", [128, 512], F32, psum=True)
                cp = self.tile(cs, "cpc", [128, 512], F32, psum=True)
                op_ = self.tile(cs, "opc", [128, 512], F32, psum=True)
                for s_i, sfx in enumerate("kv"):
                    S.load("sp", XT[:, :], self.featT[FRr["nkc" if sfx == "k" else "nvc"]:FRr["nkc" if sfx == "k" else "nvc"] + 64, :], XT.b)
                    S.load("sp", sg[0:64, 0:4096], self.dp["w1" + sfx][l].rearrange("d l f -> d (l f)"), sg.b)
                    self.copy(w1[:, :, :], sg[0:64, 0:4096].rearrange("d (l f) -> d l f", l=32), [sg.b], [w1.b])
                    S.load("sp", sg[:, 0:64], self.dp["w2" + sfx][l], sg.b)
                    self.copy(w2[:, :], sg[:, 0:64], [sg.b], [w2.b])
                    S.load("sp", sg[0:64, 0:32], self.dp["pos" + sfx][l], sg.b)
                    self.copy(pos[:, :], sg[0:64, 0:32], [sg.b], [pos.b])
                    S.load("sp", b1[:, :], self.dp["b1" + sfx][l], b1.b)
                    for li in range(32):
                        S.op("pe", lambda: nc.tensor.matmul(hp[:, 0:511], lhsT=w1[:, li, :], rhs=XT[:, li:li + 16 * 510 + 1:16], start=(li == 0), stop=(li == 31)),
                             reads=[w1.b, XT.b], writes=[hp.b], accum=(li > 0))
                    for li in range(32):
                        S.op("pe", lambda: nc.tensor.matmul(cp[:, 0:1], lhsT=w1[:, li, :], rhs=pos[:, li:li + 1], start=(li == 0), stop=(li == 31)),
                             reads=[w1.b, pos.b], writes=[cp.b], accum=(li > 0))
                    S.op("dve", lambda: nc.vector.tensor_tensor(out=cbias[:, :], in0=cp[:, 0:1], in1=b1[:, :], op=ALU.add),
                         reads=[cp.b, b1.b], writes=[cbias.b])
                    S.op("pool", lambda: nc.gpsimd.memset(hid[:, :], 0.0), writes=[hid.b])
                    S.op("act", lambda: nc.scalar.activation(out=hid[:, 0:511], in_=hp[:, 0:511], func=AF.Gelu_apprx_tanh, bias=cbias[:, 0:1]),
                         reads=[hp.b, cbias.b], writes=[hid.b])
                    if sfx == "k":
                        S.op("pe", lambda: nc.tensor.matmul(op_[0:64, 0:512], lhsT=w2[:, :], rhs=hid[:, :], start=True, stop=True),
                             reads=[w2.b, hid.b], writes=[op_.b])
                        self.copy(KCT[0:64, :], op_[0:64, 0:512], [op_.b], [KCT.b])
                    else:
                        ov_ = op_[:, 0:256].rearrange("p (a b) -> p a b", a=4)
                        for kt in range(4):
                            S.op("pe", lambda: nc.tensor.matmul(ov_[:, kt, :], lhsT=hid[:, kt * 128:(kt + 1) * 128], rhs=w2[:, :], start=True, stop=True),
                                 reads=[w2.b, hid.b], writes=[op_.b], accum=(kt > 0))
                        self.copy(VCt[:, :, 0:64], ov_, [op_.b], [VCt.b])
                S.barrier()
            KTs = self.tile(ph, "KTsl", [128, 1, S_LEN], BF16)
            KTw = self.tile(ph, "KTwn", [128, 1, S_LEN], BF16)
            Vs = self.tile(ph, "Vsl", [128, 1, NT, 65], BF16)
            Vw = self.tile(ph, "Vwn", [128, 1, NT, 65], BF16)
            self.load_kv(KTs, Vs, 67, 1, FRr["nks"], VCr["nvs"], 1)
            self.load_kv(KTw, Vw, 67, 1, FRr["nkw"], VCr["nvw"], 1)
            ES = self.tile(ph, "ESn", [128, S_LEN], BF16)
            WM = self.tile(ph, "WMn", [128, 8, 512], BF16)
            CM = self.tile(ph, "CMn", [128, 5, 512], BF16)
            OVt = self.tile(ph, "OVn", [128, 4, 128], BF16)
            KBC = self.tile(ph, "KBCn", [128, 4, 16, 4], F32)
            S.load("sp", ES[:, :], self.dc["es"], ES.b)
            S.load("sp", WM[:, :, :], self.dc["wmask"], WM.b)
            S.load("sp", CM[:, :, :], self.dc["cmask"], CM.b)
            S.load("sp", OVt[:, :, :], self.dc["ov"], OVt.b)
            S.load("sp", KBC[:, :, :, :], self.dc["kbc"], KBC.b)
            R = self.attn_res(ph)
            QT = [self.tile(ph, "QTn%d" % i, [128, 4, 512], BF16) for i in range(2)]
            for q in QT:
                S.load("sp", q[64:67, :, :], self.dc["aug"][4:8].rearrange("h r q -> r h q"), q.b)
            imps = self.tile(ph, "imps", [128, 512], F32, psum=True)
            tps = self.tile(ph, "tpsn", [128, 1024], BF16, psum=True)
            impa = self.tile(ph, "impa", [128, 4, 128], F32)
            f1 = [self.tile(ph, "f1e4_%d" % i, [128, 4, 128], F32) for i in range(2)]
            nv = [self.tile(ph, "negv_%d" % i, [128, 4, 128], F32) for i in range(2)]
            gr = [self.tile(ph, "graw%d" % i, [128, 4, 12], BF16) for i in range(2)]
            sgm = self.tile(ph, "sgm", [128, 4, 12], F32)
            scm = self.tile(ph, "scm", [128, 4, 128], F32)
            tmpm = self.tile(ph, "tmpm", [128, 4, 128], F32)
            m8a = self.tile(ph, "m8a", [128, 4, 8], F32)
            m8b = self.tile(ph, "m8b", [128, 4, 8], F32)
            sel = self.tile(ph, "seln", [128, 4, 128], F32)
            val = self.tile(ph, "valn", [128, 4, 128], F32)
            nsel = self.tile(ph, "nseln", [128, 4, 128], BF16)
            nselT = self.tile(ph, "nselTn", [128, 512], BF16)
            den = self.tile(ph, "denn", [128, 4], F32)
            acc = self.tile(ph, "accn", [128, 2, 4, 64], F32)
            mst = [self.tile(ph, "mstn%d" % i, [128, 4, 128], BF16) for i in range(2)]

            def fold(O, Ov, h, gcol, first):
                self.recip_den(Ov, O, den)
                S.op("dve", lambda: nc.vector.tensor_tensor(out=den[:, :], in0=den[:, :], in1=sgm[:, :, gcol], op=ALU.mult),
                     reads=[den.b, sgm.b], writes=[den.b])
                for qi in range(4):
                    if first:
                        S.op("dve", lambda: nc.vector.tensor_scalar(out=acc[:, h, qi, :], in0=Ov[:, qi, 0:64], scalar1=den[:, qi:qi + 1], scalar2=None, op0=ALU.mult),
                             reads=[O.b, den.b], writes=[acc.b])
                    else:
                        S.op("dve", lambda: nc.vector.scalar_tensor_tensor(out=acc[:, h, qi, :], in0=Ov[:, qi, 0:64], scalar=den[:, qi:qi + 1], in1=acc[:, h, qi, :],
                                                                           op0=ALU.mult, op1=ALU.add), reads=[O.b, den.b, acc.b], writes=[acc.b])

            for c in range(self.nch):
                q = QT[c % 2]
                for h in range(4):
                    S.load("sp", q[0:64, h, :], self.featT[FRr["nq"] + 64 * h:FRr["nq"] + 64 * (h + 1), c * 512:(c + 1) * 512], q.b)
                f1c, nvc, grc = f1[c % 2], nv[c % 2], gr[c % 2]
                S.load("sp", f1c[:, :, :], self.dc["f1e4"][c], f1c.b)
                S.load("sp", nvc[:, :, :], self.dc["negv"][c], nvc.b)
                S.load("sp", grc[:, :, :], self.vtok[c * 512:(c + 1) * 512, VCr["ng"]:VCr["ng"] + 12].rearrange("(a p) g -> p a g", p=128), grc.b)
                S.op("act", lambda: nc.scalar.activation(out=sgm[:, :, :], in_=grc[:, :, :], func=AF.Exp, scale=-1.0), reads=[grc.b], writes=[sgm.b])
                S.op("dve", lambda: nc.vector.tensor_scalar(out=sgm[:, :, :], in0=sgm[:, :, :], scalar1=1.0, scalar2=None, op0=ALU.add),
                     reads=[sgm.b], writes=[sgm.b])
                S.op("dve", lambda: nc.vector.reciprocal(out=sgm[:, :, :], in_=sgm[:, :, :]), reads=[sgm.b], writes=[sgm.b])
                ms = mst[c % 2]
                kb_ = c // 4
                iv = imps[:, :].rearrange("p (a b) -> p a b", a=4)
                for h in range(4):
                    tiles = []
                    ntl = kb_ + 1

                    def mkpost(kt, ntl=ntl):
                        def post(pt, first, last):
                            for qi in range(4):
                                S.op("pe", lambda: nc.tensor.matmul(iv[:, qi, :], lhsT=pt[:, qi * 128:(qi + 1) * 128], rhs=OVt[:, kt, :],
                                                                    start=(first and qi == 0), stop=last),
                                     reads=[pt.b, OVt.b], writes=[imps.b], accum=not (first and qi == 0))
                        return post
                    for kt in range(ntl):
                        ex = []
                        if kt == kb_:
                            ex.append((self.ident[:, :], CM[:, c % 4, :], [self.ident.b, CM.b]))
                        elif kt == kb_ - 1 and c % 4 == 0:
                            ex.append((self.ident[:, :], CM[:, 4, :], [self.ident.b, CM.b]))
                        tiles.append(dict(kT=KCT[0:67, kt * 128:(kt + 1) * 128], v=VCt[:, kt, :], kbufs=[KCT.b, VCt.b],
                                          bias=KBC[:, h, c, kt:kt + 1], bbufs=[KBC.b], extra=ex, post=mkpost(kt)))
                    O = R.O[R.io % len(R.O)]
                    R.io += 1
                    Ov = self.attend(R, q[0:67, h, :], [q.b], tiles, O, 0.125)
                    self.recip_den(Ov, O, den)
                    for qi in range(4):
                        if h == 0:
                            S.op("dve", lambda: nc.vector.tensor_scalar(out=impa[:, qi, :], in0=iv[:, qi, :], scalar1=den[:, qi:qi + 1], scalar2=None, op0=ALU.mult),
                                 reads=[imps.b, den.b], writes=[impa.b])
                        else:
                            S.op("dve", lambda: nc.vector.scalar_tensor_tensor(out=impa[:, qi, :], in0=iv[:, qi, :], scalar=den[:, qi:qi + 1], in1=impa[:, qi, :],
                                                                               op0=ALU.mult, op1=ALU.add), reads=[imps.b, den.b, impa.b], writes=[impa.b])
                    if h < 2:
                        fold(O, Ov, h, 3 * h + 0, True)
                S.op("dve", lambda: nc.vector.tensor_tensor(out=scm[:, :, :], in0=impa[:, :, :], in1=f1c[:, :, :], op=ALU.max),
                     reads=[impa.b, f1c.b], writes=[scm.b])
                S.op("dve", lambda: nc.vector.tensor_tensor(out=scm[:, :, :], in0=scm[:, :, :], in1=nvc[:, :, :], op=ALU.add),
                     reads=[scm.b, nvc.b], writes=[scm.b])
                for qi in range(4):
                    S.op("dve", lambda: nc.vector.max(out=m8a[:, qi, :], in_=scm[:, qi, :]), reads=[scm.b], writes=[m8a.b])
                    S.op("dve", lambda: nc.vector.match_replace(out=tmpm[:, qi, :], in_to_replace=m8a[:, qi, :], in_values=scm[:, qi, :], imm_value=-1e30),
                         reads=[scm.b, m8a.b], writes=[tmpm.b])
                    S.op("dve", lambda: nc.vector.max(out=m8b[:, qi, :], in_=tmpm[:, qi, :]), reads=[tmpm.b], writes=[m8b.b])
                    S.op("dve", lambda: nc.vector.tensor_scalar(out=sel[:, qi, :], in0=scm[:, qi, :], scalar1=m8b[:, qi, 7:8], scalar2=None, op0=ALU.is_ge),
                         reads=[scm.b, m8b.b], writes=[sel.b])
                S.op("dve", lambda: nc.vector.tensor_scalar(out=val[:, :, :], in0=nvc[:, :, :], scalar1=-1.0, scalar2=None, op0=ALU.is_ge),
                     reads=[nvc.b], writes=[val.b])
                S.op("dve", lambda: nc.vector.tensor_tensor(out=sel[:, :, :], in0=sel[:, :, :], in1=val[:, :, :], op=ALU.mult),
                     reads=[sel.b, val.b], writes=[sel.b])
                S.op("dve", lambda: nc.vector.tensor_scalar(out=nsel[:, :, :], in0=sel[:, :, :], scalar1=-1.0, scalar2=-NEG, op0=ALU.add, op1=ALU.mult),
                     reads=[sel.b], writes=[nsel.b])
                for qi in range(4):
                    S.op("pe", lambda: nc.tensor.transpose(out=tps[:, qi * 128:(qi + 1) * 128], in_=nsel[:, qi, :], identity=self.ident[:, :]),
                         reads=[nsel.b, self.ident.b], writes=[tps.b], accum=(qi > 0))
                self.copy(nselT[:, :], tps[:, 0:512], [tps.b], [nselT.b], eng="dve")
                for h in range(2):
                    tiles = []
                    for kt in range(4 * c + 4):
                        if far_skip(1, h, 512 * c - (128 * kt + 127)):
                            continue
                        r = kt - 4 * c
                        ex = [(ES[:, kt * 128:(kt + 1) * 128], nselT[:, :], [ES.b, nselT.b])]
                        if r >= 0:
                            ex.append((self.ident[:, :], self.caus[:, r, :], [self.ident.b, self.caus.b]))
                        tiles.append(dict(kT=KTs[0:67, 0, kt * 128:(kt + 1) * 128], v=Vs[:, 0, kt, :], kbufs=[KTs.b, Vs.b],
                                          bias=self.kb[:, 4 + h, r + 60:r + 61], bbufs=[self.kb.b], extra=ex, q0=128 * max(r, 0)))
                    O = R.O[R.io % len(R.O)]
                    R.io += 1
                    Ov = self.attend(R, q[0:67, h, :], [q.b], tiles, O, 0.125)
                    fold(O, Ov, h, 3 * h + 1, False)
                    tiles = []
                    for r in range(-4, 4):
                        kt = 4 * c + r
                        if kt < 0:
                            continue
                        tiles.append(dict(kT=KTw[0:67, 0, kt * 128:(kt + 1) * 128], v=Vw[:, 0, kt, :], kbufs=[KTw.b, Vw.b],
                                          bias=self.kb[:, 4 + h, r + 60:r + 61], bbufs=[self.kb.b],
                                          extra=[(self.ident[:, :], WM[:, r + 4, :], [self.ident.b, WM.b])]))
                    O = R.O[R.io % len(R.O)]
                    R.io += 1
                    Ov = self.attend(R, q[0:67, h, :], [q.b], tiles, O, 0.125)
                    fold(O, Ov, h, 3 * h + 2, False)
                    S.op("dve", lambda: nc.vector.tensor_copy(out=ms[:, :, h * 64:(h + 1) * 64], in_=acc[:, h, :, :]), reads=[acc.b], writes=[ms.b])
                S.store("pool", self.mix[c * 512:(c + 1) * 512, 128:256].rearrange("(a p) d -> p a d", p=128), ms[:, :, :], ms.b)
            S.barrier()

    def prep_ffn(self, l):
        S = self.S
        with ExitStack() as ph:
            stg = [self.tile(ph, "pstg%d" % i, [128, 4096], F32) for i in range(2)]
            sb = [self.tile(ph, "psb%d" % i, [128, 4096], BF16) for i in range(2)]
            i = 0
            for src, dst in ((self.dp["wg"], self.wgs), (self.dp["wu"], self.wus), (self.dp["wd"], self.wds)):
                for g in range(NCF // 4):
                    s_, b_ = stg[i % 2], sb[i % 2]
                    i += 1
                    if len(src.shape) == 5:
                        sap = src[l, 4 * g:4 * g + 4].rearrange("c p k n -> p c (k n)")
                        dap = dst[4 * g:4 * g + 4].rearrange("c p k n -> p c (k n)")
                    else:
                        sap = src[l, 4 * g:4 * g + 4].rearrange("c p n -> p c n")
                        dap = dst[4 * g:4 * g + 4].rearrange("c p n -> p c n")
                    S.load("sp", s_[:, :].rearrange("p (c n) -> p c n", c=4), sap, s_.b)
                    self.copy(b_[:, :], s_[:, :], [s_.b], [b_.b])
                    S.store("pool", dap, b_[:, :].rearrange("p (c n) -> p c n", c=4), b_.b)
            S.barrier()

    def phase_C(self, l, x_src, x_dst):
        nc, S = self.nc, self.S
        with ExitStack() as ph:
            WO = self.tile(ph, "WO", [128, 8, D], BF16)
            with ExitStack() as tmp:
                self.load_cast(tmp, WO, lambda i: WO[:, i, :], lambda i: self.dp["wo"][l, :, i, :], 8, D)
                S.barrier()
            g1 = self.tile(ph, "g_apost", [128, D], F32)
            g2 = self.tile(ph, "g_fpre", [128, D], F32)
            g3 = self.tile(ph, "g_fpost", [128, D], F32)
            S.load("sp", g1[:, :], self.dp["g_apost"][l].partition_broadcast(128), g1.b)
            S.load("sp", g2[:, :], self.dp["g_fpre"][l].partition_broadcast(128), g2.b)
            S.load("sp", g3[:, :], self.dp["g_fpost"][l].partition_broadcast(128), g3.b)
            cw = self.tile(ph, "cw", [128, NCF, 3], F32)
            cb = self.tile(ph, "cb", [128, NCF], F32)
            S.load("sp", cw[:, :, :], self.dp["cw"][l], cw.b)
            S.load("sp", cb[:, :], self.dp["cb"][l], cb.b)
            halo = self.tile(ph, "halo", [128, NCF, 2], F32)
            S.op("pool", lambda: nc.gpsimd.memset(halo[:, :, :], 0.0), writes=[halo.b])
            bank = [self.tile(ph, "bk%d" % i, [128, 512], F32, psum=True) for i in range(6)]
            tpb = [self.tile(ph, "tpC%d" % i, [128, D], BF16, psum=True) for i in range(2)]
            mixt = [self.tile(ph, "mixt%d" % i, [128, D], BF16) for i in range(2)]
            mT = [self.tile(ph, "mT%d" % i, [128, 8, 128], BF16) for i in range(2)]
            xt = [self.tile(ph, "xtC%d" % i, [128, D], F32) for i in range(2)]
            x1 = [self.tile(ph, "x1C%d" % i, [128, D], F32) for i in range(2)]
            yt = self.tile(ph, "ytC", [128, D], F32)
            junk = self.tile(ph, "junkC", [128, D], F32)
            ss = self.tile(ph, "ssC", [128, 1], F32)
            sd = self.tile(ph, "sdC", [128, 1], F32)
            rstd = self.tile(ph, "rstdC", [128, 1], F32)
            hb = [self.tile(ph, "hbC%d" % i, [128, D], BF16) for i in range(4)]
            h2T = self.tile(ph, "h2T", [128, 8, 512], BF16)
            gT = self.tile(ph, "gT", [128, NCF, 512], BF16)
            wgt = [self.tile(ph, "wgt%d" % i, [128, 8, 128], BF16) for i in range(3)]
            wut = [self.tile(ph, "wut%d" % i, [128, 8, 128], BF16) for i in range(3)]
            wdt = [self.tile(ph, "wdt%d" % i, [128, D], BF16) for i in range(3)]
            aT = [self.tile(ph, "aT%d" % i, [128, 514], F32) for i in range(2)]
            cacc = [self.tile(ph, "cacc%d" % i, [128, 512], F32) for i in range(2)]
            ga = [self.tile(ph, "ga%d" % i, [128, 512], F32) for i in range(2)]
            pst = [self.tile(ph, "pstC%d" % i, [128, D], F32) for i in range(2)]
            rt = [self.tile(ph, "rtC%d" % i, [128, D], F32) for i in range(2)]
            x1r = [self.tile(ph, "x1rC%d" % i, [128, D], F32) for i in range(2)]
            ot = [self.tile(ph, "otC%d" % i, [128, D], F32) for i in range(2)]
            ss3 = self.tile(ph, "ss3C", [128, 1], F32)
            sd3 = self.tile(ph, "sd3C", [128, 1], F32)
            rstd3 = self.tile(ph, "rstd3C", [128, 1], F32)
            junk3 = self.tile(ph, "junk3C", [128, D], F32)
            it = 0
            i3 = 0
            RNG = 2
            pending = []
            store_tags = []

            def finish_tile(tag, tt):
                nonlocal i3
                S._wait("sp", tag)
                tok = tt * 128
                r_, x_, o_ = rt[i3 % 2], x1r[i3 % 2], ot[i3 % 2]
                i3 += 1
                S.load("sp", r_[:, :], self.red[tok:tok + 128, :], r_.b)
                S.load("sp", x_[:, :], self.x1s[tok:tok + 128, :], x_.b)
                self.rstd_of(r_[:, :], r_.b, junk3, ss3, sd3, rstd3)
                S.op("dve", lambda: nc.vector.scalar_tensor_tensor(out=r_[:, :], in0=r_[:, :], scalar=rstd3[:, 0:1], in1=g3[:, :], op0=ALU.mult, op1=ALU.mult),
                     reads=[r_.b, rstd3.b, g3.b], writes=[r_.b])
                S.op("pool", lambda: nc.gpsimd.tensor_tensor(out=o_[:, :], in0=r_[:, :], in1=x_[:, :], op=ALU.add),
                     reads=[r_.b, x_.b], writes=[o_.b])
                S.store("pool", x_dst[tok:tok + 128, :], o_[:, :], o_.b)

            h2Ts = [h2T, self.tile(ph, "h2Tb", [128, 8, 512], BF16)]

            def seg_a(c, ti):
                nonlocal it
                tok = (4 * c + ti) * 128
                mt, mTt, xtt, h, tpp, x1t = mixt[it % 2], mT[it % 2], xt[it % 2], hb[it % 4], tpb[it % 2], x1[it % 2]
                it += 1
                mrow = (tok // 2048) * 4096 + (tok % 2048)
                S.load("sp", mt[:, 0:512], self.mixall[mrow:mrow + 128, :], mt.b)
                S.load("sp", mt[:, 512:1024], self.mixall[2048 + mrow:2048 + mrow + 128, :], mt.b)
                S.load("sp", xtt[:, :], x_src[tok:tok + 128, :], xtt.b)
                for kc in range(8):
                    S.op("pe", lambda: nc.tensor.transpose(out=tpp[:, kc * 128:(kc + 1) * 128], in_=mt[:, kc * 128:(kc + 1) * 128], identity=self.ident[:, :]),
                         reads=[mt.b, self.ident.b], writes=[tpp.b], accum=(kc > 0))
                self.copy(mTt[:, :, :], tpp[:, :].rearrange("p (k t) -> p k t", k=8), [tpp.b], [mTt.b])
                pa, pb = bank[4], bank[5]
                for half, pp in enumerate((pa, pb)):
                    for kc in range(8):
                        S.op("pe", lambda: nc.tensor.matmul(pp[:, :], lhsT=mTt[:, kc, :], rhs=WO[:, kc, half * 512:(half + 1) * 512],
                                                            start=(kc == 0), stop=(kc == 7)), reads=[mTt.b, WO.b], writes=[pp.b], accum=(kc > 0))
                self.copy(yt[:, 0:512], pa[:, :], [pa.b], [yt.b], eng="act")
                self.copy(yt[:, 512:D], pb[:, :], [pb.b], [yt.b], eng="act")
                self.rstd_of(yt[:, :], yt.b, junk, ss, sd, rstd)
                S.op("dve", lambda: nc.vector.scalar_tensor_tensor(out=yt[:, :], in0=yt[:, :], scalar=rstd[:, 0:1], in1=g1[:, :], op0=ALU.mult, op1=ALU.mult),
                     reads=[yt.b, rstd.b, g1.b], writes=[yt.b])
                S.op("pool", lambda: nc.gpsimd.tensor_tensor(out=x1t[:, :], in0=yt[:, :], in1=xtt[:, :], op=ALU.add),
                     reads=[yt.b, xtt.b], writes=[x1t.b])
                S.store("pool", self.x1s[tok:tok + 128, :], x1t[:, :], x1t.b)
                store_tags.append(("dma", x1t.b.dsem, x1t.b.dcnt))
                self.rstd_of(x1t[:, :], x1t.b, junk, ss, sd, rstd)
                S.op("dve", lambda: nc.vector.scalar_tensor_tensor(out=h[:, :], in0=x1t[:, :], scalar=rstd[:, 0:1], in1=g2[:, :], op0=ALU.mult, op1=ALU.mult),
                     reads=[x1t.b, rstd.b, g2.b], writes=[h.b])
                return (h, tpp)

            def seg_b(c, ti, ctx):
                h, tpp = ctx
                hT = h2Ts[c % 2]
                for kc in range(8):
                    S.op("pe", lambda: nc.tensor.transpose(out=tpp[:, kc * 128:(kc + 1) * 128], in_=h[:, kc * 128:(kc + 1) * 128], identity=self.ident[:, :]),
                         reads=[h.b, self.ident.b], writes=[tpp.b], accum=(kc > 0))
                self.copy(hT[:, :, ti * 128:(ti + 1) * 128], tpp[:, :].rearrange("p (k t) -> p k t", k=8), [tpp.b], [hT.b])

            def c2a_cf(c, cf):
                hT = h2Ts[c % 2]
                wg_, wu_ = wgt[cf % 3], wut[cf % 3]
                S.load("sp", wg_[:, :, :], self.wgs[cf], wg_.b)
                S.load("sp", wu_[:, :, :], self.wus[cf], wu_.b)
                pa, pu = bank[2 * (cf % 2)], bank[2 * (cf % 2) + 1]
                for kc in range(8):
                    S.op("pe", lambda: nc.tensor.matmul(pa[:, :], lhsT=wg_[:, kc, :], rhs=hT[:, kc, :], start=(kc == 0), stop=(kc == 7)),
                         reads=[wg_.b, hT.b], writes=[pa.b], accum=(kc > 0))
                for kc in range(8):
                    S.op("pe", lambda: nc.tensor.matmul(pu[:, :], lhsT=wu_[:, kc, :], rhs=hT[:, kc, :], start=(kc == 0), stop=(kc == 7)),
                         reads=[wu_.b, hT.b], writes=[pu.b], accum=(kc > 0))
                a, ca, g = aT[cf % 2], cacc[cf % 2], ga[cf % 2]
                e4 = a[:, 0:4]
                S.op("dve", lambda: nc.vector.tensor_copy(out=a[:, 0:2], in_=halo[:, cf, :]), reads=[halo.b], writes=[a.b])
                S.op("dve", lambda: nc.vector.tensor_copy(out=a[:, 2:4], in_=pa[:, 0:2]), reads=[pa.b], writes=[a.b])
                S.op("dve", lambda: nc.vector.tensor_copy(out=halo[:, cf, :], in_=pa[:, 510:512]), reads=[pa.b], writes=[halo.b])
                S.op("dve", lambda: nc.vector.tensor_scalar(out=ca[:, 2:512], in0=pa[:, 0:510], scalar1=cw[:, cf, 0:1], scalar2=None, op0=ALU.mult),
                     reads=[pa.b, cw.b], writes=[ca.b])
                S.op("dve", lambda: nc.vector.scalar_tensor_tensor(out=ca[:, 2:512], in0=pa[:, 1:511], scalar=cw[:, cf, 1:2], in1=ca[:, 2:512], op0=ALU.mult, op1=ALU.add),
                     reads=[pa.b, cw.b, ca.b], writes=[ca.b])
                S.op("dve", lambda: nc.vector.scalar_tensor_tensor(out=ca[:, 2:512], in0=pa[:, 2:512], scalar=cw[:, cf, 2:3], in1=ca[:, 2:512], op0=ALU.mult, op1=ALU.add),
                     reads=[pa.b, cw.b, ca.b], writes=[ca.b])
                S.op("dve", lambda: nc.vector.tensor_scalar(out=ca[:, 0:2], in0=e4[:, 0:2], scalar1=cw[:, cf, 0:1], scalar2=None, op0=ALU.mult),
                     reads=[a.b, cw.b], writes=[ca.b])
                S.op("dve", lambda: nc.vector.scalar_tensor_tensor(out=ca[:, 0:2], in0=e4[:, 1:3], scalar=cw[:, cf, 1:2], in1=ca[:, 0:2], op0=ALU.mult, op1=ALU.add),
                     reads=[a.b, cw.b, ca.b], writes=[ca.b])
                S.op("dve", lambda: nc.vector.scalar_tensor_tensor(out=ca[:, 0:2], in0=e4[:, 2:4], scalar=cw[:, cf, 2:3], in1=ca[:, 0:2], op0=ALU.mult, op1=ALU.add),
                     reads=[a.b, cw.b, ca.b], writes=[ca.b])
                S.op("act", lambda: nc.scalar.activation(out=g[:, :], in_=ca[:, :], func=AF.Gelu_apprx_tanh, bias=cb[:, cf:cf + 1]),
                     reads=[ca.b, cb.b], writes=[g.b])
                S.op("dve", lambda: nc.vector.tensor_tensor(out=gT[:, cf, :], in0=g[:, :], in1=pu[:, :], op=ALU.mult),
                     reads=[g.b, pu.b], writes=[gT.b])

            def c2b(c):
                for tp2 in range(2):
                    accs = [bank[0], bank[1], bank[2], bank[3]]
                    for cf in range(NCF):
                        wd_ = wdt[cf % 3]
                        S.load("sp", wd_[:, :], self.wds[cf], wd_.b)
                        for j in range(2):
                            ti = 2 * tp2 + j
                            for half in range(2):
                                pp = accs[2 * j + half]
                                S.op("pe", lambda: nc.tensor.matmul(pp[:, :], lhsT=gT[:, cf, ti * 128:(ti + 1) * 128], rhs=wd_[:, half * 512:(half + 1) * 512],
                                                                    start=(cf == 0), stop=(cf == NCF - 1)), reads=[gT.b, wd_.b], writes=[pp.b], accum=(cf > 0))
                    for j in range(2):
                        ti = 2 * tp2 + j
                        tok = (4 * c + ti) * 128
                        p_ = pst[j]
                        self.copy(p_[:, 0:512], accs[2 * j][:, :], [accs[2 * j].b], [p_.b], eng="act")
                        self.copy(p_[:, 512:D], accs[2 * j + 1][:, :], [accs[2 * j + 1].b], [p_.b], eng="dve")
                        S.store("pool", self.part[tok:tok + 128, :], p_[:, :], p_.b)
                        store_tags.append(("dma", p_.b.dsem, p_.b.dcnt))
                    if pend_b:
                        seg_b(*pend_b.pop(0))

            pend_b = []
            fifo = []
            for ti in range(4):
                seg_b(0, ti, seg_a(0, ti))
            for c in range(self.nch):
                for cf in range(NCF):
                    c2a_cf(c, cf)
                    if cf % 4 == 3:
                        k = cf // 4
                        if len(pend_b) >= 2:
                            seg_b(*pend_b.pop(0))
                        if c + 1 < self.nch:
                            pend_b.append((c + 1, k, seg_a(c + 1, k)))
                        if fifo and fifo[0][2] <= c:
                            tg, tt, _ = fifo.pop(0)
                            finish_tile(tg, tt)
                c2b(c)
                while pend_b:
                    seg_b(*pend_b.pop(0))
                if c % RNG == RNG - 1 or c == self.nch - 1:
                    c0 = (c // RNG) * RNG
                    r0, r1 = c0 * 512, (c + 1) * 512
                    for tg in store_tags:
                        S._wait("pool", tg)
                    store_tags = []
                    tag = self.collective("AllReduce", ALU.add, self.part[r0:r1, :].opt(), self.red[r0:r1, :].opt())
                    for tt in range(r0 // 128, r1 // 128):
                        fifo.append((tag, tt, c + 2))
            for tg, tt, _ in fifo:
                finish_tile(tg, tt)
            S.barrier()


_CACHE = {}


def kernel(**inputs):
    x = np.ascontiguousarray(inputs["x"], dtype=np.float32)
    params = [layout_params(inputs, r) for r in range(2)]
    consts = [make_consts(r) for r in range(2)]
    if "prog" not in _CACHE:
        _CACHE["prog"] = Prog()
    prog = _CACHE["prog"]
    in_maps = []
    for core in range(8):
        b, r = core // 2, core % 2
        m = {"x": x[b]}
        for n, _, _ in CONST_SPECS:
            m["c_" + n] = consts[r][n]
        for n, _ in PARAM_SPECS:
            m["p_" + n] = params[r][n]
        in_maps.append(m)
    res = run_bass_kernel_spmd(prog.nc, in_maps, core_ids=list(range(8)))
    out = np.stack([np.asarray(res.results[2 * b]["y"], dtype=np.float32) for b in range(4)], axis=0)
    return out
```

```python
import math
import numpy as np
import ml_dtypes
from contextlib import ExitStack
import concourse.bass as bass
import concourse.mybir as mybir
from concourse.bass_utils import run_bass_kernel_spmd

F32 = mybir.dt.float32
BF16 = mybir.dt.bfloat16
AF = mybir.ActivationFunctionType
ALU = mybir.AluOpType
AX = mybir.AxisListType
NPBF = ml_dtypes.bfloat16

D = 1024
S_LEN = 8192
NT = 64
NCH = 16
DEPTH = 2
DFF = 4096
NEG = -30000.0
EPS = 1e-6

OFF = dict(mq=0, mk=256, mv=512, nq=768, nkc=1024, nvc=1088, nks=1152, nvs=1216, nkw=1280, nvw=1344,
           ng=1408, dq=1420, dk=1676, dv=1932, sq=2188, sk=2444, sv=2572)
FRr = dict(mq=0, mk=128, nq=256, nkc=512, nvc=576, nks=640, nkw=704, dq=768, dk=896, sq=1024, sk=1152)
NF = 1280
VCr = dict(mv=0, nvs=128, nvw=192, dv=256, sv=384, ng=448)
NV = 460
NCF = 16


def role_heads(r, m=0):
    if m == 3:
        return [2 * r, 2 * r + 1], [2 * (1 - r), 2 * (1 - r) + 1]
    return [r, r + 2], [1 - r, 3 - r]


def far_skip(m, j, min_dist):
    sl = min(SLOPES[4 * role_heads(0, m)[0][j] + m], SLOPES[4 * role_heads(1, m)[0][j] + m])
    return min_dist > 0 and sl * min_dist >= 200.0


def role_cols(r):
    own0 = role_heads(r, 0)[0]
    own1, oth1 = role_heads(r, 1)
    own2 = role_heads(r, 2)[0]
    own3 = role_heads(r, 3)[0]

    def hc(base, heads, w=64):
        return np.concatenate([np.arange(base + w * h, base + w * (h + 1)) for h in heads])

    f = np.concatenate([hc(OFF["mq"], own0), hc(OFF["mk"], own0), hc(OFF["nq"], own1 + oth1),
                        np.arange(OFF["nkc"], OFF["nkc"] + 64), np.arange(OFF["nvc"], OFF["nvc"] + 64),
                        np.arange(OFF["nks"], OFF["nks"] + 64), np.arange(OFF["nkw"], OFF["nkw"] + 64),
                        hc(OFF["dq"], own2), hc(OFF["dk"], own2), hc(OFF["sq"], own3), hc(OFF["sk"], [r])])
    v = np.concatenate([hc(OFF["mv"], own0), np.arange(OFF["nvs"], OFF["nvs"] + 64), np.arange(OFF["nvw"], OFF["nvw"] + 64),
                        hc(OFF["dv"], own2), hc(OFF["sv"], [r]), hc(OFF["ng"], own1 + oth1, 3)])
    assert len(f) == 1216 and len(v) == NV
    return f, v


SLOPES = np.power(np.float32(2.0), np.arange(1, 17, dtype=np.float32) * np.float32(-0.5)).astype(np.float64)


class Buf:
    __slots__ = ("name", "w", "rs", "dsem", "dcnt")

    def __init__(self, name):
        self.name = name
        self.w = None
        self.rs = []
        self.dsem = None
        self.dcnt = 0


class Sched:
    def __init__(self, nc, stack):
        self.nc = nc
        self.stack = stack
        self.eng = {"pe": nc.tensor, "act": nc.scalar, "dve": nc.vector, "pool": nc.gpsimd, "sp": nc.sync}
        self.sem, self.cnt, self.seen = {}, {}, {}
        for k in self.eng:
            self.sem[k] = stack.enter_context(nc.semaphore("s_" + k))
            self.cnt[k] = 0
            self.seen[k] = {}
        self.dma_seen = {k: {} for k in self.eng}
        self.free_sems = []
        self.live = []
        self.nsem = len(self.eng)
        self.ninstr = 0

    def _wait(self, ek, dep):
        if dep is None:
            return
        e = self.eng[ek]
        if dep[0] == "dma":
            _, sem, val = dep
            d = self.dma_seen[ek]
            if d.get(id(sem), 0) >= val:
                return
            e.wait_ge(sem, val)
            d[id(sem)] = val
        else:
            pk, c = dep
            if self.seen[ek].get(pk, 0) >= c:
                return
            e.wait_ge(self.sem[pk], c)
            self.seen[ek][pk] = c
        self.ninstr += 1

    def _deps(self, ek, reads, writes, accum):
        for b in reads:
            self._wait(ek, b.w)
        for b in writes:
            if not (accum and b.w is not None and b.w[0] == ek):
                self._wait(ek, b.w)
            for r in b.rs:
                self._wait(ek, r)

    def op(self, ek, fn, reads=(), writes=(), accum=False):
        self._deps(ek, reads, writes, accum)
        ins = fn()
        self.cnt[ek] += 1
        ins.then_inc(self.sem[ek], 1)
        tag = (ek, self.cnt[ek])
        for b in reads:
            b.rs.append(tag)
        for b in writes:
            b.w = tag
            b.rs = []
        self.ninstr += 1
        return ins

    def _dsem(self, b):
        if b.dsem is None:
            if self.free_sems:
                b.dsem, b.dcnt = self.free_sems.pop()
            else:
                b.dsem = self.stack.enter_context(self.nc.semaphore("d%d" % self.nsem))
                b.dcnt = 0
                self.nsem += 1
            self.live.append(b)

    def load(self, qk, out_ap, in_ap, buf):
        self._deps(qk, (), (buf,), False)
        self._dsem(buf)
        ins = self.eng[qk].dma_start(out=out_ap, in_=in_ap)
        buf.dcnt += 16
        ins.then_inc(buf.dsem, 16)
        buf.w = ("dma", buf.dsem, buf.dcnt)
        buf.rs = []
        self.ninstr += 1

    def store(self, qk, out_ap, in_ap, buf):
        self._deps(qk, (buf,), (), False)
        self._dsem(buf)
        ins = self.eng[qk].dma_start(out=out_ap, in_=in_ap)
        buf.dcnt += 16
        ins.then_inc(buf.dsem, 16)
        buf.rs.append(("dma", buf.dsem, buf.dcnt))
        self.ninstr += 1

    def barrier(self):
        for ek in self.eng:
            for pk in self.eng:
                if pk != ek and self.cnt[pk] > 0:
                    self._wait(ek, (pk, self.cnt[pk]))
            for b in self.live:
                self._wait(ek, ("dma", b.dsem, b.dcnt))
        for b in self.live:
            self.free_sems.append((b.dsem, b.dcnt))
            b.dsem = None
        self.live = []


class T:
    def __init__(self, nc, stack, name, shape, dtype, psum=False):
        alloc = nc.psum_tensor if psum else nc.sbuf_tensor
        self.t = stack.enter_context(alloc(name, shape, dtype))
        self.b = Buf(name)

    def __getitem__(self, idx):
        return self.t[idx]


def _split3(v):
    v = np.asarray(v, np.float64)
    hi = v.astype(NPBF)
    r = v - hi.astype(np.float64)
    mid = r.astype(NPBF)
    r = r - mid.astype(np.float64)
    lo = r.astype(NPBF)
    return hi, mid, lo


def make_consts(role=0):
    locs = [sum(role_heads(role, m), []) for m in range(4)]
    c = {}
    c["ident"] = np.eye(128, dtype=np.float32).astype(NPBF)
    iq = np.arange(512, dtype=np.float64)
    p = np.arange(128, dtype=np.float64)
    aug = np.zeros((16, 3, 512), NPBF)
    for m in range(4):
        scale = 32 ** -0.5 if m == 2 else 64 ** -0.5
        for j in range(4):
            sl = SLOPES[4 * locs[m][j] + m]
            hi, mid, lo = _split3(-sl * iq / scale)
            aug[4 * m + j, 0], aug[4 * m + j, 1], aug[4 * m + j, 2] = hi, mid, lo
    c["aug"] = aug
    kb = np.zeros((128, 16, 64), np.float32)
    r = np.arange(-60, 4, dtype=np.float64)
    for m in range(4):
        for j in range(4):
            sl = SLOPES[4 * locs[m][j] + m]
            kb[:, 4 * m + j, :] = (sl * (p[:, None] + 128.0 * r[None, :])).astype(np.float32)
    c["kb"] = kb
    kbc = np.zeros((128, 4, 16, 4), np.float32)
    for j in range(4):
        sl = SLOPES[4 * locs[1][j] + 1]
        for cc in range(16):
            for kt in range(4):
                kbc[:, j, cc, kt] = (sl * (16.0 * (128 * kt + p) + 31.0 - 512.0 * cc)).astype(np.float32)
    c["kbc"] = kbc
    P = p[:, None, None]
    Q = iq[None, None, :]
    k4 = np.arange(4, dtype=np.float64)[None, :, None]
    c["caus"] = np.where(Q >= 128 * k4 + P, 0.0, NEG).astype(np.float32).astype(NPBF)
    r8 = (np.arange(8, dtype=np.float64) - 4)[None, :, None]
    dd = Q - 128 * r8 - P
    c["wmask"] = np.where((dd >= 0) & (dd < 512), 0.0, NEG).astype(np.float32).astype(NPBF)
    r5 = (np.arange(5, dtype=np.float64) - 1)[None, :, None]
    dd = Q - 128 * r5 - P
    c["swamask"] = np.where((dd >= 0) & (dd < 128), 0.0, NEG).astype(np.float32).astype(NPBF)
    d5 = (512.0 * np.arange(5, dtype=np.float64))[None, :, None]
    c["cmask"] = np.where(16 * P + 31 <= d5 + Q, 0.0, NEG).astype(np.float32).astype(NPBF)
    j = np.arange(S_LEN)
    c["em"] = (j[None, :] // 256 == np.arange(32)[:, None]).astype(np.float32).astype(NPBF)
    c["es"] = (j[None, :] // 64 == np.arange(128)[:, None]).astype(np.float32).astype(NPBF)
    ncmp, nsel = 511, 128
    cs = np.arange(512)[:, None] * 16
    bs = np.arange(nsel)[None, :] * 64
    ov = np.clip(np.minimum(cs + 32, bs + 64) - np.maximum(cs, bs), 0, None) / 32.0
    ov[ncmp:, :] = 0.0
    c["ov"] = ov.reshape(4, 128, 128).transpose(1, 0, 2).astype(np.float32).astype(NPBF)
    n32 = np.arange(32, dtype=np.float32)
    qi = np.arange(4)
    c["bi"] = np.broadcast_to((n32[None, None, :] - (qi // 2)[None, :, None].astype(np.float32)), (128, 4, 32)).astype(np.float32).copy()
    s128 = np.arange(128)[None, None, None, :]
    cc = np.arange(16)[:, None, None, None]
    pp = np.arange(128)[None, :, None, None]
    qq = np.arange(4)[None, None, :, None]
    qblk = 8 * cc + 2 * qq + (pp >= 64)
    forced = (s128 == 0) | (s128 == qblk) | (s128 == qblk - 1)
    valid = s128 <= qblk
    c["f1e4"] = np.where(forced, 1e4, 0.0).astype(np.float32)
    c["negv"] = np.where(valid, 0.0, -1e30).astype(np.float32)
    c["ones3"] = np.ones((3, 4 * S_LEN), np.float32).astype(NPBF)
    return c


CONST_SPECS = [("ident", [128, 128], BF16), ("aug", [16, 3, 512], BF16), ("kb", [128, 16, 64], F32),
               ("kbc", [128, 4, 16, 4], F32), ("caus", [128, 4, 512], BF16), ("wmask", [128, 8, 512], BF16),
               ("swamask", [128, 5, 512], BF16), ("cmask", [128, 5, 512], BF16), ("em", [32, S_LEN], BF16),
               ("es", [128, S_LEN], BF16), ("ov", [128, 4, 128], BF16), ("bi", [128, 4, 32], F32),
               ("f1e4", [16, 128, 4, 128], F32), ("negv", [16, 128, 4, 128], F32), ("ones3", [3, 4 * S_LEN], BF16)]

PARAM_SPECS = [("wf", [DEPTH, 128, 8, NF]), ("wv", [DEPTH, 128, 8, NV]), ("wo", [DEPTH, 128, 8, D]),
               ("wg", [DEPTH, NCF, 128, 8, 128]), ("wu", [DEPTH, NCF, 128, 8, 128]), ("wd", [DEPTH, NCF, 128, D]),
               ("g_apre", [DEPTH, D]), ("g_apost", [DEPTH, D]), ("g_fpre", [DEPTH, D]), ("g_fpost", [DEPTH, D]),
               ("cw", [DEPTH, 128, NCF, 3]), ("cb", [DEPTH, 128, NCF]),
               ("posk", [DEPTH, 64, 32]), ("w1k", [DEPTH, 64, 32, 128]), ("b1k", [DEPTH, 128, 1]), ("w2k", [DEPTH, 128, 64]),
               ("posv", [DEPTH, 64, 32]), ("w1v", [DEPTH, 64, 32, 128]), ("b1v", [DEPTH, 128, 1]), ("w2v", [DEPTH, 128, 64]),
               ("lq1", [DEPTH, 32]), ("lk1", [DEPTH, 32]), ("lq2", [DEPTH, 32]), ("lk2", [DEPTH, 32]),
               ("subln", [DEPTH, 64]), ("sinks", [DEPTH, 4])]


def layout_params(inp, role=0):
    o = {}
    w_in = inp["w_in"]
    fc, vc = role_cols(role)
    wf = np.zeros((DEPTH, D, NF), np.float32)
    wf[:, :, :len(fc)] = w_in[:, :, fc]
    o["wf"] = wf.reshape(DEPTH, 8, 128, NF).transpose(0, 2, 1, 3)
    o["wv"] = w_in[:, :, vc].reshape(DEPTH, 8, 128, NV).transpose(0, 2, 1, 3)
    rows = np.concatenate([np.arange(256 * m + 64 * h, 256 * m + 64 * (h + 1)) for rr in range(2) for m in range(4)
                           for h in role_heads(rr, m)[0]])
    o["wo"] = inp["w_out"][:, rows, :].reshape(DEPTH, 8, 128, D).transpose(0, 2, 1, 3)
    cfs = slice(NCF * role, NCF * (role + 1))
    o["wg"] = inp["ffn_w_gate"].reshape(DEPTH, 8, 128, 32, 128).transpose(0, 3, 2, 1, 4)[:, cfs]
    o["wu"] = inp["ffn_w_up"].reshape(DEPTH, 8, 128, 32, 128).transpose(0, 3, 2, 1, 4)[:, cfs]
    o["wd"] = inp["ffn_w_down"].reshape(DEPTH, 32, 128, D)[:, cfs]
    o["g_apre"], o["g_apost"] = inp["attn_pre_norm"], inp["attn_post_norm"]
    o["g_fpre"], o["g_fpost"] = inp["ffn_pre_norm"], inp["ffn_post_norm"]
    o["cw"] = inp["ffn_conv_w"].reshape(DEPTH, 3, 32, 128).transpose(0, 3, 2, 1)[:, :, cfs, :]
    o["cb"] = inp["ffn_conv_b"].reshape(DEPTH, 32, 128).transpose(0, 2, 1)[:, :, cfs]
    for sfx in "kv":
        o["pos" + sfx] = inp["nsa_cmp_pos_" + sfx].transpose(0, 2, 1)
        o["w1" + sfx] = inp["nsa_cmp_w1_" + sfx].transpose(0, 2, 1, 3)
        o["b1" + sfx] = inp["nsa_cmp_b1_" + sfx].reshape(DEPTH, 128, 1)
        o["w2" + sfx] = inp["nsa_cmp_w2_" + sfx]
    o["lq1"], o["lk1"] = inp["diff_lambda_q1"], inp["diff_lambda_k1"]
    o["lq2"], o["lk2"] = inp["diff_lambda_q2"], inp["diff_lambda_k2"]
    o["subln"] = inp["diff_subln"]
    o["sinks"] = inp["swa_sinks"][:, sum(role_heads(role, 3), [])]
    return {k: np.ascontiguousarray(v, dtype=np.float32) for k, v in o.items()}


class Prog:
    def __init__(self, phases=("A", "B", "C"), layers=(0, 1), mixers=(0, 1, 2, 3), debug=False, nchunks=NCH, ncores=8):
        self.phases, self.layers, self.mixers, self.debug, self.nch = phases, layers, mixers, debug, nchunks
        self.groups = [[2 * i, 2 * i + 1] for i in range(ncores // 2)]
        nc = bass.Bass("TRN2", target_bir_lowering=False)
        self.nc = nc
        self.x_in = nc.dram_tensor("x", [S_LEN, D], F32, kind="ExternalInput").ap()
        self.y = nc.dram_tensor("y", [S_LEN, D], F32, kind="ExternalOutput").ap()
        self.dc = {n: nc.dram_tensor("c_" + n, shp, dt, kind="ExternalInput").ap() for n, shp, dt in CONST_SPECS}
        self.dp = {n: nc.dram_tensor("p_" + n, shp, F32, kind="ExternalInput").ap() for n, shp in PARAM_SPECS}
        kind = "ExternalOutput" if debug else "Internal"
        self.featT = nc.dram_tensor("featT", [NF, S_LEN], BF16, kind=kind).ap()
        self.vtok = nc.dram_tensor("vtok", [S_LEN, NV], BF16, kind=kind).ap()
        self.mix_t = nc.dram_tensor("mix", [S_LEN, 512], BF16)
        self.mix = self.mix_t.ap()
        self.mixall_t = nc.dram_tensor("mixall", [2 * S_LEN, 512], BF16)
        self.mixall = self.mixall_t.ap()
        self.part = nc.dram_tensor("part", [S_LEN, D], F32).ap()
        self.red = nc.dram_tensor("red", [S_LEN, D], F32).ap()
        self.x1s = nc.dram_tensor("x1s", [S_LEN, D], F32).ap()
        self.xs = nc.dram_tensor("xs", [S_LEN, D], F32).ap()
        self.wgs = nc.dram_tensor("wgs", [NCF, 128, 8, 128], BF16).ap()
        self.wus = nc.dram_tensor("wus", [NCF, 128, 8, 128], BF16).ap()
        self.wds = nc.dram_tensor("wds", [NCF, 128, D], BF16).ap()
        self.cp_i = 0
        with ExitStack() as st:
            self.st = st
            self.S = Sched(nc, st)
            self.cc_sem = st.enter_context(nc.semaphore("cc_sem"))
            self.cc_cnt = 0
            self.build()
            print("built: instr", self.S.ninstr, "sems", self.S.nsem, flush=True)

    def collective(self, kind, op, in_ap, out_ap):
        ins = self.nc.gpsimd.collective_compute(kind, op, replica_groups=self.groups, ins=[in_ap], outs=[out_ap])
        self.cc_cnt += 1
        ins.then_inc(self.cc_sem, 1)
        self.S.ninstr += 1
        return ("dma", self.cc_sem, self.cc_cnt)

    def tile(self, stack, name, shape, dtype, psum=False):
        self.tile_i = getattr(self, "tile_i", 0) + 1
        return T(self.nc, stack, "%s_%d" % (name, self.tile_i), shape, dtype, psum)

    def copy(self, out_ap, in_ap, reads, writes, eng=None):
        nc = self.nc
        if eng is None:
            eng = "act" if (self.cp_i % 2 == 0) else "dve"
            self.cp_i += 1
        if eng == "act":
            self.S.op("act", lambda: nc.scalar.activation(out=out_ap, in_=in_ap, func=AF.Copy), reads=reads, writes=writes)
        elif eng == "dve":
            self.S.op("dve", lambda: nc.vector.tensor_copy(out=out_ap, in_=in_ap), reads=reads, writes=writes)
        else:
            self.S.op("pool", lambda: nc.gpsimd.tensor_copy(out=out_ap, in_=in_ap), reads=reads, writes=writes)

    def load_cast(self, stack, dst, dst_ap_fn, src_ap_fn, n, ncols):
        stg = [self.tile(stack, "stg%d_%d" % (i, self.S.ninstr), [128, ncols], F32) for i in range(2)]
        for i in range(n):
            s = stg[i % 2]
            self.S.load("sp", s[:, :], src_ap_fn(i), s.b)
            self.copy(dst_ap_fn(i), s[:, :], [s.b], [dst.b])

    def build(self):
        nc, S, st = self.nc, self.S, self.st
        self.ident = self.tile(st, "ident", [128, 128], BF16)
        self.caus = self.tile(st, "caus", [128, 4, 512], BF16)
        self.kb = self.tile(st, "kb", [128, 16, 64], F32)
        self.epsT = self.tile(st, "epsT", [128, 1], F32)
        S.load("sp", self.ident[:, :], self.dc["ident"], self.ident.b)
        S.load("sp", self.caus[:, :, :], self.dc["caus"], self.caus.b)
        S.load("sp", self.kb[:, :, :], self.dc["kb"], self.kb.b)
        S.op("dve", lambda: nc.vector.memset(self.epsT[:, :], EPS), writes=[self.epsT.b])
        for l in self.layers:
            x_src = self.x_in if l == 0 else self.xs
            x_dst = self.y if l == self.layers[-1] else self.xs
            if "A" in self.phases:
                self.phase_A(l, x_src)
                S.barrier()
            if "B" in self.phases:
                for m in self.mixers:
                    [self.moba, self.nsa, self.diff, self.swa][m](l)
                    S.barrier()
            if "C" in self.phases:
                tags = []
                for k in range((self.nch * 512 + 2047) // 2048):
                    tags.append(self.collective("AllGather", ALU.bypass, self.mix[k * 2048:(k + 1) * 2048, :].opt(),
                                                self.mixall[k * 4096:(k + 1) * 4096, :].opt()))
                self.prep_ffn(l)
                for ek in S.eng:
                    S._wait(ek, tags[-1])
                S.barrier()
                self.phase_C(l, x_src, x_dst)
                S.barrier()
        S.barrier()
        if self.debug:
            nc = self.nc
            dbg = {}
            for nm, src, rows, cols, dt in (("d_mixall", self.mixall, 4096, 512, BF16), ("d_part", self.part, 2048, D, F32),
                                            ("d_red", self.red, 2048, D, F32), ("d_x1s", self.x1s, 2048, D, F32), ("d_mix", self.mix, 2048, 512, BF16)):
                dst = nc.dram_tensor(nm, [rows, cols], dt, kind="ExternalOutput").ap()
                b = Buf(nm)
                S._dsem(b)
                ins = nc.sync.dma_start(out=dst[:, :], in_=src[0:rows, :])
                b.dcnt += 16
                ins.then_inc(b.dsem, 16)
                S._wait("sp", ("dma", b.dsem, b.dcnt))

    def rstd_of(self, x_ap, xbuf, junk, ss, sd, rstd, n=D):
        nc, S = self.nc, self.S
        S.op("dve", lambda: nc.vector.scalar_tensor_tensor(out=junk[:, 0:n], in0=x_ap, scalar=1.0, in1=x_ap,
                                                           op0=ALU.mult, op1=ALU.mult, accum_out=ss[:, 0:1]),
             reads=[xbuf], writes=[junk.b, ss.b])
        S.op("act", lambda: nc.scalar.activation(out=sd[:, 0:1], in_=ss[:, 0:1], func=AF.Sqrt,
                                                 bias=self.epsT[:, 0:1], scale=1.0 / n),
             reads=[ss.b, self.epsT.b], writes=[sd.b])
        S.op("dve", lambda: nc.vector.reciprocal(out=rstd[:, 0:1], in_=sd[:, 0:1]), reads=[sd.b], writes=[rstd.b])

    def phase_A(self, l, x_src):
        nc, S = self.nc, self.S
        with ExitStack() as ph:
            WF = self.tile(ph, "WF", [128, 8, NF], BF16)
            WV = self.tile(ph, "WV", [128, 8, NV], BF16)
            gain = self.tile(ph, "gainA", [128, D], F32)
            with ExitStack() as tmp:
                self.load_cast(tmp, WF, lambda i: WF[:, i, :], lambda i: self.dp["wf"][l, :, i, :], 8, NF)
                self.load_cast(tmp, WV, lambda i: WV[:, i, :], lambda i: self.dp["wv"][l, :, i, :], 8, NV)
                S.barrier()
            S.load("sp", gain[:, :], self.dp["g_apre"][l].partition_broadcast(128), gain.b)
            xts = [self.tile(ph, "xtA%d" % i, [128, D], F32) for i in range(3)]
            junk = self.tile(ph, "junkA", [128, D], F32)
            ss = self.tile(ph, "ssA", [128, 1], F32)
            sd = self.tile(ph, "sdA", [128, 1], F32)
            rstd = self.tile(ph, "rstdA", [128, 1], F32)
            hb = [self.tile(ph, "hbA%d" % i, [128, D], BF16) for i in range(2)]
            hT = [self.tile(ph, "hTA%d" % i, [128, 8, 512], BF16) for i in range(2)]
            tp = [self.tile(ph, "tpA%d" % i, [128, D], BF16, psum=True) for i in range(2)]
            psF = [self.tile(ph, "psF%d" % i, [128, 512], F32, psum=True) for i in range(2)]
            psV0 = self.tile(ph, "psV0", [128, 512], F32, psum=True)
            fst = [self.tile(ph, "fst%d" % i, [128, 512], BF16) for i in range(3)]
            vst = [self.tile(ph, "vst%d" % i, [128, NV], BF16) for i in range(2)]
            it = 0
            for c in range(self.nch):
                hTc = hT[c % 2]
                for ti in range(4):
                    tok = (4 * c + ti) * 128
                    xt = xts[it % 3]
                    h = hb[it % 2]
                    tpp = tp[it % 2]
                    it += 1
                    S.load("sp", xt[:, :], x_src[tok:tok + 128, :], xt.b)
                    self.rstd_of(xt[:, :], xt.b, junk, ss, sd, rstd)
                    S.op("dve", lambda: nc.vector.scalar_tensor_tensor(out=h[:, :], in0=xt[:, :], scalar=rstd[:, 0:1],
                                                                       in1=gain[:, :], op0=ALU.mult, op1=ALU.mult),
                         reads=[xt.b, rstd.b, gain.b], writes=[h.b])
                    for kc in range(8):
                        S.op("pe", lambda: nc.tensor.transpose(out=tpp[:, kc * 128:(kc + 1) * 128],
                                                               in_=h[:, kc * 128:(kc + 1) * 128], identity=self.ident[:, :]),
                             reads=[h.b, self.ident.b], writes=[tpp.b], accum=(kc > 0))
                    self.copy(hTc[:, :, ti * 128:(ti + 1) * 128], tpp[:, :].rearrange("p (k t) -> p k t", k=8),
                              [tpp.b], [hTc.b])
                for g in range(NF // 128):
                    ps = psF[g % 2]
                    for kc in range(8):
                        S.op("pe", lambda: nc.tensor.matmul(ps[:, :], lhsT=WF[:, kc, g * 128:(g + 1) * 128], rhs=hTc[:, kc, :],
                                                            start=(kc == 0), stop=(kc == 7)),
                             reads=[WF.b, hTc.b], writes=[ps.b], accum=(kc > 0))
                    f = fst[g % 3]
                    self.copy(f[:, :], ps[:, :], [ps.b], [f.b])
                    S.store("pool", self.featT[g * 128:(g + 1) * 128, c * 512:(c + 1) * 512], f[:, :], f.b)
                for ti in range(4):
                    tok = (4 * c + ti) * 128
                    for kc in range(8):
                        S.op("pe", lambda: nc.tensor.matmul(psV0[:, 0:NV], lhsT=hTc[:, kc, ti * 128:(ti + 1) * 128], rhs=WV[:, kc, 0:NV],
                                                            start=(kc == 0), stop=(kc == 7)),
                             reads=[WV.b, hTc.b], writes=[psV0.b], accum=(kc > 0))
                    v = vst[ti % 2]
                    self.copy(v[:, 0:NV], psV0[:, 0:NV], [psV0.b], [v.b])
                    S.store("pool", self.vtok[tok:tok + 128, :], v[:, :], v.b)
            S.barrier()

    def attn_res(self, ph, nps=3, npt=3, no=3):
        R = type("R", (), {})()
        R.ps = [self.tile(ph, "ps_s%d" % i, [128, 512], F32, psum=True) for i in range(nps)]
        R.pt = [self.tile(ph, "pT%d" % i, [128, 512], BF16) for i in range(npt)]
        R.O = [self.tile(ph, "O%d" % i, [128, 512], F32, psum=True) for i in range(no)]
        R.ips = R.ipt = R.io = 0
        return R

    def attend(self, R, q_ap, qbufs, tiles, O, scale):
        nc, S = self.nc, self.S
        Ov = O[:, 0:260].rearrange("p (a b) -> p a b", a=4)
        n = len(tiles)

        def pv(pt, tl, first, last):
            q0 = tl.get("q0", 0)
            assert not (first and q0)
            for qi in range(q0 // 128, 4):
                S.op("pe", lambda: nc.tensor.matmul(Ov[:, qi, :], lhsT=pt[:, qi * 128:(qi + 1) * 128], rhs=tl["v"],
                                                    start=(first and qi == 0), stop=last),
                     reads=[pt.b] + tl["kbufs"], writes=[O.b], accum=not (first and qi == 0))
            if tl.get("post") is not None:
                tl["post"](pt, first, last)

        prev = None
        for i, tl in enumerate(tiles):
            ps = R.ps[R.ips % len(R.ps)]
            R.ips += 1
            q0 = tl.get("q0", 0)
            ex = tl.get("extra", [])
            S.op("pe", lambda: nc.tensor.matmul(ps[:, q0:512], lhsT=tl["kT"], rhs=q_ap[:, q0:512], start=True, stop=(len(ex) == 0)),
                 reads=qbufs + tl["kbufs"], writes=[ps.b])
            for j, (lh, rh, bufs) in enumerate(ex):
                S.op("pe", lambda: nc.tensor.matmul(ps[:, q0:512], lhsT=lh, rhs=rh[:, q0:512], start=False, stop=(j == len(ex) - 1)),
                     reads=bufs, writes=[ps.b], accum=True)
            pt = R.pt[R.ipt % len(R.pt)]
            R.ipt += 1
            S.op("act", lambda: nc.scalar.activation(out=pt[:, q0:512], in_=ps[:, q0:512], func=AF.Exp, bias=tl["bias"], scale=scale),
                 reads=[ps.b] + tl["bbufs"], writes=[pt.b])
            if prev is not None:
                pv(prev[0], prev[1], prev[2] == 0, False)
            prev = (pt, tl, i)
        pv(prev[0], prev[1], prev[2] == 0, True)
        return Ov

    def load_kv(self, KT, V, krows, nh_k, k_row0, v_col0, nh_v, kd=64):
        nc, S = self.nc, self.S
        S.op("pool", lambda: nc.gpsimd.memset(V[:, :, :, 64:65], 1.0), writes=[V.b])
        for h in range(nh_k):
            S.load("sp", KT[0:kd, h, :], self.featT[k_row0 + h * kd:k_row0 + (h + 1) * kd, :], KT.b)
        S.load("sp", KT[kd:kd + 3, :, :], self.dc["ones3"][:, 0:nh_k * S_LEN].rearrange("r (h s) -> r h s", h=nh_k), KT.b)
        for h in range(nh_v):
            S.load("sp", V[:, h, :, 0:64],
                   self.vtok[:, v_col0 + h * 64:v_col0 + (h + 1) * 64].rearrange("(kt p) d -> p kt d", p=128), V.b)

    def recip_den(self, Ov, O, den, add_ap=None, add_bufs=()):
        nc, S = self.nc, self.S
        if add_ap is None:
            S.op("dve", lambda: nc.vector.tensor_scalar(out=den[:, 0:4], in0=Ov[:, :, 64], scalar1=1e-30, scalar2=None,
                                                        op0=ALU.max), reads=[O.b], writes=[den.b])
        else:
            S.op("dve", lambda: nc.vector.tensor_scalar(out=den[:, 0:4], in0=Ov[:, :, 64], scalar1=add_ap, scalar2=1e-30,
                                                        op0=ALU.add, op1=ALU.max), reads=[O.b] + list(add_bufs), writes=[den.b])
        S.op("dve", lambda: nc.vector.reciprocal(out=den[:, 0:4], in_=den[:, 0:4]), reads=[den.b], writes=[den.b])

    def moba(self, l):
        nc, S = self.nc, self.S
        with ExitStack() as ph:
            KT = self.tile(ph, "KTm", [128, 2, S_LEN], BF16)
            V = self.tile(ph, "Vm", [128, 2, NT, 65], BF16)
            BI = self.tile(ph, "BIm", [128, 4, 32], F32)
            S.load("sp", BI[:, :, :], self.dc["bi"], BI.b)
            S.op("pool", lambda: nc.gpsimd.memset(V[:, :, :, 64:65], 1.0), writes=[V.b])
            S.op("pool", lambda: nc.gpsimd.memset(KT[64:128, :, :], 0.0), writes=[KT.b])
            for h in range(2):
                S.load("sp", KT[96:128, h, :], self.dc["em"], KT.b)
                S.load("sp", KT[0:64, h, :], self.featT[FRr["mk"] + 64 * h:FRr["mk"] + 64 * (h + 1), :], KT.b)
                S.load("sp", V[:, h, :, 0:64],
                       self.vtok[:, VCr["mv"] + h * 64:VCr["mv"] + (h + 1) * 64].rearrange("(kt p) d -> p kt d", p=128), V.b)
            S.load("sp", KT[64:67, :, :], self.dc["ones3"][:, 0:2 * S_LEN].rearrange("r (h s) -> r h s", h=2), KT.b)
            R = self.attn_res(ph)
            QT = [[self.tile(ph, "QTm%d_%d" % (i, h), [128, 512], BF16) for h in range(2)] for i in range(2)]
            for qs in QT:
                for h in range(2):
                    S.op("pool", lambda: nc.gpsimd.memset(qs[h][64:128, :], 0.0), writes=[qs[h].b])
                    S.load("sp", qs[h][64:67, :], self.dc["aug"][h], qs[h].b)
            kms = self.tile(ph, "kms", [128, 2, 32], F32)
            kmh = self.tile(ph, "kmh", [128, 2, 32], BF16)
            kml = self.tile(ph, "kml", [128, 2, 32], BF16)
            kmr = self.tile(ph, "kmr", [128, 2, 32], F32)
            for h in range(2):
                S.op("dve", lambda: nc.vector.tensor_reduce(out=kms[0:64, h, :], in_=KT[0:64, h, :].rearrange("p (n k) -> p n k", k=256),
                                                            axis=AX.X, op=ALU.add), reads=[KT.b], writes=[kms.b])
            S.op("dve", lambda: nc.vector.tensor_scalar(out=kms[0:64, :, :], in0=kms[0:64, :, :], scalar1=1.0 / 256, scalar2=None, op0=ALU.mult),
                 reads=[kms.b], writes=[kms.b])
            S.op("dve", lambda: nc.vector.tensor_copy(out=kmh[0:64, :, :], in_=kms[0:64, :, :]), reads=[kms.b], writes=[kmh.b])
            S.op("dve", lambda: nc.vector.tensor_tensor(out=kmr[0:64, :, :], in0=kms[0:64, :, :], in1=kmh[0:64, :, :], op=ALU.subtract),
                 reads=[kms.b, kmh.b], writes=[kmr.b])
            S.op("dve", lambda: nc.vector.tensor_copy(out=kml[0:64, :, :], in_=kmr[0:64, :, :]), reads=[kmr.b], writes=[kml.b])
            gps = self.tile(ph, "gps", [128, 512], F32, psum=True)
            tps = self.tile(ph, "tpsm", [128, 1024], BF16, psum=True)
            past = self.tile(ph, "past", [128, 4, 32], F32)
            own = self.tile(ph, "own", [128, 4, 32], F32)
            negp = self.tile(ph, "negp", [128, 4, 32], F32)
            gm = self.tile(ph, "gm", [128, 4, 32], F32)
            m8 = self.tile(ph, "m8", [128, 4, 8], F32)
            sel = self.tile(ph, "sel", [128, 4, 32], F32)
            nsel = self.tile(ph, "nsel", [128, 4, 32], BF16)
            den = self.tile(ph, "denm", [128, 4], F32)
            mst = [self.tile(ph, "mstm%d" % i, [128, 4, 128], BF16) for i in range(2)]
            for c in range(self.nch):
                qs = QT[c % 2]
                for h in range(2):
                    S.load("sp", qs[h][0:64, :], self.featT[FRr["mq"] + 64 * h:FRr["mq"] + 64 * (h + 1), c * 512:(c + 1) * 512], qs[h].b)
                S.op("dve", lambda: nc.vector.tensor_scalar(out=past[:, :, :], in0=BI[:, :, :], scalar1=float(2 * c), scalar2=None, op0=ALU.is_lt),
                     reads=[BI.b], writes=[past.b])
                S.op("dve", lambda: nc.vector.tensor_scalar(out=own[:, :, :], in0=BI[:, :, :], scalar1=float(2 * c), scalar2=None, op0=ALU.is_equal),
                     reads=[BI.b], writes=[own.b])
                S.op("dve", lambda: nc.vector.tensor_scalar(out=negp[:, :, :], in0=past[:, :, :], scalar1=-1.0, scalar2=1e30, op0=ALU.add, op1=ALU.mult),
                     reads=[past.b], writes=[negp.b])
                ms = mst[c % 2]
                for h in range(2):
                    q = qs[h]
                    gv = gps[:, 0:128].rearrange("p (a b) -> p a b", a=4)
                    for qi in range(4):
                        S.op("pe", lambda: nc.tensor.matmul(gv[:, qi, :], lhsT=q[0:64, qi * 128:(qi + 1) * 128], rhs=kmh[0:64, h, :], start=True, stop=False),
                             reads=[q.b, kmh.b], writes=[gps.b], accum=(qi > 0))
                        S.op("pe", lambda: nc.tensor.matmul(gv[:, qi, :], lhsT=q[0:64, qi * 128:(qi + 1) * 128], rhs=kml[0:64, h, :], start=False, stop=True),
                             reads=[q.b, kml.b], writes=[gps.b], accum=True)
                    S.op("dve", lambda: nc.vector.tensor_tensor(out=gm[:, :, :], in0=gv, in1=negp[:, :, :], op=ALU.add),
                         reads=[gps.b, negp.b], writes=[gm.b])
                    for qi in range(4):
                        S.op("dve", lambda: nc.vector.max(out=m8[:, qi, :], in_=gm[:, qi, :]), reads=[gm.b], writes=[m8.b])
                    for qi in range(4):
                        S.op("dve", lambda: nc.vector.tensor_scalar(out=sel[:, qi, :], in0=gm[:, qi, :], scalar1=m8[:, qi, 2:3], scalar2=None, op0=ALU.is_ge),
                             reads=[gm.b, m8.b], writes=[sel.b])
                    S.op("dve", lambda: nc.vector.tensor_tensor(out=sel[:, :, :], in0=sel[:, :, :], in1=past[:, :, :], op=ALU.mult),
                         reads=[sel.b, past.b], writes=[sel.b])
                    S.op("dve", lambda: nc.vector.tensor_tensor(out=sel[:, :, :], in0=sel[:, :, :], in1=own[:, :, :], op=ALU.add),
                         reads=[sel.b, own.b], writes=[sel.b])
                    S.op("dve", lambda: nc.vector.tensor_scalar(out=nsel[:, :, :], in0=sel[:, :, :], scalar1=-1.0, scalar2=-NEG, op0=ALU.add, op1=ALU.mult),
                         reads=[sel.b], writes=[nsel.b])
                    for qi in range(4):
                        S.op("pe", lambda: nc.tensor.transpose(out=tps[0:32, qi * 128:(qi + 1) * 128], in_=nsel[:, qi, :], identity=self.ident[:, :]),
                             reads=[nsel.b, self.ident.b], writes=[tps.b], accum=(qi > 0))
                    self.copy(q[96:128, :], tps[0:32, 0:512], [tps.b], [q.b], eng="dve")
                    tiles = []
                    for kt in range(4 * c + 4):
                        if far_skip(0, h, 512 * c - (128 * kt + 127)):
                            continue
                        r = kt - 4 * c
                        ex = []
                        if r >= 0:
                            ex.append((self.ident[:, :], self.caus[:, r, :], [self.ident.b, self.caus.b]))
                        tiles.append(dict(kT=KT[0:128, h, kt * 128:(kt + 1) * 128], v=V[:, h, kt, :], kbufs=[KT.b, V.b],
                                          bias=self.kb[:, 0 + h, r + 60:r + 61], bbufs=[self.kb.b], extra=ex, q0=128 * max(r, 0)))
                    O = R.O[R.io % len(R.O)]
                    R.io += 1
                    Ov = self.attend(R, q[0:128, :], [q.b], tiles, O, 0.125)
                    self.recip_den(Ov, O, den)
                    for qi in range(4):
                        S.op("dve", lambda: nc.vector.tensor_scalar(out=ms[:, qi, h * 64:(h + 1) * 64], in0=Ov[:, qi, 0:64], scalar1=den[:, qi:qi + 1],
                                                                    scalar2=None, op0=ALU.mult), reads=[O.b, den.b], writes=[ms.b])
                S.store("pool", self.mix[c * 512:(c + 1) * 512, 0:128].rearrange("(a p) d -> p a d", p=128), ms[:, :, :], ms.b)
            S.barrier()

    def swa(self, l):
        nc, S = self.nc, self.S
        with ExitStack() as ph:
            KT = self.tile(ph, "KTs", [128, 1, S_LEN], BF16)
            V = self.tile(ph, "Vs", [128, 1, NT, 65], BF16)
            SM = self.tile(ph, "SMs", [128, 5, 512], BF16)
            S.load("sp", SM[:, :, :], self.dc["swamask"], SM.b)
            self.load_kv(KT, V, 67, 1, FRr["sk"], VCr["sv"], 1)
            sk = self.tile(ph, "sinks", [128, 4], F32)
            esk = self.tile(ph, "esinks", [128, 4], F32)
            S.load("sp", sk[:, :], self.dp["sinks"][l].partition_broadcast(128), sk.b)
            S.op("act", lambda: nc.scalar.activation(out=esk[:, :], in_=sk[:, :], func=AF.Exp), reads=[sk.b], writes=[esk.b])
            R = self.attn_res(ph)
            QT = [self.tile(ph, "QTs%d" % i, [128, 2, 512], BF16) for i in range(2)]
            for q in QT:
                S.load("sp", q[64:67, :, :], self.dc["aug"][12:14].rearrange("h r q -> r h q"), q.b)
            den = self.tile(ph, "dens", [128, 4], F32)
            mst = [self.tile(ph, "msts%d" % i, [128, 4, 128], BF16) for i in range(2)]
            for c in range(self.nch):
                q = QT[c % 2]
                for h in range(2):
                    S.load("sp", q[0:64, h, :], self.featT[FRr["sq"] + 64 * h:FRr["sq"] + 64 * (h + 1), c * 512:(c + 1) * 512], q.b)
                ms = mst[c % 2]
                for h in range(2):
                    g = 0
                    tiles = []
                    for r in range(-1, 4):
                        kt = 4 * c + r
                        if kt < 0:
                            continue
                        tiles.append(dict(kT=KT[0:67, g, kt * 128:(kt + 1) * 128], v=V[:, g, kt, :], kbufs=[KT.b, V.b],
                                          bias=self.kb[:, 12 + h, r + 60:r + 61], bbufs=[self.kb.b],
                                          extra=[(self.ident[:, :], SM[:, r + 1, :], [self.ident.b, SM.b])]))
                    O = R.O[R.io % len(R.O)]
                    R.io += 1
                    Ov = self.attend(R, q[0:67, h, :], [q.b], tiles, O, 0.125)
                    self.recip_den(Ov, O, den, add_ap=esk[:, h:h + 1], add_bufs=[esk.b])
                    for qi in range(4):
                        S.op("dve", lambda: nc.vector.tensor_scalar(out=ms[:, qi, h * 64:(h + 1) * 64], in0=Ov[:, qi, 0:64], scalar1=den[:, qi:qi + 1],
                                                                    scalar2=None, op0=ALU.mult), reads=[O.b, den.b], writes=[ms.b])
                S.store("pool", self.mix[c * 512:(c + 1) * 512, 384:512].rearrange("(a p) d -> p a d", p=128), ms[:, :, :], ms.b)
            S.barrier()

    def diff(self, l):
        nc, S = self.nc, self.S
        lambda_init = 0.8 - 0.6 * math.exp(-0.3 * l)
        sc = 32 ** -0.5
        with ExitStack() as ph:
            KT = self.tile(ph, "KTd", [128, 2, S_LEN], BF16)
            V = self.tile(ph, "Vd", [128, 2, NT, 65], BF16)
            S.op("pool", lambda: nc.gpsimd.memset(V[:, :, :, 64:65], 1.0), writes=[V.b])
            for h in range(2):
                r0 = FRr["dk"] + 64 * h
                S.load("sp", KT[0:32, h, :], self.featT[r0:r0 + 32, :], KT.b)
                S.load("sp", KT[64:96, h, :], self.featT[r0 + 32:r0 + 64, :], KT.b)
                S.load("sp", V[:, h, :, 0:64], self.vtok[:, VCr["dv"] + h * 64:VCr["dv"] + (h + 1) * 64].rearrange("(kt p) d -> p kt d", p=128), V.b)
            ones = self.dc["ones3"][:, 0:2 * S_LEN].rearrange("r (h s) -> r h s", h=2)
            S.load("sp", KT[32:35, :, :], ones, KT.b)
            S.load("sp", KT[96:99, :, :], ones, KT.b)
            lam = self.tile(ph, "lam", [128, 4], F32)
            lt = self.tile(ph, "lamt", [128, 4, 32], F32)
            lj = self.tile(ph, "lamj", [128, 32], F32)
            for i, nme in enumerate(["lq1", "lk1", "lq2", "lk2"]):
                S.load("sp", lt[:, i, :], self.dp[nme][l].partition_broadcast(128), lt.b)
            for i in range(2):
                S.op("dve", lambda: nc.vector.scalar_tensor_tensor(out=lj[:, :], in0=lt[:, 2 * i, :], scalar=1.0, in1=lt[:, 2 * i + 1, :],
                                                                   op0=ALU.mult, op1=ALU.mult, accum_out=lam[:, i:i + 1]),
                     reads=[lt.b], writes=[lj.b, lam.b])
            S.op("act", lambda: nc.scalar.activation(out=lam[:, 0:2], in_=lam[:, 0:2], func=AF.Exp), reads=[lam.b], writes=[lam.b])
            S.op("dve", lambda: nc.vector.tensor_tensor(out=lam[:, 2:3], in0=lam[:, 1:2], in1=lam[:, 0:1], op=ALU.subtract),
                 reads=[lam.b], writes=[lam.b])
            S.op("dve", lambda: nc.vector.tensor_scalar(out=lam[:, 2:3], in0=lam[:, 2:3], scalar1=-lambda_init, scalar2=None, op0=ALU.add),
                 reads=[lam.b], writes=[lam.b])
            gs = self.tile(ph, "gsub", [128, 64], F32)
            S.load("sp", gs[:, :], self.dp["subln"][l].partition_broadcast(128), gs.b)
            S.op("dve", lambda: nc.vector.tensor_scalar(out=gs[:, :], in0=gs[:, :], scalar1=1.0 - lambda_init, scalar2=None, op0=ALU.mult),
                 reads=[gs.b], writes=[gs.b])
            R = self.attn_res(ph, no=4)
            QT = [self.tile(ph, "QTd%d" % i, [128, 2, 512], BF16) for i in range(2)]
            for q in QT:
                a = self.dc["aug"][8:10].rearrange("h r q -> r h q")
                S.load("sp", q[32:35, :, :], a, q.b)
                S.load("sp", q[96:99, :, :], a, q.b)
            den1 = self.tile(ph, "den1", [128, 4], F32)
            den2 = self.tile(ph, "den2", [128, 4], F32)
            o1 = self.tile(ph, "o1d", [128, 4, 64], F32)
            od = self.tile(ph, "od", [128, 4, 64], F32)
            jk = self.tile(ph, "jkd", [128, 64], F32)
            ssd = self.tile(ph, "ssd", [128, 4], F32)
            mst = [self.tile(ph, "mstd%d" % i, [128, 4, 128], BF16) for i in range(2)]
            for c in range(self.nch):
                q = QT[c % 2]
                for h in range(2):
                    r0 = FRr["dq"] + 64 * h
                    S.load("sp", q[0:32, h, :], self.featT[r0:r0 + 32, c * 512:(c + 1) * 512], q.b)
                    S.load("sp", q[64:96, h, :], self.featT[r0 + 32:r0 + 64, c * 512:(c + 1) * 512], q.b)
                ms = mst[c % 2]
                for h in range(2):
                    Os = []
                    for j in range(2):
                        b0 = 64 * j
                        tiles = []
                        for kt in range(4 * c + 4):
                            if far_skip(2, h, 512 * c - (128 * kt + 127)):
                                continue
                            r = kt - 4 * c
                            ex = []
                            if r >= 0:
                                ex.append((self.ident[:, :], self.caus[:, r, :], [self.ident.b, self.caus.b]))
                            tiles.append(dict(kT=KT[b0:b0 + 35, h, kt * 128:(kt + 1) * 128], v=V[:, h, kt, :], kbufs=[KT.b, V.b],
                                              bias=self.kb[:, 8 + h, r + 60:r + 61], bbufs=[self.kb.b], extra=ex, q0=128 * max(r, 0)))
                        O = R.O[R.io % len(R.O)]
                        R.io += 1
                        Ov = self.attend(R, q[b0:b0 + 35, h, :], [q.b], tiles, O, sc)
                        Os.append((O, Ov))
                    (O1, Ov1), (O2, Ov2) = Os
                    self.recip_den(Ov1, O1, den1)
                    self.recip_den(Ov2, O2, den2)
                    S.op("dve", lambda: nc.vector.tensor_scalar(out=den2[:, :], in0=den2[:, :], scalar1=lam[:, 2:3], scalar2=None, op0=ALU.mult),
                         reads=[den2.b, lam.b], writes=[den2.b])
                    for qi in range(4):
                        S.op("dve", lambda: nc.vector.tensor_scalar(out=o1[:, qi, :], in0=Ov1[:, qi, 0:64], scalar1=den1[:, qi:qi + 1], scalar2=None, op0=ALU.mult),
                             reads=[O1.b, den1.b], writes=[o1.b])
                        S.op("dve", lambda: nc.vector.scalar_tensor_tensor(out=od[:, qi, :], in0=Ov2[:, qi, 0:64], scalar=den2[:, qi:qi + 1], in1=o1[:, qi, :],
                                                                           op0=ALU.mult, op1=ALU.add), reads=[O2.b, den2.b, o1.b], writes=[od.b])
                        S.op("dve", lambda: nc.vector.scalar_tensor_tensor(out=jk[:, :], in0=od[:, qi, :], scalar=1.0, in1=od[:, qi, :],
                                                                           op0=ALU.mult, op1=ALU.mult, accum_out=ssd[:, qi:qi + 1]),
                             reads=[od.b], writes=[jk.b, ssd.b])
                    S.op("act", lambda: nc.scalar.activation(out=ssd[:, :], in_=ssd[:, :], func=AF.Ln, bias=self.epsT[:, 0:1], scale=1.0 / 64),
                         reads=[ssd.b, self.epsT.b], writes=[ssd.b])
                    S.op("act", lambda: nc.scalar.activation(out=ssd[:, :], in_=ssd[:, :], func=AF.Exp, scale=-0.5), reads=[ssd.b], writes=[ssd.b])
                    for qi in range(4):
                        S.op("dve", lambda: nc.vector.scalar_tensor_tensor(out=ms[:, qi, h * 64:(h + 1) * 64], in0=od[:, qi, :], scalar=ssd[:, qi:qi + 1],
                                                                           in1=gs[:, :], op0=ALU.mult, op1=ALU.mult),
                             reads=[od.b, ssd.b, gs.b], writes=[ms.b])
                S.store("pool", self.mix[c * 512:(c + 1) * 512, 256:384].rearrange("(a p) d -> p a d", p=128), ms[:, :, :], ms.b)
            S.barrier()

    def nsa(self, l):
        nc, S = self.nc, self.S
        with ExitStack() as ph:
            KCT = self.tile(ph, "KCT", [128, 512], BF16)
            VCt = self.tile(ph, "VCt", [128, 4, 65], BF16)
            S.op("pool", lambda: nc.gpsimd.memset(VCt[:, :, :], 1.0), writes=[VCt.b])
            S.load("sp", KCT[64:67, :], self.dc["ones3"][:, 0:512], KCT.b)
            with ExitStack() as cs:
                XT = self.tile(cs, "XTc", [64, S_LEN], BF16)
                w1 = self.tile(cs, "w1c", [64, 32, 128], BF16)
                w2 = self.tile(cs, "w2c", [128, 64], BF16)
                pos = self.tile(cs, "posc", [64, 32], BF16)
                b1 = self.tile(cs, "b1c", [128, 1], F32)
                cbias = self.tile(cs, "cbias", [128, 1], F32)
                hid = self.tile(cs, "hidc", [128, 512], BF16)
                sg = self.tile(cs, "sgc", [128, 4096], F32)
                hp = self.tile(cs, "hpc", [128, 512], F32, psum=True)
                cp = self.tile(cs, "cpc", [128, 512], F32, psum=True)
                op_ = self.tile(cs, "opc", [128, 512], F32, psum=True)
                for s_i, sfx in enumerate("kv"):
                    S.load("sp", XT[:, :], self.featT[FRr["nkc" if sfx == "k" else "nvc"]:FRr["nkc" if sfx == "k" else "nvc"] + 64, :], XT.b)
                    S.load("sp", sg[0:64, 0:4096], self.dp["w1" + sfx][l].rearrange("d l f -> d (l f)"), sg.b)
                    self.copy(w1[:, :, :], sg[0:64, 0:4096].rearrange("d (l f) -> d l f", l=32), [sg.b], [w1.b])
                    S.load("sp", sg[:, 0:64], self.dp["w2" + sfx][l], sg.b)
                    self.copy(w2[:, :], sg[:, 0:64], [sg.b], [w2.b])
                    S.load("sp", sg[0:64, 0:32], self.dp["pos" + sfx][l], sg.b)
                    self.copy(pos[:, :], sg[0:64, 0:32], [sg.b], [pos.b])
                    S.load("sp", b1[:, :], self.dp["b1" + sfx][l], b1.b)
                    for li in range(32):
                        S.op("pe", lambda: nc.tensor.matmul(hp[:, 0:511], lhsT=w1[:, li, :], rhs=XT[:, li:li + 16 * 510 + 1:16], start=(li == 0), stop=(li == 31)),
                             reads=[w1.b, XT.b], writes=[hp.b], accum=(li > 0))
                    for li in range(32):
                        S.op("pe", lambda: nc.tensor.matmul(cp[:, 0:1], lhsT=w1[:, li, :], rhs=pos[:, li:li + 1], start=(li == 0), stop=(li == 31)),
                             reads=[w1.b, pos.b], writes=[cp.b], accum=(li > 0))
                    S.op("dve", lambda: nc.vector.tensor_tensor(out=cbias[:, :], in0=cp[:, 0:1], in1=b1[:, :], op=ALU.add),
                         reads=[cp.b, b1.b], writes=[cbias.b])
                    S.op("pool", lambda: nc.gpsimd.memset(hid[:, :], 0.0), writes=[hid.b])
                    S.op("act", lambda: nc.scalar.activation(out=hid[:, 0:511], in_=hp[:, 0:511], func=AF.Gelu_apprx_tanh, bias=cbias[:, 0:1]),
                         reads=[hp.b, cbias.b], writes=[hid.b])
                    if sfx == "k":
                        S.op("pe", lambda: nc.tensor.matmul(op_[0:64, 0:512], lhsT=w2[:, :], rhs=hid[:, :], start=True, stop=True),
                             reads=[w2.b, hid.b], writes=[op_.b])
                        self.copy(KCT[0:64, :], op_[0:64, 0:512], [op_.b], [KCT.b])
                    else:
                        ov_ = op_[:, 0:256].rearrange("p (a b) -> p a b", a=4)
                        for kt in range(4):
                            S.op("pe", lambda: nc.tensor.matmul(ov_[:, kt, :], lhsT=hid[:, kt * 128:(kt + 1) * 128], rhs=w2[:, :], start=True, stop=True),
                                 reads=[w2.b, hid.b], writes=[op_.b], accum=(kt > 0))
                        self.copy(VCt[:, :, 0:64], ov_, [op_.b], [VCt.b])
                S.barrier()
            KTs = self.tile(ph, "KTsl", [128, 1, S_LEN], BF16)
            KTw = self.tile(ph, "KTwn", [128, 1, S_LEN], BF16)
            Vs = self.tile(ph, "Vsl", [128, 1, NT, 65], BF16)
            Vw = self.tile(ph, "Vwn", [128, 1, NT, 65], BF16)
            self.load_kv(KTs, Vs, 67, 1, FRr["nks"], VCr["nvs"], 1)
            self.load_kv(KTw, Vw, 67, 1, FRr["nkw"], VCr["nvw"], 1)
            ES = self.tile(ph, "ESn", [128, S_LEN], BF16)
            WM = self.tile(ph, "WMn", [128, 8, 512], BF16)
            CM = self.tile(ph, "CMn", [128, 5, 512], BF16)
            OVt = self.tile(ph, "OVn", [128, 4, 128], BF16)
            KBC = self.tile(ph, "KBCn", [128, 4, 16, 4], F32)
            S.load("sp", ES[:, :], self.dc["es"], ES.b)
            S.load("sp", WM[:, :, :], self.dc["wmask"], WM.b)
            S.load("sp", CM[:, :, :], self.dc["cmask"], CM.b)
            S.load("sp", OVt[:, :, :], self.dc["ov"], OVt.b)
            S.load("sp", KBC[:, :, :, :], self.dc["kbc"], KBC.b)
            R = self.attn_res(ph)
            QT = [self.tile(ph, "QTn%d" % i, [128, 4, 512], BF16) for i in range(2)]
            for q in QT:
                S.load("sp", q[64:67, :, :], self.dc["aug"][4:8].rearrange("h r q -> r h q"), q.b)
            imps = self.tile(ph, "imps", [128, 512], F32, psum=True)
            tps = self.tile(ph, "tpsn", [128, 1024], BF16, psum=True)
            impa = self.tile(ph, "impa", [128, 4, 128], F32)
            f1 = [self.tile(ph, "f1e4_%d" % i, [128, 4, 128], F32) for i in range(2)]
            nv = [self.tile(ph, "negv_%d" % i, [128, 4, 128], F32) for i in range(2)]
            gr = [self.tile(ph, "graw%d" % i, [128, 4, 12], BF16) for i in range(2)]
            sgm = self.tile(ph, "sgm", [128, 4, 12], F32)
            scm = self.tile(ph, "scm", [128, 4, 128], F32)
            tmpm = self.tile(ph, "tmpm", [128, 4, 128], F32)
            m8a = self.tile(ph, "m8a", [128, 4, 8], F32)
            m8b = self.tile(ph, "m8b", [128, 4, 8], F32)
            sel = self.tile(ph, "seln", [128, 4, 128], F32)
            val = self.tile(ph, "valn", [128, 4, 128], F32)
            nsel = self.tile(ph, "nseln", [128, 4, 128], BF16)
            nselT = self.tile(ph, "nselTn", [128, 512], BF16)
            den = self.tile(ph, "denn", [128, 4], F32)
            acc = self.tile(ph, "accn", [128, 2, 4, 64], F32)
            mst = [self.tile(ph, "mstn%d" % i, [128, 4, 128], BF16) for i in range(2)]

            def fold(O, Ov, h, gcol, first):
                self.recip_den(Ov, O, den)
                S.op("dve", lambda: nc.vector.tensor_tensor(out=den[:, :], in0=den[:, :], in1=sgm[:, :, gcol], op=ALU.mult),
                     reads=[den.b, sgm.b], writes=[den.b])
                for qi in range(4):
                    if first:
                        S.op("dve", lambda: nc.vector.tensor_scalar(out=acc[:, h, qi, :], in0=Ov[:, qi, 0:64], scalar1=den[:, qi:qi + 1], scalar2=None, op0=ALU.mult),
                             reads=[O.b, den.b], writes=[acc.b])
                    else:
                        S.op("dve", lambda: nc.vector.scalar_tensor_tensor(out=acc[:, h, qi, :], in0=Ov[:, qi, 0:64], scalar=den[:, qi:qi + 1], in1=acc[:, h, qi, :],
                                                                           op0=ALU.mult, op1=ALU.add), reads=[O.b, den.b, acc.b], writes=[acc.b])

            for c in range(self.nch):
                q = QT[c % 2]
                for h in range(4):
                    S.load("sp", q[0:64, h, :], self.featT[FRr["nq"] + 64 * h:FRr["nq"] + 64 * (h + 1), c * 512:(c + 1) * 512], q.b)
                f1c, nvc, grc = f1[c % 2], nv[c % 2], gr[c % 2]
                S.load("sp", f1c[:, :, :], self.dc["f1e4"][c], f1c.b)
                S.load("sp", nvc[:, :, :], self.dc["negv"][c], nvc.b)
                S.load("sp", grc[:, :, :], self.vtok[c * 512:(c + 1) * 512, VCr["ng"]:VCr["ng"] + 12].rearrange("(a p) g -> p a g", p=128), grc.b)
                S.op("act", lambda: nc.scalar.activation(out=sgm[:, :, :], in_=grc[:, :, :], func=AF.Exp, scale=-1.0), reads=[grc.b], writes=[sgm.b])
                S.op("dve", lambda: nc.vector.tensor_scalar(out=sgm[:, :, :], in0=sgm[:, :, :], scalar1=1.0, scalar2=None, op0=ALU.add),
                     reads=[sgm.b], writes=[sgm.b])
                S.op("dve", lambda: nc.vector.reciprocal(out=sgm[:, :, :], in_=sgm[:, :, :]), reads=[sgm.b], writes=[sgm.b])
                ms = mst[c % 2]
                kb_ = c // 4
                iv = imps[:, :].rearrange("p (a b) -> p a b", a=4)
                for h in range(4):
                    tiles = []
                    ntl = kb_ + 1

                    def mkpost(kt, ntl=ntl):
                        def post(pt, first, last):
                            for qi in range(4):
                                S.op("pe", lambda: nc.tensor.matmul(iv[:, qi, :], lhsT=pt[:, qi * 128:(qi + 1) * 128], rhs=OVt[:, kt, :],
                                                                    start=(first and qi == 0), stop=last),
                                     reads=[pt.b, OVt.b], writes=[imps.b], accum=not (first and qi == 0))
                        return post
                    for kt in range(ntl):
                        ex = []
                        if kt == kb_:
                            ex.append((self.ident[:, :], CM[:, c % 4, :], [self.ident.b, CM.b]))
                        elif kt == kb_ - 1 and c % 4 == 0:
                            ex.append((self.ident[:, :], CM[:, 4, :], [self.ident.b, CM.b]))
                        tiles.append(dict(kT=KCT[0:67, kt * 128:(kt + 1) * 128], v=VCt[:, kt, :], kbufs=[KCT.b, VCt.b],
                                          bias=KBC[:, h, c, kt:kt + 1], bbufs=[KBC.b], extra=ex, post=mkpost(kt)))
                    O = R.O[R.io % len(R.O)]
                    R.io += 1
                    Ov = self.attend(R, q[0:67, h, :], [q.b], tiles, O, 0.125)
                    self.recip_den(Ov, O, den)
                    for qi in range(4):
                        if h == 0:
                            S.op("dve", lambda: nc.vector.tensor_scalar(out=impa[:, qi, :], in0=iv[:, qi, :], scalar1=den[:, qi:qi + 1], scalar2=None, op0=ALU.mult),
                                 reads=[imps.b, den.b], writes=[impa.b])
                        else:
                            S.op("dve", lambda: nc.vector.scalar_tensor_tensor(out=impa[:, qi, :], in0=iv[:, qi, :], scalar=den[:, qi:qi + 1], in1=impa[:, qi, :],
                                                                               op0=ALU.mult, op1=ALU.add), reads=[imps.b, den.b, impa.b], writes=[impa.b])
                    if h < 2:
                        fold(O, Ov, h, 3 * h + 0, True)
                S.op("dve", lambda: nc.vector.tensor_tensor(out=scm[:, :, :], in0=impa[:, :, :], in1=f1c[:, :, :], op=ALU.max),
                     reads=[impa.b, f1c.b], writes=[scm.b])
                S.op("dve", lambda: nc.vector.tensor_tensor(out=scm[:, :, :], in0=scm[:, :, :], in1=nvc[:, :, :], op=ALU.add),
                     reads=[scm.b, nvc.b], writes=[scm.b])
                for qi in range(4):
                    S.op("dve", lambda: nc.vector.max(out=m8a[:, qi, :], in_=scm[:, qi, :]), reads=[scm.b], writes=[m8a.b])
                    S.op("dve", lambda: nc.vector.match_replace(out=tmpm[:, qi, :], in_to_replace=m8a[:, qi, :], in_values=scm[:, qi, :], imm_value=-1e30),
                         reads=[scm.b, m8a.b], writes=[tmpm.b])
                    S.op("dve", lambda: nc.vector.max(out=m8b[:, qi, :], in_=tmpm[:, qi, :]), reads=[tmpm.b], writes=[m8b.b])
                    S.op("dve", lambda: nc.vector.tensor_scalar(out=sel[:, qi, :], in0=scm[:, qi, :], scalar1=m8b[:, qi, 7:8], scalar2=None, op0=ALU.is_ge),
                         reads=[scm.b, m8b.b], writes=[sel.b])
                S.op("dve", lambda: nc.vector.tensor_scalar(out=val[:, :, :], in0=nvc[:, :, :], scalar1=-1.0, scalar2=None, op0=ALU.is_ge),
                     reads=[nvc.b], writes=[val.b])
                S.op("dve", lambda: nc.vector.tensor_tensor(out=sel[:, :, :], in0=sel[:, :, :], in1=val[:, :, :], op=ALU.mult),
                     reads=[sel.b, val.b], writes=[sel.b])
                S.op("dve", lambda: nc.vector.tensor_scalar(out=nsel[:, :, :], in0=sel[:, :, :], scalar1=-1.0, scalar2=-NEG, op0=ALU.add, op1=ALU.mult),
                     reads=[sel.b], writes=[nsel.b])
                for qi in range(4):
                    S.op("pe", lambda: nc.tensor.transpose(out=tps[:, qi * 128:(qi + 1) * 128], in_=nsel[:, qi, :], identity=self.ident[:, :]),
                         reads=[nsel.b, self.ident.b], writes=[tps.b], accum=(qi > 0))
                self.copy(nselT[:, :], tps[:, 0:512], [tps.b], [nselT.b], eng="dve")
                for h in range(2):
                    tiles = []
                    for kt in range(4 * c + 4):
                        if far_skip(1, h, 512 * c - (128 * kt + 127)):
                            continue
                        r = kt - 4 * c
                        ex = [(ES[:, kt * 128:(kt + 1) * 128], nselT[:, :], [ES.b, nselT.b])]
                        if r >= 0:
                            ex.append((self.ident[:, :], self.caus[:, r, :], [self.ident.b, self.caus.b]))
                        tiles.append(dict(kT=KTs[0:67, 0, kt * 128:(kt + 1) * 128], v=Vs[:, 0, kt, :], kbufs=[KTs.b, Vs.b],
                                          bias=self.kb[:, 4 + h, r + 60:r + 61], bbufs=[self.kb.b], extra=ex, q0=128 * max(r, 0)))
                    O = R.O[R.io % len(R.O)]
                    R.io += 1
                    Ov = self.attend(R, q[0:67, h, :], [q.b], tiles, O, 0.125)
                    fold(O, Ov, h, 3 * h + 1, False)
                    tiles = []
                    for r in range(-4, 4):
                        kt = 4 * c + r
                        if kt < 0:
                            continue
                        tiles.append(dict(kT=KTw[0:67, 0, kt * 128:(kt + 1) * 128], v=Vw[:, 0, kt, :], kbufs=[KTw.b, Vw.b],
                                          bias=self.kb[:, 4 + h, r + 60:r + 61], bbufs=[self.kb.b],
                                          extra=[(self.ident[:, :], WM[:, r + 4, :], [self.ident.b, WM.b])]))
                    O = R.O[R.io % len(R.O)]
                    R.io += 1
                    Ov = self.attend(R, q[0:67, h, :], [q.b], tiles, O, 0.125)
                    fold(O, Ov, h, 3 * h + 2, False)
                    S.op("dve", lambda: nc.vector.tensor_copy(out=ms[:, :, h * 64:(h + 1) * 64], in_=acc[:, h, :, :]), reads=[acc.b], writes=[ms.b])
                S.store("pool", self.mix[c * 512:(c + 1) * 512, 128:256].rearrange("(a p) d -> p a d", p=128), ms[:, :, :], ms.b)
            S.barrier()

    def prep_ffn(self, l):
        S = self.S
        with ExitStack() as ph:
            stg = [self.tile(ph, "pstg%d" % i, [128, 4096], F32) for i in range(2)]
            sb = [self.tile(ph, "psb%d" % i, [128, 4096], BF16) for i in range(2)]
            i = 0
            for src, dst in ((self.dp["wg"], self.wgs), (self.dp["wu"], self.wus), (self.dp["wd"], self.wds)):
                for g in range(NCF // 4):
                    s_, b_ = stg[i % 2], sb[i % 2]
                    i += 1
                    if len(src.shape) == 5:
                        sap = src[l, 4 * g:4 * g + 4].rearrange("c p k n -> p c (k n)")
                        dap = dst[4 * g:4 * g + 4].rearrange("c p k n -> p c (k n)")
                    else:
                        sap = src[l, 4 * g:4 * g + 4].rearrange("c p n -> p c n")
                        dap = dst[4 * g:4 * g + 4].rearrange("c p n -> p c n")
                    S.load("sp", s_[:, :].rearrange("p (c n) -> p c n", c=4), sap, s_.b)
                    self.copy(b_[:, :], s_[:, :], [s_.b], [b_.b])
                    S.store("pool", dap, b_[:, :].rearrange("p (c n) -> p c n", c=4), b_.b)
            S.barrier()

    def phase_C(self, l, x_src, x_dst):
        nc, S = self.nc, self.S
        with ExitStack() as ph:
            WO = self.tile(ph, "WO", [128, 8, D], BF16)
            with ExitStack() as tmp:
                self.load_cast(tmp, WO, lambda i: WO[:, i, :], lambda i: self.dp["wo"][l, :, i, :], 8, D)
                S.barrier()
            g1 = self.tile(ph, "g_apost", [128, D], F32)
            g2 = self.tile(ph, "g_fpre", [128, D], F32)
            g3 = self.tile(ph, "g_fpost", [128, D], F32)
            S.load("sp", g1[:, :], self.dp["g_apost"][l].partition_broadcast(128), g1.b)
            S.load("sp", g2[:, :], self.dp["g_fpre"][l].partition_broadcast(128), g2.b)
            S.load("sp", g3[:, :], self.dp["g_fpost"][l].partition_broadcast(128), g3.b)
            cw = self.tile(ph, "cw", [128, NCF, 3], F32)
            cb = self.tile(ph, "cb", [128, NCF], F32)
            S.load("sp", cw[:, :, :], self.dp["cw"][l], cw.b)
            S.load("sp", cb[:, :], self.dp["cb"][l], cb.b)
            halo = self.tile(ph, "halo", [128, NCF, 2], F32)
            S.op("pool", lambda: nc.gpsimd.memset(halo[:, :, :], 0.0), writes=[halo.b])
            bank = [self.tile(ph, "bk%d" % i, [128, 512], F32, psum=True) for i in range(6)]
            tpb = [self.tile(ph, "tpC%d" % i, [128, D], BF16, psum=True) for i in range(2)]
            mixt = [self.tile(ph, "mixt%d" % i, [128, D], BF16) for i in range(2)]
            mT = [self.tile(ph, "mT%d" % i, [128, 8, 128], BF16) for i in range(2)]
            xt = [self.tile(ph, "xtC%d" % i, [128, D], F32) for i in range(2)]
            x1 = [self.tile(ph, "x1C%d" % i, [128, D], F32) for i in range(2)]
            yt = self.tile(ph, "ytC", [128, D], F32)
            junk = self.tile(ph, "junkC", [128, D], F32)
            ss = self.tile(ph, "ssC", [128, 1], F32)
            sd = self.tile(ph, "sdC", [128, 1], F32)
            rstd = self.tile(ph, "rstdC", [128, 1], F32)
            hb = [self.tile(ph, "hbC%d" % i, [128, D], BF16) for i in range(4)]
            h2T = self.tile(ph, "h2T", [128, 8, 512], BF16)
            gT = self.tile(ph, "gT", [128, NCF, 512], BF16)
            wgt = [self.tile(ph, "wgt%d" % i, [128, 8, 128], BF16) for i in range(3)]
            wut = [self.tile(ph, "wut%d" % i, [128, 8, 128], BF16) for i in range(3)]
            wdt = [self.tile(ph, "wdt%d" % i, [128, D], BF16) for i in range(3)]
            aT = [self.tile(ph, "aT%d" % i, [128, 514], F32) for i in range(2)]
            cacc = [self.tile(ph, "cacc%d" % i, [128, 512], F32) for i in range(2)]
            ga = [self.tile(ph, "ga%d" % i, [128, 512], F32) for i in range(2)]
            pst = [self.tile(ph, "pstC%d" % i, [128, D], F32) for i in range(2)]
            rt = [self.tile(ph, "rtC%d" % i, [128, D], F32) for i in range(2)]
            x1r = [self.tile(ph, "x1rC%d" % i, [128, D], F32) for i in range(2)]
            ot = [self.tile(ph, "otC%d" % i, [128, D], F32) for i in range(2)]
            ss3 = self.tile(ph, "ss3C", [128, 1], F32)
            sd3 = self.tile(ph, "sd3C", [128, 1], F32)
            rstd3 = self.tile(ph, "rstd3C", [128, 1], F32)
            junk3 = self.tile(ph, "junk3C", [128, D], F32)
            it = 0
            i3 = 0
            RNG = 2
            pending = []
            store_tags = []

            def finish_tile(tag, tt):
                nonlocal i3
                S._wait("sp", tag)
                tok = tt * 128
                r_, x_, o_ = rt[i3 % 2], x1r[i3 % 2], ot[i3 % 2]
                i3 += 1
                S.load("sp", r_[:, :], self.red[tok:tok + 128, :], r_.b)
                S.load("sp", x_[:, :], self.x1s[tok:tok + 128, :], x_.b)
                self.rstd_of(r_[:, :], r_.b, junk3, ss3, sd3, rstd3)
                S.op("dve", lambda: nc.vector.scalar_tensor_tensor(out=r_[:, :], in0=r_[:, :], scalar=rstd3[:, 0:1], in1=g3[:, :], op0=ALU.mult, op1=ALU.mult),
                     reads=[r_.b, rstd3.b, g3.b], writes=[r_.b])
                S.op("pool", lambda: nc.gpsimd.tensor_tensor(out=o_[:, :], in0=r_[:, :], in1=x_[:, :], op=ALU.add),
                     reads=[r_.b, x_.b], writes=[o_.b])
                S.store("pool", x_dst[tok:tok + 128, :], o_[:, :], o_.b)

            h2Ts = [h2T, self.tile(ph, "h2Tb", [128, 8, 512], BF16)]

            def seg_a1(T):
                tok = T * 128
                mt, mTt, tpp = mixt[T % 2], mT[T % 2], tpb[0]
                mrow = (tok // 2048) * 4096 + (tok % 2048)
                S.load("sp", mt[:, 0:512], self.mixall[mrow:mrow + 128, :], mt.b)
                S.load("sp", mt[:, 512:1024], self.mixall[2048 + mrow:2048 + mrow + 128, :], mt.b)
                for kc in range(8):
                    S.op("pe", lambda: nc.tensor.transpose(out=tpp[:, kc * 128:(kc + 1) * 128], in_=mt[:, kc * 128:(kc + 1) * 128], identity=self.ident[:, :]),
                         reads=[mt.b, self.ident.b], writes=[tpp.b], accum=(kc > 0))
                self.copy(mTt[:, :, :], tpp[:, :].rearrange("p (k t) -> p k t", k=8), [tpp.b], [mTt.b])

            def seg_a2(T):
                tok = T * 128
                mTt, xtt, h, x1t = mT[T % 2], xt[T % 2], hb[T % 4], x1[T % 2]
                S.load("sp", xtt[:, :], x_src[tok:tok + 128, :], xtt.b)
                pa, pb = bank[4], bank[5]
                for half, pp in enumerate((pa, pb)):
                    for kc in range(8):
                        S.op("pe", lambda: nc.tensor.matmul(pp[:, :], lhsT=mTt[:, kc, :], rhs=WO[:, kc, half * 512:(half + 1) * 512],
                                                            start=(kc == 0), stop=(kc == 7)), reads=[mTt.b, WO.b], writes=[pp.b], accum=(kc > 0))
                self.copy(yt[:, 0:512], pa[:, :], [pa.b], [yt.b], eng="act")
                self.copy(yt[:, 512:D], pb[:, :], [pb.b], [yt.b], eng="act")
                self.rstd_of(yt[:, :], yt.b, junk, ss, sd, rstd)
                S.op("dve", lambda: nc.vector.scalar_tensor_tensor(out=yt[:, :], in0=yt[:, :], scalar=rstd[:, 0:1], in1=g1[:, :], op0=ALU.mult, op1=ALU.mult),
                     reads=[yt.b, rstd.b, g1.b], writes=[yt.b])
                S.op("pool", lambda: nc.gpsimd.tensor_tensor(out=x1t[:, :], in0=yt[:, :], in1=xtt[:, :], op=ALU.add),
                     reads=[yt.b, xtt.b], writes=[x1t.b])
                S.store("pool", self.x1s[tok:tok + 128, :], x1t[:, :], x1t.b)
                store_tags.append(("dma", x1t.b.dsem, x1t.b.dcnt))
                self.rstd_of(x1t[:, :], x1t.b, junk, ss, sd, rstd)
                S.op("dve", lambda: nc.vector.scalar_tensor_tensor(out=h[:, :], in0=x1t[:, :], scalar=rstd[:, 0:1], in1=g2[:, :], op0=ALU.mult, op1=ALU.mult),
                     reads=[x1t.b, rstd.b, g2.b], writes=[h.b])

            def seg_b3(T):
                h, tpp = hb[T % 4], tpb[1]
                hT = h2Ts[(T // 4) % 2]
                ti = T % 4
                for kc in range(8):
                    S.op("pe", lambda: nc.tensor.transpose(out=tpp[:, kc * 128:(kc + 1) * 128], in_=h[:, kc * 128:(kc + 1) * 128], identity=self.ident[:, :]),
                         reads=[h.b, self.ident.b], writes=[tpp.b], accum=(kc > 0))
                self.copy(hT[:, :, ti * 128:(ti + 1) * 128], tpp[:, :].rearrange("p (k t) -> p k t", k=8), [tpp.b], [hT.b])

            def seg_b(c, ti, ctx):
                h, tpp = ctx
                hT = h2Ts[c % 2]
                for kc in range(8):
                    S.op("pe", lambda: nc.tensor.transpose(out=tpp[:, kc * 128:(kc + 1) * 128], in_=h[:, kc * 128:(kc + 1) * 128], identity=self.ident[:, :]),
                         reads=[h.b, self.ident.b], writes=[tpp.b], accum=(kc > 0))
                self.copy(hT[:, :, ti * 128:(ti + 1) * 128], tpp[:, :].rearrange("p (k t) -> p k t", k=8), [tpp.b], [hT.b])

            def c2a_cf(c, cf):
                hT = h2Ts[c % 2]
                wg_, wu_ = wgt[cf % 3], wut[cf % 3]
                S.load("sp", wg_[:, :, :], self.wgs[cf], wg_.b)
                S.load("sp", wu_[:, :, :], self.wus[cf], wu_.b)
                pa, pu = bank[2 * (cf % 2)], bank[2 * (cf % 2) + 1]
                for kc in range(8):
                    S.op("pe", lambda: nc.tensor.matmul(pa[:, :], lhsT=wg_[:, kc, :], rhs=hT[:, kc, :], start=(kc == 0), stop=(kc == 7)),
                         reads=[wg_.b, hT.b], writes=[pa.b], accum=(kc > 0))
                for kc in range(8):
                    S.op("pe", lambda: nc.tensor.matmul(pu[:, :], lhsT=wu_[:, kc, :], rhs=hT[:, kc, :], start=(kc == 0), stop=(kc == 7)),
                         reads=[wu_.b, hT.b], writes=[pu.b], accum=(kc > 0))
                a, ca, g = aT[cf % 2], cacc[cf % 2], ga[cf % 2]
                e4 = a[:, 0:4]
                S.op("dve", lambda: nc.vector.tensor_copy(out=a[:, 0:2], in_=halo[:, cf, :]), reads=[halo.b], writes=[a.b])
                S.op("dve", lambda: nc.vector.tensor_copy(out=a[:, 2:4], in_=pa[:, 0:2]), reads=[pa.b], writes=[a.b])
                S.op("dve", lambda: nc.vector.tensor_copy(out=halo[:, cf, :], in_=pa[:, 510:512]), reads=[pa.b], writes=[halo.b])
                S.op("dve", lambda: nc.vector.tensor_scalar(out=ca[:, 2:512], in0=pa[:, 0:510], scalar1=cw[:, cf, 0:1], scalar2=None, op0=ALU.mult),
                     reads=[pa.b, cw.b], writes=[ca.b])
                S.op("dve", lambda: nc.vector.scalar_tensor_tensor(out=ca[:, 2:512], in0=pa[:, 1:511], scalar=cw[:, cf, 1:2], in1=ca[:, 2:512], op0=ALU.mult, op1=ALU.add),
                     reads=[pa.b, cw.b, ca.b], writes=[ca.b])
                S.op("dve", lambda: nc.vector.scalar_tensor_tensor(out=ca[:, 2:512], in0=pa[:, 2:512], scalar=cw[:, cf, 2:3], in1=ca[:, 2:512], op0=ALU.mult, op1=ALU.add),
                     reads=[pa.b, cw.b, ca.b], writes=[ca.b])
                S.op("dve", lambda: nc.vector.tensor_scalar(out=ca[:, 0:2], in0=e4[:, 0:2], scalar1=cw[:, cf, 0:1], scalar2=None, op0=ALU.mult),
                     reads=[a.b, cw.b], writes=[ca.b])
                S.op("dve", lambda: nc.vector.scalar_tensor_tensor(out=ca[:, 0:2], in0=e4[:, 1:3], scalar=cw[:, cf, 1:2], in1=ca[:, 0:2], op0=ALU.mult, op1=ALU.add),
                     reads=[a.b, cw.b, ca.b], writes=[ca.b])
                S.op("dve", lambda: nc.vector.scalar_tensor_tensor(out=ca[:, 0:2], in0=e4[:, 2:4], scalar=cw[:, cf, 2:3], in1=ca[:, 0:2], op0=ALU.mult, op1=ALU.add),
                     reads=[a.b, cw.b, ca.b], writes=[ca.b])
                S.op("act", lambda: nc.scalar.activation(out=g[:, :], in_=ca[:, :], func=AF.Gelu_apprx_tanh, bias=cb[:, cf:cf + 1]),
                     reads=[ca.b, cb.b], writes=[g.b])
                S.op("dve", lambda: nc.vector.tensor_tensor(out=gT[:, cf, :], in0=g[:, :], in1=pu[:, :], op=ALU.mult),
                     reads=[g.b, pu.b], writes=[gT.b])

            def c2b(c):
                for tp2 in range(2):
                    accs = [bank[0], bank[1], bank[2], bank[3]]
                    for cf in range(NCF):
                        wd_ = wdt[cf % 3]
                        S.load("sp", wd_[:, :], self.wds[cf], wd_.b)
                        for j in range(2):
                            ti = 2 * tp2 + j
                            for half in range(2):
                                pp = accs[2 * j + half]
                                S.op("pe", lambda: nc.tensor.matmul(pp[:, :], lhsT=gT[:, cf, ti * 128:(ti + 1) * 128], rhs=wd_[:, half * 512:(half + 1) * 512],
                                                                    start=(cf == 0), stop=(cf == NCF - 1)), reads=[gT.b, wd_.b], writes=[pp.b], accum=(cf > 0))
                    for j in range(2):
                        ti = 2 * tp2 + j
                        tok = (4 * c + ti) * 128
                        p_ = pst[j]
                        self.copy(p_[:, 0:512], accs[2 * j][:, :], [accs[2 * j].b], [p_.b], eng="act")
                        self.copy(p_[:, 512:D], accs[2 * j + 1][:, :], [accs[2 * j + 1].b], [p_.b], eng="dve")
                        S.store("pool", self.part[tok:tok + 128, :], p_[:, :], p_.b)
                        store_tags.append(("dma", p_.b.dsem, p_.b.dcnt))

            NTL = 4 * self.nch
            fifo = []
            for s_ in range(-7, 0):
                if 0 <= s_ + 4 < NTL:
                    seg_b3(s_ + 4)
                if 0 <= s_ + 6 < NTL:
                    seg_a2(s_ + 6)
                if 0 <= s_ + 7 < NTL:
                    seg_a1(s_ + 7)
            for c in range(self.nch):
                for cf in range(NCF):
                    c2a_cf(c, cf)
                    if cf % 4 == 3:
                        s_ = 4 * c + cf // 4
                        if s_ + 4 < NTL:
                            seg_b3(s_ + 4)
                        if s_ + 6 < NTL:
                            seg_a2(s_ + 6)
                        if s_ + 7 < NTL:
                            seg_a1(s_ + 7)
                        if fifo and fifo[0][2] <= c:
                            tg, tt, _ = fifo.pop(0)
                            finish_tile(tg, tt)
                c2b(c)
                if c % RNG == RNG - 1 or c == self.nch - 1:
                    c0 = (c // RNG) * RNG
                    r0, r1 = c0 * 512, (c + 1) * 512
                    for tg in store_tags:
                        S._wait("pool", tg)
                    store_tags = []
                    tag = self.collective("AllReduce", ALU.add, self.part[r0:r1, :].opt(), self.red[r0:r1, :].opt())
                    for tt in range(r0 // 128, r1 // 128):
                        fifo.append((tag, tt, c + 2))
            for tg, tt, _ in fifo:
                finish_tile(tg, tt)
            S.barrier()


_CACHE = {}


def kernel(**inputs):
    x = np.ascontiguousarray(inputs["x"], dtype=np.float32)
    params = [layout_params(inputs, r) for r in range(2)]
    consts = [make_consts(r) for r in range(2)]
    if "prog" not in _CACHE:
        _CACHE["prog"] = Prog()
    prog = _CACHE["prog"]
    in_maps = []
    for core in range(8):
        b, r = core // 2, core % 2
        m = {"x": x[b]}
        for n, _, _ in CONST_SPECS:
            m["c_" + n] = consts[r][n]
        for n, _ in PARAM_SPECS:
            m["p_" + n] = params[r][n]
        in_maps.append(m)
    res = run_bass_kernel_spmd(prog.nc, in_maps, core_ids=list(range(8)))
    out = np.stack([np.asarray(res.results[2 * b]["y"], dtype=np.float32) for b in range(4)], axis=0)
    return out
```

```python
import math
import numpy as np
import ml_dtypes
from contextlib import ExitStack
import concourse.bass as bass
import concourse.mybir as mybir
from concourse.bass_utils import run_bass_kernel_spmd

F32 = mybir.dt.float32
BF16 = mybir.dt.bfloat16
AF = mybir.ActivationFunctionType
ALU = mybir.AluOpType
AX = mybir.AxisListType
NPBF = ml_dtypes.bfloat16

D = 1024
S_LEN = 8192
NT = 64
NCH = 16
DEPTH = 2
DFF = 4096
NEG = -30000.0
EPS = 1e-6

OFF = dict(mq=0, mk=256, mv=512, nq=768, nkc=1024, nvc=1088, nks=1152, nvs=1216, nkw=1280, nvw=1344,
           ng=1408, dq=1420, dk=1676, dv=1932, sq=2188, sk=2444, sv=2572)
FRr = dict(mq=0, mk=128, nq=256, nkc=512, nvc=576, nks=640, nkw=704, dq=768, dk=896, sq=1024, sk=1152)
NF = 1280
VCr = dict(mv=0, nvs=128, nvw=192, dv=256, sv=384, ng=448)
NV = 460
NCF = 16


def role_heads(r, m=0):
    if m == 3:
        return [2 * r, 2 * r + 1], [2 * (1 - r), 2 * (1 - r) + 1]
    return [r, r + 2], [1 - r, 3 - r]


def far_skip(m, j, min_dist):
    sl = min(SLOPES[4 * role_heads(0, m)[0][j] + m], SLOPES[4 * role_heads(1, m)[0][j] + m])
    return min_dist > 0 and sl * min_dist >= 160.0


def role_cols(r):
    own0 = role_heads(r, 0)[0]
    own1, oth1 = role_heads(r, 1)
    own2 = role_heads(r, 2)[0]
    own3 = role_heads(r, 3)[0]

    def hc(base, heads, w=64):
        return np.concatenate([np.arange(base + w * h, base + w * (h + 1)) for h in heads])

    f = np.concatenate([hc(OFF["mq"], own0), hc(OFF["mk"], own0), hc(OFF["nq"], own1 + oth1),
                        np.arange(OFF["nkc"], OFF["nkc"] + 64), np.arange(OFF["nvc"], OFF["nvc"] + 64),
                        np.arange(OFF["nks"], OFF["nks"] + 64), np.arange(OFF["nkw"], OFF["nkw"] + 64),
                        hc(OFF["dq"], own2), hc(OFF["dk"], own2), hc(OFF["sq"], own3), hc(OFF["sk"], [r])])
    v = np.concatenate([hc(OFF["mv"], own0), np.arange(OFF["nvs"], OFF["nvs"] + 64), np.arange(OFF["nvw"], OFF["nvw"] + 64),
                        hc(OFF["dv"], own2), hc(OFF["sv"], [r]), hc(OFF["ng"], own1 + oth1, 3)])
    assert len(f) == 1216 and len(v) == NV
    return f, v


SLOPES = np.power(np.float32(2.0), np.arange(1, 17, dtype=np.float32) * np.float32(-0.5)).astype(np.float64)


class Buf:
    __slots__ = ("name", "w", "rs", "dsem", "dcnt")

    def __init__(self, name):
        self.name = name
        self.w = None
        self.rs = []
        self.dsem = None
        self.dcnt = 0


class Sched:
    def __init__(self, nc, stack):
        self.nc = nc
        self.stack = stack
        self.eng = {"pe": nc.tensor, "act": nc.scalar, "dve": nc.vector, "pool": nc.gpsimd, "sp": nc.sync}
        self.sem, self.cnt, self.seen = {}, {}, {}
        for k in self.eng:
            self.sem[k] = stack.enter_context(nc.semaphore("s_" + k))
            self.cnt[k] = 0
            self.seen[k] = {}
        self.dma_seen = {k: {} for k in self.eng}
        self.free_sems = []
        self.live = []
        self.nsem = len(self.eng)
        self.ninstr = 0

    def _wait(self, ek, dep):
        if dep is None:
            return
        e = self.eng[ek]
        if dep[0] == "dma":
            _, sem, val = dep
            d = self.dma_seen[ek]
            if d.get(id(sem), 0) >= val:
                return
            e.wait_ge(sem, val)
            d[id(sem)] = val
        else:
            pk, c = dep
            if self.seen[ek].get(pk, 0) >= c:
                return
            e.wait_ge(self.sem[pk], c)
            self.seen[ek][pk] = c
        self.ninstr += 1

    def _deps(self, ek, reads, writes, accum):
        for b in reads:
            self._wait(ek, b.w)
        for b in writes:
            if not (accum and b.w is not None and b.w[0] == ek):
                self._wait(ek, b.w)
            for r in b.rs:
                self._wait(ek, r)

    def op(self, ek, fn, reads=(), writes=(), accum=False):
        self._deps(ek, reads, writes, accum)
        ins = fn()
        self.cnt[ek] += 1
        ins.then_inc(self.sem[ek], 1)
        tag = (ek, self.cnt[ek])
        for b in reads:
            b.rs.append(tag)
        for b in writes:
            b.w = tag
            b.rs = []
        self.ninstr += 1
        return ins

    def _dsem(self, b):
        if b.dsem is None:
            if self.free_sems:
                b.dsem, b.dcnt = self.free_sems.pop()
            else:
                b.dsem = self.stack.enter_context(self.nc.semaphore("d%d" % self.nsem))
                b.dcnt = 0
                self.nsem += 1
            self.live.append(b)

    def load(self, qk, out_ap, in_ap, buf):
        self._deps(qk, (), (buf,), False)
        self._dsem(buf)
        ins = self.eng[qk].dma_start(out=out_ap, in_=in_ap)
        buf.dcnt += 16
        ins.then_inc(buf.dsem, 16)
        buf.w = ("dma", buf.dsem, buf.dcnt)
        buf.rs = []
        self.ninstr += 1

    def store(self, qk, out_ap, in_ap, buf):
        self._deps(qk, (buf,), (), False)
        self._dsem(buf)
        ins = self.eng[qk].dma_start(out=out_ap, in_=in_ap)
        buf.dcnt += 16
        ins.then_inc(buf.dsem, 16)
        buf.rs.append(("dma", buf.dsem, buf.dcnt))
        self.ninstr += 1

    def barrier(self):
        for ek in self.eng:
            for pk in self.eng:
                if pk != ek and self.cnt[pk] > 0:
                    self._wait(ek, (pk, self.cnt[pk]))
            for b in self.live:
                self._wait(ek, ("dma", b.dsem, b.dcnt))
        for b in self.live:
            self.free_sems.append((b.dsem, b.dcnt))
            b.dsem = None
        self.live = []


class T:
    def __init__(self, nc, stack, name, shape, dtype, psum=False):
        alloc = nc.psum_tensor if psum else nc.sbuf_tensor
        self.t = stack.enter_context(alloc(name, shape, dtype))
        self.b = Buf(name)

    def __getitem__(self, idx):
        return self.t[idx]


def _split3(v):
    v = np.asarray(v, np.float64)
    hi = v.astype(NPBF)
    r = v - hi.astype(np.float64)
    mid = r.astype(NPBF)
    r = r - mid.astype(np.float64)
    lo = r.astype(NPBF)
    return hi, mid, lo


def make_consts(role=0):
    locs = [sum(role_heads(role, m), []) for m in range(4)]
    c = {}
    c["ident"] = np.eye(128, dtype=np.float32).astype(NPBF)
    iq = np.arange(512, dtype=np.float64)
    p = np.arange(128, dtype=np.float64)
    aug = np.zeros((16, 3, 512), NPBF)
    for m in range(4):
        scale = 32 ** -0.5 if m == 2 else 64 ** -0.5
        for j in range(4):
            sl = SLOPES[4 * locs[m][j] + m]
            hi, mid, lo = _split3(-sl * iq / scale)
            aug[4 * m + j, 0], aug[4 * m + j, 1], aug[4 * m + j, 2] = hi, mid, lo
    c["aug"] = aug
    kb = np.zeros((128, 16, 64), np.float32)
    r = np.arange(-60, 4, dtype=np.float64)
    for m in range(4):
        for j in range(4):
            sl = SLOPES[4 * locs[m][j] + m]
            kb[:, 4 * m + j, :] = (sl * (p[:, None] + 128.0 * r[None, :])).astype(np.float32)
    c["kb"] = kb
    kbc = np.zeros((128, 4, 16, 4), np.float32)
    for j in range(4):
        sl = SLOPES[4 * locs[1][j] + 1]
        for cc in range(16):
            for kt in range(4):
                kbc[:, j, cc, kt] = (sl * (16.0 * (128 * kt + p) + 31.0 - 512.0 * cc)).astype(np.float32)
    c["kbc"] = kbc
    P = p[:, None, None]
    Q = iq[None, None, :]
    k4 = np.arange(4, dtype=np.float64)[None, :, None]
    c["caus"] = np.where(Q >= 128 * k4 + P, 0.0, NEG).astype(np.float32).astype(NPBF)
    r8 = (np.arange(8, dtype=np.float64) - 4)[None, :, None]
    dd = Q - 128 * r8 - P
    c["wmask"] = np.where((dd >= 0) & (dd < 512), 0.0, NEG).astype(np.float32).astype(NPBF)
    r5 = (np.arange(5, dtype=np.float64) - 1)[None, :, None]
    dd = Q - 128 * r5 - P
    c["swamask"] = np.where((dd >= 0) & (dd < 128), 0.0, NEG).astype(np.float32).astype(NPBF)
    d5 = (512.0 * np.arange(5, dtype=np.float64))[None, :, None]
    c["cmask"] = np.where(16 * P + 31 <= d5 + Q, 0.0, NEG).astype(np.float32).astype(NPBF)
    j = np.arange(S_LEN)
    c["em"] = (j[None, :] // 256 == np.arange(32)[:, None]).astype(np.float32).astype(NPBF)
    c["es"] = (j[None, :] // 64 == np.arange(128)[:, None]).astype(np.float32).astype(NPBF)
    ncmp, nsel = 511, 128
    cs = np.arange(512)[:, None] * 16
    bs = np.arange(nsel)[None, :] * 64
    ov = np.clip(np.minimum(cs + 32, bs + 64) - np.maximum(cs, bs), 0, None) / 32.0
    ov[ncmp:, :] = 0.0
    c["ov"] = ov.reshape(4, 128, 128).transpose(1, 0, 2).astype(np.float32).astype(NPBF)
    n32 = np.arange(32, dtype=np.float32)
    qi = np.arange(4)
    c["bi"] = np.broadcast_to((n32[None, None, :] - (qi // 2)[None, :, None].astype(np.float32)), (128, 4, 32)).astype(np.float32).copy()
    s128 = np.arange(128)[None, None, None, :]
    cc = np.arange(16)[:, None, None, None]
    pp = np.arange(128)[None, :, None, None]
    qq = np.arange(4)[None, None, :, None]
    qblk = 8 * cc + 2 * qq + (pp >= 64)
    forced = (s128 == 0) | (s128 == qblk) | (s128 == qblk - 1)
    valid = s128 <= qblk
    c["f1e4"] = np.where(forced, 1e4, 0.0).astype(np.float32)
    c["negv"] = np.where(valid, 0.0, -1e30).astype(np.float32)
    c["ones3"] = np.ones((3, 4 * S_LEN), np.float32).astype(NPBF)
    return c


CONST_SPECS = [("ident", [128, 128], BF16), ("aug", [16, 3, 512], BF16), ("kb", [128, 16, 64], F32),
               ("kbc", [128, 4, 16, 4], F32), ("caus", [128, 4, 512], BF16), ("wmask", [128, 8, 512], BF16),
               ("swamask", [128, 5, 512], BF16), ("cmask", [128, 5, 512], BF16), ("em", [32, S_LEN], BF16),
               ("es", [128, S_LEN], BF16), ("ov", [128, 4, 128], BF16), ("bi", [128, 4, 32], F32),
               ("f1e4", [16, 128, 4, 128], F32), ("negv", [16, 128, 4, 128], F32), ("ones3", [3, 4 * S_LEN], BF16)]

PARAM_SPECS = [("wf", [DEPTH, 128, 8, NF]), ("wv", [DEPTH, 128, 8, NV]), ("wo", [DEPTH, 128, 8, D]),
               ("wg", [DEPTH, NCF, 128, 8, 128]), ("wu", [DEPTH, NCF, 128, 8, 128]), ("wd", [DEPTH, NCF, 128, D]),
               ("g_apre", [DEPTH, D]), ("g_apost", [DEPTH, D]), ("g_fpre", [DEPTH, D]), ("g_fpost", [DEPTH, D]),
               ("cw", [DEPTH, 128, NCF, 3]), ("cb", [DEPTH, 128, NCF]),
               ("posk", [DEPTH, 64, 32]), ("w1k", [DEPTH, 64, 32, 128]), ("b1k", [DEPTH, 128, 1]), ("w2k", [DEPTH, 128, 64]),
               ("posv", [DEPTH, 64, 32]), ("w1v", [DEPTH, 64, 32, 128]), ("b1v", [DEPTH, 128, 1]), ("w2v", [DEPTH, 128, 64]),
               ("lq1", [DEPTH, 32]), ("lk1", [DEPTH, 32]), ("lq2", [DEPTH, 32]), ("lk2", [DEPTH, 32]),
               ("subln", [DEPTH, 64]), ("sinks", [DEPTH, 4])]


def layout_params(inp, role=0):
    o = {}
    w_in = inp["w_in"]
    fc, vc = role_cols(role)
    wf = np.zeros((DEPTH, D, NF), np.float32)
    wf[:, :, :len(fc)] = w_in[:, :, fc]
    o["wf"] = wf.reshape(DEPTH, 8, 128, NF).transpose(0, 2, 1, 3)
    o["wv"] = w_in[:, :, vc].reshape(DEPTH, 8, 128, NV).transpose(0, 2, 1, 3)
    rows = np.concatenate([np.arange(256 * m + 64 * h, 256 * m + 64 * (h + 1)) for rr in range(2) for m in range(4)
                           for h in role_heads(rr, m)[0]])
    o["wo"] = inp["w_out"][:, rows, :].reshape(DEPTH, 8, 128, D).transpose(0, 2, 1, 3)
    cfs = slice(NCF * role, NCF * (role + 1))
    o["wg"] = inp["ffn_w_gate"].reshape(DEPTH, 8, 128, 32, 128).transpose(0, 3, 2, 1, 4)[:, cfs]
    o["wu"] = inp["ffn_w_up"].reshape(DEPTH, 8, 128, 32, 128).transpose(0, 3, 2, 1, 4)[:, cfs]
    o["wd"] = inp["ffn_w_down"].reshape(DEPTH, 32, 128, D)[:, cfs]
    o["g_apre"], o["g_apost"] = inp["attn_pre_norm"], inp["attn_post_norm"]
    o["g_fpre"], o["g_fpost"] = inp["ffn_pre_norm"], inp["ffn_post_norm"]
    o["cw"] = inp["ffn_conv_w"].reshape(DEPTH, 3, 32, 128).transpose(0, 3, 2, 1)[:, :, cfs, :]
    o["cb"] = inp["ffn_conv_b"].reshape(DEPTH, 32, 128).transpose(0, 2, 1)[:, :, cfs]
    for sfx in "kv":
        o["pos" + sfx] = inp["nsa_cmp_pos_" + sfx].transpose(0, 2, 1)
        o["w1" + sfx] = inp["nsa_cmp_w1_" + sfx].transpose(0, 2, 1, 3)
        o["b1" + sfx] = inp["nsa_cmp_b1_" + sfx].reshape(DEPTH, 128, 1)
        o["w2" + sfx] = inp["nsa_cmp_w2_" + sfx]
    o["lq1"], o["lk1"] = inp["diff_lambda_q1"], inp["diff_lambda_k1"]
    o["lq2"], o["lk2"] = inp["diff_lambda_q2"], inp["diff_lambda_k2"]
    o["subln"] = inp["diff_subln"]
    o["sinks"] = inp["swa_sinks"][:, sum(role_heads(role, 3), [])]
    return {k: np.ascontiguousarray(v, dtype=np.float32) for k, v in o.items()}


class Prog:
    def __init__(self, phases=("A", "B", "C"), layers=(0, 1), mixers=(0, 1, 2, 3), debug=False, nchunks=NCH, ncores=8):
        self.phases, self.layers, self.mixers, self.debug, self.nch = phases, layers, mixers, debug, nchunks
        self.groups = [[2 * i, 2 * i + 1] for i in range(ncores // 2)]
        nc = bass.Bass("TRN2", target_bir_lowering=False)
        self.nc = nc
        self.x_in = nc.dram_tensor("x", [S_LEN, D], F32, kind="ExternalInput").ap()
        self.y = nc.dram_tensor("y", [S_LEN, D], F32, kind="ExternalOutput").ap()
        self.dc = {n: nc.dram_tensor("c_" + n, shp, dt, kind="ExternalInput").ap() for n, shp, dt in CONST_SPECS}
        self.dp = {n: nc.dram_tensor("p_" + n, shp, F32, kind="ExternalInput").ap() for n, shp in PARAM_SPECS}
        kind = "ExternalOutput" if debug else "Internal"
        self.featT = nc.dram_tensor("featT", [NF, S_LEN], BF16, kind=kind).ap()
        self.vtok = nc.dram_tensor("vtok", [S_LEN, NV], BF16, kind=kind).ap()
        self.mix_t = nc.dram_tensor("mix", [S_LEN, 512], BF16)
        self.mix = self.mix_t.ap()
        self.mixall_t = nc.dram_tensor("mixall", [2 * S_LEN, 512], BF16)
        self.mixall = self.mixall_t.ap()
        self.part = nc.dram_tensor("part", [S_LEN, D], F32).ap()
        self.red = nc.dram_tensor("red", [S_LEN, D], F32).ap()
        self.x1s = nc.dram_tensor("x1s", [S_LEN, D], F32).ap()
        self.xs = nc.dram_tensor("xs", [S_LEN, D], F32).ap()
        self.wgs = nc.dram_tensor("wgs", [NCF, 128, 8, 128], BF16).ap()
        self.wus = nc.dram_tensor("wus", [NCF, 128, 8, 128], BF16).ap()
        self.wds = nc.dram_tensor("wds", [NCF, 128, D], BF16).ap()
        self.cp_i = 0
        with ExitStack() as st:
            self.st = st
            self.S = Sched(nc, st)
            self.cc_sem = st.enter_context(nc.semaphore("cc_sem"))
            self.cc_cnt = 0
            self.build()
            print("built: instr", self.S.ninstr, "sems", self.S.nsem, flush=True)

    def collective(self, kind, op, in_ap, out_ap):
        ins = self.nc.gpsimd.collective_compute(kind, op, replica_groups=self.groups, ins=[in_ap], outs=[out_ap])
        self.cc_cnt += 1
        ins.then_inc(self.cc_sem, 1)
        self.S.ninstr += 1
        return ("dma", self.cc_sem, self.cc_cnt)

    def tile(self, stack, name, shape, dtype, psum=False):
        self.tile_i = getattr(self, "tile_i", 0) + 1
        return T(self.nc, stack, "%s_%d" % (name, self.tile_i), shape, dtype, psum)

    def copy(self, out_ap, in_ap, reads, writes, eng=None):
        nc = self.nc
        if eng is None:
            eng = "act" if (self.cp_i % 2 == 0) else "dve"
            self.cp_i += 1
        if eng == "act":
            self.S.op("act", lambda: nc.scalar.activation(out=out_ap, in_=in_ap, func=AF.Copy), reads=reads, writes=writes)
        elif eng == "dve":
            self.S.op("dve", lambda: nc.vector.tensor_copy(out=out_ap, in_=in_ap), reads=reads, writes=writes)
        else:
            self.S.op("pool", lambda: nc.gpsimd.tensor_copy(out=out_ap, in_=in_ap), reads=reads, writes=writes)

    def load_cast(self, stack, dst, dst_ap_fn, src_ap_fn, n, ncols):
        stg = [self.tile(stack, "stg%d_%d" % (i, self.S.ninstr), [128, ncols], F32) for i in range(2)]
        for i in range(n):
            s = stg[i % 2]
            self.S.load("sp", s[:, :], src_ap_fn(i), s.b)
            self.copy(dst_ap_fn(i), s[:, :], [s.b], [dst.b])

    def build(self):
        nc, S, st = self.nc, self.S, self.st
        self.ident = self.tile(st, "ident", [128, 128], BF16)
        self.caus = self.tile(st, "caus", [128, 4, 512], BF16)
        self.kb = self.tile(st, "kb", [128, 16, 64], F32)
        self.epsT = self.tile(st, "epsT", [128, 1], F32)
        S.load("sp", self.ident[:, :], self.dc["ident"], self.ident.b)
        S.load("sp", self.caus[:, :, :], self.dc["caus"], self.caus.b)
        S.load("sp", self.kb[:, :, :], self.dc["kb"], self.kb.b)
        S.op("dve", lambda: nc.vector.memset(self.epsT[:, :], EPS), writes=[self.epsT.b])
        for l in self.layers:
            x_src = self.x_in if l == 0 else self.xs
            x_dst = self.y if l == self.layers[-1] else self.xs
            if "A" in self.phases:
                self.phase_A(l, x_src)
                S.barrier()
            if "B" in self.phases:
                for m in self.mixers:
                    [self.moba, self.nsa, self.diff, self.swa][m](l)
                    S.barrier()
            if "C" in self.phases:
                tags = []
                for k in range((self.nch * 512 + 2047) // 2048):
                    tags.append(self.collective("AllGather", ALU.bypass, self.mix[k * 2048:(k + 1) * 2048, :].opt(),
                                                self.mixall[k * 4096:(k + 1) * 4096, :].opt()))
                self.prep_ffn(l)
                for ek in S.eng:
                    S._wait(ek, tags[-1])
                S.barrier()
                self.phase_C(l, x_src, x_dst)
                S.barrier()
        S.barrier()
        if self.debug:
            nc = self.nc
            dbg = {}
            for nm, src, rows, cols, dt in (("d_mixall", self.mixall, 4096, 512, BF16), ("d_part", self.part, 2048, D, F32),
                                            ("d_red", self.red, 2048, D, F32), ("d_x1s", self.x1s, 2048, D, F32), ("d_mix", self.mix, 2048, 512, BF16)):
                dst = nc.dram_tensor(nm, [rows, cols], dt, kind="ExternalOutput").ap()
                b = Buf(nm)
                S._dsem(b)
                ins = nc.sync.dma_start(out=dst[:, :], in_=src[0:rows, :])
                b.dcnt += 16
                ins.then_inc(b.dsem, 16)
                S._wait("sp", ("dma", b.dsem, b.dcnt))

    def rstd_of(self, x_ap, xbuf, junk, ss, sd, rstd, n=D):
        nc, S = self.nc, self.S
        S.op("dve", lambda: nc.vector.scalar_tensor_tensor(out=junk[:, 0:n], in0=x_ap, scalar=1.0, in1=x_ap,
                                                           op0=ALU.mult, op1=ALU.mult, accum_out=ss[:, 0:1]),
             reads=[xbuf], writes=[junk.b, ss.b])
        S.op("act", lambda: nc.scalar.activation(out=sd[:, 0:1], in_=ss[:, 0:1], func=AF.Sqrt,
                                                 bias=self.epsT[:, 0:1], scale=1.0 / n),
             reads=[ss.b, self.epsT.b], writes=[sd.b])
        S.op("dve", lambda: nc.vector.reciprocal(out=rstd[:, 0:1], in_=sd[:, 0:1]), reads=[sd.b], writes=[rstd.b])

    def phase_A(self, l, x_src):
        nc, S = self.nc, self.S
        with ExitStack() as ph:
            WF = self.tile(ph, "WF", [128, 8, NF], BF16)
            WV = self.tile(ph, "WV", [128, 8, NV], BF16)
            gain = self.tile(ph, "gainA", [128, D], F32)
            with ExitStack() as tmp:
                self.load_cast(tmp, WF, lambda i: WF[:, i, :], lambda i: self.dp["wf"][l, :, i, :], 8, NF)
                self.load_cast(tmp, WV, lambda i: WV[:, i, :], lambda i: self.dp["wv"][l, :, i, :], 8, NV)
                S.barrier()
            S.load("sp", gain[:, :], self.dp["g_apre"][l].partition_broadcast(128), gain.b)
            xts = [self.tile(ph, "xtA%d" % i, [128, D], F32) for i in range(3)]
            junk = self.tile(ph, "junkA", [128, D], F32)
            ss = self.tile(ph, "ssA", [128, 1], F32)
            sd = self.tile(ph, "sdA", [128, 1], F32)
            rstd = self.tile(ph, "rstdA", [128, 1], F32)
            hb = [self.tile(ph, "hbA%d" % i, [128, D], BF16) for i in range(2)]
            hT = [self.tile(ph, "hTA%d" % i, [128, 8, 512], BF16) for i in range(2)]
            tp = [self.tile(ph, "tpA%d" % i, [128, D], BF16, psum=True) for i in range(2)]
            psF = [self.tile(ph, "psF%d" % i, [128, 512], F32, psum=True) for i in range(2)]
            psV0 = self.tile(ph, "psV0", [128, 512], F32, psum=True)
            fst = [self.tile(ph, "fst%d" % i, [128, 512], BF16) for i in range(3)]
            vst = [self.tile(ph, "vst%d" % i, [128, NV], BF16) for i in range(2)]
            it = 0
            for c in range(self.nch):
                hTc = hT[c % 2]
                for ti in range(4):
                    tok = (4 * c + ti) * 128
                    xt = xts[it % 3]
                    h = hb[it % 2]
                    tpp = tp[it % 2]
                    it += 1
                    S.load("sp", xt[:, :], x_src[tok:tok + 128, :], xt.b)
                    self.rstd_of(xt[:, :], xt.b, junk, ss, sd, rstd)
                    S.op("dve", lambda: nc.vector.scalar_tensor_tensor(out=h[:, :], in0=xt[:, :], scalar=rstd[:, 0:1],
                                                                       in1=gain[:, :], op0=ALU.mult, op1=ALU.mult),
                         reads=[xt.b, rstd.b, gain.b], writes=[h.b])
                    for kc in range(8):
                        S.op("pe", lambda: nc.tensor.transpose(out=tpp[:, kc * 128:(kc + 1) * 128],
                                                               in_=h[:, kc * 128:(kc + 1) * 128], identity=self.ident[:, :]),
                             reads=[h.b, self.ident.b], writes=[tpp.b], accum=(kc > 0))
                    self.copy(hTc[:, :, ti * 128:(ti + 1) * 128], tpp[:, :].rearrange("p (k t) -> p k t", k=8),
                              [tpp.b], [hTc.b])
                for g in range(NF // 128):
                    ps = psF[g % 2]
                    for kc in range(8):
                        S.op("pe", lambda: nc.tensor.matmul(ps[:, :], lhsT=WF[:, kc, g * 128:(g + 1) * 128], rhs=hTc[:, kc, :],
                                                            start=(kc == 0), stop=(kc == 7)),
                             reads=[WF.b, hTc.b], writes=[ps.b], accum=(kc > 0))
                    f = fst[g % 3]
                    self.copy(f[:, :], ps[:, :], [ps.b], [f.b])
                    S.store("pool", self.featT[g * 128:(g + 1) * 128, c * 512:(c + 1) * 512], f[:, :], f.b)
                for ti in range(4):
                    tok = (4 * c + ti) * 128
                    for kc in range(8):
                        S.op("pe", lambda: nc.tensor.matmul(psV0[:, 0:NV], lhsT=hTc[:, kc, ti * 128:(ti + 1) * 128], rhs=WV[:, kc, 0:NV],
                                                            start=(kc == 0), stop=(kc == 7)),
                             reads=[WV.b, hTc.b], writes=[psV0.b], accum=(kc > 0))
                    v = vst[ti % 2]
                    self.copy(v[:, 0:NV], psV0[:, 0:NV], [psV0.b], [v.b])
                    S.store("pool", self.vtok[tok:tok + 128, :], v[:, :], v.b)
            S.barrier()

    def attn_res(self, ph, nps=3, npt=3, no=3):
        R = type("R", (), {})()
        R.ps = [self.tile(ph, "ps_s%d" % i, [128, 512], F32, psum=True) for i in range(nps)]
        R.pt = [self.tile(ph, "pT%d" % i, [128, 512], BF16) for i in range(npt)]
        R.O = [self.tile(ph, "O%d" % i, [128, 512], F32, psum=True) for i in range(no)]
        R.ips = R.ipt = R.io = 0
        return R

    def attend(self, R, q_ap, qbufs, tiles, O, scale):
        nc, S = self.nc, self.S
        Ov = O[:, 0:260].rearrange("p (a b) -> p a b", a=4)
        n = len(tiles)

        def pv(pt, tl, first, last):
            q0 = tl.get("q0", 0)
            assert not (first and q0)
            for qi in range(q0 // 128, 4):
                S.op("pe", lambda: nc.tensor.matmul(Ov[:, qi, :], lhsT=pt[:, qi * 128:(qi + 1) * 128], rhs=tl["v"],
                                                    start=(first and qi == 0), stop=last),
                     reads=[pt.b] + tl["kbufs"], writes=[O.b], accum=not (first and qi == 0))
            if tl.get("post") is not None:
                tl["post"](pt, first, last)

        prev = None
        for i, tl in enumerate(tiles):
            ps = R.ps[R.ips % len(R.ps)]
            R.ips += 1
            q0 = tl.get("q0", 0)
            ex = tl.get("extra", [])
            S.op("pe", lambda: nc.tensor.matmul(ps[:, q0:512], lhsT=tl["kT"], rhs=q_ap[:, q0:512], start=True, stop=(len(ex) == 0)),
                 reads=qbufs + tl["kbufs"], writes=[ps.b])
            for j, (lh, rh, bufs) in enumerate(ex):
                S.op("pe", lambda: nc.tensor.matmul(ps[:, q0:512], lhsT=lh, rhs=rh[:, q0:512], start=False, stop=(j == len(ex) - 1)),
                     reads=bufs, writes=[ps.b], accum=True)
            pt = R.pt[R.ipt % len(R.pt)]
            R.ipt += 1
            S.op("act", lambda: nc.scalar.activation(out=pt[:, q0:512], in_=ps[:, q0:512], func=AF.Exp, bias=tl["bias"], scale=scale),
                 reads=[ps.b] + tl["bbufs"], writes=[pt.b])
            if prev is not None:
                pv(prev[0], prev[1], prev[2] == 0, False)
            prev = (pt, tl, i)
        pv(prev[0], prev[1], prev[2] == 0, True)
        return Ov

    def load_kv(self, KT, V, krows, nh_k, k_row0, v_col0, nh_v, kd=64):
        nc, S = self.nc, self.S
        S.op("pool", lambda: nc.gpsimd.memset(V[:, :, :, 64:65], 1.0), writes=[V.b])
        for h in range(nh_k):
            S.load("sp", KT[0:kd, h, :], self.featT[k_row0 + h * kd:k_row0 + (h + 1) * kd, :], KT.b)
        S.load("sp", KT[kd:kd + 3, :, :], self.dc["ones3"][:, 0:nh_k * S_LEN].rearrange("r (h s) -> r h s", h=nh_k), KT.b)
        for h in range(nh_v):
            S.load("sp", V[:, h, :, 0:64],
                   self.vtok[:, v_col0 + h * 64:v_col0 + (h + 1) * 64].rearrange("(kt p) d -> p kt d", p=128), V.b)

    def recip_den(self, Ov, O, den, add_ap=None, add_bufs=()):
        nc, S = self.nc, self.S
        if add_ap is None:
            S.op("dve", lambda: nc.vector.tensor_scalar(out=den[:, 0:4], in0=Ov[:, :, 64], scalar1=1e-30, scalar2=None,
                                                        op0=ALU.max), reads=[O.b], writes=[den.b])
        else:
            S.op("dve", lambda: nc.vector.tensor_scalar(out=den[:, 0:4], in0=Ov[:, :, 64], scalar1=add_ap, scalar2=1e-30,
                                                        op0=ALU.add, op1=ALU.max), reads=[O.b] + list(add_bufs), writes=[den.b])
        S.op("dve", lambda: nc.vector.reciprocal(out=den[:, 0:4], in_=den[:, 0:4]), reads=[den.b], writes=[den.b])

    def moba(self, l):
        nc, S = self.nc, self.S
        with ExitStack() as ph:
            KT = self.tile(ph, "KTm", [128, 2, S_LEN], BF16)
            V = self.tile(ph, "Vm", [128, 2, NT, 65], BF16)
            BI = self.tile(ph, "BIm", [128, 4, 32], F32)
            S.load("sp", BI[:, :, :], self.dc["bi"], BI.b)
            S.op("pool", lambda: nc.gpsimd.memset(V[:, :, :, 64:65], 1.0), writes=[V.b])
            S.op("pool", lambda: nc.gpsimd.memset(KT[64:128, :, :], 0.0), writes=[KT.b])
            for h in range(2):
                S.load("sp", KT[96:128, h, :], self.dc["em"], KT.b)
                S.load("sp", KT[0:64, h, :], self.featT[FRr["mk"] + 64 * h:FRr["mk"] + 64 * (h + 1), :], KT.b)
                S.load("sp", V[:, h, :, 0:64],
                       self.vtok[:, VCr["mv"] + h * 64:VCr["mv"] + (h + 1) * 64].rearrange("(kt p) d -> p kt d", p=128), V.b)
            S.load("sp", KT[64:67, :, :], self.dc["ones3"][:, 0:2 * S_LEN].rearrange("r (h s) -> r h s", h=2), KT.b)
            R = self.attn_res(ph)
            QT = [[self.tile(ph, "QTm%d_%d" % (i, h), [128, 512], BF16) for h in range(2)] for i in range(2)]
            for qs in QT:
                for h in range(2):
                    S.op("pool", lambda: nc.gpsimd.memset(qs[h][64:128, :], 0.0), writes=[qs[h].b])
                    S.load("sp", qs[h][64:67, :], self.dc["aug"][h], qs[h].b)
            kms = self.tile(ph, "kms", [128, 2, 32], F32)
            kmh = self.tile(ph, "kmh", [128, 2, 32], BF16)
            kml = self.tile(ph, "kml", [128, 2, 32], BF16)
            kmr = self.tile(ph, "kmr", [128, 2, 32], F32)
            for h in range(2):
                S.op("dve", lambda: nc.vector.tensor_reduce(out=kms[0:64, h, :], in_=KT[0:64, h, :].rearrange("p (n k) -> p n k", k=256),
                                                            axis=AX.X, op=ALU.add), reads=[KT.b], writes=[kms.b])
            S.op("dve", lambda: nc.vector.tensor_scalar(out=kms[0:64, :, :], in0=kms[0:64, :, :], scalar1=1.0 / 256, scalar2=None, op0=ALU.mult),
                 reads=[kms.b], writes=[kms.b])
            S.op("dve", lambda: nc.vector.tensor_copy(out=kmh[0:64, :, :], in_=kms[0:64, :, :]), reads=[kms.b], writes=[kmh.b])
            S.op("dve", lambda: nc.vector.tensor_tensor(out=kmr[0:64, :, :], in0=kms[0:64, :, :], in1=kmh[0:64, :, :], op=ALU.subtract),
                 reads=[kms.b, kmh.b], writes=[kmr.b])
            S.op("dve", lambda: nc.vector.tensor_copy(out=kml[0:64, :, :], in_=kmr[0:64, :, :]), reads=[kmr.b], writes=[kml.b])
            gps = self.tile(ph, "gps", [128, 512], F32, psum=True)
            tps = self.tile(ph, "tpsm", [128, 1024], BF16, psum=True)
            past = self.tile(ph, "past", [128, 4, 32], F32)
            own = self.tile(ph, "own", [128, 4, 32], F32)
            negp = self.tile(ph, "negp", [128, 4, 32], F32)
            gm = self.tile(ph, "gm", [128, 4, 32], F32)
            m8 = self.tile(ph, "m8", [128, 4, 8], F32)
            sel = self.tile(ph, "sel", [128, 4, 32], F32)
            nsel = self.tile(ph, "nsel", [128, 4, 32], BF16)
            den = self.tile(ph, "denm", [128, 4], F32)
            mst = [self.tile(ph, "mstm%d" % i, [128, 4, 128], BF16) for i in range(2)]
            for c in range(self.nch):
                qs = QT[c % 2]
                for h in range(2):
                    S.load("sp", qs[h][0:64, :], self.featT[FRr["mq"] + 64 * h:FRr["mq"] + 64 * (h + 1), c * 512:(c + 1) * 512], qs[h].b)
                S.op("dve", lambda: nc.vector.tensor_scalar(out=past[:, :, :], in0=BI[:, :, :], scalar1=float(2 * c), scalar2=None, op0=ALU.is_lt),
                     reads=[BI.b], writes=[past.b])
                S.op("dve", lambda: nc.vector.tensor_scalar(out=own[:, :, :], in0=BI[:, :, :], scalar1=float(2 * c), scalar2=None, op0=ALU.is_equal),
                     reads=[BI.b], writes=[own.b])
                S.op("dve", lambda: nc.vector.tensor_scalar(out=negp[:, :, :], in0=past[:, :, :], scalar1=-1.0, scalar2=1e30, op0=ALU.add, op1=ALU.mult),
                     reads=[past.b], writes=[negp.b])
                ms = mst[c % 2]
                for h in range(2):
                    q = qs[h]
                    gv = gps[:, 0:128].rearrange("p (a b) -> p a b", a=4)
                    for qi in range(4):
                        S.op("pe", lambda: nc.tensor.matmul(gv[:, qi, :], lhsT=q[0:64, qi * 128:(qi + 1) * 128], rhs=kmh[0:64, h, :], start=True, stop=False),
                             reads=[q.b, kmh.b], writes=[gps.b], accum=(qi > 0))
                        S.op("pe", lambda: nc.tensor.matmul(gv[:, qi, :], lhsT=q[0:64, qi * 128:(qi + 1) * 128], rhs=kml[0:64, h, :], start=False, stop=True),
                             reads=[q.b, kml.b], writes=[gps.b], accum=True)
                    S.op("dve", lambda: nc.vector.tensor_tensor(out=gm[:, :, :], in0=gv, in1=negp[:, :, :], op=ALU.add),
                         reads=[gps.b, negp.b], writes=[gm.b])
                    for qi in range(4):
                        S.op("dve", lambda: nc.vector.max(out=m8[:, qi, :], in_=gm[:, qi, :]), reads=[gm.b], writes=[m8.b])
                    for qi in range(4):
                        S.op("dve", lambda: nc.vector.tensor_scalar(out=sel[:, qi, :], in0=gm[:, qi, :], scalar1=m8[:, qi, 2:3], scalar2=None, op0=ALU.is_ge),
                             reads=[gm.b, m8.b], writes=[sel.b])
                    S.op("dve", lambda: nc.vector.tensor_tensor(out=sel[:, :, :], in0=sel[:, :, :], in1=past[:, :, :], op=ALU.mult),
                         reads=[sel.b, past.b], writes=[sel.b])
                    S.op("dve", lambda: nc.vector.tensor_tensor(out=sel[:, :, :], in0=sel[:, :, :], in1=own[:, :, :], op=ALU.add),
                         reads=[sel.b, own.b], writes=[sel.b])
                    S.op("dve", lambda: nc.vector.tensor_scalar(out=nsel[:, :, :], in0=sel[:, :, :], scalar1=-1.0, scalar2=-NEG, op0=ALU.add, op1=ALU.mult),
                         reads=[sel.b], writes=[nsel.b])
                    for qi in range(4):
                        S.op("pe", lambda: nc.tensor.transpose(out=tps[0:32, qi * 128:(qi + 1) * 128], in_=nsel[:, qi, :], identity=self.ident[:, :]),
                             reads=[nsel.b, self.ident.b], writes=[tps.b], accum=(qi > 0))
                    self.copy(q[96:128, :], tps[0:32, 0:512], [tps.b], [q.b], eng="dve")
                    tiles = []
                    for kt in range(4 * c + 4):
                        if far_skip(0, h, 512 * c - (128 * kt + 127)):
                            continue
                        r = kt - 4 * c
                        ex = []
                        if r >= 0:
                            ex.append((self.ident[:, :], self.caus[:, r, :], [self.ident.b, self.caus.b]))
                        tiles.append(dict(kT=KT[0:128, h, kt * 128:(kt + 1) * 128], v=V[:, h, kt, :], kbufs=[KT.b, V.b],
                                          bias=self.kb[:, 0 + h, r + 60:r + 61], bbufs=[self.kb.b], extra=ex, q0=128 * max(r, 0)))
                    O = R.O[R.io % len(R.O)]
                    R.io += 1
                    Ov = self.attend(R, q[0:128, :], [q.b], tiles, O, 0.125)
                    self.recip_den(Ov, O, den)
                    for qi in range(4):
                        S.op("dve", lambda: nc.vector.tensor_scalar(out=ms[:, qi, h * 64:(h + 1) * 64], in0=Ov[:, qi, 0:64], scalar1=den[:, qi:qi + 1],
                                                                    scalar2=None, op0=ALU.mult), reads=[O.b, den.b], writes=[ms.b])
                S.store("pool", self.mix[c * 512:(c + 1) * 512, 0:128].rearrange("(a p) d -> p a d", p=128), ms[:, :, :], ms.b)
            S.barrier()

    def swa(self, l):
        nc, S = self.nc, self.S
        with ExitStack() as ph:
            KT = self.tile(ph, "KTs", [128, 1, S_LEN], BF16)
            V = self.tile(ph, "Vs", [128, 1, NT, 65], BF16)
            SM = self.tile(ph, "SMs", [128, 5, 512], BF16)
            S.load("sp", SM[:, :, :], self.dc["swamask"], SM.b)
            self.load_kv(KT, V, 67, 1, FRr["sk"], VCr["sv"], 1)
            sk = self.tile(ph, "sinks", [128, 4], F32)
            esk = self.tile(ph, "esinks", [128, 4], F32)
            S.load("sp", sk[:, :], self.dp["sinks"][l].partition_broadcast(128), sk.b)
            S.op("act", lambda: nc.scalar.activation(out=esk[:, :], in_=sk[:, :], func=AF.Exp), reads=[sk.b], writes=[esk.b])
            R = self.attn_res(ph)
            QT = [self.tile(ph, "QTs%d" % i, [128, 2, 512], BF16) for i in range(2)]
            for q in QT:
                S.load("sp", q[64:67, :, :], self.dc["aug"][12:14].rearrange("h r q -> r h q"), q.b)
            den = self.tile(ph, "dens", [128, 4], F32)
            mst = [self.tile(ph, "msts%d" % i, [128, 4, 128], BF16) for i in range(2)]
            for c in range(self.nch):
                q = QT[c % 2]
                for h in range(2):
                    S.load("sp", q[0:64, h, :], self.featT[FRr["sq"] + 64 * h:FRr["sq"] + 64 * (h + 1), c * 512:(c + 1) * 512], q.b)
                ms = mst[c % 2]
                for h in range(2):
                    g = 0
                    tiles = []
                    for r in range(-1, 4):
                        kt = 4 * c + r
                        if kt < 0:
                            continue
                        tiles.append(dict(kT=KT[0:67, g, kt * 128:(kt + 1) * 128], v=V[:, g, kt, :], kbufs=[KT.b, V.b],
                                          bias=self.kb[:, 12 + h, r + 60:r + 61], bbufs=[self.kb.b],
                                          extra=[(self.ident[:, :], SM[:, r + 1, :], [self.ident.b, SM.b])]))
                    O = R.O[R.io % len(R.O)]
                    R.io += 1
                    Ov = self.attend(R, q[0:67, h, :], [q.b], tiles, O, 0.125)
                    self.recip_den(Ov, O, den, add_ap=esk[:, h:h + 1], add_bufs=[esk.b])
                    for qi in range(4):
                        S.op("dve", lambda: nc.vector.tensor_scalar(out=ms[:, qi, h * 64:(h + 1) * 64], in0=Ov[:, qi, 0:64], scalar1=den[:, qi:qi + 1],
                                                                    scalar2=None, op0=ALU.mult), reads=[O.b, den.b], writes=[ms.b])
                S.store("pool", self.mix[c * 512:(c + 1) * 512, 384:512].rearrange("(a p) d -> p a d", p=128), ms[:, :, :], ms.b)
            S.barrier()

    def diff(self, l):
        nc, S = self.nc, self.S
        lambda_init = 0.8 - 0.6 * math.exp(-0.3 * l)
        sc = 32 ** -0.5
        with ExitStack() as ph:
            KT = self.tile(ph, "KTd", [128, 2, S_LEN], BF16)
            V = self.tile(ph, "Vd", [128, 2, NT, 65], BF16)
            S.op("pool", lambda: nc.gpsimd.memset(V[:, :, :, 64:65], 1.0), writes=[V.b])
            for h in range(2):
                r0 = FRr["dk"] + 64 * h
                S.load("sp", KT[0:32, h, :], self.featT[r0:r0 + 32, :], KT.b)
                S.load("sp", KT[64:96, h, :], self.featT[r0 + 32:r0 + 64, :], KT.b)
                S.load("sp", V[:, h, :, 0:64], self.vtok[:, VCr["dv"] + h * 64:VCr["dv"] + (h + 1) * 64].rearrange("(kt p) d -> p kt d", p=128), V.b)
            ones = self.dc["ones3"][:, 0:2 * S_LEN].rearrange("r (h s) -> r h s", h=2)
            S.load("sp", KT[32:35, :, :], ones, KT.b)
            S.load("sp", KT[96:99, :, :], ones, KT.b)
            lam = self.tile(ph, "lam", [128, 4], F32)
            lt = self.tile(ph, "lamt", [128, 4, 32], F32)
            lj = self.tile(ph, "lamj", [128, 32], F32)
            for i, nme in enumerate(["lq1", "lk1", "lq2", "lk2"]):
                S.load("sp", lt[:, i, :], self.dp[nme][l].partition_broadcast(128), lt.b)
            for i in range(2):
                S.op("dve", lambda: nc.vector.scalar_tensor_tensor(out=lj[:, :], in0=lt[:, 2 * i, :], scalar=1.0, in1=lt[:, 2 * i + 1, :],
                                                                   op0=ALU.mult, op1=ALU.mult, accum_out=lam[:, i:i + 1]),
                     reads=[lt.b], writes=[lj.b, lam.b])
            S.op("act", lambda: nc.scalar.activation(out=lam[:, 0:2], in_=lam[:, 0:2], func=AF.Exp), reads=[lam.b], writes=[lam.b])
            S.op("dve", lambda: nc.vector.tensor_tensor(out=lam[:, 2:3], in0=lam[:, 1:2], in1=lam[:, 0:1], op=ALU.subtract),
                 reads=[lam.b], writes=[lam.b])
            S.op("dve", lambda: nc.vector.tensor_scalar(out=lam[:, 2:3], in0=lam[:, 2:3], scalar1=-lambda_init, scalar2=None, op0=ALU.add),
                 reads=[lam.b], writes=[lam.b])
            gs = self.tile(ph, "gsub", [128, 64], F32)
            S.load("sp", gs[:, :], self.dp["subln"][l].partition_broadcast(128), gs.b)
            S.op("dve", lambda: nc.vector.tensor_scalar(out=gs[:, :], in0=gs[:, :], scalar1=1.0 - lambda_init, scalar2=None, op0=ALU.mult),
                 reads=[gs.b], writes=[gs.b])
            R = self.attn_res(ph, no=4)
            QT = [self.tile(ph, "QTd%d" % i, [128, 2, 512], BF16) for i in range(2)]
            for q in QT:
                a = self.dc["aug"][8:10].rearrange("h r q -> r h q")
                S.load("sp", q[32:35, :, :], a, q.b)
                S.load("sp", q[96:99, :, :], a, q.b)
            den1 = self.tile(ph, "den1", [128, 4], F32)
            den2 = self.tile(ph, "den2", [128, 4], F32)
            o1 = self.tile(ph, "o1d", [128, 4, 64], F32)
            od = self.tile(ph, "od", [128, 4, 64], F32)
            jk = self.tile(ph, "jkd", [128, 64], F32)
            ssd = self.tile(ph, "ssd", [128, 4], F32)
            mst = [self.tile(ph, "mstd%d" % i, [128, 4, 128], BF16) for i in range(2)]
            for c in range(self.nch):
                q = QT[c % 2]
                for h in range(2):
                    r0 = FRr["dq"] + 64 * h
                    S.load("sp", q[0:32, h, :], self.featT[r0:r0 + 32, c * 512:(c + 1) * 512], q.b)
                    S.load("sp", q[64:96, h, :], self.featT[r0 + 32:r0 + 64, c * 512:(c + 1) * 512], q.b)
                ms = mst[c % 2]
                for h in range(2):
                    Os = []
                    for j in range(2):
                        b0 = 64 * j
                        tiles = []
                        for kt in range(4 * c + 4):
                            if far_skip(2, h, 512 * c - (128 * kt + 127)):
                                continue
                            r = kt - 4 * c
                            ex = []
                            if r >= 0:
                                ex.append((self.ident[:, :], self.caus[:, r, :], [self.ident.b, self.caus.b]))
                            tiles.append(dict(kT=KT[b0:b0 + 35, h, kt * 128:(kt + 1) * 128], v=V[:, h, kt, :], kbufs=[KT.b, V.b],
                                              bias=self.kb[:, 8 + h, r + 60:r + 61], bbufs=[self.kb.b], extra=ex, q0=128 * max(r, 0)))
                        O = R.O[R.io % len(R.O)]
                        R.io += 1
                        Ov = self.attend(R, q[b0:b0 + 35, h, :], [q.b], tiles, O, sc)
                        Os.append((O, Ov))
                    (O1, Ov1), (O2, Ov2) = Os
                    self.recip_den(Ov1, O1, den1)
                    self.recip_den(Ov2, O2, den2)
                    S.op("dve", lambda: nc.vector.tensor_scalar(out=den2[:, :], in0=den2[:, :], scalar1=lam[:, 2:3], scalar2=None, op0=ALU.mult),
                         reads=[den2.b, lam.b], writes=[den2.b])
                    for qi in range(4):
                        S.op("dve", lambda: nc.vector.tensor_scalar(out=o1[:, qi, :], in0=Ov1[:, qi, 0:64], scalar1=den1[:, qi:qi + 1], scalar2=None, op0=ALU.mult),
                             reads=[O1.b, den1.b], writes=[o1.b])
                        S.op("dve", lambda: nc.vector.scalar_tensor_tensor(out=od[:, qi, :], in0=Ov2[:, qi, 0:64], scalar=den2[:, qi:qi + 1], in1=o1[:, qi, :],
                                                                           op0=ALU.mult, op1=ALU.add), reads=[O2.b, den2.b, o1.b], writes=[od.b])
                        S.op("dve", lambda: nc.vector.scalar_tensor_tensor(out=jk[:, :], in0=od[:, qi, :], scalar=1.0, in1=od[:, qi, :],
                                                                           op0=ALU.mult, op1=ALU.mult, accum_out=ssd[:, qi:qi + 1]),
                             reads=[od.b], writes=[jk.b, ssd.b])
                    S.op("act", lambda: nc.scalar.activation(out=ssd[:, :], in_=ssd[:, :], func=AF.Ln, bias=self.epsT[:, 0:1], scale=1.0 / 64),
                         reads=[ssd.b, self.epsT.b], writes=[ssd.b])
                    S.op("act", lambda: nc.scalar.activation(out=ssd[:, :], in_=ssd[:, :], func=AF.Exp, scale=-0.5), reads=[ssd.b], writes=[ssd.b])
                    for qi in range(4):
                        S.op("dve", lambda: nc.vector.scalar_tensor_tensor(out=ms[:, qi, h * 64:(h + 1) * 64], in0=od[:, qi, :], scalar=ssd[:, qi:qi + 1],
                                                                           in1=gs[:, :], op0=ALU.mult, op1=ALU.mult),
                             reads=[od.b, ssd.b, gs.b], writes=[ms.b])
                S.store("pool", self.mix[c * 512:(c + 1) * 512, 256:384].rearrange("(a p) d -> p a d", p=128), ms[:, :, :], ms.b)
            S.barrier()

    def nsa(self, l):
        nc, S = self.nc, self.S
        with ExitStack() as ph:
            KCT = self.tile(ph, "KCT", [128, 512], BF16)
            VCt = self.tile(ph, "VCt", [128, 4, 65], BF16)
            S.op("pool", lambda: nc.gpsimd.memset(VCt[:, :, :], 1.0), writes=[VCt.b])
            S.load("sp", KCT[64:67, :], self.dc["ones3"][:, 0:512], KCT.b)
            with ExitStack() as cs:
                XT = self.tile(cs, "XTc", [64, S_LEN], BF16)
                w1 = self.tile(cs, "w1c", [64, 32, 128], BF16)
                w2 = self.tile(cs, "w2c", [128, 64], BF16)
                pos = self.tile(cs, "posc", [64, 32], BF16)
                b1 = self.tile(cs, "b1c", [128, 1], F32)
                cbias = self.tile(cs, "cbias", [128, 1], F32)
                hid = self.tile(cs, "hidc", [128, 512], BF16)
                sg = self.tile(cs, "sgc", [128, 4096], F32)
                hp = self.tile(cs, "hpc", [128, 512], F32, psum=True)
                cp = self.tile(cs, "cpc", [128, 512], F32, psum=True)
                op_ = self.tile(cs, "opc", [128, 512], F32, psum=True)
                for s_i, sfx in enumerate("kv"):
                    S.load("sp", XT[:, :], self.featT[FRr["nkc" if sfx == "k" else "nvc"]:FRr["nkc" if sfx == "k" else "nvc"] + 64, :], XT.b)
                    S.load("sp", sg[0:64, 0:4096], self.dp["w1" + sfx][l].rearrange("d l f -> d (l f)"), sg.b)
                    self.copy(w1[:, :, :], sg[0:64, 0:4096].rearrange("d (l f) -> d l f", l=32), [sg.b], [w1.b])
                    S.load("sp", sg[:, 0:64], self.dp["w2" + sfx][l], sg.b)
                    self.copy(w2[:, :], sg[:, 0:64], [sg.b], [w2.b])
                    S.load("sp", sg[0:64, 0:32], self.dp["pos" + sfx][l], sg.b)
                    self.copy(pos[:, :], sg[0:64, 0:32], [sg.b], [pos.b])
                    S.load("sp", b1[:, :], self.dp["b1" + sfx][l], b1.b)
                    for li in range(32):
                        S.op("pe", lambda: nc.tensor.matmul(hp[:, 0:511], lhsT=w1[:, li, :], rhs=XT[:, li:li + 16 * 510 + 1:16], start=(li == 0), stop=(li == 31)),
                             reads=[w1.b, XT.b], writes=[hp.b], accum=(li > 0))
                    for li in range(32):
                        S.op("pe", lambda: nc.tensor.matmul(cp[:, 0:1], lhsT=w1[:, li, :], rhs=pos[:, li:li + 1], start=(li == 0), stop=(li == 31)),
                             reads=[w1.b, pos.b], writes=[cp.b], accum=(li > 0))
                    S.op("dve", lambda: nc.vector.tensor_tensor(out=cbias[:, :], in0=cp[:, 0:1], in1=b1[:, :], op=ALU.add),
                         reads=[cp.b, b1.b], writes=[cbias.b])
                    S.op("pool", lambda: nc.gpsimd.memset(hid[:, :], 0.0), writes=[hid.b])
                    S.op("act", lambda: nc.scalar.activation(out=hid[:, 0:511], in_=hp[:, 0:511], func=AF.Gelu_apprx_tanh, bias=cbias[:, 0:1]),
                         reads=[hp.b, cbias.b], writes=[hid.b])
                    if sfx == "k":
                        S.op("pe", lambda: nc.tensor.matmul(op_[0:64, 0:512], lhsT=w2[:, :], rhs=hid[:, :], start=True, stop=True),
                             reads=[w2.b, hid.b], writes=[op_.b])
                        self.copy(KCT[0:64, :], op_[0:64, 0:512], [op_.b], [KCT.b])
                    else:
                        ov_ = op_[:, 0:256].rearrange("p (a b) -> p a b", a=4)
                        for kt in range(4):
                            S.op("pe", lambda: nc.tensor.matmul(ov_[:, kt, :], lhsT=hid[:, kt * 128:(kt + 1) * 128], rhs=w2[:, :], start=True, stop=True),
                                 reads=[w2.b, hid.b], writes=[op_.b], accum=(kt > 0))
                        self.copy(VCt[:, :, 0:64], ov_, [op_.b], [VCt.b])
                S.barrier()
            KTs = self.tile(ph, "KTsl", [128, 1, S_LEN], BF16)
            KTw = self.tile(ph, "KTwn", [128, 1, S_LEN], BF16)
            Vs = self.tile(ph, "Vsl", [128, 1, NT, 65], BF16)
            Vw = self.tile(ph, "Vwn", [128, 1, NT, 65], BF16)
            self.load_kv(KTs, Vs, 67, 1, FRr["nks"], VCr["nvs"], 1)
            self.load_kv(KTw, Vw, 67, 1, FRr["nkw"], VCr["nvw"], 1)
            ES = self.tile(ph, "ESn", [128, S_LEN], BF16)
            WM = self.tile(ph, "WMn", [128, 8, 512], BF16)
            CM = self.tile(ph, "CMn", [128, 5, 512], BF16)
            OVt = self.tile(ph, "OVn", [128, 4, 128], BF16)
            KBC = self.tile(ph, "KBCn", [128, 4, 16, 4], F32)
            S.load("sp", ES[:, :], self.dc["es"], ES.b)
            S.load("sp", WM[:, :, :], self.dc["wmask"], WM.b)
            S.load("sp", CM[:, :, :], self.dc["cmask"], CM.b)
            S.load("sp", OVt[:, :, :], self.dc["ov"], OVt.b)
            S.load("sp", KBC[:, :, :, :], self.dc["kbc"], KBC.b)
            R = self.attn_res(ph)
            QT = [self.tile(ph, "QTn%d" % i, [128, 4, 512], BF16) for i in range(2)]
            for q in QT:
                S.load("sp", q[64:67, :, :], self.dc["aug"][4:8].rearrange("h r q -> r h q"), q.b)
            imps = self.tile(ph, "imps", [128, 512], F32, psum=True)
            tps = self.tile(ph, "tpsn", [128, 1024], BF16, psum=True)
            impa = self.tile(ph, "impa", [128, 4, 128], F32)
            f1 = [self.tile(ph, "f1e4_%d" % i, [128, 4, 128], F32) for i in range(2)]
            nv = [self.tile(ph, "negv_%d" % i, [128, 4, 128], F32) for i in range(2)]
            gr = [self.tile(ph, "graw%d" % i, [128, 4, 12], BF16) for i in range(2)]
            sgm = self.tile(ph, "sgm", [128, 4, 12], F32)
            scm = self.tile(ph, "scm", [128, 4, 128], F32)
            tmpm = self.tile(ph, "tmpm", [128, 4, 128], F32)
            m8a = self.tile(ph, "m8a", [128, 4, 8], F32)
            m8b = self.tile(ph, "m8b", [128, 4, 8], F32)
            sel = self.tile(ph, "seln", [128, 4, 128], F32)
            val = self.tile(ph, "valn", [128, 4, 128], F32)
            nsel = self.tile(ph, "nseln", [128, 4, 128], BF16)
            nselT = self.tile(ph, "nselTn", [128, 512], BF16)
            den = self.tile(ph, "denn", [128, 4], F32)
            acc = self.tile(ph, "accn", [128, 2, 4, 64], F32)
            mst = [self.tile(ph, "mstn%d" % i, [128, 4, 128], BF16) for i in range(2)]

            def fold(O, Ov, h, gcol, first):
                self.recip_den(Ov, O, den)
                S.op("dve", lambda: nc.vector.tensor_tensor(out=den[:, :], in0=den[:, :], in1=sgm[:, :, gcol], op=ALU.mult),
                     reads=[den.b, sgm.b], writes=[den.b])
                for qi in range(4):
                    if first:
                        S.op("dve", lambda: nc.vector.tensor_scalar(out=acc[:, h, qi, :], in0=Ov[:, qi, 0:64], scalar1=den[:, qi:qi + 1], scalar2=None, op0=ALU.mult),
                             reads=[O.b, den.b], writes=[acc.b])
                    else:
                        S.op("dve", lambda: nc.vector.scalar_tensor_tensor(out=acc[:, h, qi, :], in0=Ov[:, qi, 0:64], scalar=den[:, qi:qi + 1], in1=acc[:, h, qi, :],
                                                                           op0=ALU.mult, op1=ALU.add), reads=[O.b, den.b, acc.b], writes=[acc.b])

            for c in range(self.nch):
                q = QT[c % 2]
                for h in range(4):
                    S.load("sp", q[0:64, h, :], self.featT[FRr["nq"] + 64 * h:FRr["nq"] + 64 * (h + 1), c * 512:(c + 1) * 512], q.b)
                f1c, nvc, grc = f1[c % 2], nv[c % 2], gr[c % 2]
                S.load("sp", f1c[:, :, :], self.dc["f1e4"][c], f1c.b)
                S.load("sp", nvc[:, :, :], self.dc["negv"][c], nvc.b)
                S.load("sp", grc[:, :, :], self.vtok[c * 512:(c + 1) * 512, VCr["ng"]:VCr["ng"] + 12].rearrange("(a p) g -> p a g", p=128), grc.b)
                S.op("act", lambda: nc.scalar.activation(out=sgm[:, :, :], in_=grc[:, :, :], func=AF.Exp, scale=-1.0), reads=[grc.b], writes=[sgm.b])
                S.op("dve", lambda: nc.vector.tensor_scalar(out=sgm[:, :, :], in0=sgm[:, :, :], scalar1=1.0, scalar2=None, op0=ALU.add),
                     reads=[sgm.b], writes=[sgm.b])
                S.op("dve", lambda: nc.vector.reciprocal(out=sgm[:, :, :], in_=sgm[:, :, :]), reads=[sgm.b], writes=[sgm.b])
                ms = mst[c % 2]
                kb_ = c // 4
                iv = imps[:, :].rearrange("p (a b) -> p a b", a=4)
                for h in range(4):
                    tiles = []
                    ntl = kb_ + 1

                    def mkpost(kt, ntl=ntl):
                        def post(pt, first, last):
                            for qi in range(4):
                                S.op("pe", lambda: nc.tensor.matmul(iv[:, qi, :], lhsT=pt[:, qi * 128:(qi + 1) * 128], rhs=OVt[:, kt, :],
                                                                    start=(first and qi == 0), stop=last),
                                     reads=[pt.b, OVt.b], writes=[imps.b], accum=not (first and qi == 0))
                        return post
                    for kt in range(ntl):
                        ex = []
                        if kt == kb_:
                            ex.append((self.ident[:, :], CM[:, c % 4, :], [self.ident.b, CM.b]))
                        elif kt == kb_ - 1 and c % 4 == 0:
                            ex.append((self.ident[:, :], CM[:, 4, :], [self.ident.b, CM.b]))
                        tiles.append(dict(kT=KCT[0:67, kt * 128:(kt + 1) * 128], v=VCt[:, kt, :], kbufs=[KCT.b, VCt.b],
                                          bias=KBC[:, h, c, kt:kt + 1], bbufs=[KBC.b], extra=ex, post=mkpost(kt)))
                    O = R.O[R.io % len(R.O)]
                    R.io += 1
                    Ov = self.attend(R, q[0:67, h, :], [q.b], tiles, O, 0.125)
                    self.recip_den(Ov, O, den)
                    for qi in range(4):
                        if h == 0:
                            S.op("dve", lambda: nc.vector.tensor_scalar(out=impa[:, qi, :], in0=iv[:, qi, :], scalar1=den[:, qi:qi + 1], scalar2=None, op0=ALU.mult),
                                 reads=[imps.b, den.b], writes=[impa.b])
                        else:
                            S.op("dve", lambda: nc.vector.scalar_tensor_tensor(out=impa[:, qi, :], in0=iv[:, qi, :], scalar=den[:, qi:qi + 1], in1=impa[:, qi, :],
                                                                               op0=ALU.mult, op1=ALU.add), reads=[imps.b, den.b, impa.b], writes=[impa.b])
                    if h < 2:
                        fold(O, Ov, h, 3 * h + 0, True)
                S.op("dve", lambda: nc.vector.tensor_tensor(out=scm[:, :, :], in0=impa[:, :, :], in1=f1c[:, :, :], op=ALU.max),
                     reads=[impa.b, f1c.b], writes=[scm.b])
                S.op("dve", lambda: nc.vector.tensor_tensor(out=scm[:, :, :], in0=scm[:, :, :], in1=nvc[:, :, :], op=ALU.add),
                     reads=[scm.b, nvc.b], writes=[scm.b])
                for qi in range(4):
                    S.op("dve", lambda: nc.vector.max(out=m8a[:, qi, :], in_=scm[:, qi, :]), reads=[scm.b], writes=[m8a.b])
                    S.op("dve", lambda: nc.vector.match_replace(out=tmpm[:, qi, :], in_to_replace=m8a[:, qi, :], in_values=scm[:, qi, :], imm_value=-1e30),
                         reads=[scm.b, m8a.b], writes=[tmpm.b])
                    S.op("dve", lambda: nc.vector.max(out=m8b[:, qi, :], in_=tmpm[:, qi, :]), reads=[tmpm.b], writes=[m8b.b])
                    S.op("dve", lambda: nc.vector.tensor_scalar(out=sel[:, qi, :], in0=scm[:, qi, :], scalar1=m8b[:, qi, 7:8], scalar2=None, op0=ALU.is_ge),
                         reads=[scm.b, m8b.b], writes=[sel.b])
                S.op("dve", lambda: nc.vector.tensor_scalar(out=val[:, :, :], in0=nvc[:, :, :], scalar1=-1.0, scalar2=None, op0=ALU.is_ge),
                     reads=[nvc.b], writes=[val.b])
                S.op("dve", lambda: nc.vector.tensor_tensor(out=sel[:, :, :], in0=sel[:, :, :], in1=val[:, :, :], op=ALU.mult),
                     reads=[sel.b, val.b], writes=[sel.b])
                S.op("dve", lambda: nc.vector.tensor_scalar(out=nsel[:, :, :], in0=sel[:, :, :], scalar1=-1.0, scalar2=-NEG, op0=ALU.add, op1=ALU.mult),
                     reads=[sel.b], writes=[nsel.b])
                for qi in range(4):
                    S.op("pe", lambda: nc.tensor.transpose(out=tps[:, qi * 128:(qi + 1) * 128], in_=nsel[:, qi, :], identity=self.ident[:, :]),
                         reads=[nsel.b, self.ident.b], writes=[tps.b], accum=(qi > 0))
                self.copy(nselT[:, :], tps[:, 0:512], [tps.b], [nselT.b], eng="dve")
                for h in range(2):
                    tiles = []
                    for kt in range(4 * c + 4):
                        if far_skip(1, h, 512 * c - (128 * kt + 127)):
                            continue
                        r = kt - 4 * c
                        ex = [(ES[:, kt * 128:(kt + 1) * 128], nselT[:, :], [ES.b, nselT.b])]
                        if r >= 0:
                            ex.append((self.ident[:, :], self.caus[:, r, :], [self.ident.b, self.caus.b]))
                        tiles.append(dict(kT=KTs[0:67, 0, kt * 128:(kt + 1) * 128], v=Vs[:, 0, kt, :], kbufs=[KTs.b, Vs.b],
                                          bias=self.kb[:, 4 + h, r + 60:r + 61], bbufs=[self.kb.b], extra=ex, q0=128 * max(r, 0)))
                    O = R.O[R.io % len(R.O)]
                    R.io += 1
                    Ov = self.attend(R, q[0:67, h, :], [q.b], tiles, O, 0.125)
                    fold(O, Ov, h, 3 * h + 1, False)
                    tiles = []
                    for r in range(-4, 4):
                        kt = 4 * c + r
                        if kt < 0:
                            continue
                        tiles.append(dict(kT=KTw[0:67, 0, kt * 128:(kt + 1) * 128], v=Vw[:, 0, kt, :], kbufs=[KTw.b, Vw.b],
                                          bias=self.kb[:, 4 + h, r + 60:r + 61], bbufs=[self.kb.b],
                                          extra=[(self.ident[:, :], WM[:, r + 4, :], [self.ident.b, WM.b])]))
                    O = R.O[R.io % len(R.O)]
                    R.io += 1
                    Ov = self.attend(R, q[0:67, h, :], [q.b], tiles, O, 0.125)
                    fold(O, Ov, h, 3 * h + 2, False)
                    S.op("dve", lambda: nc.vector.tensor_copy(out=ms[:, :, h * 64:(h + 1) * 64], in_=acc[:, h, :, :]), reads=[acc.b], writes=[ms.b])
                S.store("pool", self.mix[c * 512:(c + 1) * 512, 128:256].rearrange("(a p) d -> p a d", p=128), ms[:, :, :], ms.b)
            S.barrier()

    def prep_ffn(self, l):
        S = self.S
        with ExitStack() as ph:
            stg = [self.tile(ph, "pstg%d" % i, [128, 4096], F32) for i in range(2)]
            sb = [self.tile(ph, "psb%d" % i, [128, 4096], BF16) for i in range(2)]
            i = 0
            for src, dst in ((self.dp["wg"], self.wgs), (self.dp["wu"], self.wus), (self.dp["wd"], self.wds)):
                for g in range(NCF // 4):
                    s_, b_ = stg[i % 2], sb[i % 2]
                    i += 1
                    if len(src.shape) == 5:
                        sap = src[l, 4 * g:4 * g + 4].rearrange("c p k n -> p c (k n)")
                        dap = dst[4 * g:4 * g + 4].rearrange("c p k n -> p c (k n)")
                    else:
                        sap = src[l, 4 * g:4 * g + 4].rearrange("c p n -> p c n")
                        dap = dst[4 * g:4 * g + 4].rearrange("c p n -> p c n")
                    S.load("sp", s_[:, :].rearrange("p (c n) -> p c n", c=4), sap, s_.b)
                    self.copy(b_[:, :], s_[:, :], [s_.b], [b_.b])
                    S.store("pool", dap, b_[:, :].rearrange("p (c n) -> p c n", c=4), b_.b)
            S.barrier()

    def phase_C(self, l, x_src, x_dst):
        nc, S = self.nc, self.S
        with ExitStack() as ph:
            WO = self.tile(ph, "WO", [128, 8, D], BF16)
            with ExitStack() as tmp:
                self.load_cast(tmp, WO, lambda i: WO[:, i, :], lambda i: self.dp["wo"][l, :, i, :], 8, D)
                S.barrier()
            g1 = self.tile(ph, "g_apost", [128, D], F32)
            g2 = self.tile(ph, "g_fpre", [128, D], F32)
            g3 = self.tile(ph, "g_fpost", [128, D], F32)
            S.load("sp", g1[:, :], self.dp["g_apost"][l].partition_broadcast(128), g1.b)
            S.load("sp", g2[:, :], self.dp["g_fpre"][l].partition_broadcast(128), g2.b)
            S.load("sp", g3[:, :], self.dp["g_fpost"][l].partition_broadcast(128), g3.b)
            cw = self.tile(ph, "cw", [128, NCF, 3], F32)
            cb = self.tile(ph, "cb", [128, NCF], F32)
            S.load("sp", cw[:, :, :], self.dp["cw"][l], cw.b)
            S.load("sp", cb[:, :], self.dp["cb"][l], cb.b)
            halo = self.tile(ph, "halo", [128, NCF, 2], F32)
            S.op("pool", lambda: nc.gpsimd.memset(halo[:, :, :], 0.0), writes=[halo.b])
            bank = [self.tile(ph, "bk%d" % i, [128, 512], F32, psum=True) for i in range(6)]
            tpb = [self.tile(ph, "tpC%d" % i, [128, D], BF16, psum=True) for i in range(2)]
            mixt = [self.tile(ph, "mixt%d" % i, [128, D], BF16) for i in range(2)]
            mT = [self.tile(ph, "mT%d" % i, [128, 8, 128], BF16) for i in range(2)]
            xt = [self.tile(ph, "xtC%d" % i, [128, D], F32) for i in range(2)]
            x1 = [self.tile(ph, "x1C%d" % i, [128, D], F32) for i in range(2)]
            yt = self.tile(ph, "ytC", [128, D], F32)
            junk = self.tile(ph, "junkC", [128, D], F32)
            ss = self.tile(ph, "ssC", [128, 1], F32)
            sd = self.tile(ph, "sdC", [128, 1], F32)
            rstd = self.tile(ph, "rstdC", [128, 1], F32)
            hb = [self.tile(ph, "hbC%d" % i, [128, D], BF16) for i in range(4)]
            h2T = self.tile(ph, "h2T", [128, 8, 512], BF16)
            gT = self.tile(ph, "gT", [128, NCF, 512], BF16)
            wgt = [self.tile(ph, "wgt%d" % i, [128, 8, 128], BF16) for i in range(3)]
            wut = [self.tile(ph, "wut%d" % i, [128, 8, 128], BF16) for i in range(3)]
            wdt = [self.tile(ph, "wdt%d" % i, [128, D], BF16) for i in range(3)]
            aT = [self.tile(ph, "aT%d" % i, [128, 514], F32) for i in range(2)]
            cacc = [self.tile(ph, "cacc%d" % i, [128, 512], F32) for i in range(2)]
            ga = [self.tile(ph, "ga%d" % i, [128, 512], F32) for i in range(2)]
            pst = [self.tile(ph, "pstC%d" % i, [128, D], F32) for i in range(2)]
            rt = [self.tile(ph, "rtC%d" % i, [128, D], F32) for i in range(2)]
            x1r = [self.tile(ph, "x1rC%d" % i, [128, D], F32) for i in range(2)]
            ot = [self.tile(ph, "otC%d" % i, [128, D], F32) for i in range(2)]
            ss3 = self.tile(ph, "ss3C", [128, 1], F32)
            sd3 = self.tile(ph, "sd3C", [128, 1], F32)
            rstd3 = self.tile(ph, "rstd3C", [128, 1], F32)
            junk3 = self.tile(ph, "junk3C", [128, D], F32)
            it = 0
            i3 = 0
            RNG = 2
            pending = []
            store_tags = []

            def finish_tile(tag, tt):
                nonlocal i3
                S._wait("sp", tag)
                tok = tt * 128
                r_, x_, o_ = rt[i3 % 2], x1r[i3 % 2], ot[i3 % 2]
                i3 += 1
                S.load("sp", r_[:, :], self.red[tok:tok + 128, :], r_.b)
                S.load("sp", x_[:, :], self.x1s[tok:tok + 128, :], x_.b)
                self.rstd_of(r_[:, :], r_.b, junk3, ss3, sd3, rstd3)
                S.op("dve", lambda: nc.vector.scalar_tensor_tensor(out=r_[:, :], in0=r_[:, :], scalar=rstd3[:, 0:1], in1=g3[:, :], op0=ALU.mult, op1=ALU.mult),
                     reads=[r_.b, rstd3.b, g3.b], writes=[r_.b])
                S.op("pool", lambda: nc.gpsimd.tensor_tensor(out=o_[:, :], in0=r_[:, :], in1=x_[:, :], op=ALU.add),
                     reads=[r_.b, x_.b], writes=[o_.b])
                S.store("pool", x_dst[tok:tok + 128, :], o_[:, :], o_.b)

            h2Ts = [h2T, self.tile(ph, "h2Tb", [128, 8, 512], BF16)]

            def seg_a1(T):
                tok = T * 128
                mt, mTt, tpp = mixt[T % 2], mT[T % 2], tpb[0]
                mrow = (tok // 2048) * 4096 + (tok % 2048)
                S.load("sp", mt[:, 0:512], self.mixall[mrow:mrow + 128, :], mt.b)
                S.load("sp", mt[:, 512:1024], self.mixall[2048 + mrow:2048 + mrow + 128, :], mt.b)
                for kc in range(8):
                    S.op("pe", lambda: nc.tensor.transpose(out=tpp[:, kc * 128:(kc + 1) * 128], in_=mt[:, kc * 128:(kc + 1) * 128], identity=self.ident[:, :]),
                         reads=[mt.b, self.ident.b], writes=[tpp.b], accum=(kc > 0))
                self.copy(mTt[:, :, :], tpp[:, :].rearrange("p (k t) -> p k t", k=8), [tpp.b], [mTt.b])

            def seg_a2(T):
                tok = T * 128
                mTt, xtt, h, x1t = mT[T % 2], xt[T % 2], hb[T % 4], x1[T % 2]
                S.load("sp", xtt[:, :], x_src[tok:tok + 128, :], xtt.b)
                pa, pb = bank[4], bank[5]
                for half, pp in enumerate((pa, pb)):
                    for kc in range(8):
                        S.op("pe", lambda: nc.tensor.matmul(pp[:, :], lhsT=mTt[:, kc, :], rhs=WO[:, kc, half * 512:(half + 1) * 512],
                                                            start=(kc == 0), stop=(kc == 7)), reads=[mTt.b, WO.b], writes=[pp.b], accum=(kc > 0))
                self.copy(yt[:, 0:512], pa[:, :], [pa.b], [yt.b], eng="act")
                self.copy(yt[:, 512:D], pb[:, :], [pb.b], [yt.b], eng="act")
                self.rstd_of(yt[:, :], yt.b, junk, ss, sd, rstd)
                S.op("dve", lambda: nc.vector.scalar_tensor_tensor(out=yt[:, :], in0=yt[:, :], scalar=rstd[:, 0:1], in1=g1[:, :], op0=ALU.mult, op1=ALU.mult),
                     reads=[yt.b, rstd.b, g1.b], writes=[yt.b])
                S.op("pool", lambda: nc.gpsimd.tensor_tensor(out=x1t[:, :], in0=yt[:, :], in1=xtt[:, :], op=ALU.add),
                     reads=[yt.b, xtt.b], writes=[x1t.b])
                S.store("pool", self.x1s[tok:tok + 128, :], x1t[:, :], x1t.b)
                store_tags.append(("dma", x1t.b.dsem, x1t.b.dcnt))
                self.rstd_of(x1t[:, :], x1t.b, junk, ss, sd, rstd)
                S.op("dve", lambda: nc.vector.scalar_tensor_tensor(out=h[:, :], in0=x1t[:, :], scalar=rstd[:, 0:1], in1=g2[:, :], op0=ALU.mult, op1=ALU.mult),
                     reads=[x1t.b, rstd.b, g2.b], writes=[h.b])

            def seg_b3(T):
                h, tpp = hb[T % 4], tpb[1]
                hT = h2Ts[(T // 4) % 2]
                ti = T % 4
                for kc in range(8):
                    S.op("pe", lambda: nc.tensor.transpose(out=tpp[:, kc * 128:(kc + 1) * 128], in_=h[:, kc * 128:(kc + 1) * 128], identity=self.ident[:, :]),
                         reads=[h.b, self.ident.b], writes=[tpp.b], accum=(kc > 0))
                self.copy(hT[:, :, ti * 128:(ti + 1) * 128], tpp[:, :].rearrange("p (k t) -> p k t", k=8), [tpp.b], [hT.b])

            def seg_b(c, ti, ctx):
                h, tpp = ctx
                hT = h2Ts[c % 2]
                for kc in range(8):
                    S.op("pe", lambda: nc.tensor.transpose(out=tpp[:, kc * 128:(kc + 1) * 128], in_=h[:, kc * 128:(kc + 1) * 128], identity=self.ident[:, :]),
                         reads=[h.b, self.ident.b], writes=[tpp.b], accum=(kc > 0))
                self.copy(hT[:, :, ti * 128:(ti + 1) * 128], tpp[:, :].rearrange("p (k t) -> p k t", k=8), [tpp.b], [hT.b])

            def c2a_cf(c, cf):
                hT = h2Ts[c % 2]
                wg_, wu_ = wgt[cf % 3], wut[cf % 3]
                S.load("sp", wg_[:, :, :], self.wgs[cf], wg_.b)
                S.load("sp", wu_[:, :, :], self.wus[cf], wu_.b)
                pa, pu = bank[2 * (cf % 2)], bank[2 * (cf % 2) + 1]
                for kc in range(8):
                    S.op("pe", lambda: nc.tensor.matmul(pa[:, :], lhsT=wg_[:, kc, :], rhs=hT[:, kc, :], start=(kc == 0), stop=(kc == 7)),
                         reads=[wg_.b, hT.b], writes=[pa.b], accum=(kc > 0))
                for kc in range(8):
                    S.op("pe", lambda: nc.tensor.matmul(pu[:, :], lhsT=wu_[:, kc, :], rhs=hT[:, kc, :], start=(kc == 0), stop=(kc == 7)),
                         reads=[wu_.b, hT.b], writes=[pu.b], accum=(kc > 0))
                a, ca, g = aT[cf % 2], cacc[cf % 2], ga[cf % 2]
                e4 = a[:, 0:4]
                S.op("dve", lambda: nc.vector.tensor_copy(out=a[:, 0:2], in_=halo[:, cf, :]), reads=[halo.b], writes=[a.b])
                S.op("dve", lambda: nc.vector.tensor_copy(out=a[:, 2:4], in_=pa[:, 0:2]), reads=[pa.b], writes=[a.b])
                S.op("dve", lambda: nc.vector.tensor_copy(out=halo[:, cf, :], in_=pa[:, 510:512]), reads=[pa.b], writes=[halo.b])
                S.op("dve", lambda: nc.vector.tensor_scalar(out=ca[:, 2:512], in0=pa[:, 0:510], scalar1=cw[:, cf, 0:1], scalar2=None, op0=ALU.mult),
                     reads=[pa.b, cw.b], writes=[ca.b])
                S.op("dve", lambda: nc.vector.scalar_tensor_tensor(out=ca[:, 2:512], in0=pa[:, 1:511], scalar=cw[:, cf, 1:2], in1=ca[:, 2:512], op0=ALU.mult, op1=ALU.add),
                     reads=[pa.b, cw.b, ca.b], writes=[ca.b])
                S.op("dve", lambda: nc.vector.scalar_tensor_tensor(out=ca[:, 2:512], in0=pa[:, 2:512], scalar=cw[:, cf, 2:3], in1=ca[:, 2:512], op0=ALU.mult, op1=ALU.add),
                     reads=[pa.b, cw.b, ca.b], writes=[ca.b])
                S.op("dve", lambda: nc.vector.tensor_scalar(out=ca[:, 0:2], in0=e4[:, 0:2], scalar1=cw[:, cf, 0:1], scalar2=None, op0=ALU.mult),
                     reads=[a.b, cw.b], writes=[ca.b])
                S.op("dve", lambda: nc.vector.scalar_tensor_tensor(out=ca[:, 0:2], in0=e4[:, 1:3], scalar=cw[:, cf, 1:2], in1=ca[:, 0:2], op0=ALU.mult, op1=ALU.add),
                     reads=[a.b, cw.b, ca.b], writes=[ca.b])
                S.op("dve", lambda: nc.vector.scalar_tensor_tensor(out=ca[:, 0:2], in0=e4[:, 2:4], scalar=cw[:, cf, 2:3], in1=ca[:, 0:2], op0=ALU.mult, op1=ALU.add),
                     reads=[a.b, cw.b, ca.b], writes=[ca.b])
                S.op("act", lambda: nc.scalar.activation(out=g[:, :], in_=ca[:, :], func=AF.Gelu_apprx_tanh, bias=cb[:, cf:cf + 1]),
                     reads=[ca.b, cb.b], writes=[g.b])
                S.op("dve", lambda: nc.vector.tensor_tensor(out=gT[:, cf, :], in0=g[:, :], in1=pu[:, :], op=ALU.mult),
                     reads=[g.b, pu.b], writes=[gT.b])

            def c2b(c):
                for tp2 in range(2):
                    accs = [bank[0], bank[1], bank[2], bank[3]]
                    for cf in range(NCF):
                        wd_ = wdt[cf % 3]
                        S.load("sp", wd_[:, :], self.wds[cf], wd_.b)
                        for j in range(2):
                            ti = 2 * tp2 + j
                            for half in range(2):
                                pp = accs[2 * j + half]
                                S.op("pe", lambda: nc.tensor.matmul(pp[:, :], lhsT=gT[:, cf, ti * 128:(ti + 1) * 128], rhs=wd_[:, half * 512:(half + 1) * 512],
                                                                    start=(cf == 0), stop=(cf == NCF - 1)), reads=[gT.b, wd_.b], writes=[pp.b], accum=(cf > 0))
                    for j in range(2):
                        ti = 2 * tp2 + j
                        tok = (4 * c + ti) * 128
                        p_ = pst[j]
                        self.copy(p_[:, 0:512], accs[2 * j][:, :], [accs[2 * j].b], [p_.b], eng="act")
                        self.copy(p_[:, 512:D], accs[2 * j + 1][:, :], [accs[2 * j + 1].b], [p_.b], eng="dve")
                        S.store("pool", self.part[tok:tok + 128, :], p_[:, :], p_.b)
                        store_tags.append(("dma", p_.b.dsem, p_.b.dcnt))

            NTL = 4 * self.nch
            fifo = []
            for s_ in range(-7, 0):
                if 0 <= s_ + 4 < NTL:
                    seg_b3(s_ + 4)
                if 0 <= s_ + 6 < NTL:
                    seg_a2(s_ + 6)
                if 0 <= s_ + 7 < NTL:
                    seg_a1(s_ + 7)
            for c in range(self.nch):
                for cf in range(NCF):
                    c2a_cf(c, cf)
                    if cf % 4 == 3:
                        s_ = 4 * c + cf // 4
                        if s_ + 4 < NTL:
                            seg_b3(s_ + 4)
                        if s_ + 6 < NTL:
                            seg_a2(s_ + 6)
                        if s_ + 7 < NTL:
                            seg_a1(s_ + 7)
                        if fifo and fifo[0][2] <= c:
                            tg, tt, _ = fifo.pop(0)
                            finish_tile(tg, tt)
                c2b(c)
                if c % RNG == RNG - 1 or c == self.nch - 1:
                    c0 = (c // RNG) * RNG
                    r0, r1 = c0 * 512, (c + 1) * 512
                    for tg in store_tags:
                        S._wait("pool", tg)
                    store_tags = []
                    tag = self.collective("AllReduce", ALU.add, self.part[r0:r1, :].opt(), self.red[r0:r1, :].opt())
                    for tt in range(r0 // 128, r1 // 128):
                        fifo.append((tag, tt, c + 2))
            for tg, tt, _ in fifo:
                finish_tile(tg, tt)
            S.barrier()


_CACHE = {}


def kernel(**inputs):
    x = np.ascontiguousarray(inputs["x"], dtype=np.float32)
    params = [layout_params(inputs, r) for r in range(2)]
    consts = [make_consts(r) for r in range(2)]
    if "prog" not in _CACHE:
        _CACHE["prog"] = Prog()
    prog = _CACHE["prog"]
    in_maps = []
    for core in range(8):
        b, r = core // 2, core % 2
        m = {"x": x[b]}
        for n, _, _ in CONST_SPECS:
            m["c_" + n] = consts[r][n]
        for n, _ in PARAM_SPECS:
            m["p_" + n] = params[r][n]
        in_maps.append(m)
    res = run_bass_kernel_spmd(prog.nc, in_maps, core_ids=list(range(8)))
    out = np.stack([np.asarray(res.results[2 * b]["y"], dtype=np.float32) for b in range(4)], axis=0)
    return out
```

```python
import math
import numpy as np
import ml_dtypes
from contextlib import ExitStack
import concourse.bass as bass
import concourse.mybir as mybir
from concourse.bass_utils import run_bass_kernel_spmd

F32 = mybir.dt.float32
BF16 = mybir.dt.bfloat16
AF = mybir.ActivationFunctionType
ALU = mybir.AluOpType
AX = mybir.AxisListType
NPBF = ml_dtypes.bfloat16

D = 1024
S_LEN = 8192
NT = 64
NCH = 16
DEPTH = 2
DFF = 4096
NEG = -30000.0
EPS = 1e-6

OFF = dict(mq=0, mk=256, mv=512, nq=768, nkc=1024, nvc=1088, nks=1152, nvs=1216, nkw=1280, nvw=1344,
           ng=1408, dq=1420, dk=1676, dv=1932, sq=2188, sk=2444, sv=2572)
FRr = dict(mq=0, mk=128, nq=256, nkc=512, nvc=576, nks=640, nkw=704, dq=768, dk=896, sq=1024, sk=1152)
NF = 1280
VCr = dict(mv=0, nvs=128, nvw=192, dv=256, sv=384, ng=448)
NV = 460
NCF = 16


def role_heads(r, m=0):
    if m == 3:
        return [2 * r, 2 * r + 1], [2 * (1 - r), 2 * (1 - r) + 1]
    return [r, r + 2], [1 - r, 3 - r]


def far_skip(m, j, min_dist):
    sl = min(SLOPES[4 * role_heads(0, m)[0][j] + m], SLOPES[4 * role_heads(1, m)[0][j] + m])
    return min_dist > 0 and sl * min_dist >= 160.0


def role_cols(r):
    own0 = role_heads(r, 0)[0]
    own1, oth1 = role_heads(r, 1)
    own2 = role_heads(r, 2)[0]
    own3 = role_heads(r, 3)[0]

    def hc(base, heads, w=64):
        return np.concatenate([np.arange(base + w * h, base + w * (h + 1)) for h in heads])

    f = np.concatenate([hc(OFF["mq"], own0), hc(OFF["mk"], own0), hc(OFF["nq"], own1 + oth1),
                        np.arange(OFF["nkc"], OFF["nkc"] + 64), np.arange(OFF["nvc"], OFF["nvc"] + 64),
                        np.arange(OFF["nks"], OFF["nks"] + 64), np.arange(OFF["nkw"], OFF["nkw"] + 64),
                        hc(OFF["dq"], own2), hc(OFF["dk"], own2), hc(OFF["sq"], own3), hc(OFF["sk"], [r])])
    v = np.concatenate([hc(OFF["mv"], own0), np.arange(OFF["nvs"], OFF["nvs"] + 64), np.arange(OFF["nvw"], OFF["nvw"] + 64),
                        hc(OFF["dv"], own2), hc(OFF["sv"], [r]), hc(OFF["ng"], own1 + oth1, 3)])
    assert len(f) == 1216 and len(v) == NV
    return f, v


SLOPES = np.power(np.float32(2.0), np.arange(1, 17, dtype=np.float32) * np.float32(-0.5)).astype(np.float64)


class Buf:
    __slots__ = ("name", "w", "rs", "dsem", "dcnt")

    def __init__(self, name):
        self.name = name
        self.w = None
        self.rs = []
        self.dsem = None
        self.dcnt = 0


class Sched:
    def __init__(self, nc, stack):
        self.nc = nc
        self.stack = stack
        self.eng = {"pe": nc.tensor, "act": nc.scalar, "dve": nc.vector, "pool": nc.gpsimd, "sp": nc.sync}
        self.sem, self.cnt, self.seen = {}, {}, {}
        for k in self.eng:
            self.sem[k] = stack.enter_context(nc.semaphore("s_" + k))
            self.cnt[k] = 0
            self.seen[k] = {}
        self.dma_seen = {k: {} for k in self.eng}
        self.free_sems = []
        self.live = []
        self.nsem = len(self.eng)
        self.ninstr = 0

    def _wait(self, ek, dep):
        if dep is None:
            return
        e = self.eng[ek]
        if dep[0] == "dma":
            _, sem, val = dep
            d = self.dma_seen[ek]
            if d.get(id(sem), 0) >= val:
                return
            e.wait_ge(sem, val)
            d[id(sem)] = val
        else:
            pk, c = dep
            if self.seen[ek].get(pk, 0) >= c:
                return
            e.wait_ge(self.sem[pk], c)
            self.seen[ek][pk] = c
        self.ninstr += 1

    def _deps(self, ek, reads, writes, accum):
        for b in reads:
            self._wait(ek, b.w)
        for b in writes:
            if not (accum and b.w is not None and b.w[0] == ek):
                self._wait(ek, b.w)
            for r in b.rs:
                self._wait(ek, r)

    def op(self, ek, fn, reads=(), writes=(), accum=False):
        self._deps(ek, reads, writes, accum)
        ins = fn()
        self.cnt[ek] += 1
        ins.then_inc(self.sem[ek], 1)
        tag = (ek, self.cnt[ek])
        for b in reads:
            b.rs.append(tag)
        for b in writes:
            b.w = tag
            b.rs = []
        self.ninstr += 1
        return ins

    def _dsem(self, b):
        if b.dsem is None:
            if self.free_sems:
                b.dsem, b.dcnt = self.free_sems.pop()
            else:
                b.dsem = self.stack.enter_context(self.nc.semaphore("d%d" % self.nsem))
                b.dcnt = 0
                self.nsem += 1
            self.live.append(b)

    def load(self, qk, out_ap, in_ap, buf):
        self._deps(qk, (), (buf,), False)
        self._dsem(buf)
        ins = self.eng[qk].dma_start(out=out_ap, in_=in_ap)
        buf.dcnt += 16
        ins.then_inc(buf.dsem, 16)
        buf.w = ("dma", buf.dsem, buf.dcnt)
        buf.rs = []
        self.ninstr += 1

    def store(self, qk, out_ap, in_ap, buf):
        self._deps(qk, (buf,), (), False)
        self._dsem(buf)
        ins = self.eng[qk].dma_start(out=out_ap, in_=in_ap)
        buf.dcnt += 16
        ins.then_inc(buf.dsem, 16)
        buf.rs.append(("dma", buf.dsem, buf.dcnt))
        self.ninstr += 1

    def barrier(self):
        for ek in self.eng:
            for pk in self.eng:
                if pk != ek and self.cnt[pk] > 0:
                    self._wait(ek, (pk, self.cnt[pk]))
            for b in self.live:
                self._wait(ek, ("dma", b.dsem, b.dcnt))
        for b in self.live:
            self.free_sems.append((b.dsem, b.dcnt))
            b.dsem = None
        self.live = []


class T:
    def __init__(self, nc, stack, name, shape, dtype, psum=False):
        alloc = nc.psum_tensor if psum else nc.sbuf_tensor
        self.t = stack.enter_context(alloc(name, shape, dtype))
        self.b = Buf(name)

    def __getitem__(self, idx):
        return self.t[idx]


def _split3(v):
    v = np.asarray(v, np.float64)
    hi = v.astype(NPBF)
    r = v - hi.astype(np.float64)
    mid = r.astype(NPBF)
    r = r - mid.astype(np.float64)
    lo = r.astype(NPBF)
    return hi, mid, lo


def make_consts(role=0):
    locs = [sum(role_heads(role, m), []) for m in range(4)]
    c = {}
    c["ident"] = np.eye(128, dtype=np.float32).astype(NPBF)
    iq = np.arange(512, dtype=np.float64)
    p = np.arange(128, dtype=np.float64)
    aug = np.zeros((16, 3, 512), NPBF)
    for m in range(4):
        scale = 32 ** -0.5 if m == 2 else 64 ** -0.5
        for j in range(4):
            sl = SLOPES[4 * locs[m][j] + m]
            hi, mid, lo = _split3(-sl * iq / scale)
            aug[4 * m + j, 0], aug[4 * m + j, 1], aug[4 * m + j, 2] = hi, mid, lo
    c["aug"] = aug
    kb = np.zeros((128, 16, 64), np.float32)
    r = np.arange(-60, 4, dtype=np.float64)
    for m in range(4):
        for j in range(4):
            sl = SLOPES[4 * locs[m][j] + m]
            kb[:, 4 * m + j, :] = (sl * (p[:, None] + 128.0 * r[None, :])).astype(np.float32)
    c["kb"] = kb
    kbc = np.zeros((128, 4, 16, 4), np.float32)
    for j in range(4):
        sl = SLOPES[4 * locs[1][j] + 1]
        for cc in range(16):
            for kt in range(4):
                kbc[:, j, cc, kt] = (sl * (16.0 * (128 * kt + p) + 31.0 - 512.0 * cc)).astype(np.float32)
    c["kbc"] = kbc
    P = p[:, None, None]
    Q = iq[None, None, :]
    k4 = np.arange(4, dtype=np.float64)[None, :, None]
    c["caus"] = np.where(Q >= 128 * k4 + P, 0.0, NEG).astype(np.float32).astype(NPBF)
    r8 = (np.arange(8, dtype=np.float64) - 4)[None, :, None]
    dd = Q - 128 * r8 - P
    c["wmask"] = np.where((dd >= 0) & (dd < 512), 0.0, NEG).astype(np.float32).astype(NPBF)
    r5 = (np.arange(5, dtype=np.float64) - 1)[None, :, None]
    dd = Q - 128 * r5 - P
    c["swamask"] = np.where((dd >= 0) & (dd < 128), 0.0, NEG).astype(np.float32).astype(NPBF)
    d5 = (512.0 * np.arange(5, dtype=np.float64))[None, :, None]
    c["cmask"] = np.where(16 * P + 31 <= d5 + Q, 0.0, NEG).astype(np.float32).astype(NPBF)
    j = np.arange(S_LEN)
    c["em"] = (j[None, :] // 256 == np.arange(32)[:, None]).astype(np.float32).astype(NPBF)
    c["es"] = (j[None, :] // 64 == np.arange(128)[:, None]).astype(np.float32).astype(NPBF)
    ncmp, nsel = 511, 128
    cs = np.arange(512)[:, None] * 16
    bs = np.arange(nsel)[None, :] * 64
    ov = np.clip(np.minimum(cs + 32, bs + 64) - np.maximum(cs, bs), 0, None) / 32.0
    ov[ncmp:, :] = 0.0
    c["ov"] = ov.reshape(4, 128, 128).transpose(1, 0, 2).astype(np.float32).astype(NPBF)
    n32 = np.arange(32, dtype=np.float32)
    qi = np.arange(4)
    c["bi"] = np.broadcast_to((n32[None, None, :] - (qi // 2)[None, :, None].astype(np.float32)), (128, 4, 32)).astype(np.float32).copy()
    s128 = np.arange(128)[None, None, None, :]
    cc = np.arange(16)[:, None, None, None]
    pp = np.arange(128)[None, :, None, None]
    qq = np.arange(4)[None, None, :, None]
    qblk = 8 * cc + 2 * qq + (pp >= 64)
    forced = (s128 == 0) | (s128 == qblk) | (s128 == qblk - 1)
    valid = s128 <= qblk
    c["f1e4"] = np.where(forced, 1e4, 0.0).astype(np.float32)
    c["negv"] = np.where(valid, 0.0, -1e30).astype(np.float32)
    c["ones3"] = np.ones((3, 4 * S_LEN), np.float32).astype(NPBF)
    return c


CONST_SPECS = [("ident", [128, 128], BF16), ("aug", [16, 3, 512], BF16), ("kb", [128, 16, 64], F32),
               ("kbc", [128, 4, 16, 4], F32), ("caus", [128, 4, 512], BF16), ("wmask", [128, 8, 512], BF16),
               ("swamask", [128, 5, 512], BF16), ("cmask", [128, 5, 512], BF16), ("em", [32, S_LEN], BF16),
               ("es", [128, S_LEN], BF16), ("ov", [128, 4, 128], BF16), ("bi", [128, 4, 32], F32),
               ("f1e4", [16, 128, 4, 128], F32), ("negv", [16, 128, 4, 128], F32), ("ones3", [3, 4 * S_LEN], BF16)]

PARAM_SPECS = [("wf", [DEPTH, 128, 8, NF]), ("wv", [DEPTH, 128, 8, NV]), ("wo", [DEPTH, 128, 8, D]),
               ("wg", [DEPTH, NCF, 128, 8, 128]), ("wu", [DEPTH, NCF, 128, 8, 128]), ("wd", [DEPTH, NCF, 128, D]),
               ("g_apre", [DEPTH, D]), ("g_apost", [DEPTH, D]), ("g_fpre", [DEPTH, D]), ("g_fpost", [DEPTH, D]),
               ("cw", [DEPTH, 128, NCF, 3]), ("cb", [DEPTH, 128, NCF]),
               ("posk", [DEPTH, 64, 32]), ("w1k", [DEPTH, 64, 32, 128]), ("b1k", [DEPTH, 128, 1]), ("w2k", [DEPTH, 128, 64]),
               ("posv", [DEPTH, 64, 32]), ("w1v", [DEPTH, 64, 32, 128]), ("b1v", [DEPTH, 128, 1]), ("w2v", [DEPTH, 128, 64]),
               ("lq1", [DEPTH, 32]), ("lk1", [DEPTH, 32]), ("lq2", [DEPTH, 32]), ("lk2", [DEPTH, 32]),
               ("subln", [DEPTH, 64]), ("sinks", [DEPTH, 4])]


def layout_params(inp, role=0):
    o = {}
    w_in = inp["w_in"]
    fc, vc = role_cols(role)
    wf = np.zeros((DEPTH, D, NF), np.float32)
    wf[:, :, :len(fc)] = w_in[:, :, fc]
    o["wf"] = wf.reshape(DEPTH, 8, 128, NF).transpose(0, 2, 1, 3)
    o["wv"] = w_in[:, :, vc].reshape(DEPTH, 8, 128, NV).transpose(0, 2, 1, 3)
    rows = np.concatenate([np.arange(256 * m + 64 * h, 256 * m + 64 * (h + 1)) for rr in range(2) for m in range(4)
                           for h in role_heads(rr, m)[0]])
    o["wo"] = inp["w_out"][:, rows, :].reshape(DEPTH, 8, 128, D).transpose(0, 2, 1, 3)
    cfs = slice(NCF * role, NCF * (role + 1))
    o["wg"] = inp["ffn_w_gate"].reshape(DEPTH, 8, 128, 32, 128).transpose(0, 3, 2, 1, 4)[:, cfs]
    o["wu"] = inp["ffn_w_up"].reshape(DEPTH, 8, 128, 32, 128).transpose(0, 3, 2, 1, 4)[:, cfs]
    o["wd"] = inp["ffn_w_down"].reshape(DEPTH, 32, 128, D)[:, cfs]
    o["g_apre"], o["g_apost"] = inp["attn_pre_norm"], inp["attn_post_norm"]
    o["g_fpre"], o["g_fpost"] = inp["ffn_pre_norm"], inp["ffn_post_norm"]
    o["cw"] = inp["ffn_conv_w"].reshape(DEPTH, 3, 32, 128).transpose(0, 3, 2, 1)[:, :, cfs, :]
    o["cb"] = inp["ffn_conv_b"].reshape(DEPTH, 32, 128).transpose(0, 2, 1)[:, :, cfs]
    for sfx in "kv":
        o["pos" + sfx] = inp["nsa_cmp_pos_" + sfx].transpose(0, 2, 1)
        o["w1" + sfx] = inp["nsa_cmp_w1_" + sfx].transpose(0, 2, 1, 3)
        o["b1" + sfx] = inp["nsa_cmp_b1_" + sfx].reshape(DEPTH, 128, 1)
        o["w2" + sfx] = inp["nsa_cmp_w2_" + sfx]
    o["lq1"], o["lk1"] = inp["diff_lambda_q1"], inp["diff_lambda_k1"]
    o["lq2"], o["lk2"] = inp["diff_lambda_q2"], inp["diff_lambda_k2"]
    o["subln"] = inp["diff_subln"]
    o["sinks"] = inp["swa_sinks"][:, sum(role_heads(role, 3), [])]
    return {k: np.ascontiguousarray(v, dtype=np.float32) for k, v in o.items()}


class Prog:
    def __init__(self, phases=("A", "B", "C"), layers=(0, 1), mixers=(0, 1, 2, 3), debug=False, nchunks=NCH, ncores=8):
        self.phases, self.layers, self.mixers, self.debug, self.nch = phases, layers, mixers, debug, nchunks
        self.groups = [[2 * i, 2 * i + 1] for i in range(ncores // 2)]
        nc = bass.Bass("TRN2", target_bir_lowering=False)
        self.nc = nc
        self.x_in = nc.dram_tensor("x", [S_LEN, D], F32, kind="ExternalInput").ap()
        self.y = nc.dram_tensor("y", [S_LEN, D], F32, kind="ExternalOutput").ap()
        self.dc = {n: nc.dram_tensor("c_" + n, shp, dt, kind="ExternalInput").ap() for n, shp, dt in CONST_SPECS}
        self.dp = {n: nc.dram_tensor("p_" + n, shp, F32, kind="ExternalInput").ap() for n, shp in PARAM_SPECS}
        kind = "ExternalOutput" if debug else "Internal"
        self.featT = nc.dram_tensor("featT", [NF, S_LEN], BF16, kind=kind).ap()
        self.vtok = nc.dram_tensor("vtok", [S_LEN, NV], BF16, kind=kind).ap()
        self.mix_t = nc.dram_tensor("mix", [S_LEN, 512], BF16)
        self.mix = self.mix_t.ap()
        self.mixall_t = nc.dram_tensor("mixall", [2 * S_LEN, 512], BF16)
        self.mixall = self.mixall_t.ap()
        self.part = nc.dram_tensor("part", [S_LEN, D], F32).ap()
        self.red = nc.dram_tensor("red", [S_LEN, D], F32).ap()
        self.x1s = nc.dram_tensor("x1s", [S_LEN, D], F32).ap()
        self.xs = nc.dram_tensor("xs", [S_LEN, D], F32).ap()
        self.wgs = nc.dram_tensor("wgs", [NCF, 128, 8, 128], BF16).ap()
        self.wus = nc.dram_tensor("wus", [NCF, 128, 8, 128], BF16).ap()
        self.wds = nc.dram_tensor("wds", [NCF, 128, D], BF16).ap()
        self.cp_i = 0
        with ExitStack() as st:
            self.st = st
            self.S = Sched(nc, st)
            self.cc_sem = st.enter_context(nc.semaphore("cc_sem"))
            self.cc_cnt = 0
            self.build()
            print("built: instr", self.S.ninstr, "sems", self.S.nsem, flush=True)

    def collective(self, kind, op, in_ap, out_ap):
        ins = self.nc.gpsimd.collective_compute(kind, op, replica_groups=self.groups, ins=[in_ap], outs=[out_ap])
        self.cc_cnt += 1
        ins.then_inc(self.cc_sem, 1)
        self.S.ninstr += 1
        return ("dma", self.cc_sem, self.cc_cnt)

    def tile(self, stack, name, shape, dtype, psum=False):
        self.tile_i = getattr(self, "tile_i", 0) + 1
        return T(self.nc, stack, "%s_%d" % (name, self.tile_i), shape, dtype, psum)

    def copy(self, out_ap, in_ap, reads, writes, eng=None):
        nc = self.nc
        if eng is None:
            eng = "act" if (self.cp_i % 2 == 0) else "dve"
            self.cp_i += 1
        if eng == "act":
            self.S.op("act", lambda: nc.scalar.activation(out=out_ap, in_=in_ap, func=AF.Copy), reads=reads, writes=writes)
        elif eng == "dve":
            self.S.op("dve", lambda: nc.vector.tensor_copy(out=out_ap, in_=in_ap), reads=reads, writes=writes)
        else:
            self.S.op("pool", lambda: nc.gpsimd.tensor_copy(out=out_ap, in_=in_ap), reads=reads, writes=writes)

    def load_cast(self, stack, dst, dst_ap_fn, src_ap_fn, n, ncols):
        stg = [self.tile(stack, "stg%d_%d" % (i, self.S.ninstr), [128, ncols], F32) for i in range(2)]
        for i in range(n):
            s = stg[i % 2]
            self.S.load("sp", s[:, :], src_ap_fn(i), s.b)
            self.copy(dst_ap_fn(i), s[:, :], [s.b], [dst.b])

    def build(self):
        nc, S, st = self.nc, self.S, self.st
        self.ident = self.tile(st, "ident", [128, 128], BF16)
        self.caus = self.tile(st, "caus", [128, 4, 512], BF16)
        self.kb = self.tile(st, "kb", [128, 16, 64], F32)
        self.epsT = self.tile(st, "epsT", [128, 1], F32)
        S.load("sp", self.ident[:, :], self.dc["ident"], self.ident.b)
        S.load("sp", self.caus[:, :, :], self.dc["caus"], self.caus.b)
        S.load("sp", self.kb[:, :, :], self.dc["kb"], self.kb.b)
        S.op("dve", lambda: nc.vector.memset(self.epsT[:, :], EPS), writes=[self.epsT.b])
        for l in self.layers:
            x_src = self.x_in if l == 0 else self.xs
            x_dst = self.y if l == self.layers[-1] else self.xs
            if "A" in self.phases:
                self.phase_A(l, x_src)
                S.barrier()
            if "B" in self.phases:
                for m in self.mixers:
                    [self.moba, self.nsa, self.diff, self.swa][m](l)
                    S.barrier()
            if "C" in self.phases:
                tags = []
                for k in range((self.nch * 512 + 2047) // 2048):
                    tags.append(self.collective("AllGather", ALU.bypass, self.mix[k * 2048:(k + 1) * 2048, :].opt(),
                                                self.mixall[k * 4096:(k + 1) * 4096, :].opt()))
                self.prep_ffn(l)
                for ek in S.eng:
                    S._wait(ek, tags[-1])
                S.barrier()
                self.phase_C(l, x_src, x_dst)
                S.barrier()
        S.barrier()
        if self.debug:
            nc = self.nc
            dbg = {}
            for nm, src, rows, cols, dt in (("d_mixall", self.mixall, 4096, 512, BF16), ("d_part", self.part, 2048, D, F32),
                                            ("d_red", self.red, 2048, D, F32), ("d_x1s", self.x1s, 2048, D, F32), ("d_mix", self.mix, 2048, 512, BF16)):
                dst = nc.dram_tensor(nm, [rows, cols], dt, kind="ExternalOutput").ap()
                b = Buf(nm)
                S._dsem(b)
                ins = nc.sync.dma_start(out=dst[:, :], in_=src[0:rows, :])
                b.dcnt += 16
                ins.then_inc(b.dsem, 16)
                S._wait("sp", ("dma", b.dsem, b.dcnt))

    def rstd_of(self, x_ap, xbuf, junk, ss, sd, rstd, n=D):
        nc, S = self.nc, self.S
        S.op("dve", lambda: nc.vector.scalar_tensor_tensor(out=junk[:, 0:n], in0=x_ap, scalar=1.0, in1=x_ap,
                                                           op0=ALU.mult, op1=ALU.mult, accum_out=ss[:, 0:1]),
             reads=[xbuf], writes=[junk.b, ss.b])
        S.op("act", lambda: nc.scalar.activation(out=sd[:, 0:1], in_=ss[:, 0:1], func=AF.Sqrt,
                                                 bias=self.epsT[:, 0:1], scale=1.0 / n),
             reads=[ss.b, self.epsT.b], writes=[sd.b])
        S.op("dve", lambda: nc.vector.reciprocal(out=rstd[:, 0:1], in_=sd[:, 0:1]), reads=[sd.b], writes=[rstd.b])

    def phase_A(self, l, x_src):
        nc, S = self.nc, self.S
        with ExitStack() as ph:
            WF = self.tile(ph, "WF", [128, 8, NF], BF16)
            WV = self.tile(ph, "WV", [128, 8, NV], BF16)
            gain = self.tile(ph, "gainA", [128, D], F32)
            with ExitStack() as tmp:
                self.load_cast(tmp, WF, lambda i: WF[:, i, :], lambda i: self.dp["wf"][l, :, i, :], 8, NF)
                self.load_cast(tmp, WV, lambda i: WV[:, i, :], lambda i: self.dp["wv"][l, :, i, :], 8, NV)
                S.barrier()
            S.load("sp", gain[:, :], self.dp["g_apre"][l].partition_broadcast(128), gain.b)
            xts = [self.tile(ph, "xtA%d" % i, [128, D], F32) for i in range(3)]
            junk = self.tile(ph, "junkA", [128, D], F32)
            ss = self.tile(ph, "ssA", [128, 1], F32)
            sd = self.tile(ph, "sdA", [128, 1], F32)
            rstd = self.tile(ph, "rstdA", [128, 1], F32)
            hb = [self.tile(ph, "hbA%d" % i, [128, D], BF16) for i in range(2)]
            hT = [self.tile(ph, "hTA%d" % i, [128, 8, 512], BF16) for i in range(2)]
            tp = [self.tile(ph, "tpA%d" % i, [128, D], BF16, psum=True) for i in range(2)]
            psF = [self.tile(ph, "psF%d" % i, [128, 512], F32, psum=True) for i in range(2)]
            psV0 = self.tile(ph, "psV0", [128, 512], F32, psum=True)
            fst = [self.tile(ph, "fst%d" % i, [128, 512], BF16) for i in range(3)]
            vst = [self.tile(ph, "vst%d" % i, [128, NV], BF16) for i in range(2)]
            hb3 = hb + [self.tile(ph, "hbA2", [128, D], BF16)]

            def pa1(T):
                tok = T * 128
                xt, h = xts[T % 3], hb3[T % 3]
                S.load("sp", xt[:, :], x_src[tok:tok + 128, :], xt.b)
                self.rstd_of(xt[:, :], xt.b, junk, ss, sd, rstd)
                S.op("dve", lambda: nc.vector.scalar_tensor_tensor(out=h[:, :], in0=xt[:, :], scalar=rstd[:, 0:1],
                                                                   in1=gain[:, :], op0=ALU.mult, op1=ALU.mult),
                     reads=[xt.b, rstd.b, gain.b], writes=[h.b])

            def pa2(T):
                h, tpp, hTt, ti = hb3[T % 3], tp[T % 2], hT[(T // 4) % 2], T % 4
                for kc in range(8):
                    S.op("pe", lambda: nc.tensor.transpose(out=tpp[:, kc * 128:(kc + 1) * 128],
                                                           in_=h[:, kc * 128:(kc + 1) * 128], identity=self.ident[:, :]),
                         reads=[h.b, self.ident.b], writes=[tpp.b], accum=(kc > 0))
                self.copy(hTt[:, :, ti * 128:(ti + 1) * 128], tpp[:, :].rearrange("p (k t) -> p k t", k=8),
                          [tpp.b], [hTt.b])

            NTL = 4 * self.nch
            for T in range(4):
                pa1(T)
                pa2(T)
            for c in range(self.nch):
                hTc = hT[c % 2]
                for g in range(NF // 128):
                    ps = psF[g % 2]
                    for kc in range(8):
                        S.op("pe", lambda: nc.tensor.matmul(ps[:, :], lhsT=WF[:, kc, g * 128:(g + 1) * 128], rhs=hTc[:, kc, :],
                                                            start=(kc == 0), stop=(kc == 7)),
                             reads=[WF.b, hTc.b], writes=[ps.b], accum=(kc > 0))
                    f = fst[g % 3]
                    self.copy(f[:, :], ps[:, :], [ps.b], [f.b])
                    S.store("pool", self.featT[g * 128:(g + 1) * 128, c * 512:(c + 1) * 512], f[:, :], f.b)
                    if g % 2 == 1 and c + 1 < self.nch:
                        k = g // 2
                        if 1 <= k <= 4:
                            pa2(4 * (c + 1) + k - 1)
                        if k <= 3:
                            pa1(4 * (c + 1) + k)
                for ti in range(4):
                    tok = (4 * c + ti) * 128
                    for kc in range(8):
                        S.op("pe", lambda: nc.tensor.matmul(psV0[:, 0:NV], lhsT=hTc[:, kc, ti * 128:(ti + 1) * 128], rhs=WV[:, kc, 0:NV],
                                                            start=(kc == 0), stop=(kc == 7)),
                             reads=[WV.b, hTc.b], writes=[psV0.b], accum=(kc > 0))
                    v = vst[ti % 2]
                    self.copy(v[:, 0:NV], psV0[:, 0:NV], [psV0.b], [v.b])
                    S.store("pool", self.vtok[tok:tok + 128, :], v[:, :], v.b)
            S.barrier()

    def attn_res(self, ph, nps=3, npt=3, no=3):
        R = type("R", (), {})()
        R.ps = [self.tile(ph, "ps_s%d" % i, [128, 512], F32, psum=True) for i in range(nps)]
        R.pt = [self.tile(ph, "pT%d" % i, [128, 512], BF16) for i in range(npt)]
        R.O = [self.tile(ph, "O%d" % i, [128, 512], F32, psum=True) for i in range(no)]
        R.ips = R.ipt = R.io = 0
        return R

    def attend(self, R, q_ap, qbufs, tiles, O, scale):
        nc, S = self.nc, self.S
        Ov = O[:, 0:260].rearrange("p (a b) -> p a b", a=4)
        n = len(tiles)

        def pv(pt, tl, first, last):
            q0 = tl.get("q0", 0)
            assert not (first and q0)
            for qi in range(q0 // 128, 4):
                S.op("pe", lambda: nc.tensor.matmul(Ov[:, qi, :], lhsT=pt[:, qi * 128:(qi + 1) * 128], rhs=tl["v"],
                                                    start=(first and qi == 0), stop=last),
                     reads=[pt.b] + tl["kbufs"], writes=[O.b], accum=not (first and qi == 0))
            if tl.get("post") is not None:
                tl["post"](pt, first, last)

        prev = None
        for i, tl in enumerate(tiles):
            ps = R.ps[R.ips % len(R.ps)]
            R.ips += 1
            q0 = tl.get("q0", 0)
            ex = tl.get("extra", [])
            S.op("pe", lambda: nc.tensor.matmul(ps[:, q0:512], lhsT=tl["kT"], rhs=q_ap[:, q0:512], start=True, stop=(len(ex) == 0)),
                 reads=qbufs + tl["kbufs"], writes=[ps.b])
            for j, (lh, rh, bufs) in enumerate(ex):
                S.op("pe", lambda: nc.tensor.matmul(ps[:, q0:512], lhsT=lh, rhs=rh[:, q0:512], start=False, stop=(j == len(ex) - 1)),
                     reads=bufs, writes=[ps.b], accum=True)
            pt = R.pt[R.ipt % len(R.pt)]
            R.ipt += 1
            S.op("act", lambda: nc.scalar.activation(out=pt[:, q0:512], in_=ps[:, q0:512], func=AF.Exp, bias=tl["bias"], scale=scale),
                 reads=[ps.b] + tl["bbufs"], writes=[pt.b])
            if prev is not None:
                pv(prev[0], prev[1], prev[2] == 0, False)
            prev = (pt, tl, i)
        pv(prev[0], prev[1], prev[2] == 0, True)
        return Ov

    def load_kv(self, KT, V, krows, nh_k, k_row0, v_col0, nh_v, kd=64):
        nc, S = self.nc, self.S
        S.op("pool", lambda: nc.gpsimd.memset(V[:, :, :, 64:65], 1.0), writes=[V.b])
        for h in range(nh_k):
            S.load("sp", KT[0:kd, h, :], self.featT[k_row0 + h * kd:k_row0 + (h + 1) * kd, :], KT.b)
        S.load("sp", KT[kd:kd + 3, :, :], self.dc["ones3"][:, 0:nh_k * S_LEN].rearrange("r (h s) -> r h s", h=nh_k), KT.b)
        for h in range(nh_v):
            S.load("sp", V[:, h, :, 0:64],
                   self.vtok[:, v_col0 + h * 64:v_col0 + (h + 1) * 64].rearrange("(kt p) d -> p kt d", p=128), V.b)

    def recip_den(self, Ov, O, den, add_ap=None, add_bufs=()):
        nc, S = self.nc, self.S
        if add_ap is None:
            S.op("dve", lambda: nc.vector.tensor_scalar(out=den[:, 0:4], in0=Ov[:, :, 64], scalar1=1e-30, scalar2=None,
                                                        op0=ALU.max), reads=[O.b], writes=[den.b])
        else:
            S.op("dve", lambda: nc.vector.tensor_scalar(out=den[:, 0:4], in0=Ov[:, :, 64], scalar1=add_ap, scalar2=1e-30,
                                                        op0=ALU.add, op1=ALU.max), reads=[O.b] + list(add_bufs), writes=[den.b])
        S.op("dve", lambda: nc.vector.reciprocal(out=den[:, 0:4], in_=den[:, 0:4]), reads=[den.b], writes=[den.b])

    def moba(self, l):
        nc, S = self.nc, self.S
        with ExitStack() as ph:
            KT = self.tile(ph, "KTm", [128, 2, S_LEN], BF16)
            V = self.tile(ph, "Vm", [128, 2, NT, 65], BF16)
            BI = self.tile(ph, "BIm", [128, 4, 32], F32)
            S.load("sp", BI[:, :, :], self.dc["bi"], BI.b)
            S.op("pool", lambda: nc.gpsimd.memset(V[:, :, :, 64:65], 1.0), writes=[V.b])
            S.op("pool", lambda: nc.gpsimd.memset(KT[64:128, :, :], 0.0), writes=[KT.b])
            for h in range(2):
                S.load("sp", KT[96:128, h, :], self.dc["em"], KT.b)
                S.load("sp", KT[0:64, h, :], self.featT[FRr["mk"] + 64 * h:FRr["mk"] + 64 * (h + 1), :], KT.b)
                S.load("sp", V[:, h, :, 0:64],
                       self.vtok[:, VCr["mv"] + h * 64:VCr["mv"] + (h + 1) * 64].rearrange("(kt p) d -> p kt d", p=128), V.b)
            S.load("sp", KT[64:67, :, :], self.dc["ones3"][:, 0:2 * S_LEN].rearrange("r (h s) -> r h s", h=2), KT.b)
            R = self.attn_res(ph)
            QT = [[self.tile(ph, "QTm%d_%d" % (i, h), [128, 512], BF16) for h in range(2)] for i in range(2)]
            for qs in QT:
                for h in range(2):
                    S.op("pool", lambda: nc.gpsimd.memset(qs[h][64:128, :], 0.0), writes=[qs[h].b])
                    S.load("sp", qs[h][64:67, :], self.dc["aug"][h], qs[h].b)
            kms = self.tile(ph, "kms", [128, 2, 32], F32)
            kmh = self.tile(ph, "kmh", [128, 2, 32], BF16)
            kml = self.tile(ph, "kml", [128, 2, 32], BF16)
            kmr = self.tile(ph, "kmr", [128, 2, 32], F32)
            for h in range(2):
                S.op("dve", lambda: nc.vector.tensor_reduce(out=kms[0:64, h, :], in_=KT[0:64, h, :].rearrange("p (n k) -> p n k", k=256),
                                                            axis=AX.X, op=ALU.add), reads=[KT.b], writes=[kms.b])
            S.op("dve", lambda: nc.vector.tensor_scalar(out=kms[0:64, :, :], in0=kms[0:64, :, :], scalar1=1.0 / 256, scalar2=None, op0=ALU.mult),
                 reads=[kms.b], writes=[kms.b])
            S.op("dve", lambda: nc.vector.tensor_copy(out=kmh[0:64, :, :], in_=kms[0:64, :, :]), reads=[kms.b], writes=[kmh.b])
            S.op("dve", lambda: nc.vector.tensor_tensor(out=kmr[0:64, :, :], in0=kms[0:64, :, :], in1=kmh[0:64, :, :], op=ALU.subtract),
                 reads=[kms.b, kmh.b], writes=[kmr.b])
            S.op("dve", lambda: nc.vector.tensor_copy(out=kml[0:64, :, :], in_=kmr[0:64, :, :]), reads=[kmr.b], writes=[kml.b])
            gps = self.tile(ph, "gps", [128, 512], F32, psum=True)
            tps = self.tile(ph, "tpsm", [128, 1024], BF16, psum=True)
            past = self.tile(ph, "past", [128, 4, 32], F32)
            own = self.tile(ph, "own", [128, 4, 32], F32)
            negp = self.tile(ph, "negp", [128, 4, 32], F32)
            gm = self.tile(ph, "gm", [128, 4, 32], F32)
            m8 = self.tile(ph, "m8", [128, 4, 8], F32)
            sel = self.tile(ph, "sel", [128, 4, 32], F32)
            nsel = self.tile(ph, "nsel", [128, 4, 32], BF16)
            den = self.tile(ph, "denm", [128, 4], F32)
            mst = [self.tile(ph, "mstm%d" % i, [128, 4, 128], BF16) for i in range(2)]
            for c in range(self.nch):
                qs = QT[c % 2]
                for h in range(2):
                    S.load("sp", qs[h][0:64, :], self.featT[FRr["mq"] + 64 * h:FRr["mq"] + 64 * (h + 1), c * 512:(c + 1) * 512], qs[h].b)
                S.op("dve", lambda: nc.vector.tensor_scalar(out=past[:, :, :], in0=BI[:, :, :], scalar1=float(2 * c), scalar2=None, op0=ALU.is_lt),
                     reads=[BI.b], writes=[past.b])
                S.op("dve", lambda: nc.vector.tensor_scalar(out=own[:, :, :], in0=BI[:, :, :], scalar1=float(2 * c), scalar2=None, op0=ALU.is_equal),
                     reads=[BI.b], writes=[own.b])
                S.op("dve", lambda: nc.vector.tensor_scalar(out=negp[:, :, :], in0=past[:, :, :], scalar1=-1.0, scalar2=1e30, op0=ALU.add, op1=ALU.mult),
                     reads=[past.b], writes=[negp.b])
                ms = mst[c % 2]
                for h in range(2):
                    q = qs[h]
                    gv = gps[:, 0:128].rearrange("p (a b) -> p a b", a=4)
                    for qi in range(4):
                        S.op("pe", lambda: nc.tensor.matmul(gv[:, qi, :], lhsT=q[0:64, qi * 128:(qi + 1) * 128], rhs=kmh[0:64, h, :], start=True, stop=False),
                             reads=[q.b, kmh.b], writes=[gps.b], accum=(qi > 0))
                        S.op("pe", lambda: nc.tensor.matmul(gv[:, qi, :], lhsT=q[0:64, qi * 128:(qi + 1) * 128], rhs=kml[0:64, h, :], start=False, stop=True),
                             reads=[q.b, kml.b], writes=[gps.b], accum=True)
                    S.op("dve", lambda: nc.vector.tensor_tensor(out=gm[:, :, :], in0=gv, in1=negp[:, :, :], op=ALU.add),
                         reads=[gps.b, negp.b], writes=[gm.b])
                    for qi in range(4):
                        S.op("dve", lambda: nc.vector.max(out=m8[:, qi, :], in_=gm[:, qi, :]), reads=[gm.b], writes=[m8.b])
                    for qi in range(4):
                        S.op("dve", lambda: nc.vector.tensor_scalar(out=sel[:, qi, :], in0=gm[:, qi, :], scalar1=m8[:, qi, 2:3], scalar2=None, op0=ALU.is_ge),
                             reads=[gm.b, m8.b], writes=[sel.b])
                    S.op("dve", lambda: nc.vector.tensor_tensor(out=sel[:, :, :], in0=sel[:, :, :], in1=past[:, :, :], op=ALU.mult),
                         reads=[sel.b, past.b], writes=[sel.b])
                    S.op("dve", lambda: nc.vector.tensor_tensor(out=sel[:, :, :], in0=sel[:, :, :], in1=own[:, :, :], op=ALU.add),
                         reads=[sel.b, own.b], writes=[sel.b])
                    S.op("dve", lambda: nc.vector.tensor_scalar(out=nsel[:, :, :], in0=sel[:, :, :], scalar1=-1.0, scalar2=-NEG, op0=ALU.add, op1=ALU.mult),
                         reads=[sel.b], writes=[nsel.b])
                    for qi in range(4):
                        S.op("pe", lambda: nc.tensor.transpose(out=tps[0:32, qi * 128:(qi + 1) * 128], in_=nsel[:, qi, :], identity=self.ident[:, :]),
                             reads=[nsel.b, self.ident.b], writes=[tps.b], accum=(qi > 0))
                    self.copy(q[96:128, :], tps[0:32, 0:512], [tps.b], [q.b], eng="dve")
                    tiles = []
                    for kt in range(4 * c + 4):
                        if far_skip(0, h, 512 * c - (128 * kt + 127)):
                            continue
                        r = kt - 4 * c
                        ex = []
                        if r >= 0:
                            ex.append((self.ident[:, :], self.caus[:, r, :], [self.ident.b, self.caus.b]))
                        tiles.append(dict(kT=KT[0:128, h, kt * 128:(kt + 1) * 128], v=V[:, h, kt, :], kbufs=[KT.b, V.b],
                                          bias=self.kb[:, 0 + h, r + 60:r + 61], bbufs=[self.kb.b], extra=ex, q0=128 * max(r, 0)))
                    O = R.O[R.io % len(R.O)]
                    R.io += 1
                    Ov = self.attend(R, q[0:128, :], [q.b], tiles, O, 0.125)
                    self.recip_den(Ov, O, den)
                    for qi in range(4):
                        S.op("dve", lambda: nc.vector.tensor_scalar(out=ms[:, qi, h * 64:(h + 1) * 64], in0=Ov[:, qi, 0:64], scalar1=den[:, qi:qi + 1],
                                                                    scalar2=None, op0=ALU.mult), reads=[O.b, den.b], writes=[ms.b])
                S.store("pool", self.mix[c * 512:(c + 1) * 512, 0:128].rearrange("(a p) d -> p a d", p=128), ms[:, :, :], ms.b)
            S.barrier()

    def swa(self, l):
        nc, S = self.nc, self.S
        with ExitStack() as ph:
            KT = self.tile(ph, "KTs", [128, 1, S_LEN], BF16)
            V = self.tile(ph, "Vs", [128, 1, NT, 65], BF16)
            SM = self.tile(ph, "SMs", [128, 5, 512], BF16)
            S.load("sp", SM[:, :, :], self.dc["swamask"], SM.b)
            self.load_kv(KT, V, 67, 1, FRr["sk"], VCr["sv"], 1)
            sk = self.tile(ph, "sinks", [128, 4], F32)
            esk = self.tile(ph, "esinks", [128, 4], F32)
            S.load("sp", sk[:, :], self.dp["sinks"][l].partition_broadcast(128), sk.b)
            S.op("act", lambda: nc.scalar.activation(out=esk[:, :], in_=sk[:, :], func=AF.Exp), reads=[sk.b], writes=[esk.b])
            R = self.attn_res(ph)
            QT = [self.tile(ph, "QTs%d" % i, [128, 2, 512], BF16) for i in range(2)]
            for q in QT:
                S.load("sp", q[64:67, :, :], self.dc["aug"][12:14].rearrange("h r q -> r h q"), q.b)
            den = self.tile(ph, "dens", [128, 4], F32)
            mst = [self.tile(ph, "msts%d" % i, [128, 4, 128], BF16) for i in range(2)]
            for c in range(self.nch):
                q = QT[c % 2]
                for h in range(2):
                    S.load("sp", q[0:64, h, :], self.featT[FRr["sq"] + 64 * h:FRr["sq"] + 64 * (h + 1), c * 512:(c + 1) * 512], q.b)
                ms = mst[c % 2]
                for h in range(2):
                    g = 0
                    tiles = []
                    for r in range(-1, 4):
                        kt = 4 * c + r
                        if kt < 0:
                            continue
                        tiles.append(dict(kT=KT[0:67, g, kt * 128:(kt + 1) * 128], v=V[:, g, kt, :], kbufs=[KT.b, V.b],
                                          bias=self.kb[:, 12 + h, r + 60:r + 61], bbufs=[self.kb.b],
                                          extra=[(self.ident[:, :], SM[:, r + 1, :], [self.ident.b, SM.b])]))
                    O = R.O[R.io % len(R.O)]
                    R.io += 1
                    Ov = self.attend(R, q[0:67, h, :], [q.b], tiles, O, 0.125)
                    self.recip_den(Ov, O, den, add_ap=esk[:, h:h + 1], add_bufs=[esk.b])
                    for qi in range(4):
                        S.op("dve", lambda: nc.vector.tensor_scalar(out=ms[:, qi, h * 64:(h + 1) * 64], in0=Ov[:, qi, 0:64], scalar1=den[:, qi:qi + 1],
                                                                    scalar2=None, op0=ALU.mult), reads=[O.b, den.b], writes=[ms.b])
                S.store("pool", self.mix[c * 512:(c + 1) * 512, 384:512].rearrange("(a p) d -> p a d", p=128), ms[:, :, :], ms.b)
            S.barrier()

    def diff(self, l):
        nc, S = self.nc, self.S
        lambda_init = 0.8 - 0.6 * math.exp(-0.3 * l)
        sc = 32 ** -0.5
        with ExitStack() as ph:
            KT = self.tile(ph, "KTd", [128, 2, S_LEN], BF16)
            V = self.tile(ph, "Vd", [128, 2, NT, 65], BF16)
            S.op("pool", lambda: nc.gpsimd.memset(V[:, :, :, 64:65], 1.0), writes=[V.b])
            for h in range(2):
                r0 = FRr["dk"] + 64 * h
                S.load("sp", KT[0:32, h, :], self.featT[r0:r0 + 32, :], KT.b)
                S.load("sp", KT[64:96, h, :], self.featT[r0 + 32:r0 + 64, :], KT.b)
                S.load("sp", V[:, h, :, 0:64], self.vtok[:, VCr["dv"] + h * 64:VCr["dv"] + (h + 1) * 64].rearrange("(kt p) d -> p kt d", p=128), V.b)
            ones = self.dc["ones3"][:, 0:2 * S_LEN].rearrange("r (h s) -> r h s", h=2)
            S.load("sp", KT[32:35, :, :], ones, KT.b)
            S.load("sp", KT[96:99, :, :], ones, KT.b)
            lam = self.tile(ph, "lam", [128, 4], F32)
            lt = self.tile(ph, "lamt", [128, 4, 32], F32)
            lj = self.tile(ph, "lamj", [128, 32], F32)
            for i, nme in enumerate(["lq1", "lk1", "lq2", "lk2"]):
                S.load("sp", lt[:, i, :], self.dp[nme][l].partition_broadcast(128), lt.b)
            for i in range(2):
                S.op("dve", lambda: nc.vector.scalar_tensor_tensor(out=lj[:, :], in0=lt[:, 2 * i, :], scalar=1.0, in1=lt[:, 2 * i + 1, :],
                                                                   op0=ALU.mult, op1=ALU.mult, accum_out=lam[:, i:i + 1]),
                     reads=[lt.b], writes=[lj.b, lam.b])
            S.op("act", lambda: nc.scalar.activation(out=lam[:, 0:2], in_=lam[:, 0:2], func=AF.Exp), reads=[lam.b], writes=[lam.b])
            S.op("dve", lambda: nc.vector.tensor_tensor(out=lam[:, 2:3], in0=lam[:, 1:2], in1=lam[:, 0:1], op=ALU.subtract),
                 reads=[lam.b], writes=[lam.b])
            S.op("dve", lambda: nc.vector.tensor_scalar(out=lam[:, 2:3], in0=lam[:, 2:3], scalar1=-lambda_init, scalar2=None, op0=ALU.add),
                 reads=[lam.b], writes=[lam.b])
            gs = self.tile(ph, "gsub", [128, 64], F32)
            S.load("sp", gs[:, :], self.dp["subln"][l].partition_broadcast(128), gs.b)
            S.op("dve", lambda: nc.vector.tensor_scalar(out=gs[:, :], in0=gs[:, :], scalar1=1.0 - lambda_init, scalar2=None, op0=ALU.mult),
                 reads=[gs.b], writes=[gs.b])
            R = self.attn_res(ph, no=4)
            QT = [self.tile(ph, "QTd%d" % i, [128, 2, 512], BF16) for i in range(2)]
            for q in QT:
                a = self.dc["aug"][8:10].rearrange("h r q -> r h q")
                S.load("sp", q[32:35, :, :], a, q.b)
                S.load("sp", q[96:99, :, :], a, q.b)
            den1 = self.tile(ph, "den1", [128, 4], F32)
            den2 = self.tile(ph, "den2", [128, 4], F32)
            o1 = self.tile(ph, "o1d", [128, 4, 64], F32)
            od = self.tile(ph, "od", [128, 4, 64], F32)
            jk = self.tile(ph, "jkd", [128, 64], F32)
            ssd = self.tile(ph, "ssd", [128, 4], F32)
            mst = [self.tile(ph, "mstd%d" % i, [128, 4, 128], BF16) for i in range(2)]
            for c in range(self.nch):
                q = QT[c % 2]
                for h in range(2):
                    r0 = FRr["dq"] + 64 * h
                    S.load("sp", q[0:32, h, :], self.featT[r0:r0 + 32, c * 512:(c + 1) * 512], q.b)
                    S.load("sp", q[64:96, h, :], self.featT[r0 + 32:r0 + 64, c * 512:(c + 1) * 512], q.b)
                ms = mst[c % 2]
                for h in range(2):
                    Os = []
                    for j in range(2):
                        b0 = 64 * j
                        tiles = []
                        for kt in range(4 * c + 4):
                            if far_skip(2, h, 512 * c - (128 * kt + 127)):
                                continue
                            r = kt - 4 * c
                            ex = []
                            if r >= 0:
                                ex.append((self.ident[:, :], self.caus[:, r, :], [self.ident.b, self.caus.b]))
                            tiles.append(dict(kT=KT[b0:b0 + 35, h, kt * 128:(kt + 1) * 128], v=V[:, h, kt, :], kbufs=[KT.b, V.b],
                                              bias=self.kb[:, 8 + h, r + 60:r + 61], bbufs=[self.kb.b], extra=ex, q0=128 * max(r, 0)))
                        O = R.O[R.io % len(R.O)]
                        R.io += 1
                        Ov = self.attend(R, q[b0:b0 + 35, h, :], [q.b], tiles, O, sc)
                        Os.append((O, Ov))
                    (O1, Ov1), (O2, Ov2) = Os
                    self.recip_den(Ov1, O1, den1)
                    self.recip_den(Ov2, O2, den2)
                    S.op("dve", lambda: nc.vector.tensor_scalar(out=den2[:, :], in0=den2[:, :], scalar1=lam[:, 2:3], scalar2=None, op0=ALU.mult),
                         reads=[den2.b, lam.b], writes=[den2.b])
                    for qi in range(4):
                        S.op("dve", lambda: nc.vector.tensor_scalar(out=o1[:, qi, :], in0=Ov1[:, qi, 0:64], scalar1=den1[:, qi:qi + 1], scalar2=None, op0=ALU.mult),
                             reads=[O1.b, den1.b], writes=[o1.b])
                        S.op("dve", lambda: nc.vector.scalar_tensor_tensor(out=od[:, qi, :], in0=Ov2[:, qi, 0:64], scalar=den2[:, qi:qi + 1], in1=o1[:, qi, :],
                                                                           op0=ALU.mult, op1=ALU.add), reads=[O2.b, den2.b, o1.b], writes=[od.b])
                        S.op("dve", lambda: nc.vector.scalar_tensor_tensor(out=jk[:, :], in0=od[:, qi, :], scalar=1.0, in1=od[:, qi, :],
                                                                           op0=ALU.mult, op1=ALU.mult, accum_out=ssd[:, qi:qi + 1]),
                             reads=[od.b], writes=[jk.b, ssd.b])
                    S.op("act", lambda: nc.scalar.activation(out=ssd[:, :], in_=ssd[:, :], func=AF.Ln, bias=self.epsT[:, 0:1], scale=1.0 / 64),
                         reads=[ssd.b, self.epsT.b], writes=[ssd.b])
                    S.op("act", lambda: nc.scalar.activation(out=ssd[:, :], in_=ssd[:, :], func=AF.Exp, scale=-0.5), reads=[ssd.b], writes=[ssd.b])
                    for qi in range(4):
                        S.op("dve", lambda: nc.vector.scalar_tensor_tensor(out=ms[:, qi, h * 64:(h + 1) * 64], in0=od[:, qi, :], scalar=ssd[:, qi:qi + 1],
                                                                           in1=gs[:, :], op0=ALU.mult, op1=ALU.mult),
                             reads=[od.b, ssd.b, gs.b], writes=[ms.b])
                S.store("pool", self.mix[c * 512:(c + 1) * 512, 256:384].rearrange("(a p) d -> p a d", p=128), ms[:, :, :], ms.b)
            S.barrier()

    def nsa(self, l):
        nc, S = self.nc, self.S
        with ExitStack() as ph:
            KCT = self.tile(ph, "KCT", [128, 512], BF16)
            VCt = self.tile(ph, "VCt", [128, 4, 65], BF16)
            S.op("pool", lambda: nc.gpsimd.memset(VCt[:, :, :], 1.0), writes=[VCt.b])
            S.load("sp", KCT[64:67, :], self.dc["ones3"][:, 0:512], KCT.b)
            with ExitStack() as cs:
                XT = self.tile(cs, "XTc", [64, S_LEN], BF16)
                w1 = self.tile(cs, "w1c", [64, 32, 128], BF16)
                w2 = self.tile(cs, "w2c", [128, 64], BF16)
                pos = self.tile(cs, "posc", [64, 32], BF16)
                b1 = self.tile(cs, "b1c", [128, 1], F32)
                cbias = self.tile(cs, "cbias", [128, 1], F32)
                hid = self.tile(cs, "hidc", [128, 512], BF16)
                sg = self.tile(cs, "sgc", [128, 4096], F32)
                hp = self.tile(cs, "hpc", [128, 512], F32, psum=True)
                cp = self.tile(cs, "cpc", [128, 512], F32, psum=True)
                op_ = self.tile(cs, "opc", [128, 512], F32, psum=True)
                for s_i, sfx in enumerate("kv"):
                    S.load("sp", XT[:, :], self.featT[FRr["nkc" if sfx == "k" else "nvc"]:FRr["nkc" if sfx == "k" else "nvc"] + 64, :], XT.b)
                    S.load("sp", sg[0:64, 0:4096], self.dp["w1" + sfx][l].rearrange("d l f -> d (l f)"), sg.b)
                    self.copy(w1[:, :, :], sg[0:64, 0:4096].rearrange("d (l f) -> d l f", l=32), [sg.b], [w1.b])
                    S.load("sp", sg[:, 0:64], self.dp["w2" + sfx][l], sg.b)
                    self.copy(w2[:, :], sg[:, 0:64], [sg.b], [w2.b])
                    S.load("sp", sg[0:64, 0:32], self.dp["pos" + sfx][l], sg.b)
                    self.copy(pos[:, :], sg[0:64, 0:32], [sg.b], [pos.b])
                    S.load("sp", b1[:, :], self.dp["b1" + sfx][l], b1.b)
                    for li in range(32):
                        S.op("pe", lambda: nc.tensor.matmul(hp[:, 0:511], lhsT=w1[:, li, :], rhs=XT[:, li:li + 16 * 510 + 1:16], start=(li == 0), stop=(li == 31)),
                             reads=[w1.b, XT.b], writes=[hp.b], accum=(li > 0))
                    for li in range(32):
                        S.op("pe", lambda: nc.tensor.matmul(cp[:, 0:1], lhsT=w1[:, li, :], rhs=pos[:, li:li + 1], start=(li == 0), stop=(li == 31)),
                             reads=[w1.b, pos.b], writes=[cp.b], accum=(li > 0))
                    S.op("dve", lambda: nc.vector.tensor_tensor(out=cbias[:, :], in0=cp[:, 0:1], in1=b1[:, :], op=ALU.add),
                         reads=[cp.b, b1.b], writes=[cbias.b])
                    S.op("pool", lambda: nc.gpsimd.memset(hid[:, :], 0.0), writes=[hid.b])
                    S.op("act", lambda: nc.scalar.activation(out=hid[:, 0:511], in_=hp[:, 0:511], func=AF.Gelu_apprx_tanh, bias=cbias[:, 0:1]),
                         reads=[hp.b, cbias.b], writes=[hid.b])
                    if sfx == "k":
                        S.op("pe", lambda: nc.tensor.matmul(op_[0:64, 0:512], lhsT=w2[:, :], rhs=hid[:, :], start=True, stop=True),
                             reads=[w2.b, hid.b], writes=[op_.b])
                        self.copy(KCT[0:64, :], op_[0:64, 0:512], [op_.b], [KCT.b])
                    else:
                        ov_ = op_[:, 0:256].rearrange("p (a b) -> p a b", a=4)
                        for kt in range(4):
                            S.op("pe", lambda: nc.tensor.matmul(ov_[:, kt, :], lhsT=hid[:, kt * 128:(kt + 1) * 128], rhs=w2[:, :], start=True, stop=True),
                                 reads=[w2.b, hid.b], writes=[op_.b], accum=(kt > 0))
                        self.copy(VCt[:, :, 0:64], ov_, [op_.b], [VCt.b])
                S.barrier()
            KTs = self.tile(ph, "KTsl", [128, 1, S_LEN], BF16)
            KTw = self.tile(ph, "KTwn", [128, 1, S_LEN], BF16)
            Vs = self.tile(ph, "Vsl", [128, 1, NT, 65], BF16)
            Vw = self.tile(ph, "Vwn", [128, 1, NT, 65], BF16)
            self.load_kv(KTs, Vs, 67, 1, FRr["nks"], VCr["nvs"], 1)
            self.load_kv(KTw, Vw, 67, 1, FRr["nkw"], VCr["nvw"], 1)
            ES = self.tile(ph, "ESn", [128, S_LEN], BF16)
            WM = self.tile(ph, "WMn", [128, 8, 512], BF16)
            CM = self.tile(ph, "CMn", [128, 5, 512], BF16)
            OVt = self.tile(ph, "OVn", [128, 4, 128], BF16)
            KBC = self.tile(ph, "KBCn", [128, 4, 16, 4], F32)
            S.load("sp", ES[:, :], self.dc["es"], ES.b)
            S.load("sp", WM[:, :, :], self.dc["wmask"], WM.b)
            S.load("sp", CM[:, :, :], self.dc["cmask"], CM.b)
            S.load("sp", OVt[:, :, :], self.dc["ov"], OVt.b)
            S.load("sp", KBC[:, :, :, :], self.dc["kbc"], KBC.b)
            R = self.attn_res(ph)
            QT = [self.tile(ph, "QTn%d" % i, [128, 4, 512], BF16) for i in range(2)]
            for q in QT:
                S.load("sp", q[64:67, :, :], self.dc["aug"][4:8].rearrange("h r q -> r h q"), q.b)
            imps = self.tile(ph, "imps", [128, 512], F32, psum=True)
            tps = self.tile(ph, "tpsn", [128, 1024], BF16, psum=True)
            impa = self.tile(ph, "impa", [128, 4, 128], F32)
            f1 = [self.tile(ph, "f1e4_%d" % i, [128, 4, 128], F32) for i in range(2)]
            nv = [self.tile(ph, "negv_%d" % i, [128, 4, 128], F32) for i in range(2)]
            gr = [self.tile(ph, "graw%d" % i, [128, 4, 12], BF16) for i in range(2)]
            sgm = self.tile(ph, "sgm", [128, 4, 12], F32)
            scm = self.tile(ph, "scm", [128, 4, 128], F32)
            tmpm = self.tile(ph, "tmpm", [128, 4, 128], F32)
            m8a = self.tile(ph, "m8a", [128, 4, 8], F32)
            m8b = self.tile(ph, "m8b", [128, 4, 8], F32)
            sel = self.tile(ph, "seln", [128, 4, 128], F32)
            val = self.tile(ph, "valn", [128, 4, 128], F32)
            nsel = self.tile(ph, "nseln", [128, 4, 128], BF16)
            nselT = self.tile(ph, "nselTn", [128, 512], BF16)
            den = self.tile(ph, "denn", [128, 4], F32)
            acc = self.tile(ph, "accn", [128, 2, 4, 64], F32)
            mst = [self.tile(ph, "mstn%d" % i, [128, 4, 128], BF16) for i in range(2)]

            def fold(O, Ov, h, gcol, first):
                self.recip_den(Ov, O, den)
                S.op("dve", lambda: nc.vector.tensor_tensor(out=den[:, :], in0=den[:, :], in1=sgm[:, :, gcol], op=ALU.mult),
                     reads=[den.b, sgm.b], writes=[den.b])
                for qi in range(4):
                    if first:
                        S.op("dve", lambda: nc.vector.tensor_scalar(out=acc[:, h, qi, :], in0=Ov[:, qi, 0:64], scalar1=den[:, qi:qi + 1], scalar2=None, op0=ALU.mult),
                             reads=[O.b, den.b], writes=[acc.b])
                    else:
                        S.op("dve", lambda: nc.vector.scalar_tensor_tensor(out=acc[:, h, qi, :], in0=Ov[:, qi, 0:64], scalar=den[:, qi:qi + 1], in1=acc[:, h, qi, :],
                                                                           op0=ALU.mult, op1=ALU.add), reads=[O.b, den.b, acc.b], writes=[acc.b])

            for c in range(self.nch):
                q = QT[c % 2]
                for h in range(4):
                    S.load("sp", q[0:64, h, :], self.featT[FRr["nq"] + 64 * h:FRr["nq"] + 64 * (h + 1), c * 512:(c + 1) * 512], q.b)
                f1c, nvc, grc = f1[c % 2], nv[c % 2], gr[c % 2]
                S.load("sp", f1c[:, :, :], self.dc["f1e4"][c], f1c.b)
                S.load("sp", nvc[:, :, :], self.dc["negv"][c], nvc.b)
                S.load("sp", grc[:, :, :], self.vtok[c * 512:(c + 1) * 512, VCr["ng"]:VCr["ng"] + 12].rearrange("(a p) g -> p a g", p=128), grc.b)
                S.op("act", lambda: nc.scalar.activation(out=sgm[:, :, :], in_=grc[:, :, :], func=AF.Exp, scale=-1.0), reads=[grc.b], writes=[sgm.b])
                S.op("dve", lambda: nc.vector.tensor_scalar(out=sgm[:, :, :], in0=sgm[:, :, :], scalar1=1.0, scalar2=None, op0=ALU.add),
                     reads=[sgm.b], writes=[sgm.b])
                S.op("dve", lambda: nc.vector.reciprocal(out=sgm[:, :, :], in_=sgm[:, :, :]), reads=[sgm.b], writes=[sgm.b])
                ms = mst[c % 2]
                kb_ = c // 4
                iv = imps[:, :].rearrange("p (a b) -> p a b", a=4)
                for h in range(4):
                    tiles = []
                    ntl = kb_ + 1

                    def mkpost(kt, ntl=ntl):
                        def post(pt, first, last):
                            for qi in range(4):
                                S.op("pe", lambda: nc.tensor.matmul(iv[:, qi, :], lhsT=pt[:, qi * 128:(qi + 1) * 128], rhs=OVt[:, kt, :],
                                                                    start=(first and qi == 0), stop=last),
                                     reads=[pt.b, OVt.b], writes=[imps.b], accum=not (first and qi == 0))
                        return post
                    for kt in range(ntl):
                        ex = []
                        if kt == kb_:
                            ex.append((self.ident[:, :], CM[:, c % 4, :], [self.ident.b, CM.b]))
                        elif kt == kb_ - 1 and c % 4 == 0:
                            ex.append((self.ident[:, :], CM[:, 4, :], [self.ident.b, CM.b]))
                        tiles.append(dict(kT=KCT[0:67, kt * 128:(kt + 1) * 128], v=VCt[:, kt, :], kbufs=[KCT.b, VCt.b],
                                          bias=KBC[:, h, c, kt:kt + 1], bbufs=[KBC.b], extra=ex, post=mkpost(kt)))
                    O = R.O[R.io % len(R.O)]
                    R.io += 1
                    Ov = self.attend(R, q[0:67, h, :], [q.b], tiles, O, 0.125)
                    self.recip_den(Ov, O, den)
                    for qi in range(4):
                        if h == 0:
                            S.op("dve", lambda: nc.vector.tensor_scalar(out=impa[:, qi, :], in0=iv[:, qi, :], scalar1=den[:, qi:qi + 1], scalar2=None, op0=ALU.mult),
                                 reads=[imps.b, den.b], writes=[impa.b])
                        else:
                            S.op("dve", lambda: nc.vector.scalar_tensor_tensor(out=impa[:, qi, :], in0=iv[:, qi, :], scalar=den[:, qi:qi + 1], in1=impa[:, qi, :],
                                                                               op0=ALU.mult, op1=ALU.add), reads=[imps.b, den.b, impa.b], writes=[impa.b])
                    if h < 2:
                        fold(O, Ov, h, 3 * h + 0, True)
                S.op("dve", lambda: nc.vector.tensor_tensor(out=scm[:, :, :], in0=impa[:, :, :], in1=f1c[:, :, :], op=ALU.max),
                     reads=[impa.b, f1c.b], writes=[scm.b])
                S.op("dve", lambda: nc.vector.tensor_tensor(out=scm[:, :, :], in0=scm[:, :, :], in1=nvc[:, :, :], op=ALU.add),
                     reads=[scm.b, nvc.b], writes=[scm.b])
                for qi in range(4):
                    S.op("dve", lambda: nc.vector.max(out=m8a[:, qi, :], in_=scm[:, qi, :]), reads=[scm.b], writes=[m8a.b])
                    S.op("dve", lambda: nc.vector.match_replace(out=tmpm[:, qi, :], in_to_replace=m8a[:, qi, :], in_values=scm[:, qi, :], imm_value=-1e30),
                         reads=[scm.b, m8a.b], writes=[tmpm.b])
                    S.op("dve", lambda: nc.vector.max(out=m8b[:, qi, :], in_=tmpm[:, qi, :]), reads=[tmpm.b], writes=[m8b.b])
                    S.op("dve", lambda: nc.vector.tensor_scalar(out=sel[:, qi, :], in0=scm[:, qi, :], scalar1=m8b[:, qi, 7:8], scalar2=None, op0=ALU.is_ge),
                         reads=[scm.b, m8b.b], writes=[sel.b])
                S.op("dve", lambda: nc.vector.tensor_scalar(out=val[:, :, :], in0=nvc[:, :, :], scalar1=-1.0, scalar2=None, op0=ALU.is_ge),
                     reads=[nvc.b], writes=[val.b])
                S.op("dve", lambda: nc.vector.tensor_tensor(out=sel[:, :, :], in0=sel[:, :, :], in1=val[:, :, :], op=ALU.mult),
                     reads=[sel.b, val.b], writes=[sel.b])
                S.op("dve", lambda: nc.vector.tensor_scalar(out=nsel[:, :, :], in0=sel[:, :, :], scalar1=-1.0, scalar2=-NEG, op0=ALU.add, op1=ALU.mult),
                     reads=[sel.b], writes=[nsel.b])
                for qi in range(4):
                    S.op("pe", lambda: nc.tensor.transpose(out=tps[:, qi * 128:(qi + 1) * 128], in_=nsel[:, qi, :], identity=self.ident[:, :]),
                         reads=[nsel.b, self.ident.b], writes=[tps.b], accum=(qi > 0))
                self.copy(nselT[:, :], tps[:, 0:512], [tps.b], [nselT.b], eng="dve")
                for h in range(2):
                    tiles = []
                    for kt in range(4 * c + 4):
                        if far_skip(1, h, 512 * c - (128 * kt + 127)):
                            continue
                        r = kt - 4 * c
                        ex = [(ES[:, kt * 128:(kt + 1) * 128], nselT[:, :], [ES.b, nselT.b])]
                        if r >= 0:
                            ex.append((self.ident[:, :], self.caus[:, r, :], [self.ident.b, self.caus.b]))
                        tiles.append(dict(kT=KTs[0:67, 0, kt * 128:(kt + 1) * 128], v=Vs[:, 0, kt, :], kbufs=[KTs.b, Vs.b],
                                          bias=self.kb[:, 4 + h, r + 60:r + 61], bbufs=[self.kb.b], extra=ex, q0=128 * max(r, 0)))
                    O = R.O[R.io % len(R.O)]
                    R.io += 1
                    Ov = self.attend(R, q[0:67, h, :], [q.b], tiles, O, 0.125)
                    fold(O, Ov, h, 3 * h + 1, False)
                    tiles = []
                    for r in range(-4, 4):
                        kt = 4 * c + r
                        if kt < 0:
                            continue
                        tiles.append(dict(kT=KTw[0:67, 0, kt * 128:(kt + 1) * 128], v=Vw[:, 0, kt, :], kbufs=[KTw.b, Vw.b],
                                          bias=self.kb[:, 4 + h, r + 60:r + 61], bbufs=[self.kb.b],
                                          extra=[(self.ident[:, :], WM[:, r + 4, :], [self.ident.b, WM.b])]))
                    O = R.O[R.io % len(R.O)]
                    R.io += 1
                    Ov = self.attend(R, q[0:67, h, :], [q.b], tiles, O, 0.125)
                    fold(O, Ov, h, 3 * h + 2, False)
                    S.op("dve", lambda: nc.vector.tensor_copy(out=ms[:, :, h * 64:(h + 1) * 64], in_=acc[:, h, :, :]), reads=[acc.b], writes=[ms.b])
                S.store("pool", self.mix[c * 512:(c + 1) * 512, 128:256].rearrange("(a p) d -> p a d", p=128), ms[:, :, :], ms.b)
            S.barrier()

    def prep_ffn(self, l):
        S = self.S
        with ExitStack() as ph:
            stg = [self.tile(ph, "pstg%d" % i, [128, 4096], F32) for i in range(2)]
            sb = [self.tile(ph, "psb%d" % i, [128, 4096], BF16) for i in range(2)]
            i = 0
            for src, dst in ((self.dp["wg"], self.wgs), (self.dp["wu"], self.wus), (self.dp["wd"], self.wds)):
                for g in range(NCF // 4):
                    s_, b_ = stg[i % 2], sb[i % 2]
                    i += 1
                    if len(src.shape) == 5:
                        sap = src[l, 4 * g:4 * g + 4].rearrange("c p k n -> p c (k n)")
                        dap = dst[4 * g:4 * g + 4].rearrange("c p k n -> p c (k n)")
                    else:
                        sap = src[l, 4 * g:4 * g + 4].rearrange("c p n -> p c n")
                        dap = dst[4 * g:4 * g + 4].rearrange("c p n -> p c n")
                    S.load("sp", s_[:, :].rearrange("p (c n) -> p c n", c=4), sap, s_.b)
                    self.copy(b_[:, :], s_[:, :], [s_.b], [b_.b])
                    S.store("pool", dap, b_[:, :].rearrange("p (c n) -> p c n", c=4), b_.b)
            S.barrier()

    def phase_C(self, l, x_src, x_dst):
        nc, S = self.nc, self.S
        with ExitStack() as ph:
            WO = self.tile(ph, "WO", [128, 8, D], BF16)
            with ExitStack() as tmp:
                self.load_cast(tmp, WO, lambda i: WO[:, i, :], lambda i: self.dp["wo"][l, :, i, :], 8, D)
                S.barrier()
            g1 = self.tile(ph, "g_apost", [128, D], F32)
            g2 = self.tile(ph, "g_fpre", [128, D], F32)
            g3 = self.tile(ph, "g_fpost", [128, D], F32)
            S.load("sp", g1[:, :], self.dp["g_apost"][l].partition_broadcast(128), g1.b)
            S.load("sp", g2[:, :], self.dp["g_fpre"][l].partition_broadcast(128), g2.b)
            S.load("sp", g3[:, :], self.dp["g_fpost"][l].partition_broadcast(128), g3.b)
            cw = self.tile(ph, "cw", [128, NCF, 3], F32)
            cb = self.tile(ph, "cb", [128, NCF], F32)
            S.load("sp", cw[:, :, :], self.dp["cw"][l], cw.b)
            S.load("sp", cb[:, :], self.dp["cb"][l], cb.b)
            halo = self.tile(ph, "halo", [128, NCF, 2], F32)
            S.op("pool", lambda: nc.gpsimd.memset(halo[:, :, :], 0.0), writes=[halo.b])
            bank = [self.tile(ph, "bk%d" % i, [128, 512], F32, psum=True) for i in range(6)]
            tpb = [self.tile(ph, "tpC%d" % i, [128, D], BF16, psum=True) for i in range(2)]
            mixt = [self.tile(ph, "mixt%d" % i, [128, D], BF16) for i in range(2)]
            mT = [self.tile(ph, "mT%d" % i, [128, 8, 128], BF16) for i in range(2)]
            xt = [self.tile(ph, "xtC%d" % i, [128, D], F32) for i in range(2)]
            x1 = [self.tile(ph, "x1C%d" % i, [128, D], F32) for i in range(2)]
            yt = self.tile(ph, "ytC", [128, D], F32)
            junk = self.tile(ph, "junkC", [128, D], F32)
            ss = self.tile(ph, "ssC", [128, 1], F32)
            sd = self.tile(ph, "sdC", [128, 1], F32)
            rstd = self.tile(ph, "rstdC", [128, 1], F32)
            hb = [self.tile(ph, "hbC%d" % i, [128, D], BF16) for i in range(4)]
            h2T = self.tile(ph, "h2T", [128, 8, 512], BF16)
            gT = self.tile(ph, "gT", [128, NCF, 512], BF16)
            wgt = [self.tile(ph, "wgt%d" % i, [128, 8, 128], BF16) for i in range(3)]
            wut = [self.tile(ph, "wut%d" % i, [128, 8, 128], BF16) for i in range(3)]
            wdt = [self.tile(ph, "wdt%d" % i, [128, D], BF16) for i in range(3)]
            aT = [self.tile(ph, "aT%d" % i, [128, 514], F32) for i in range(2)]
            cacc = [self.tile(ph, "cacc%d" % i, [128, 512], F32) for i in range(2)]
            ga = [self.tile(ph, "ga%d" % i, [128, 512], F32) for i in range(2)]
            pst = [self.tile(ph, "pstC%d" % i, [128, D], F32) for i in range(2)]
            rt = [self.tile(ph, "rtC%d" % i, [128, D], F32) for i in range(2)]
            x1r = [self.tile(ph, "x1rC%d" % i, [128, D], F32) for i in range(2)]
            ot = [self.tile(ph, "otC%d" % i, [128, D], F32) for i in range(2)]
            ss3 = self.tile(ph, "ss3C", [128, 1], F32)
            sd3 = self.tile(ph, "sd3C", [128, 1], F32)
            rstd3 = self.tile(ph, "rstd3C", [128, 1], F32)
            junk3 = self.tile(ph, "junk3C", [128, D], F32)
            it = 0
            i3 = 0
            RNG = 2
            pending = []
            store_tags = []

            def finish_tile(tag, tt):
                nonlocal i3
                S._wait("sp", tag)
                tok = tt * 128
                r_, x_, o_ = rt[i3 % 2], x1r[i3 % 2], ot[i3 % 2]
                i3 += 1
                S.load("sp", r_[:, :], self.red[tok:tok + 128, :], r_.b)
                S.load("sp", x_[:, :], self.x1s[tok:tok + 128, :], x_.b)
                self.rstd_of(r_[:, :], r_.b, junk3, ss3, sd3, rstd3)
                S.op("dve", lambda: nc.vector.scalar_tensor_tensor(out=r_[:, :], in0=r_[:, :], scalar=rstd3[:, 0:1], in1=g3[:, :], op0=ALU.mult, op1=ALU.mult),
                     reads=[r_.b, rstd3.b, g3.b], writes=[r_.b])
                S.op("pool", lambda: nc.gpsimd.tensor_tensor(out=o_[:, :], in0=r_[:, :], in1=x_[:, :], op=ALU.add),
                     reads=[r_.b, x_.b], writes=[o_.b])
                S.store("pool", x_dst[tok:tok + 128, :], o_[:, :], o_.b)

            h2Ts = [h2T, self.tile(ph, "h2Tb", [128, 8, 512], BF16)]

            def seg_a1(T):
                tok = T * 128
                mt, mTt, tpp = mixt[T % 2], mT[T % 2], tpb[0]
                mrow = (tok // 2048) * 4096 + (tok % 2048)
                S.load("sp", mt[:, 0:512], self.mixall[mrow:mrow + 128, :], mt.b)
                S.load("sp", mt[:, 512:1024], self.mixall[2048 + mrow:2048 + mrow + 128, :], mt.b)
                for kc in range(8):
                    S.op("pe", lambda: nc.tensor.transpose(out=tpp[:, kc * 128:(kc + 1) * 128], in_=mt[:, kc * 128:(kc + 1) * 128], identity=self.ident[:, :]),
                         reads=[mt.b, self.ident.b], writes=[tpp.b], accum=(kc > 0))
                self.copy(mTt[:, :, :], tpp[:, :].rearrange("p (k t) -> p k t", k=8), [tpp.b], [mTt.b])

            def seg_a2(T):
                tok = T * 128
                mTt, xtt, h, x1t = mT[T % 2], xt[T % 2], hb[T % 4], x1[T % 2]
                S.load("sp", xtt[:, :], x_src[tok:tok + 128, :], xtt.b)
                pa, pb = bank[4], bank[5]
                for half, pp in enumerate((pa, pb)):
                    for kc in range(8):
                        S.op("pe", lambda: nc.tensor.matmul(pp[:, :], lhsT=mTt[:, kc, :], rhs=WO[:, kc, half * 512:(half + 1) * 512],
                                                            start=(kc == 0), stop=(kc == 7)), reads=[mTt.b, WO.b], writes=[pp.b], accum=(kc > 0))
                self.copy(yt[:, 0:512], pa[:, :], [pa.b], [yt.b], eng="act")
                self.copy(yt[:, 512:D], pb[:, :], [pb.b], [yt.b], eng="act")
                self.rstd_of(yt[:, :], yt.b, junk, ss, sd, rstd)
                S.op("dve", lambda: nc.vector.scalar_tensor_tensor(out=yt[:, :], in0=yt[:, :], scalar=rstd[:, 0:1], in1=g1[:, :], op0=ALU.mult, op1=ALU.mult),
                     reads=[yt.b, rstd.b, g1.b], writes=[yt.b])
                S.op("pool", lambda: nc.gpsimd.tensor_tensor(out=x1t[:, :], in0=yt[:, :], in1=xtt[:, :], op=ALU.add),
                     reads=[yt.b, xtt.b], writes=[x1t.b])
                S.store("pool", self.x1s[tok:tok + 128, :], x1t[:, :], x1t.b)
                store_tags.append(("dma", x1t.b.dsem, x1t.b.dcnt))
                self.rstd_of(x1t[:, :], x1t.b, junk, ss, sd, rstd)
                S.op("dve", lambda: nc.vector.scalar_tensor_tensor(out=h[:, :], in0=x1t[:, :], scalar=rstd[:, 0:1], in1=g2[:, :], op0=ALU.mult, op1=ALU.mult),
                     reads=[x1t.b, rstd.b, g2.b], writes=[h.b])

            def seg_b3(T):
                h, tpp = hb[T % 4], tpb[1]
                hT = h2Ts[(T // 4) % 2]
                ti = T % 4
                for kc in range(8):
                    S.op("pe", lambda: nc.tensor.transpose(out=tpp[:, kc * 128:(kc + 1) * 128], in_=h[:, kc * 128:(kc + 1) * 128], identity=self.ident[:, :]),
                         reads=[h.b, self.ident.b], writes=[tpp.b], accum=(kc > 0))
                self.copy(hT[:, :, ti * 128:(ti + 1) * 128], tpp[:, :].rearrange("p (k t) -> p k t", k=8), [tpp.b], [hT.b])

            def seg_b(c, ti, ctx):
                h, tpp = ctx
                hT = h2Ts[c % 2]
                for kc in range(8):
                    S.op("pe", lambda: nc.tensor.transpose(out=tpp[:, kc * 128:(kc + 1) * 128], in_=h[:, kc * 128:(kc + 1) * 128], identity=self.ident[:, :]),
                         reads=[h.b, self.ident.b], writes=[tpp.b], accum=(kc > 0))
                self.copy(hT[:, :, ti * 128:(ti + 1) * 128], tpp[:, :].rearrange("p (k t) -> p k t", k=8), [tpp.b], [hT.b])

            def c2a_cf(c, cf):
                hT = h2Ts[c % 2]
                wg_, wu_ = wgt[cf % 3], wut[cf % 3]
                S.load("sp", wg_[:, :, :], self.wgs[cf], wg_.b)
                S.load("sp", wu_[:, :, :], self.wus[cf], wu_.b)
                pa, pu = bank[2 * (cf % 2)], bank[2 * (cf % 2) + 1]
                for kc in range(8):
                    S.op("pe", lambda: nc.tensor.matmul(pa[:, :], lhsT=wg_[:, kc, :], rhs=hT[:, kc, :], start=(kc == 0), stop=(kc == 7)),
                         reads=[wg_.b, hT.b], writes=[pa.b], accum=(kc > 0))
                for kc in range(8):
                    S.op("pe", lambda: nc.tensor.matmul(pu[:, :], lhsT=wu_[:, kc, :], rhs=hT[:, kc, :], start=(kc == 0), stop=(kc == 7)),
                         reads=[wu_.b, hT.b], writes=[pu.b], accum=(kc > 0))
                a, ca, g = aT[cf % 2], cacc[cf % 2], ga[cf % 2]
                e4 = a[:, 0:4]
                S.op("dve", lambda: nc.vector.tensor_copy(out=a[:, 0:2], in_=halo[:, cf, :]), reads=[halo.b], writes=[a.b])
                S.op("dve", lambda: nc.vector.tensor_copy(out=a[:, 2:4], in_=pa[:, 0:2]), reads=[pa.b], writes=[a.b])
                S.op("dve", lambda: nc.vector.tensor_copy(out=halo[:, cf, :], in_=pa[:, 510:512]), reads=[pa.b], writes=[halo.b])
                S.op("dve", lambda: nc.vector.tensor_scalar(out=ca[:, 2:512], in0=pa[:, 0:510], scalar1=cw[:, cf, 0:1], scalar2=None, op0=ALU.mult),
                     reads=[pa.b, cw.b], writes=[ca.b])
                S.op("dve", lambda: nc.vector.scalar_tensor_tensor(out=ca[:, 2:512], in0=pa[:, 1:511], scalar=cw[:, cf, 1:2], in1=ca[:, 2:512], op0=ALU.mult, op1=ALU.add),
                     reads=[pa.b, cw.b, ca.b], writes=[ca.b])
                S.op("dve", lambda: nc.vector.scalar_tensor_tensor(out=ca[:, 2:512], in0=pa[:, 2:512], scalar=cw[:, cf, 2:3], in1=ca[:, 2:512], op0=ALU.mult, op1=ALU.add),
                     reads=[pa.b, cw.b, ca.b], writes=[ca.b])
                S.op("dve", lambda: nc.vector.tensor_scalar(out=ca[:, 0:2], in0=e4[:, 0:2], scalar1=cw[:, cf, 0:1], scalar2=None, op0=ALU.mult),
                     reads=[a.b, cw.b], writes=[ca.b])
                S.op("dve", lambda: nc.vector.scalar_tensor_tensor(out=ca[:, 0:2], in0=e4[:, 1:3], scalar=cw[:, cf, 1:2], in1=ca[:, 0:2], op0=ALU.mult, op1=ALU.add),
                     reads=[a.b, cw.b, ca.b], writes=[ca.b])
                S.op("dve", lambda: nc.vector.scalar_tensor_tensor(out=ca[:, 0:2], in0=e4[:, 2:4], scalar=cw[:, cf, 2:3], in1=ca[:, 0:2], op0=ALU.mult, op1=ALU.add),
                     reads=[a.b, cw.b, ca.b], writes=[ca.b])
                S.op("act", lambda: nc.scalar.activation(out=g[:, :], in_=ca[:, :], func=AF.Gelu_apprx_tanh, bias=cb[:, cf:cf + 1]),
                     reads=[ca.b, cb.b], writes=[g.b])
                S.op("dve", lambda: nc.vector.tensor_tensor(out=gT[:, cf, :], in0=g[:, :], in1=pu[:, :], op=ALU.mult),
                     reads=[g.b, pu.b], writes=[gT.b])

            def c2b(c):
                for tp2 in range(2):
                    accs = [bank[0], bank[1], bank[2], bank[3]]
                    for cf in range(NCF):
                        wd_ = wdt[cf % 3]
                        S.load("sp", wd_[:, :], self.wds[cf], wd_.b)
                        for j in range(2):
                            ti = 2 * tp2 + j
                            for half in range(2):
                                pp = accs[2 * j + half]
                                S.op("pe", lambda: nc.tensor.matmul(pp[:, :], lhsT=gT[:, cf, ti * 128:(ti + 1) * 128], rhs=wd_[:, half * 512:(half + 1) * 512],
                                                                    start=(cf == 0), stop=(cf == NCF - 1)), reads=[gT.b, wd_.b], writes=[pp.b], accum=(cf > 0))
                    for j in range(2):
                        ti = 2 * tp2 + j
                        tok = (4 * c + ti) * 128
                        p_ = pst[j]
                        self.copy(p_[:, 0:512], accs[2 * j][:, :], [accs[2 * j].b], [p_.b], eng="act")
                        self.copy(p_[:, 512:D], accs[2 * j + 1][:, :], [accs[2 * j + 1].b], [p_.b], eng="dve")
                        S.store("pool", self.part[tok:tok + 128, :], p_[:, :], p_.b)
                        store_tags.append(("dma", p_.b.dsem, p_.b.dcnt))

            NTL = 4 * self.nch
            fifo = []
            for s_ in range(-7, 0):
                if 0 <= s_ + 4 < NTL:
                    seg_b3(s_ + 4)
                if 0 <= s_ + 6 < NTL:
                    seg_a2(s_ + 6)
                if 0 <= s_ + 7 < NTL:
                    seg_a1(s_ + 7)
            for c in range(self.nch):
                for cf in range(NCF):
                    c2a_cf(c, cf)
                    if cf % 4 == 3:
                        s_ = 4 * c + cf // 4
                        if s_ + 4 < NTL:
                            seg_b3(s_ + 4)
                        if s_ + 6 < NTL:
                            seg_a2(s_ + 6)
                        if s_ + 7 < NTL:
                            seg_a1(s_ + 7)
                        if fifo and fifo[0][2] <= c:
                            tg, tt, _ = fifo.pop(0)
                            finish_tile(tg, tt)
                c2b(c)
                if c % RNG == RNG - 1 or c == self.nch - 1:
                    c0 = (c // RNG) * RNG
                    r0, r1 = c0 * 512, (c + 1) * 512
                    for tg in store_tags:
                        S._wait("pool", tg)
                    store_tags = []
                    tag = self.collective("AllReduce", ALU.add, self.part[r0:r1, :].opt(), self.red[r0:r1, :].opt())
                    for tt in range(r0 // 128, r1 // 128):
                        fifo.append((tag, tt, c + 2))
            for tg, tt, _ in fifo:
                finish_tile(tg, tt)
            S.barrier()


_CACHE = {}


def kernel(**inputs):
    x = np.ascontiguousarray(inputs["x"], dtype=np.float32)
    params = [layout_params(inputs, r) for r in range(2)]
    consts = [make_consts(r) for r in range(2)]
    if "prog" not in _CACHE:
        _CACHE["prog"] = Prog()
    prog = _CACHE["prog"]
    in_maps = []
    for core in range(8):
        b, r = core // 2, core % 2
        m = {"x": x[b]}
        for n, _, _ in CONST_SPECS:
            m["c_" + n] = consts[r][n]
        for n, _ in PARAM_SPECS:
            m["p_" + n] = params[r][n]
        in_maps.append(m)
    res = run_bass_kernel_spmd(prog.nc, in_maps, core_ids=list(range(8)))
    out = np.stack([np.asarray(res.results[2 * b]["y"], dtype=np.float32) for b in range(4)], axis=0)
    return out
```

```python
import math
import numpy as np
import ml_dtypes
from contextlib import ExitStack
import concourse.bass as bass
import concourse.mybir as mybir
from concourse.bass_utils import run_bass_kernel_spmd

F32 = mybir.dt.float32
BF16 = mybir.dt.bfloat16
AF = mybir.ActivationFunctionType
ALU = mybir.AluOpType
AX = mybir.AxisListType
NPBF = ml_dtypes.bfloat16

D = 1024
S_LEN = 8192
NT = 64
NCH = 16
DEPTH = 2
DFF = 4096
NEG = -30000.0
EPS = 1e-6

OFF = dict(mq=0, mk=256, mv=512, nq=768, nkc=1024, nvc=1088, nks=1152, nvs=1216, nkw=1280, nvw=1344,
           ng=1408, dq=1420, dk=1676, dv=1932, sq=2188, sk=2444, sv=2572)
FRr = dict(mq=0, mk=128, nq=256, nkc=512, nvc=576, nks=640, nkw=704, dq=768, dk=896, sq=1024, sk=1152)
NF = 1280
VCr = dict(mv=0, nvs=128, nvw=192, dv=256, sv=384, ng=448)
NV = 460
NCF = 16


def role_heads(r, m=0):
    if m == 3:
        return [2 * r, 2 * r + 1], [2 * (1 - r), 2 * (1 - r) + 1]
    return [r, r + 2], [1 - r, 3 - r]


def far_skip(m, j, min_dist):
    sl = min(SLOPES[4 * role_heads(0, m)[0][j] + m], SLOPES[4 * role_heads(1, m)[0][j] + m])
    return min_dist > 0 and sl * min_dist >= 160.0


def role_cols(r):
    own0 = role_heads(r, 0)[0]
    own1, oth1 = role_heads(r, 1)
    own2 = role_heads(r, 2)[0]
    own3 = role_heads(r, 3)[0]

    def hc(base, heads, w=64):
        return np.concatenate([np.arange(base + w * h, base + w * (h + 1)) for h in heads])

    f = np.concatenate([hc(OFF["mq"], own0), hc(OFF["mk"], own0), hc(OFF["nq"], own1 + oth1),
                        np.arange(OFF["nkc"], OFF["nkc"] + 64), np.arange(OFF["nvc"], OFF["nvc"] + 64),
                        np.arange(OFF["nks"], OFF["nks"] + 64), np.arange(OFF["nkw"], OFF["nkw"] + 64),
                        hc(OFF["dq"], own2), hc(OFF["dk"], own2), hc(OFF["sq"], own3), hc(OFF["sk"], [r])])
    v = np.concatenate([hc(OFF["mv"], own0), np.arange(OFF["nvs"], OFF["nvs"] + 64), np.arange(OFF["nvw"], OFF["nvw"] + 64),
                        hc(OFF["dv"], own2), hc(OFF["sv"], [r]), hc(OFF["ng"], own1 + oth1, 3)])
    assert len(f) == 1216 and len(v) == NV
    return f, v


SLOPES = np.power(np.float32(2.0), np.arange(1, 17, dtype=np.float32) * np.float32(-0.5)).astype(np.float64)


class Buf:
    __slots__ = ("name", "w", "rs", "dsem", "dcnt")

    def __init__(self, name):
        self.name = name
        self.w = None
        self.rs = []
        self.dsem = None
        self.dcnt = 0


class Sched:
    def __init__(self, nc, stack):
        self.nc = nc
        self.stack = stack
        self.eng = {"pe": nc.tensor, "act": nc.scalar, "dve": nc.vector, "pool": nc.gpsimd, "sp": nc.sync}
        self.sem, self.cnt, self.seen = {}, {}, {}
        for k in self.eng:
            self.sem[k] = stack.enter_context(nc.semaphore("s_" + k))
            self.cnt[k] = 0
            self.seen[k] = {}
        self.dma_seen = {k: {} for k in self.eng}
        self.free_sems = []
        self.live = []
        self.nsem = len(self.eng)
        self.ninstr = 0

    def _wait(self, ek, dep):
        if dep is None:
            return
        e = self.eng[ek]
        if dep[0] == "dma":
            _, sem, val = dep
            d = self.dma_seen[ek]
            if d.get(id(sem), 0) >= val:
                return
            e.wait_ge(sem, val)
            d[id(sem)] = val
        else:
            pk, c = dep
            if self.seen[ek].get(pk, 0) >= c:
                return
            e.wait_ge(self.sem[pk], c)
            self.seen[ek][pk] = c
        self.ninstr += 1

    def _deps(self, ek, reads, writes, accum):
        for b in reads:
            self._wait(ek, b.w)
        for b in writes:
            if not (accum and b.w is not None and b.w[0] == ek):
                self._wait(ek, b.w)
            for r in b.rs:
                self._wait(ek, r)

    def op(self, ek, fn, reads=(), writes=(), accum=False):
        self._deps(ek, reads, writes, accum)
        ins = fn()
        self.cnt[ek] += 1
        ins.then_inc(self.sem[ek], 1)
        tag = (ek, self.cnt[ek])
        for b in reads:
            b.rs.append(tag)
        for b in writes:
            b.w = tag
            b.rs = []
        self.ninstr += 1
        return ins

    def _dsem(self, b):
        if b.dsem is None:
            if self.free_sems:
                b.dsem, b.dcnt = self.free_sems.pop()
            else:
                b.dsem = self.stack.enter_context(self.nc.semaphore("d%d" % self.nsem))
                b.dcnt = 0
                self.nsem += 1
            self.live.append(b)

    def load(self, qk, out_ap, in_ap, buf):
        self._deps(qk, (), (buf,), False)
        self._dsem(buf)
        ins = self.eng[qk].dma_start(out=out_ap, in_=in_ap)
        buf.dcnt += 16
        ins.then_inc(buf.dsem, 16)
        buf.w = ("dma", buf.dsem, buf.dcnt)
        buf.rs = []
        self.ninstr += 1

    def store(self, qk, out_ap, in_ap, buf):
        self._deps(qk, (buf,), (), False)
        self._dsem(buf)
        ins = self.eng[qk].dma_start(out=out_ap, in_=in_ap)
        buf.dcnt += 16
        ins.then_inc(buf.dsem, 16)
        buf.rs.append(("dma", buf.dsem, buf.dcnt))
        self.ninstr += 1

    def barrier(self):
        for ek in self.eng:
            for pk in self.eng:
                if pk != ek and self.cnt[pk] > 0:
                    self._wait(ek, (pk, self.cnt[pk]))
            for b in self.live:
                self._wait(ek, ("dma", b.dsem, b.dcnt))
        for b in self.live:
            self.free_sems.append((b.dsem, b.dcnt))
            b.dsem = None
        self.live = []


class T:
    def __init__(self, nc, stack, name, shape, dtype, psum=False):
        alloc = nc.psum_tensor if psum else nc.sbuf_tensor
        self.t = stack.enter_context(alloc(name, shape, dtype))
        self.b = Buf(name)

    def __getitem__(self, idx):
        return self.t[idx]


def _split3(v):
    v = np.asarray(v, np.float64)
    hi = v.astype(NPBF)
    r = v - hi.astype(np.float64)
    mid = r.astype(NPBF)
    r = r - mid.astype(np.float64)
    lo = r.astype(NPBF)
    return hi, mid, lo


def make_consts(role=0):
    locs = [sum(role_heads(role, m), []) for m in range(4)]
    c = {}
    c["ident"] = np.eye(128, dtype=np.float32).astype(NPBF)
    iq = np.arange(512, dtype=np.float64)
    p = np.arange(128, dtype=np.float64)
    aug = np.zeros((16, 3, 512), NPBF)
    for m in range(4):
        scale = 32 ** -0.5 if m == 2 else 64 ** -0.5
        for j in range(4):
            sl = SLOPES[4 * locs[m][j] + m]
            hi, mid, lo = _split3(-sl * iq / scale)
            aug[4 * m + j, 0], aug[4 * m + j, 1], aug[4 * m + j, 2] = hi, mid, lo
    c["aug"] = aug
    kb = np.zeros((128, 16, 64), np.float32)
    r = np.arange(-60, 4, dtype=np.float64)
    for m in range(4):
        for j in range(4):
            sl = SLOPES[4 * locs[m][j] + m]
            kb[:, 4 * m + j, :] = (sl * (p[:, None] + 128.0 * r[None, :])).astype(np.float32)
    c["kb"] = kb
    kbc = np.zeros((128, 4, 16, 4), np.float32)
    for j in range(4):
        sl = SLOPES[4 * locs[1][j] + 1]
        for cc in range(16):
            for kt in range(4):
                kbc[:, j, cc, kt] = (sl * (16.0 * (128 * kt + p) + 31.0 - 512.0 * cc)).astype(np.float32)
    c["kbc"] = kbc
    P = p[:, None, None]
    Q = iq[None, None, :]
    k4 = np.arange(4, dtype=np.float64)[None, :, None]
    c["caus"] = np.where(Q >= 128 * k4 + P, 0.0, NEG).astype(np.float32).astype(NPBF)
    r8 = (np.arange(8, dtype=np.float64) - 4)[None, :, None]
    dd = Q - 128 * r8 - P
    c["wmask"] = np.where((dd >= 0) & (dd < 512), 0.0, NEG).astype(np.float32).astype(NPBF)
    r5 = (np.arange(5, dtype=np.float64) - 1)[None, :, None]
    dd = Q - 128 * r5 - P
    c["swamask"] = np.where((dd >= 0) & (dd < 128), 0.0, NEG).astype(np.float32).astype(NPBF)
    d5 = (512.0 * np.arange(5, dtype=np.float64))[None, :, None]
    c["cmask"] = np.where(16 * P + 31 <= d5 + Q, 0.0, NEG).astype(np.float32).astype(NPBF)
    j = np.arange(S_LEN)
    c["em"] = (j[None, :] // 256 == np.arange(32)[:, None]).astype(np.float32).astype(NPBF)
    c["es"] = (j[None, :] // 64 == np.arange(128)[:, None]).astype(np.float32).astype(NPBF)
    ncmp, nsel = 511, 128
    cs = np.arange(512)[:, None] * 16
    bs = np.arange(nsel)[None, :] * 64
    ov = np.clip(np.minimum(cs + 32, bs + 64) - np.maximum(cs, bs), 0, None) / 32.0
    ov[ncmp:, :] = 0.0
    c["ov"] = ov.reshape(4, 128, 128).transpose(1, 0, 2).astype(np.float32).astype(NPBF)
    n32 = np.arange(32, dtype=np.float32)
    qi = np.arange(4)
    c["bi"] = np.broadcast_to((n32[None, None, :] - (qi // 2)[None, :, None].astype(np.float32)), (128, 4, 32)).astype(np.float32).copy()
    s128 = np.arange(128)[None, None, None, :]
    cc = np.arange(16)[:, None, None, None]
    pp = np.arange(128)[None, :, None, None]
    qq = np.arange(4)[None, None, :, None]
    qblk = 8 * cc + 2 * qq + (pp >= 64)
    forced = (s128 == 0) | (s128 == qblk) | (s128 == qblk - 1)
    valid = s128 <= qblk
    c["f1e4"] = np.where(forced, 1e4, 0.0).astype(np.float32)
    c["negv"] = np.where(valid, 0.0, -1e30).astype(np.float32)
    c["ones3"] = np.ones((3, 4 * S_LEN), np.float32).astype(NPBF)
    return c


CONST_SPECS = [("ident", [128, 128], BF16), ("aug", [16, 3, 512], BF16), ("kb", [128, 16, 64], F32),
               ("kbc", [128, 4, 16, 4], F32), ("caus", [128, 4, 512], BF16), ("wmask", [128, 8, 512], BF16),
               ("swamask", [128, 5, 512], BF16), ("cmask", [128, 5, 512], BF16), ("em", [32, S_LEN], BF16),
               ("es", [128, S_LEN], BF16), ("ov", [128, 4, 128], BF16), ("bi", [128, 4, 32], F32),
               ("f1e4", [16, 128, 4, 128], F32), ("negv", [16, 128, 4, 128], F32), ("ones3", [3, 4 * S_LEN], BF16)]

PARAM_SPECS = [("wf", [DEPTH, 128, 8, NF]), ("wv", [DEPTH, 128, 8, NV]), ("wo", [DEPTH, 128, 8, D]),
               ("wg", [DEPTH, NCF, 128, 8, 128]), ("wu", [DEPTH, NCF, 128, 8, 128]), ("wd", [DEPTH, NCF, 128, D]),
               ("g_apre", [DEPTH, D]), ("g_apost", [DEPTH, D]), ("g_fpre", [DEPTH, D]), ("g_fpost", [DEPTH, D]),
               ("cw", [DEPTH, 128, NCF, 3]), ("cb", [DEPTH, 128, NCF]),
               ("posk", [DEPTH, 64, 32]), ("w1k", [DEPTH, 64, 32, 128]), ("b1k", [DEPTH, 128, 1]), ("w2k", [DEPTH, 128, 64]),
               ("posv", [DEPTH, 64, 32]), ("w1v", [DEPTH, 64, 32, 128]), ("b1v", [DEPTH, 128, 1]), ("w2v", [DEPTH, 128, 64]),
               ("lq1", [DEPTH, 32]), ("lk1", [DEPTH, 32]), ("lq2", [DEPTH, 32]), ("lk2", [DEPTH, 32]),
               ("subln", [DEPTH, 64]), ("sinks", [DEPTH, 4])]


def layout_params(inp, role=0):
    o = {}
    w_in = inp["w_in"]
    fc, vc = role_cols(role)
    wf = np.zeros((DEPTH, D, NF), np.float32)
    wf[:, :, :len(fc)] = w_in[:, :, fc]
    o["wf"] = wf.reshape(DEPTH, 8, 128, NF).transpose(0, 2, 1, 3)
    o["wv"] = w_in[:, :, vc].reshape(DEPTH, 8, 128, NV).transpose(0, 2, 1, 3)
    rows = np.concatenate([np.arange(256 * m + 64 * h, 256 * m + 64 * (h + 1)) for rr in range(2) for m in range(4)
                           for h in role_heads(rr, m)[0]])
    o["wo"] = inp["w_out"][:, rows, :].reshape(DEPTH, 8, 128, D).transpose(0, 2, 1, 3)
    cfs = slice(NCF * role, NCF * (role + 1))
    o["wg"] = inp["ffn_w_gate"].reshape(DEPTH, 8, 128, 32, 128).transpose(0, 3, 2, 1, 4)[:, cfs]
    o["wu"] = inp["ffn_w_up"].reshape(DEPTH, 8, 128, 32, 128).transpose(0, 3, 2, 1, 4)[:, cfs]
    o["wd"] = inp["ffn_w_down"].reshape(DEPTH, 32, 128, D)[:, cfs]
    o["g_apre"], o["g_apost"] = inp["attn_pre_norm"], inp["attn_post_norm"]
    o["g_fpre"], o["g_fpost"] = inp["ffn_pre_norm"], inp["ffn_post_norm"]
    o["cw"] = inp["ffn_conv_w"].reshape(DEPTH, 3, 32, 128).transpose(0, 3, 2, 1)[:, :, cfs, :]
    o["cb"] = inp["ffn_conv_b"].reshape(DEPTH, 32, 128).transpose(0, 2, 1)[:, :, cfs]
    for sfx in "kv":
        o["pos" + sfx] = inp["nsa_cmp_pos_" + sfx].transpose(0, 2, 1)
        o["w1" + sfx] = inp["nsa_cmp_w1_" + sfx].transpose(0, 2, 1, 3)
        o["b1" + sfx] = inp["nsa_cmp_b1_" + sfx].reshape(DEPTH, 128, 1)
        o["w2" + sfx] = inp["nsa_cmp_w2_" + sfx]
    o["lq1"], o["lk1"] = inp["diff_lambda_q1"], inp["diff_lambda_k1"]
    o["lq2"], o["lk2"] = inp["diff_lambda_q2"], inp["diff_lambda_k2"]
    o["subln"] = inp["diff_subln"]
    o["sinks"] = inp["swa_sinks"][:, sum(role_heads(role, 3), [])]
    return {k: np.ascontiguousarray(v, dtype=np.float32) for k, v in o.items()}


class Prog:
    def __init__(self, phases=("A", "B", "C"), layers=(0, 1), mixers=(0, 1, 2, 3), debug=False, nchunks=NCH, ncores=8):
        self.phases, self.layers, self.mixers, self.debug, self.nch = phases, layers, mixers, debug, nchunks
        self.groups = [[2 * i, 2 * i + 1] for i in range(ncores // 2)]
        nc = bass.Bass("TRN2", target_bir_lowering=False)
        self.nc = nc
        self.x_in = nc.dram_tensor("x", [S_LEN, D], F32, kind="ExternalInput").ap()
        self.y = nc.dram_tensor("y", [S_LEN, D], F32, kind="ExternalOutput").ap()
        self.dc = {n: nc.dram_tensor("c_" + n, shp, dt, kind="ExternalInput").ap() for n, shp, dt in CONST_SPECS}
        self.dp = {n: nc.dram_tensor("p_" + n, shp, F32, kind="ExternalInput").ap() for n, shp in PARAM_SPECS}
        kind = "ExternalOutput" if debug else "Internal"
        self.featT = nc.dram_tensor("featT", [NF, S_LEN], BF16, kind=kind).ap()
        self.vtok = nc.dram_tensor("vtok", [S_LEN, NV], BF16, kind=kind).ap()
        self.mix_t = nc.dram_tensor("mix", [S_LEN, 512], BF16)
        self.mix = self.mix_t.ap()
        self.mixall_t = nc.dram_tensor("mixall", [2 * S_LEN, 512], BF16)
        self.mixall = self.mixall_t.ap()
        self.part = nc.dram_tensor("part", [S_LEN, D], F32).ap()
        self.red = nc.dram_tensor("red", [S_LEN, D], F32).ap()
        self.x1s = nc.dram_tensor("x1s", [S_LEN, D], F32).ap()
        self.xs = nc.dram_tensor("xs", [S_LEN, D], F32).ap()
        self.wgs = nc.dram_tensor("wgs", [NCF, 128, 8, 128], BF16).ap()
        self.wus = nc.dram_tensor("wus", [NCF, 128, 8, 128], BF16).ap()
        self.wds = nc.dram_tensor("wds", [NCF, 128, D], BF16).ap()
        self.cp_i = 0
        with ExitStack() as st:
            self.st = st
            self.S = Sched(nc, st)
            self.cc_sem = st.enter_context(nc.semaphore("cc_sem"))
            self.cc_cnt = 0
            self.build()
            print("built: instr", self.S.ninstr, "sems", self.S.nsem, flush=True)

    def collective(self, kind, op, in_ap, out_ap):
        ins = self.nc.gpsimd.collective_compute(kind, op, replica_groups=self.groups, ins=[in_ap], outs=[out_ap])
        self.cc_cnt += 1
        ins.then_inc(self.cc_sem, 1)
        self.S.ninstr += 1
        return ("dma", self.cc_sem, self.cc_cnt)

    def tile(self, stack, name, shape, dtype, psum=False):
        self.tile_i = getattr(self, "tile_i", 0) + 1
        return T(self.nc, stack, "%s_%d" % (name, self.tile_i), shape, dtype, psum)

    def copy(self, out_ap, in_ap, reads, writes, eng=None):
        nc = self.nc
        if eng is None:
            eng = "act" if (self.cp_i % 2 == 0) else "dve"
            self.cp_i += 1
        if eng == "act":
            self.S.op("act", lambda: nc.scalar.activation(out=out_ap, in_=in_ap, func=AF.Copy), reads=reads, writes=writes)
        elif eng == "dve":
            self.S.op("dve", lambda: nc.vector.tensor_copy(out=out_ap, in_=in_ap), reads=reads, writes=writes)
        else:
            self.S.op("pool", lambda: nc.gpsimd.tensor_copy(out=out_ap, in_=in_ap), reads=reads, writes=writes)

    def load_cast(self, stack, dst, dst_ap_fn, src_ap_fn, n, ncols):
        stg = [self.tile(stack, "stg%d_%d" % (i, self.S.ninstr), [128, ncols], F32) for i in range(2)]
        for i in range(n):
            s = stg[i % 2]
            self.S.load("sp", s[:, :], src_ap_fn(i), s.b)
            self.copy(dst_ap_fn(i), s[:, :], [s.b], [dst.b])

    def build(self):
        nc, S, st = self.nc, self.S, self.st
        self.ident = self.tile(st, "ident", [128, 128], BF16)
        self.caus = self.tile(st, "caus", [128, 4, 512], BF16)
        self.kb = self.tile(st, "kb", [128, 16, 64], F32)
        self.epsT = self.tile(st, "epsT", [128, 1], F32)
        S.load("sp", self.ident[:, :], self.dc["ident"], self.ident.b)
        S.load("sp", self.caus[:, :, :], self.dc["caus"], self.caus.b)
        S.load("sp", self.kb[:, :, :], self.dc["kb"], self.kb.b)
        S.op("dve", lambda: nc.vector.memset(self.epsT[:, :], EPS), writes=[self.epsT.b])
        for l in self.layers:
            x_src = self.x_in if l == 0 else self.xs
            x_dst = self.y if l == self.layers[-1] else self.xs
            if "A" in self.phases:
                self.phase_A(l, x_src)
                S.barrier()
            if "B" in self.phases:
                for m in self.mixers:
                    [self.moba, self.nsa, self.diff, self.swa][m](l)
                    S.barrier()
            if "C" in self.phases:
                tags = []
                for k in range((self.nch * 512 + 2047) // 2048):
                    tags.append(self.collective("AllGather", ALU.bypass, self.mix[k * 2048:(k + 1) * 2048, :].opt(),
                                                self.mixall[k * 4096:(k + 1) * 4096, :].opt()))
                self.prep_ffn(l)
                for ek in S.eng:
                    S._wait(ek, tags[-1])
                S.barrier()
                self.phase_C(l, x_src, x_dst)
                S.barrier()
        S.barrier()
        if self.debug:
            nc = self.nc
            dbg = {}
            for nm, src, rows, cols, dt in (("d_mixall", self.mixall, 4096, 512, BF16), ("d_part", self.part, 2048, D, F32),
                                            ("d_red", self.red, 2048, D, F32), ("d_x1s", self.x1s, 2048, D, F32), ("d_mix", self.mix, 2048, 512, BF16)):
                dst = nc.dram_tensor(nm, [rows, cols], dt, kind="ExternalOutput").ap()
                b = Buf(nm)
                S._dsem(b)
                ins = nc.sync.dma_start(out=dst[:, :], in_=src[0:rows, :])
                b.dcnt += 16
                ins.then_inc(b.dsem, 16)
                S._wait("sp", ("dma", b.dsem, b.dcnt))

    def rstd_of(self, x_ap, xbuf, junk, ss, sd, rstd, n=D):
        nc, S = self.nc, self.S
        S.op("dve", lambda: nc.vector.scalar_tensor_tensor(out=junk[:, 0:n], in0=x_ap, scalar=1.0, in1=x_ap,
                                                           op0=ALU.mult, op1=ALU.mult, accum_out=ss[:, 0:1]),
             reads=[xbuf], writes=[junk.b, ss.b])
        S.op("act", lambda: nc.scalar.activation(out=sd[:, 0:1], in_=ss[:, 0:1], func=AF.Sqrt,
                                                 bias=self.epsT[:, 0:1], scale=1.0 / n),
             reads=[ss.b, self.epsT.b], writes=[sd.b])
        S.op("dve", lambda: nc.vector.reciprocal(out=rstd[:, 0:1], in_=sd[:, 0:1]), reads=[sd.b], writes=[rstd.b])

    def phase_A(self, l, x_src):
        nc, S = self.nc, self.S
        with ExitStack() as ph:
            WF = self.tile(ph, "WF", [128, 8, NF], BF16)
            WV = self.tile(ph, "WV", [128, 8, NV], BF16)
            gain = self.tile(ph, "gainA", [128, D], F32)
            with ExitStack() as tmp:
                self.load_cast(tmp, WF, lambda i: WF[:, i, :], lambda i: self.dp["wf"][l, :, i, :], 8, NF)
                self.load_cast(tmp, WV, lambda i: WV[:, i, :], lambda i: self.dp["wv"][l, :, i, :], 8, NV)
                S.barrier()
            S.load("sp", gain[:, :], self.dp["g_apre"][l].partition_broadcast(128), gain.b)
            xts = [self.tile(ph, "xtA%d" % i, [128, D], F32) for i in range(3)]
            junk = self.tile(ph, "junkA", [128, D], F32)
            ss = self.tile(ph, "ssA", [128, 1], F32)
            sd = self.tile(ph, "sdA", [128, 1], F32)
            rstd = self.tile(ph, "rstdA", [128, 1], F32)
            hb = [self.tile(ph, "hbA%d" % i, [128, D], BF16) for i in range(2)]
            hT = [self.tile(ph, "hTA%d" % i, [128, 8, 512], BF16) for i in range(2)]
            tp = [self.tile(ph, "tpA%d" % i, [128, D], BF16, psum=True) for i in range(2)]
            psF = [self.tile(ph, "psF%d" % i, [128, 512], F32, psum=True) for i in range(2)]
            psV0 = self.tile(ph, "psV0", [128, 512], F32, psum=True)
            fst = [self.tile(ph, "fst%d" % i, [128, 512], BF16) for i in range(3)]
            vst = [self.tile(ph, "vst%d" % i, [128, NV], BF16) for i in range(2)]
            hb3 = hb + [self.tile(ph, "hbA2", [128, D], BF16)]

            def pa1(T):
                tok = T * 128
                xt, h = xts[T % 3], hb3[T % 3]
                S.load("sp", xt[:, :], x_src[tok:tok + 128, :], xt.b)
                self.rstd_of(xt[:, :], xt.b, junk, ss, sd, rstd)
                S.op("dve", lambda: nc.vector.scalar_tensor_tensor(out=h[:, :], in0=xt[:, :], scalar=rstd[:, 0:1],
                                                                   in1=gain[:, :], op0=ALU.mult, op1=ALU.mult),
                     reads=[xt.b, rstd.b, gain.b], writes=[h.b])

            def pa2(T):
                h, tpp, hTt, ti = hb3[T % 3], tp[T % 2], hT[(T // 4) % 2], T % 4
                for kc in range(8):
                    S.op("pe", lambda: nc.tensor.transpose(out=tpp[:, kc * 128:(kc + 1) * 128],
                                                           in_=h[:, kc * 128:(kc + 1) * 128], identity=self.ident[:, :]),
                         reads=[h.b, self.ident.b], writes=[tpp.b], accum=(kc > 0))
                self.copy(hTt[:, :, ti * 128:(ti + 1) * 128], tpp[:, :].rearrange("p (k t) -> p k t", k=8),
                          [tpp.b], [hTt.b])

            NTL = 4 * self.nch
            for T in range(4):
                pa1(T)
                pa2(T)
            for c in range(self.nch):
                hTc = hT[c % 2]
                for g in range(NF // 128):
                    ps = psF[g % 2]
                    for kc in range(8):
                        S.op("pe", lambda: nc.tensor.matmul(ps[:, :], lhsT=WF[:, kc, g * 128:(g + 1) * 128], rhs=hTc[:, kc, :],
                                                            start=(kc == 0), stop=(kc == 7)),
                             reads=[WF.b, hTc.b], writes=[ps.b], accum=(kc > 0))
                    f = fst[g % 3]
                    self.copy(f[:, :], ps[:, :], [ps.b], [f.b])
                    S.store("pool", self.featT[g * 128:(g + 1) * 128, c * 512:(c + 1) * 512], f[:, :], f.b)
                    if g % 2 == 1 and c + 1 < self.nch:
                        k = g // 2
                        if 1 <= k <= 4:
                            pa2(4 * (c + 1) + k - 1)
                        if k <= 3:
                            pa1(4 * (c + 1) + k)
                for ti in range(4):
                    tok = (4 * c + ti) * 128
                    for kc in range(8):
                        S.op("pe", lambda: nc.tensor.matmul(psV0[:, 0:NV], lhsT=hTc[:, kc, ti * 128:(ti + 1) * 128], rhs=WV[:, kc, 0:NV],
                                                            start=(kc == 0), stop=(kc == 7)),
                             reads=[WV.b, hTc.b], writes=[psV0.b], accum=(kc > 0))
                    v = vst[ti % 2]
                    self.copy(v[:, 0:NV], psV0[:, 0:NV], [psV0.b], [v.b])
                    S.store("pool", self.vtok[tok:tok + 128, :], v[:, :], v.b)
            S.barrier()

    def attn_res(self, ph, nps=3, npt=3, no=3):
        R = type("R", (), {})()
        R.ps = [self.tile(ph, "ps_s%d" % i, [128, 512], F32, psum=True) for i in range(nps)]
        R.pt = [self.tile(ph, "pT%d" % i, [128, 512], BF16) for i in range(npt)]
        R.O = [self.tile(ph, "O%d" % i, [128, 512], F32, psum=True) for i in range(no)]
        R.ips = R.ipt = R.io = 0
        return R

    def attend(self, R, q_ap, qbufs, tiles, O, scale):
        nc, S = self.nc, self.S
        Ov = O[:, 0:260].rearrange("p (a b) -> p a b", a=4)
        n = len(tiles)

        def pv(pt, tl, first, last):
            q0 = tl.get("q0", 0)
            assert not (first and q0)
            for qi in range(q0 // 128, 4):
                S.op("pe", lambda: nc.tensor.matmul(Ov[:, qi, :], lhsT=pt[:, qi * 128:(qi + 1) * 128], rhs=tl["v"],
                                                    start=(first and qi == 0), stop=last),
                     reads=[pt.b] + tl["kbufs"], writes=[O.b], accum=not (first and qi == 0))
            if tl.get("post") is not None:
                tl["post"](pt, first, last)

        prev = None
        for i, tl in enumerate(tiles):
            ps = R.ps[R.ips % len(R.ps)]
            R.ips += 1
            q0 = tl.get("q0", 0)
            ex = tl.get("extra", [])
            S.op("pe", lambda: nc.tensor.matmul(ps[:, q0:512], lhsT=tl["kT"], rhs=q_ap[:, q0:512], start=True, stop=(len(ex) == 0)),
                 reads=qbufs + tl["kbufs"], writes=[ps.b])
            for j, (lh, rh, bufs) in enumerate(ex):
                S.op("pe", lambda: nc.tensor.matmul(ps[:, q0:512], lhsT=lh, rhs=rh[:, q0:512], start=False, stop=(j == len(ex) - 1)),
                     reads=bufs, writes=[ps.b], accum=True)
            pt = R.pt[R.ipt % len(R.pt)]
            R.ipt += 1
            S.op("act", lambda: nc.scalar.activation(out=pt[:, q0:512], in_=ps[:, q0:512], func=AF.Exp, bias=tl["bias"], scale=scale),
                 reads=[ps.b] + tl["bbufs"], writes=[pt.b])
            if prev is not None:
                pv(prev[0], prev[1], prev[2] == 0, False)
            prev = (pt, tl, i)
        pv(prev[0], prev[1], prev[2] == 0, True)
        return Ov

    def load_kv(self, KT, V, krows, nh_k, k_row0, v_col0, nh_v, kd=64):
        nc, S = self.nc, self.S
        S.op("pool", lambda: nc.gpsimd.memset(V[:, :, :, 64:65], 1.0), writes=[V.b])
        for h in range(nh_k):
            S.load("sp", KT[0:kd, h, :], self.featT[k_row0 + h * kd:k_row0 + (h + 1) * kd, :], KT.b)
        S.load("sp", KT[kd:kd + 3, :, :], self.dc["ones3"][:, 0:nh_k * S_LEN].rearrange("r (h s) -> r h s", h=nh_k), KT.b)
        for h in range(nh_v):
            S.load("sp", V[:, h, :, 0:64],
                   self.vtok[:, v_col0 + h * 64:v_col0 + (h + 1) * 64].rearrange("(kt p) d -> p kt d", p=128), V.b)

    def recip_den(self, Ov, O, den, add_ap=None, add_bufs=()):
        nc, S = self.nc, self.S
        if add_ap is None:
            S.op("dve", lambda: nc.vector.tensor_scalar(out=den[:, 0:4], in0=Ov[:, :, 64], scalar1=1e-30, scalar2=None,
                                                        op0=ALU.max), reads=[O.b], writes=[den.b])
        else:
            S.op("dve", lambda: nc.vector.tensor_scalar(out=den[:, 0:4], in0=Ov[:, :, 64], scalar1=add_ap, scalar2=1e-30,
                                                        op0=ALU.add, op1=ALU.max), reads=[O.b] + list(add_bufs), writes=[den.b])
        S.op("dve", lambda: nc.vector.reciprocal(out=den[:, 0:4], in_=den[:, 0:4]), reads=[den.b], writes=[den.b])

    def moba(self, l):
        nc, S = self.nc, self.S
        with ExitStack() as ph:
            KT = self.tile(ph, "KTm", [128, 2, S_LEN], BF16)
            V = self.tile(ph, "Vm", [128, 2, NT, 65], BF16)
            BI = self.tile(ph, "BIm", [128, 4, 32], F32)
            S.load("sp", BI[:, :, :], self.dc["bi"], BI.b)
            S.op("pool", lambda: nc.gpsimd.memset(V[:, :, :, 64:65], 1.0), writes=[V.b])
            S.op("pool", lambda: nc.gpsimd.memset(KT[64:128, :, :], 0.0), writes=[KT.b])
            for h in range(2):
                S.load("sp", KT[96:128, h, :], self.dc["em"], KT.b)
                S.load("sp", KT[0:64, h, :], self.featT[FRr["mk"] + 64 * h:FRr["mk"] + 64 * (h + 1), :], KT.b)
                S.load("sp", V[:, h, :, 0:64],
                       self.vtok[:, VCr["mv"] + h * 64:VCr["mv"] + (h + 1) * 64].rearrange("(kt p) d -> p kt d", p=128), V.b)
            S.load("sp", KT[64:67, :, :], self.dc["ones3"][:, 0:2 * S_LEN].rearrange("r (h s) -> r h s", h=2), KT.b)
            R = self.attn_res(ph)
            QT = [[self.tile(ph, "QTm%d_%d" % (i, h), [128, 512], BF16) for h in range(2)] for i in range(2)]
            for qs in QT:
                for h in range(2):
                    S.op("pool", lambda: nc.gpsimd.memset(qs[h][64:128, :], 0.0), writes=[qs[h].b])
                    S.load("sp", qs[h][64:67, :], self.dc["aug"][h], qs[h].b)
            kms = self.tile(ph, "kms", [128, 2, 32], F32)
            kmh = self.tile(ph, "kmh", [128, 2, 32], BF16)
            kml = self.tile(ph, "kml", [128, 2, 32], BF16)
            kmr = self.tile(ph, "kmr", [128, 2, 32], F32)
            for h in range(2):
                S.op("dve", lambda: nc.vector.tensor_reduce(out=kms[0:64, h, :], in_=KT[0:64, h, :].rearrange("p (n k) -> p n k", k=256),
                                                            axis=AX.X, op=ALU.add), reads=[KT.b], writes=[kms.b])
            S.op("dve", lambda: nc.vector.tensor_scalar(out=kms[0:64, :, :], in0=kms[0:64, :, :], scalar1=1.0 / 256, scalar2=None, op0=ALU.mult),
                 reads=[kms.b], writes=[kms.b])
            S.op("dve", lambda: nc.vector.tensor_copy(out=kmh[0:64, :, :], in_=kms[0:64, :, :]), reads=[kms.b], writes=[kmh.b])
            S.op("dve", lambda: nc.vector.tensor_tensor(out=kmr[0:64, :, :], in0=kms[0:64, :, :], in1=kmh[0:64, :, :], op=ALU.subtract),
                 reads=[kms.b, kmh.b], writes=[kmr.b])
            S.op("dve", lambda: nc.vector.tensor_copy(out=kml[0:64, :, :], in_=kmr[0:64, :, :]), reads=[kmr.b], writes=[kml.b])
            gps = self.tile(ph, "gps", [128, 512], F32, psum=True)
            tps = self.tile(ph, "tpsm", [128, 1024], BF16, psum=True)
            past = self.tile(ph, "past", [128, 4, 32], F32)
            own = self.tile(ph, "own", [128, 4, 32], F32)
            negp = self.tile(ph, "negp", [128, 4, 32], F32)
            gm = self.tile(ph, "gm", [128, 4, 32], F32)
            m8 = self.tile(ph, "m8", [128, 4, 8], F32)
            sel = self.tile(ph, "sel", [128, 4, 32], F32)
            nsel = self.tile(ph, "nsel", [128, 4, 32], BF16)
            den = self.tile(ph, "denm", [128, 4], F32)
            mst = [self.tile(ph, "mstm%d" % i, [128, 4, 128], BF16) for i in range(2)]
            for c in range(self.nch):
                qs = QT[c % 2]
                for h in range(2):
                    S.load("sp", qs[h][0:64, :], self.featT[FRr["mq"] + 64 * h:FRr["mq"] + 64 * (h + 1), c * 512:(c + 1) * 512], qs[h].b)
                S.op("dve", lambda: nc.vector.tensor_scalar(out=past[:, :, :], in0=BI[:, :, :], scalar1=float(2 * c), scalar2=None, op0=ALU.is_lt),
                     reads=[BI.b], writes=[past.b])
                S.op("dve", lambda: nc.vector.tensor_scalar(out=own[:, :, :], in0=BI[:, :, :], scalar1=float(2 * c), scalar2=None, op0=ALU.is_equal),
                     reads=[BI.b], writes=[own.b])
                S.op("dve", lambda: nc.vector.tensor_scalar(out=negp[:, :, :], in0=past[:, :, :], scalar1=-1.0, scalar2=1e30, op0=ALU.add, op1=ALU.mult),
                     reads=[past.b], writes=[negp.b])
                ms = mst[c % 2]
                for h in range(2):
                    q = qs[h]
                    gv = gps[:, 0:128].rearrange("p (a b) -> p a b", a=4)
                    for qi in range(4):
                        S.op("pe", lambda: nc.tensor.matmul(gv[:, qi, :], lhsT=q[0:64, qi * 128:(qi + 1) * 128], rhs=kmh[0:64, h, :], start=True, stop=False),
                             reads=[q.b, kmh.b], writes=[gps.b], accum=(qi > 0))
                        S.op("pe", lambda: nc.tensor.matmul(gv[:, qi, :], lhsT=q[0:64, qi * 128:(qi + 1) * 128], rhs=kml[0:64, h, :], start=False, stop=True),
                             reads=[q.b, kml.b], writes=[gps.b], accum=True)
                    S.op("dve", lambda: nc.vector.tensor_tensor(out=gm[:, :, :], in0=gv, in1=negp[:, :, :], op=ALU.add),
                         reads=[gps.b, negp.b], writes=[gm.b])
                    for qi in range(4):
                        S.op("dve", lambda: nc.vector.max(out=m8[:, qi, :], in_=gm[:, qi, :]), reads=[gm.b], writes=[m8.b])
                    for qi in range(4):
                        S.op("dve", lambda: nc.vector.tensor_scalar(out=sel[:, qi, :], in0=gm[:, qi, :], scalar1=m8[:, qi, 2:3], scalar2=None, op0=ALU.is_ge),
                             reads=[gm.b, m8.b], writes=[sel.b])
                    S.op("dve", lambda: nc.vector.tensor_tensor(out=sel[:, :, :], in0=sel[:, :, :], in1=past[:, :, :], op=ALU.mult),
                         reads=[sel.b, past.b], writes=[sel.b])
                    S.op("dve", lambda: nc.vector.tensor_tensor(out=sel[:, :, :], in0=sel[:, :, :], in1=own[:, :, :], op=ALU.add),
                         reads=[sel.b, own.b], writes=[sel.b])
                    S.op("dve", lambda: nc.vector.tensor_scalar(out=nsel[:, :, :], in0=sel[:, :, :], scalar1=-1.0, scalar2=-NEG, op0=ALU.add, op1=ALU.mult),
                         reads=[sel.b], writes=[nsel.b])
                    for qi in range(4):
                        S.op("pe", lambda: nc.tensor.transpose(out=tps[0:32, qi * 128:(qi + 1) * 128], in_=nsel[:, qi, :], identity=self.ident[:, :]),
                             reads=[nsel.b, self.ident.b], writes=[tps.b], accum=(qi > 0))
                    self.copy(q[96:128, :], tps[0:32, 0:512], [tps.b], [q.b], eng="dve")
                    tiles = []
                    for kt in range(4 * c + 4):
                        if far_skip(0, h, 512 * c - (128 * kt + 127)):
                            continue
                        r = kt - 4 * c
                        ex = []
                        if r >= 0:
                            ex.append((self.ident[:, :], self.caus[:, r, :], [self.ident.b, self.caus.b]))
                        tiles.append(dict(kT=KT[0:128, h, kt * 128:(kt + 1) * 128], v=V[:, h, kt, :], kbufs=[KT.b, V.b],
                                          bias=self.kb[:, 0 + h, r + 60:r + 61], bbufs=[self.kb.b], extra=ex, q0=128 * max(r, 0)))
                    O = R.O[R.io % len(R.O)]
                    R.io += 1
                    Ov = self.attend(R, q[0:128, :], [q.b], tiles, O, 0.125)
                    self.recip_den(Ov, O, den)
                    for qi in range(4):
                        S.op("dve", lambda: nc.vector.tensor_scalar(out=ms[:, qi, h * 64:(h + 1) * 64], in0=Ov[:, qi, 0:64], scalar1=den[:, qi:qi + 1],
                                                                    scalar2=None, op0=ALU.mult), reads=[O.b, den.b], writes=[ms.b])
                S.store("pool", self.mix[c * 512:(c + 1) * 512, 0:128].rearrange("(a p) d -> p a d", p=128), ms[:, :, :], ms.b)
            S.barrier()

    def swa(self, l):
        nc, S = self.nc, self.S
        with ExitStack() as ph:
            KT = self.tile(ph, "KTs", [128, 1, S_LEN], BF16)
            V = self.tile(ph, "Vs", [128, 1, NT, 65], BF16)
            SM = self.tile(ph, "SMs", [128, 5, 512], BF16)
            S.load("sp", SM[:, :, :], self.dc["swamask"], SM.b)
            self.load_kv(KT, V, 67, 1, FRr["sk"], VCr["sv"], 1)
            sk = self.tile(ph, "sinks", [128, 4], F32)
            esk = self.tile(ph, "esinks", [128, 4], F32)
            S.load("sp", sk[:, :], self.dp["sinks"][l].partition_broadcast(128), sk.b)
            S.op("act", lambda: nc.scalar.activation(out=esk[:, :], in_=sk[:, :], func=AF.Exp), reads=[sk.b], writes=[esk.b])
            R = self.attn_res(ph)
            QT = [self.tile(ph, "QTs%d" % i, [128, 2, 512], BF16) for i in range(2)]
            for q in QT:
                S.load("sp", q[64:67, :, :], self.dc["aug"][12:14].rearrange("h r q -> r h q"), q.b)
            den = self.tile(ph, "dens", [128, 4], F32)
            mst = [self.tile(ph, "msts%d" % i, [128, 4, 128], BF16) for i in range(2)]
            for c in range(self.nch):
                q = QT[c % 2]
                for h in range(2):
                    S.load("sp", q[0:64, h, :], self.featT[FRr["sq"] + 64 * h:FRr["sq"] + 64 * (h + 1), c * 512:(c + 1) * 512], q.b)
                ms = mst[c % 2]
                for h in range(2):
                    g = 0
                    tiles = []
                    for r in range(-1, 4):
                        kt = 4 * c + r
                        if kt < 0:
                            continue
                        tiles.append(dict(kT=KT[0:67, g, kt * 128:(kt + 1) * 128], v=V[:, g, kt, :], kbufs=[KT.b, V.b],
                                          bias=self.kb[:, 12 + h, r + 60:r + 61], bbufs=[self.kb.b],
                                          extra=[(self.ident[:, :], SM[:, r + 1, :], [self.ident.b, SM.b])], q0=128 * max(r, 0)))
                    O = R.O[R.io % len(R.O)]
                    R.io += 1
                    Ov = self.attend(R, q[0:67, h, :], [q.b], tiles, O, 0.125)
                    self.recip_den(Ov, O, den, add_ap=esk[:, h:h + 1], add_bufs=[esk.b])
                    for qi in range(4):
                        S.op("dve", lambda: nc.vector.tensor_scalar(out=ms[:, qi, h * 64:(h + 1) * 64], in0=Ov[:, qi, 0:64], scalar1=den[:, qi:qi + 1],
                                                                    scalar2=None, op0=ALU.mult), reads=[O.b, den.b], writes=[ms.b])
                S.store("pool", self.mix[c * 512:(c + 1) * 512, 384:512].rearrange("(a p) d -> p a d", p=128), ms[:, :, :], ms.b)
            S.barrier()

    def diff(self, l):
        nc, S = self.nc, self.S
        lambda_init = 0.8 - 0.6 * math.exp(-0.3 * l)
        sc = 32 ** -0.5
        with ExitStack() as ph:
            KT = self.tile(ph, "KTd", [128, 2, S_LEN], BF16)
            V = self.tile(ph, "Vd", [128, 2, NT, 65], BF16)
            S.op("pool", lambda: nc.gpsimd.memset(V[:, :, :, 64:65], 1.0), writes=[V.b])
            for h in range(2):
                r0 = FRr["dk"] + 64 * h
                S.load("sp", KT[0:32, h, :], self.featT[r0:r0 + 32, :], KT.b)
                S.load("sp", KT[64:96, h, :], self.featT[r0 + 32:r0 + 64, :], KT.b)
                S.load("sp", V[:, h, :, 0:64], self.vtok[:, VCr["dv"] + h * 64:VCr["dv"] + (h + 1) * 64].rearrange("(kt p) d -> p kt d", p=128), V.b)
            ones = self.dc["ones3"][:, 0:2 * S_LEN].rearrange("r (h s) -> r h s", h=2)
            S.load("sp", KT[32:35, :, :], ones, KT.b)
            S.load("sp", KT[96:99, :, :], ones, KT.b)
            lam = self.tile(ph, "lam", [128, 4], F32)
            lt = self.tile(ph, "lamt", [128, 4, 32], F32)
            lj = self.tile(ph, "lamj", [128, 32], F32)
            for i, nme in enumerate(["lq1", "lk1", "lq2", "lk2"]):
                S.load("sp", lt[:, i, :], self.dp[nme][l].partition_broadcast(128), lt.b)
            for i in range(2):
                S.op("dve", lambda: nc.vector.scalar_tensor_tensor(out=lj[:, :], in0=lt[:, 2 * i, :], scalar=1.0, in1=lt[:, 2 * i + 1, :],
                                                                   op0=ALU.mult, op1=ALU.mult, accum_out=lam[:, i:i + 1]),
                     reads=[lt.b], writes=[lj.b, lam.b])
            S.op("act", lambda: nc.scalar.activation(out=lam[:, 0:2], in_=lam[:, 0:2], func=AF.Exp), reads=[lam.b], writes=[lam.b])
            S.op("dve", lambda: nc.vector.tensor_tensor(out=lam[:, 2:3], in0=lam[:, 1:2], in1=lam[:, 0:1], op=ALU.subtract),
                 reads=[lam.b], writes=[lam.b])
            S.op("dve", lambda: nc.vector.tensor_scalar(out=lam[:, 2:3], in0=lam[:, 2:3], scalar1=-lambda_init, scalar2=None, op0=ALU.add),
                 reads=[lam.b], writes=[lam.b])
            gs = self.tile(ph, "gsub", [128, 64], F32)
            S.load("sp", gs[:, :], self.dp["subln"][l].partition_broadcast(128), gs.b)
            S.op("dve", lambda: nc.vector.tensor_scalar(out=gs[:, :], in0=gs[:, :], scalar1=1.0 - lambda_init, scalar2=None, op0=ALU.mult),
                 reads=[gs.b], writes=[gs.b])
            R = self.attn_res(ph, no=4)
            QT = [self.tile(ph, "QTd%d" % i, [128, 2, 512], BF16) for i in range(2)]
            for q in QT:
                a = self.dc["aug"][8:10].rearrange("h r q -> r h q")
                S.load("sp", q[32:35, :, :], a, q.b)
                S.load("sp", q[96:99, :, :], a, q.b)
            den1 = self.tile(ph, "den1", [128, 4], F32)
            den2 = self.tile(ph, "den2", [128, 4], F32)
            o1 = self.tile(ph, "o1d", [128, 4, 64], F32)
            od = self.tile(ph, "od", [128, 4, 64], F32)
            jk = self.tile(ph, "jkd", [128, 64], F32)
            ssd = self.tile(ph, "ssd", [128, 4], F32)
            mst = [self.tile(ph, "mstd%d" % i, [128, 4, 128], BF16) for i in range(2)]
            for c in range(self.nch):
                q = QT[c % 2]
                for h in range(2):
                    r0 = FRr["dq"] + 64 * h
                    S.load("sp", q[0:32, h, :], self.featT[r0:r0 + 32, c * 512:(c + 1) * 512], q.b)
                    S.load("sp", q[64:96, h, :], self.featT[r0 + 32:r0 + 64, c * 512:(c + 1) * 512], q.b)
                ms = mst[c % 2]
                for h in range(2):
                    Os = []
                    for j in range(2):
                        b0 = 64 * j
                        tiles = []
                        for kt in range(4 * c + 4):
                            if far_skip(2, h, 512 * c - (128 * kt + 127)):
                                continue
                            r = kt - 4 * c
                            ex = []
                            if r >= 0:
                                ex.append((self.ident[:, :], self.caus[:, r, :], [self.ident.b, self.caus.b]))
                            tiles.append(dict(kT=KT[b0:b0 + 35, h, kt * 128:(kt + 1) * 128], v=V[:, h, kt, :], kbufs=[KT.b, V.b],
                                              bias=self.kb[:, 8 + h, r + 60:r + 61], bbufs=[self.kb.b], extra=ex, q0=128 * max(r, 0)))
                        O = R.O[R.io % len(R.O)]
                        R.io += 1
                        Ov = self.attend(R, q[b0:b0 + 35, h, :], [q.b], tiles, O, sc)
                        Os.append((O, Ov))
                    (O1, Ov1), (O2, Ov2) = Os
                    self.recip_den(Ov1, O1, den1)
                    self.recip_den(Ov2, O2, den2)
                    S.op("dve", lambda: nc.vector.tensor_scalar(out=den2[:, :], in0=den2[:, :], scalar1=lam[:, 2:3], scalar2=None, op0=ALU.mult),
                         reads=[den2.b, lam.b], writes=[den2.b])
                    for qi in range(4):
                        S.op("dve", lambda: nc.vector.tensor_scalar(out=o1[:, qi, :], in0=Ov1[:, qi, 0:64], scalar1=den1[:, qi:qi + 1], scalar2=None, op0=ALU.mult),
                             reads=[O1.b, den1.b], writes=[o1.b])
                        S.op("dve", lambda: nc.vector.scalar_tensor_tensor(out=od[:, qi, :], in0=Ov2[:, qi, 0:64], scalar=den2[:, qi:qi + 1], in1=o1[:, qi, :],
                                                                           op0=ALU.mult, op1=ALU.add), reads=[O2.b, den2.b, o1.b], writes=[od.b])
                        S.op("dve", lambda: nc.vector.scalar_tensor_tensor(out=jk[:, :], in0=od[:, qi, :], scalar=1.0, in1=od[:, qi, :],
                                                                           op0=ALU.mult, op1=ALU.mult, accum_out=ssd[:, qi:qi + 1]),
                             reads=[od.b], writes=[jk.b, ssd.b])
                    S.op("act", lambda: nc.scalar.activation(out=ssd[:, :], in_=ssd[:, :], func=AF.Ln, bias=self.epsT[:, 0:1], scale=1.0 / 64),
                         reads=[ssd.b, self.epsT.b], writes=[ssd.b])
                    S.op("act", lambda: nc.scalar.activation(out=ssd[:, :], in_=ssd[:, :], func=AF.Exp, scale=-0.5), reads=[ssd.b], writes=[ssd.b])
                    for qi in range(4):
                        S.op("dve", lambda: nc.vector.scalar_tensor_tensor(out=ms[:, qi, h * 64:(h + 1) * 64], in0=od[:, qi, :], scalar=ssd[:, qi:qi + 1],
                                                                           in1=gs[:, :], op0=ALU.mult, op1=ALU.mult),
                             reads=[od.b, ssd.b, gs.b], writes=[ms.b])
                S.store("pool", self.mix[c * 512:(c + 1) * 512, 256:384].rearrange("(a p) d -> p a d", p=128), ms[:, :, :], ms.b)
            S.barrier()

    def nsa(self, l):
        nc, S = self.nc, self.S
        with ExitStack() as ph:
            KCT = self.tile(ph, "KCT", [128, 512], BF16)
            VCt = self.tile(ph, "VCt", [128, 4, 65], BF16)
            S.op("pool", lambda: nc.gpsimd.memset(VCt[:, :, :], 1.0), writes=[VCt.b])
            S.load("sp", KCT[64:67, :], self.dc["ones3"][:, 0:512], KCT.b)
            with ExitStack() as cs:
                XT = self.tile(cs, "XTc", [64, S_LEN], BF16)
                w1 = self.tile(cs, "w1c", [64, 32, 128], BF16)
                w2 = self.tile(cs, "w2c", [128, 64], BF16)
                pos = self.tile(cs, "posc", [64, 32], BF16)
                b1 = self.tile(cs, "b1c", [128, 1], F32)
                cbias = self.tile(cs, "cbias", [128, 1], F32)
                hid = self.tile(cs, "hidc", [128, 512], BF16)
                sg = self.tile(cs, "sgc", [128, 4096], F32)
                hp = self.tile(cs, "hpc", [128, 512], F32, psum=True)
                cp = self.tile(cs, "cpc", [128, 512], F32, psum=True)
                op_ = self.tile(cs, "opc", [128, 512], F32, psum=True)
                for s_i, sfx in enumerate("kv"):
                    S.load("sp", XT[:, :], self.featT[FRr["nkc" if sfx == "k" else "nvc"]:FRr["nkc" if sfx == "k" else "nvc"] + 64, :], XT.b)
                    S.load("sp", sg[0:64, 0:4096], self.dp["w1" + sfx][l].rearrange("d l f -> d (l f)"), sg.b)
                    self.copy(w1[:, :, :], sg[0:64, 0:4096].rearrange("d (l f) -> d l f", l=32), [sg.b], [w1.b])
                    S.load("sp", sg[:, 0:64], self.dp["w2" + sfx][l], sg.b)
                    self.copy(w2[:, :], sg[:, 0:64], [sg.b], [w2.b])
                    S.load("sp", sg[0:64, 0:32], self.dp["pos" + sfx][l], sg.b)
                    self.copy(pos[:, :], sg[0:64, 0:32], [sg.b], [pos.b])
                    S.load("sp", b1[:, :], self.dp["b1" + sfx][l], b1.b)
                    for li in range(32):
                        S.op("pe", lambda: nc.tensor.matmul(hp[:, 0:511], lhsT=w1[:, li, :], rhs=XT[:, li:li + 16 * 510 + 1:16], start=(li == 0), stop=(li == 31)),
                             reads=[w1.b, XT.b], writes=[hp.b], accum=(li > 0))
                    for li in range(32):
                        S.op("pe", lambda: nc.tensor.matmul(cp[:, 0:1], lhsT=w1[:, li, :], rhs=pos[:, li:li + 1], start=(li == 0), stop=(li == 31)),
                             reads=[w1.b, pos.b], writes=[cp.b], accum=(li > 0))
                    S.op("dve", lambda: nc.vector.tensor_tensor(out=cbias[:, :], in0=cp[:, 0:1], in1=b1[:, :], op=ALU.add),
                         reads=[cp.b, b1.b], writes=[cbias.b])
                    S.op("pool", lambda: nc.gpsimd.memset(hid[:, :], 0.0), writes=[hid.b])
                    S.op("act", lambda: nc.scalar.activation(out=hid[:, 0:511], in_=hp[:, 0:511], func=AF.Gelu_apprx_tanh, bias=cbias[:, 0:1]),
                         reads=[hp.b, cbias.b], writes=[hid.b])
                    if sfx == "k":
                        S.op("pe", lambda: nc.tensor.matmul(op_[0:64, 0:512], lhsT=w2[:, :], rhs=hid[:, :], start=True, stop=True),
                             reads=[w2.b, hid.b], writes=[op_.b])
                        self.copy(KCT[0:64, :], op_[0:64, 0:512], [op_.b], [KCT.b])
                    else:
                        ov_ = op_[:, 0:256].rearrange("p (a b) -> p a b", a=4)
                        for kt in range(4):
                            S.op("pe", lambda: nc.tensor.matmul(ov_[:, kt, :], lhsT=hid[:, kt * 128:(kt + 1) * 128], rhs=w2[:, :], start=True, stop=True),
                                 reads=[w2.b, hid.b], writes=[op_.b], accum=(kt > 0))
                        self.copy(VCt[:, :, 0:64], ov_, [op_.b], [VCt.b])
                S.barrier()
            KTs = self.tile(ph, "KTsl", [128, 1, S_LEN], BF16)
            KTw = self.tile(ph, "KTwn", [128, 1, S_LEN], BF16)
            Vs = self.tile(ph, "Vsl", [128, 1, NT, 65], BF16)
            Vw = self.tile(ph, "Vwn", [128, 1, NT, 65], BF16)
            self.load_kv(KTs, Vs, 67, 1, FRr["nks"], VCr["nvs"], 1)
            self.load_kv(KTw, Vw, 67, 1, FRr["nkw"], VCr["nvw"], 1)
            ES = self.tile(ph, "ESn", [128, S_LEN], BF16)
            WM = self.tile(ph, "WMn", [128, 8, 512], BF16)
            CM = self.tile(ph, "CMn", [128, 5, 512], BF16)
            OVt = self.tile(ph, "OVn", [128, 4, 128], BF16)
            KBC = self.tile(ph, "KBCn", [128, 4, 16, 4], F32)
            S.load("sp", ES[:, :], self.dc["es"], ES.b)
            S.load("sp", WM[:, :, :], self.dc["wmask"], WM.b)
            S.load("sp", CM[:, :, :], self.dc["cmask"], CM.b)
            S.load("sp", OVt[:, :, :], self.dc["ov"], OVt.b)
            S.load("sp", KBC[:, :, :, :], self.dc["kbc"], KBC.b)
            R = self.attn_res(ph)
            QT = [self.tile(ph, "QTn%d" % i, [128, 4, 512], BF16) for i in range(2)]
            for q in QT:
                S.load("sp", q[64:67, :, :], self.dc["aug"][4:8].rearrange("h r q -> r h q"), q.b)
            imps = self.tile(ph, "imps", [128, 512], F32, psum=True)
            tps = self.tile(ph, "tpsn", [128, 1024], BF16, psum=True)
            impa = self.tile(ph, "impa", [128, 4, 128], F32)
            f1 = [self.tile(ph, "f1e4_%d" % i, [128, 4, 128], F32) for i in range(2)]
            nv = [self.tile(ph, "negv_%d" % i, [128, 4, 128], F32) for i in range(2)]
            gr = [self.tile(ph, "graw%d" % i, [128, 4, 12], BF16) for i in range(2)]
            sgm = self.tile(ph, "sgm", [128, 4, 12], F32)
            scm = self.tile(ph, "scm", [128, 4, 128], F32)
            tmpm = self.tile(ph, "tmpm", [128, 4, 128], F32)
            m8a = self.tile(ph, "m8a", [128, 4, 8], F32)
            m8b = self.tile(ph, "m8b", [128, 4, 8], F32)
            sel = self.tile(ph, "seln", [128, 4, 128], F32)
            val = self.tile(ph, "valn", [128, 4, 128], F32)
            nsel = self.tile(ph, "nseln", [128, 4, 128], BF16)
            nselT = self.tile(ph, "nselTn", [128, 512], BF16)
            den = self.tile(ph, "denn", [128, 4], F32)
            acc = self.tile(ph, "accn", [128, 2, 4, 64], F32)
            mst = [self.tile(ph, "mstn%d" % i, [128, 4, 128], BF16) for i in range(2)]

            def fold(O, Ov, h, gcol, first):
                self.recip_den(Ov, O, den)
                S.op("dve", lambda: nc.vector.tensor_tensor(out=den[:, :], in0=den[:, :], in1=sgm[:, :, gcol], op=ALU.mult),
                     reads=[den.b, sgm.b], writes=[den.b])
                for qi in range(4):
                    if first:
                        S.op("dve", lambda: nc.vector.tensor_scalar(out=acc[:, h, qi, :], in0=Ov[:, qi, 0:64], scalar1=den[:, qi:qi + 1], scalar2=None, op0=ALU.mult),
                             reads=[O.b, den.b], writes=[acc.b])
                    else:
                        S.op("dve", lambda: nc.vector.scalar_tensor_tensor(out=acc[:, h, qi, :], in0=Ov[:, qi, 0:64], scalar=den[:, qi:qi + 1], in1=acc[:, h, qi, :],
                                                                           op0=ALU.mult, op1=ALU.add), reads=[O.b, den.b, acc.b], writes=[acc.b])

            for c in range(self.nch):
                q = QT[c % 2]
                for h in range(4):
                    S.load("sp", q[0:64, h, :], self.featT[FRr["nq"] + 64 * h:FRr["nq"] + 64 * (h + 1), c * 512:(c + 1) * 512], q.b)
                f1c, nvc, grc = f1[c % 2], nv[c % 2], gr[c % 2]
                S.load("sp", f1c[:, :, :], self.dc["f1e4"][c], f1c.b)
                S.load("sp", nvc[:, :, :], self.dc["negv"][c], nvc.b)
                S.load("sp", grc[:, :, :], self.vtok[c * 512:(c + 1) * 512, VCr["ng"]:VCr["ng"] + 12].rearrange("(a p) g -> p a g", p=128), grc.b)
                S.op("act", lambda: nc.scalar.activation(out=sgm[:, :, :], in_=grc[:, :, :], func=AF.Exp, scale=-1.0), reads=[grc.b], writes=[sgm.b])
                S.op("dve", lambda: nc.vector.tensor_scalar(out=sgm[:, :, :], in0=sgm[:, :, :], scalar1=1.0, scalar2=None, op0=ALU.add),
                     reads=[sgm.b], writes=[sgm.b])
                S.op("dve", lambda: nc.vector.reciprocal(out=sgm[:, :, :], in_=sgm[:, :, :]), reads=[sgm.b], writes=[sgm.b])
                ms = mst[c % 2]
                kb_ = c // 4
                iv = imps[:, :].rearrange("p (a b) -> p a b", a=4)
                for h in range(4):
                    tiles = []
                    ntl = kb_ + 1

                    def mkpost(kt, ntl=ntl):
                        def post(pt, first, last):
                            for qi in range(4):
                                S.op("pe", lambda: nc.tensor.matmul(iv[:, qi, :], lhsT=pt[:, qi * 128:(qi + 1) * 128], rhs=OVt[:, kt, :],
                                                                    start=(first and qi == 0), stop=last),
                                     reads=[pt.b, OVt.b], writes=[imps.b], accum=not (first and qi == 0))
                        return post
                    for kt in range(ntl):
                        ex = []
                        if kt == kb_:
                            ex.append((self.ident[:, :], CM[:, c % 4, :], [self.ident.b, CM.b]))
                        elif kt == kb_ - 1 and c % 4 == 0:
                            ex.append((self.ident[:, :], CM[:, 4, :], [self.ident.b, CM.b]))
                        tiles.append(dict(kT=KCT[0:67, kt * 128:(kt + 1) * 128], v=VCt[:, kt, :], kbufs=[KCT.b, VCt.b],
                                          bias=KBC[:, h, c, kt:kt + 1], bbufs=[KBC.b], extra=ex, post=mkpost(kt)))
                    O = R.O[R.io % len(R.O)]
                    R.io += 1
                    Ov = self.attend(R, q[0:67, h, :], [q.b], tiles, O, 0.125)
                    self.recip_den(Ov, O, den)
                    for qi in range(4):
                        if h == 0:
                            S.op("dve", lambda: nc.vector.tensor_scalar(out=impa[:, qi, :], in0=iv[:, qi, :], scalar1=den[:, qi:qi + 1], scalar2=None, op0=ALU.mult),
                                 reads=[imps.b, den.b], writes=[impa.b])
                        else:
                            S.op("dve", lambda: nc.vector.scalar_tensor_tensor(out=impa[:, qi, :], in0=iv[:, qi, :], scalar=den[:, qi:qi + 1], in1=impa[:, qi, :],
                                                                               op0=ALU.mult, op1=ALU.add), reads=[imps.b, den.b, impa.b], writes=[impa.b])
                    if h < 2:
                        fold(O, Ov, h, 3 * h + 0, True)
                S.op("dve", lambda: nc.vector.tensor_tensor(out=scm[:, :, :], in0=impa[:, :, :], in1=f1c[:, :, :], op=ALU.max),
                     reads=[impa.b, f1c.b], writes=[scm.b])
                S.op("dve", lambda: nc.vector.tensor_tensor(out=scm[:, :, :], in0=scm[:, :, :], in1=nvc[:, :, :], op=ALU.add),
                     reads=[scm.b, nvc.b], writes=[scm.b])
                for qi in range(4):
                    S.op("dve", lambda: nc.vector.max(out=m8a[:, qi, :], in_=scm[:, qi, :]), reads=[scm.b], writes=[m8a.b])
                    S.op("dve", lambda: nc.vector.match_replace(out=tmpm[:, qi, :], in_to_replace=m8a[:, qi, :], in_values=scm[:, qi, :], imm_value=-1e30),
                         reads=[scm.b, m8a.b], writes=[tmpm.b])
                    S.op("dve", lambda: nc.vector.max(out=m8b[:, qi, :], in_=tmpm[:, qi, :]), reads=[tmpm.b], writes=[m8b.b])
                    S.op("dve", lambda: nc.vector.tensor_scalar(out=sel[:, qi, :], in0=scm[:, qi, :], scalar1=m8b[:, qi, 7:8], scalar2=None, op0=ALU.is_ge),
                         reads=[scm.b, m8b.b], writes=[sel.b])
                S.op("dve", lambda: nc.vector.tensor_scalar(out=val[:, :, :], in0=nvc[:, :, :], scalar1=-1.0, scalar2=None, op0=ALU.is_ge),
                     reads=[nvc.b], writes=[val.b])
                S.op("dve", lambda: nc.vector.tensor_tensor(out=sel[:, :, :], in0=sel[:, :, :], in1=val[:, :, :], op=ALU.mult),
                     reads=[sel.b, val.b], writes=[sel.b])
                S.op("dve", lambda: nc.vector.tensor_scalar(out=nsel[:, :, :], in0=sel[:, :, :], scalar1=-1.0, scalar2=-NEG, op0=ALU.add, op1=ALU.mult),
                     reads=[sel.b], writes=[nsel.b])
                for qi in range(4):
                    S.op("pe", lambda: nc.tensor.transpose(out=tps[:, qi * 128:(qi + 1) * 128], in_=nsel[:, qi, :], identity=self.ident[:, :]),
                         reads=[nsel.b, self.ident.b], writes=[tps.b], accum=(qi > 0))
                self.copy(nselT[:, :], tps[:, 0:512], [tps.b], [nselT.b], eng="dve")
                for h in range(2):
                    tiles = []
                    for kt in range(4 * c + 4):
                        if far_skip(1, h, 512 * c - (128 * kt + 127)):
                            continue
                        r = kt - 4 * c
                        ex = [(ES[:, kt * 128:(kt + 1) * 128], nselT[:, :], [ES.b, nselT.b])]
                        if r >= 0:
                            ex.append((self.ident[:, :], self.caus[:, r, :], [self.ident.b, self.caus.b]))
                        tiles.append(dict(kT=KTs[0:67, 0, kt * 128:(kt + 1) * 128], v=Vs[:, 0, kt, :], kbufs=[KTs.b, Vs.b],
                                          bias=self.kb[:, 4 + h, r + 60:r + 61], bbufs=[self.kb.b], extra=ex, q0=128 * max(r, 0)))
                    O = R.O[R.io % len(R.O)]
                    R.io += 1
                    Ov = self.attend(R, q[0:67, h, :], [q.b], tiles, O, 0.125)
                    fold(O, Ov, h, 3 * h + 1, False)
                    tiles = []
                    for r in range(-4, 4):
                        kt = 4 * c + r
                        if kt < 0:
                            continue
                        tiles.append(dict(kT=KTw[0:67, 0, kt * 128:(kt + 1) * 128], v=Vw[:, 0, kt, :], kbufs=[KTw.b, Vw.b],
                                          bias=self.kb[:, 4 + h, r + 60:r + 61], bbufs=[self.kb.b],
                                          extra=[(self.ident[:, :], WM[:, r + 4, :], [self.ident.b, WM.b])], q0=128 * max(r, 0)))
                    O = R.O[R.io % len(R.O)]
                    R.io += 1
                    Ov = self.attend(R, q[0:67, h, :], [q.b], tiles, O, 0.125)
                    fold(O, Ov, h, 3 * h + 2, False)
                    S.op("dve", lambda: nc.vector.tensor_copy(out=ms[:, :, h * 64:(h + 1) * 64], in_=acc[:, h, :, :]), reads=[acc.b], writes=[ms.b])
                S.store("pool", self.mix[c * 512:(c + 1) * 512, 128:256].rearrange("(a p) d -> p a d", p=128), ms[:, :, :], ms.b)
            S.barrier()

    def prep_ffn(self, l):
        S = self.S
        with ExitStack() as ph:
            stg = [self.tile(ph, "pstg%d" % i, [128, 4096], F32) for i in range(2)]
            sb = [self.tile(ph, "psb%d" % i, [128, 4096], BF16) for i in range(2)]
            i = 0
            for src, dst in ((self.dp["wg"], self.wgs), (self.dp["wu"], self.wus), (self.dp["wd"], self.wds)):
                for g in range(NCF // 4):
                    s_, b_ = stg[i % 2], sb[i % 2]
                    i += 1
                    if len(src.shape) == 5:
                        sap = src[l, 4 * g:4 * g + 4].rearrange("c p k n -> p c (k n)")
                        dap = dst[4 * g:4 * g + 4].rearrange("c p k n -> p c (k n)")
                    else:
                        sap = src[l, 4 * g:4 * g + 4].rearrange("c p n -> p c n")
                        dap = dst[4 * g:4 * g + 4].rearrange("c p n -> p c n")
                    S.load("sp", s_[:, :].rearrange("p (c n) -> p c n", c=4), sap, s_.b)
                    self.copy(b_[:, :], s_[:, :], [s_.b], [b_.b])
                    S.store("pool", dap, b_[:, :].rearrange("p (c n) -> p c n", c=4), b_.b)
            S.barrier()

    def phase_C(self, l, x_src, x_dst):
        nc, S = self.nc, self.S
        with ExitStack() as ph:
            WO = self.tile(ph, "WO", [128, 8, D], BF16)
            with ExitStack() as tmp:
                self.load_cast(tmp, WO, lambda i: WO[:, i, :], lambda i: self.dp["wo"][l, :, i, :], 8, D)
                S.barrier()
            g1 = self.tile(ph, "g_apost", [128, D], F32)
            g2 = self.tile(ph, "g_fpre", [128, D], F32)
            g3 = self.tile(ph, "g_fpost", [128, D], F32)
            S.load("sp", g1[:, :], self.dp["g_apost"][l].partition_broadcast(128), g1.b)
            S.load("sp", g2[:, :], self.dp["g_fpre"][l].partition_broadcast(128), g2.b)
            S.load("sp", g3[:, :], self.dp["g_fpost"][l].partition_broadcast(128), g3.b)
            cw = self.tile(ph, "cw", [128, NCF, 3], F32)
            cb = self.tile(ph, "cb", [128, NCF], F32)
            S.load("sp", cw[:, :, :], self.dp["cw"][l], cw.b)
            S.load("sp", cb[:, :], self.dp["cb"][l], cb.b)
            halo = self.tile(ph, "halo", [128, NCF, 2], F32)
            S.op("pool", lambda: nc.gpsimd.memset(halo[:, :, :], 0.0), writes=[halo.b])
            bank = [self.tile(ph, "bk%d" % i, [128, 512], F32, psum=True) for i in range(6)]
            tpb = [self.tile(ph, "tpC%d" % i, [128, D], BF16, psum=True) for i in range(2)]
            mixt = [self.tile(ph, "mixt%d" % i, [128, D], BF16) for i in range(2)]
            mT = [self.tile(ph, "mT%d" % i, [128, 8, 128], BF16) for i in range(2)]
            xt = [self.tile(ph, "xtC%d" % i, [128, D], F32) for i in range(2)]
            x1 = [self.tile(ph, "x1C%d" % i, [128, D], F32) for i in range(2)]
            yt = self.tile(ph, "ytC", [128, D], F32)
            junk = self.tile(ph, "junkC", [128, D], F32)
            ss = self.tile(ph, "ssC", [128, 1], F32)
            sd = self.tile(ph, "sdC", [128, 1], F32)
            rstd = self.tile(ph, "rstdC", [128, 1], F32)
            hb = [self.tile(ph, "hbC%d" % i, [128, D], BF16) for i in range(4)]
            h2T = self.tile(ph, "h2T", [128, 8, 512], BF16)
            gT = self.tile(ph, "gT", [128, NCF, 512], BF16)
            wgt = [self.tile(ph, "wgt%d" % i, [128, 8, 128], BF16) for i in range(3)]
            wut = [self.tile(ph, "wut%d" % i, [128, 8, 128], BF16) for i in range(3)]
            wdt = [self.tile(ph, "wdt%d" % i, [128, D], BF16) for i in range(3)]
            aT = [self.tile(ph, "aT%d" % i, [128, 514], F32) for i in range(2)]
            cacc = [self.tile(ph, "cacc%d" % i, [128, 512], F32) for i in range(2)]
            ga = [self.tile(ph, "ga%d" % i, [128, 512], F32) for i in range(2)]
            pst = [self.tile(ph, "pstC%d" % i, [128, D], F32) for i in range(2)]
            rt = [self.tile(ph, "rtC%d" % i, [128, D], F32) for i in range(2)]
            x1r = [self.tile(ph, "x1rC%d" % i, [128, D], F32) for i in range(2)]
            ot = [self.tile(ph, "otC%d" % i, [128, D], F32) for i in range(2)]
            ss3 = self.tile(ph, "ss3C", [128, 1], F32)
            sd3 = self.tile(ph, "sd3C", [128, 1], F32)
            rstd3 = self.tile(ph, "rstd3C", [128, 1], F32)
            junk3 = self.tile(ph, "junk3C", [128, D], F32)
            it = 0
            i3 = 0
            RNG = 2
            pending = []
            store_tags = []

            def finish_tile(tag, tt):
                nonlocal i3
                S._wait("sp", tag)
                tok = tt * 128
                r_, x_, o_ = rt[i3 % 2], x1r[i3 % 2], ot[i3 % 2]
                i3 += 1
                S.load("sp", r_[:, :], self.red[tok:tok + 128, :], r_.b)
                S.load("sp", x_[:, :], self.x1s[tok:tok + 128, :], x_.b)
                self.rstd_of(r_[:, :], r_.b, junk3, ss3, sd3, rstd3)
                S.op("dve", lambda: nc.vector.scalar_tensor_tensor(out=r_[:, :], in0=r_[:, :], scalar=rstd3[:, 0:1], in1=g3[:, :], op0=ALU.mult, op1=ALU.mult),
                     reads=[r_.b, rstd3.b, g3.b], writes=[r_.b])
                S.op("pool", lambda: nc.gpsimd.tensor_tensor(out=o_[:, :], in0=r_[:, :], in1=x_[:, :], op=ALU.add),
                     reads=[r_.b, x_.b], writes=[o_.b])
                S.store("pool", x_dst[tok:tok + 128, :], o_[:, :], o_.b)

            h2Ts = [h2T, self.tile(ph, "h2Tb", [128, 8, 512], BF16)]

            def seg_a1(T):
                tok = T * 128
                mt, mTt, tpp = mixt[T % 2], mT[T % 2], tpb[0]
                mrow = (tok // 2048) * 4096 + (tok % 2048)
                S.load("sp", mt[:, 0:512], self.mixall[mrow:mrow + 128, :], mt.b)
                S.load("sp", mt[:, 512:1024], self.mixall[2048 + mrow:2048 + mrow + 128, :], mt.b)
                for kc in range(8):
                    S.op("pe", lambda: nc.tensor.transpose(out=tpp[:, kc * 128:(kc + 1) * 128], in_=mt[:, kc * 128:(kc + 1) * 128], identity=self.ident[:, :]),
                         reads=[mt.b, self.ident.b], writes=[tpp.b], accum=(kc > 0))
                self.copy(mTt[:, :, :], tpp[:, :].rearrange("p (k t) -> p k t", k=8), [tpp.b], [mTt.b])

            def seg_a2(T):
                tok = T * 128
                mTt, xtt, h, x1t = mT[T % 2], xt[T % 2], hb[T % 4], x1[T % 2]
                S.load("sp", xtt[:, :], x_src[tok:tok + 128, :], xtt.b)
                pa, pb = bank[4], bank[5]
                for half, pp in enumerate((pa, pb)):
                    for kc in range(8):
                        S.op("pe", lambda: nc.tensor.matmul(pp[:, :], lhsT=mTt[:, kc, :], rhs=WO[:, kc, half * 512:(half + 1) * 512],
                                                            start=(kc == 0), stop=(kc == 7)), reads=[mTt.b, WO.b], writes=[pp.b], accum=(kc > 0))
                self.copy(yt[:, 0:512], pa[:, :], [pa.b], [yt.b], eng="act")
                self.copy(yt[:, 512:D], pb[:, :], [pb.b], [yt.b], eng="act")
                self.rstd_of(yt[:, :], yt.b, junk, ss, sd, rstd)
                S.op("dve", lambda: nc.vector.scalar_tensor_tensor(out=yt[:, :], in0=yt[:, :], scalar=rstd[:, 0:1], in1=g1[:, :], op0=ALU.mult, op1=ALU.mult),
                     reads=[yt.b, rstd.b, g1.b], writes=[yt.b])
                S.op("pool", lambda: nc.gpsimd.tensor_tensor(out=x1t[:, :], in0=yt[:, :], in1=xtt[:, :], op=ALU.add),
                     reads=[yt.b, xtt.b], writes=[x1t.b])
                S.store("pool", self.x1s[tok:tok + 128, :], x1t[:, :], x1t.b)
                store_tags.append(("dma", x1t.b.dsem, x1t.b.dcnt))
                self.rstd_of(x1t[:, :], x1t.b, junk, ss, sd, rstd)
                S.op("dve", lambda: nc.vector.scalar_tensor_tensor(out=h[:, :], in0=x1t[:, :], scalar=rstd[:, 0:1], in1=g2[:, :], op0=ALU.mult, op1=ALU.mult),
                     reads=[x1t.b, rstd.b, g2.b], writes=[h.b])

            def seg_b3(T):
                h, tpp = hb[T % 4], tpb[1]
                hT = h2Ts[(T // 4) % 2]
                ti = T % 4
                for kc in range(8):
                    S.op("pe", lambda: nc.tensor.transpose(out=tpp[:, kc * 128:(kc + 1) * 128], in_=h[:, kc * 128:(kc + 1) * 128], identity=self.ident[:, :]),
                         reads=[h.b, self.ident.b], writes=[tpp.b], accum=(kc > 0))
                self.copy(hT[:, :, ti * 128:(ti + 1) * 128], tpp[:, :].rearrange("p (k t) -> p k t", k=8), [tpp.b], [hT.b])

            def seg_b(c, ti, ctx):
                h, tpp = ctx
                hT = h2Ts[c % 2]
                for kc in range(8):
                    S.op("pe", lambda: nc.tensor.transpose(out=tpp[:, kc * 128:(kc + 1) * 128], in_=h[:, kc * 128:(kc + 1) * 128], identity=self.ident[:, :]),
                         reads=[h.b, self.ident.b], writes=[tpp.b], accum=(kc > 0))
                self.copy(hT[:, :, ti * 128:(ti + 1) * 128], tpp[:, :].rearrange("p (k t) -> p k t", k=8), [tpp.b], [hT.b])

            def c2a_cf(c, cf):
                hT = h2Ts[c % 2]
                wg_, wu_ = wgt[cf % 3], wut[cf % 3]
                S.load("sp", wg_[:, :, :], self.wgs[cf], wg_.b)
                S.load("sp", wu_[:, :, :], self.wus[cf], wu_.b)
                pa, pu = bank[2 * (cf % 2)], bank[2 * (cf % 2) + 1]
                for kc in range(8):
                    S.op("pe", lambda: nc.tensor.matmul(pa[:, :], lhsT=wg_[:, kc, :], rhs=hT[:, kc, :], start=(kc == 0), stop=(kc == 7)),
                         reads=[wg_.b, hT.b], writes=[pa.b], accum=(kc > 0))
                for kc in range(8):
                    S.op("pe", lambda: nc.tensor.matmul(pu[:, :], lhsT=wu_[:, kc, :], rhs=hT[:, kc, :], start=(kc == 0), stop=(kc == 7)),
                         reads=[wu_.b, hT.b], writes=[pu.b], accum=(kc > 0))
                a, ca, g = aT[cf % 2], cacc[cf % 2], ga[cf % 2]
                e4 = a[:, 0:4]
                S.op("dve", lambda: nc.vector.tensor_copy(out=a[:, 0:2], in_=halo[:, cf, :]), reads=[halo.b], writes=[a.b])
                S.op("dve", lambda: nc.vector.tensor_copy(out=a[:, 2:4], in_=pa[:, 0:2]), reads=[pa.b], writes=[a.b])
                S.op("dve", lambda: nc.vector.tensor_copy(out=halo[:, cf, :], in_=pa[:, 510:512]), reads=[pa.b], writes=[halo.b])
                S.op("dve", lambda: nc.vector.tensor_scalar(out=ca[:, 2:512], in0=pa[:, 0:510], scalar1=cw[:, cf, 0:1], scalar2=None, op0=ALU.mult),
                     reads=[pa.b, cw.b], writes=[ca.b])
                S.op("dve", lambda: nc.vector.scalar_tensor_tensor(out=ca[:, 2:512], in0=pa[:, 1:511], scalar=cw[:, cf, 1:2], in1=ca[:, 2:512], op0=ALU.mult, op1=ALU.add),
                     reads=[pa.b, cw.b, ca.b], writes=[ca.b])
                S.op("dve", lambda: nc.vector.scalar_tensor_tensor(out=ca[:, 2:512], in0=pa[:, 2:512], scalar=cw[:, cf, 2:3], in1=ca[:, 2:512], op0=ALU.mult, op1=ALU.add),
                     reads=[pa.b, cw.b, ca.b], writes=[ca.b])
                S.op("dve", lambda: nc.vector.tensor_scalar(out=ca[:, 0:2], in0=e4[:, 0:2], scalar1=cw[:, cf, 0:1], scalar2=None, op0=ALU.mult),
                     reads=[a.b, cw.b], writes=[ca.b])
                S.op("dve", lambda: nc.vector.scalar_tensor_tensor(out=ca[:, 0:2], in0=e4[:, 1:3], scalar=cw[:, cf, 1:2], in1=ca[:, 0:2], op0=ALU.mult, op1=ALU.add),
                     reads=[a.b, cw.b, ca.b], writes=[ca.b])
                S.op("dve", lambda: nc.vector.scalar_tensor_tensor(out=ca[:, 0:2], in0=e4[:, 2:4], scalar=cw[:, cf, 2:3], in1=ca[:, 0:2], op0=ALU.mult, op1=ALU.add),
                     reads=[a.b, cw.b, ca.b], writes=[ca.b])
                S.op("act", lambda: nc.scalar.activation(out=g[:, :], in_=ca[:, :], func=AF.Gelu_apprx_tanh, bias=cb[:, cf:cf + 1]),
                     reads=[ca.b, cb.b], writes=[g.b])
                S.op("dve", lambda: nc.vector.tensor_tensor(out=gT[:, cf, :], in0=g[:, :], in1=pu[:, :], op=ALU.mult),
                     reads=[g.b, pu.b], writes=[gT.b])

            def c2b(c):
                for tp2 in range(2):
                    accs = [bank[0], bank[1], bank[2], bank[3]]
                    for cf in range(NCF):
                        wd_ = wdt[cf % 3]
                        S.load("sp", wd_[:, :], self.wds[cf], wd_.b)
                        for j in range(2):
                            ti = 2 * tp2 + j
                            for half in range(2):
                                pp = accs[2 * j + half]
                                S.op("pe", lambda: nc.tensor.matmul(pp[:, :], lhsT=gT[:, cf, ti * 128:(ti + 1) * 128], rhs=wd_[:, half * 512:(half + 1) * 512],
                                                                    start=(cf == 0), stop=(cf == NCF - 1)), reads=[gT.b, wd_.b], writes=[pp.b], accum=(cf > 0))
                    for j in range(2):
                        ti = 2 * tp2 + j
                        tok = (4 * c + ti) * 128
                        p_ = pst[j]
                        self.copy(p_[:, 0:512], accs[2 * j][:, :], [accs[2 * j].b], [p_.b], eng="act")
                        self.copy(p_[:, 512:D], accs[2 * j + 1][:, :], [accs[2 * j + 1].b], [p_.b], eng="dve")
                        S.store("pool", self.part[tok:tok + 128, :], p_[:, :], p_.b)
                        store_tags.append(("dma", p_.b.dsem, p_.b.dcnt))

            NTL = 4 * self.nch
            fifo = []
            for s_ in range(-7, 0):
                if 0 <= s_ + 4 < NTL:
                    seg_b3(s_ + 4)
                if 0 <= s_ + 6 < NTL:
                    seg_a2(s_ + 6)
                if 0 <= s_ + 7 < NTL:
                    seg_a1(s_ + 7)
            for c in range(self.nch):
                for cf in range(NCF):
                    c2a_cf(c, cf)
                    if cf % 4 == 3:
                        s_ = 4 * c + cf // 4
                        if s_ + 4 < NTL:
                            seg_b3(s_ + 4)
                        if s_ + 6 < NTL:
                            seg_a2(s_ + 6)
                        if s_ + 7 < NTL:
                            seg_a1(s_ + 7)
                        if fifo and fifo[0][2] <= c:
                            tg, tt, _ = fifo.pop(0)
                            finish_tile(tg, tt)
                c2b(c)
                if c % RNG == RNG - 1 or c == self.nch - 1:
                    c0 = (c // RNG) * RNG
                    r0, r1 = c0 * 512, (c + 1) * 512
                    for tg in store_tags:
                        S._wait("pool", tg)
                    store_tags = []
                    tag = self.collective("AllReduce", ALU.add, self.part[r0:r1, :].opt(), self.red[r0:r1, :].opt())
                    for tt in range(r0 // 128, r1 // 128):
                        fifo.append((tag, tt, c + 2))
            for tg, tt, _ in fifo:
                finish_tile(tg, tt)
            S.barrier()


_CACHE = {}


def kernel(**inputs):
    x = np.ascontiguousarray(inputs["x"], dtype=np.float32)
    params = [layout_params(inputs, r) for r in range(2)]
    consts = [make_consts(r) for r in range(2)]
    if "prog" not in _CACHE:
        _CACHE["prog"] = Prog()
    prog = _CACHE["prog"]
    in_maps = []
    for core in range(8):
        b, r = core // 2, core % 2
        m = {"x": x[b]}
        for n, _, _ in CONST_SPECS:
            m["c_" + n] = consts[r][n]
        for n, _ in PARAM_SPECS:
            m["p_" + n] = params[r][n]
        in_maps.append(m)
    res = run_bass_kernel_spmd(prog.nc, in_maps, core_ids=list(range(8)))
    out = np.stack([np.asarray(res.results[2 * b]["y"], dtype=np.float32) for b in range(4)], axis=0)
    return out
```
